# Optimizing a Trainium2 kernel written in Bass

```python
import math
import jax, jax.numpy as jnp
from jax import lax
import numpy as np

D_MODEL = 4096
BATCH = 4
SEQ = 2048
DEPTH = 1

HEAD_DIM = 128
ATTN_HEADS = D_MODEL // 2 // HEAD_DIM
ATTN_KV_HEADS = 2
ATTN_GROUP = ATTN_HEADS // ATTN_KV_HEADS
WINDOW = 128
BLOCK = 128
N_BUCKETS = 32
MAX_DISTANCE = 128
GLA_HEADS = 4
GLA_DV = D_MODEL // 2 // GLA_HEADS
GLA_DK = GLA_DV // 2
GLA_RANK = 16
GLA_TAU = 16.0
GLA_CHUNK = 64
ATTN_Q = ATTN_HEADS * HEAD_DIM
ATTN_KV = ATTN_KV_HEADS * HEAD_DIM
GLA_K = GLA_HEADS * GLA_DK
GLA_V = GLA_HEADS * GLA_DV
MIX_WIDTH = ATTN_Q + GLA_V
IN_WIDTH = ATTN_Q + 2 * ATTN_KV + 2 * GLA_K + GLA_V + GLA_RANK + GLA_V
PEER_HEADS = 8
N_KEYS = 128
N_EXPERTS = N_KEYS * N_KEYS
PEER_TOPK = 16
PEER_DKEY = 256
PEER_TOKEN_BLOCK = 128
N_MOD = 6
EPS = 1e-6
NEG_INF = -1e30

kernel_name = "hybrid_swa_gla_peer_adaln"


def rmsnorm(x, g):
    xf = x.astype(jnp.float32)
    y = xf * lax.rsqrt(jnp.mean(xf * xf, axis=-1, keepdims=True) + EPS)
    return (y * g.astype(jnp.float32)).astype(x.dtype)


def t5_causal_bucket(dist):
    max_exact = N_BUCKETS // 2
    d = jnp.maximum(dist, 0)
    log_ratio = jnp.log(jnp.maximum(d, 1).astype(jnp.float32) / max_exact) / math.log(MAX_DISTANCE / max_exact)
    large = max_exact + (log_ratio * (N_BUCKETS - max_exact)).astype(jnp.int32)
    large = jnp.minimum(large, N_BUCKETS - 1)
    return jnp.where(d < max_exact, d, large)


def sliding_window_attention(q, k, v, sinks, rel_bias):
    B, T = q.shape[0], q.shape[1]
    nb = T // BLOCK
    qb = q.reshape(B, nb, BLOCK, ATTN_KV_HEADS, ATTN_GROUP, HEAD_DIM)

    def with_prev(a):
        a = a.reshape(B, nb, BLOCK, ATTN_KV_HEADS, HEAD_DIM)
        prev = jnp.pad(a, ((0, 0), (1, 0), (0, 0), (0, 0), (0, 0)))[:, :-1]
        return jnp.concatenate([prev, a], axis=2)

    kb, vb = with_prev(k), with_prev(v)
    s = jnp.einsum('bnqhgd,bnkhd->bnhgqk', qb, kb,
                   preferred_element_type=jnp.float32) * (HEAD_DIM ** -0.5)
    qi = jnp.arange(BLOCK)[:, None]
    kj = jnp.arange(2 * BLOCK)[None, :]
    dist = qi + BLOCK - kj
    in_window = (dist >= 0) & (dist < WINDOW)
    bias = rel_bias.astype(jnp.float32)[t5_causal_bucket(dist)]
    bias = bias.transpose(2, 0, 1).reshape(ATTN_KV_HEADS, ATTN_GROUP, BLOCK, 2 * BLOCK)
    kpos = jnp.arange(nb)[:, None] * BLOCK - BLOCK + jnp.arange(2 * BLOCK)[None, :]
    mask = in_window[None] & (kpos >= 0)[:, None, :]
    s = jnp.where(mask[None, :, None, None], s + bias[None, None], NEG_INF)
    sink = sinks.astype(jnp.float32).reshape(ATTN_KV_HEADS, ATTN_GROUP)[None, None, :, :, None, None]
    m = jnp.maximum(jnp.max(s, axis=-1, keepdims=True), sink)
    p = jnp.exp(s - m)
    p = p / (jnp.sum(p, axis=-1, keepdims=True) + jnp.exp(sink - m))
    o = jnp.einsum('bnhgqk,bnkhd->bnqhgd', p.astype(v.dtype), vb)
    return o.reshape(B, T, ATTN_Q)


def gated_linear_attention(q, k, v, log_a):
    B, T, H, dk = q.shape
    dv = v.shape[-1]
    nc, C = T // GLA_CHUNK, GLA_CHUNK

    def chunked(a):
        return a.astype(jnp.float32).reshape(B, nc, C, H, a.shape[-1]).transpose(0, 1, 3, 2, 4)

    q, k, v, log_a = chunked(q) * (dk ** -0.5), chunked(k), chunked(v), chunked(log_a)
    b = jnp.cumsum(log_a, axis=3)
    b_last = b[:, :, :, -1:, :]
    q_dec = q * jnp.exp(b)
    k_inv = k * jnp.exp(-b)
    k_dec = k * jnp.exp(b_last - b)
    causal = jnp.tril(jnp.ones((C, C), dtype=bool))
    attn = jnp.where(causal, jnp.einsum('bnhid,bnhjd->bnhij', q_dec, k_inv), 0.0)
    o_intra = jnp.einsum('bnhij,bnhjv->bnhiv', attn, v)

    def step(S, inp):
        qd, kd, vc, dl = inp
        o = jnp.einsum('bhcd,bhdv->bhcv', qd, S)
        S = dl[..., None] * S + jnp.einsum('bhcd,bhcv->bhdv', kd, vc)
        return S, o

    S0 = jnp.zeros((B, H, dk, dv), jnp.float32)
    xs = (q_dec.swapaxes(0, 1), k_dec.swapaxes(0, 1), v.swapaxes(0, 1),
          jnp.exp(b_last[:, :, :, 0, :]).swapaxes(0, 1))
    _, o_inter = lax.scan(step, S0, xs)
    o = o_intra + o_inter.swapaxes(0, 1)
    return o.transpose(0, 1, 3, 2, 4).reshape(B, T, H, dv)


def peer(h, w_q, sub_keys, u, v):
    B, T, D = h.shape
    tokens = h.reshape(B * T, D)
    q = (tokens @ w_q).astype(jnp.float32).reshape(B * T, PEER_HEADS, 2, PEER_DKEY // 2)
    s = jnp.einsum('nhpd,hpkd->nhpk', q, sub_keys.astype(jnp.float32))
    s1, i1 = lax.top_k(s[:, :, 0], PEER_TOPK)
    s2, i2 = lax.top_k(s[:, :, 1], PEER_TOPK)
    cand_s = (s1[..., :, None] + s2[..., None, :]).reshape(B * T, PEER_HEADS, PEER_TOPK * PEER_TOPK)
    cand_i = (i1[..., :, None] * N_KEYS + i2[..., None, :]).reshape(B * T, PEER_HEADS, PEER_TOPK * PEER_TOPK)
    top_s, pos = lax.top_k(cand_s, PEER_TOPK)
    idx = jnp.take_along_axis(cand_i, pos, axis=-1)
    gates = jax.nn.softmax(top_s, axis=-1)
    nblk = (B * T) // PEER_TOKEN_BLOCK

    def block(args):
        xt, ib, gb = args
        act = jnp.einsum('phkd,pd->phk', u[ib], xt, preferred_element_type=jnp.float32)
        w = (gb * jax.nn.gelu(act, approximate=False)).astype(xt.dtype)
        return jnp.einsum('phk,phkd->pd', w, v[ib])

    out = lax.map(block, (tokens.reshape(nblk, PEER_TOKEN_BLOCK, D),
                          idx.reshape(nblk, PEER_TOKEN_BLOCK, PEER_HEADS, PEER_TOPK),
                          gates.reshape(nblk, PEER_TOKEN_BLOCK, PEER_HEADS, PEER_TOPK)))
    return out.reshape(B, T, D)


def hybrid_layer(x, mod, norm1_g, norm2_g, w_in, sinks, rel_bias, gk2_w, gk2_b,
                 gla_norm_g, w_out, peer_w_q, peer_keys, peer_u, peer_v):
    B, T, _ = x.shape
    shift1, scale1, gate1, shift2, scale2, gate2 = jnp.split(mod[:, None, :], N_MOD, axis=-1)
    h = rmsnorm(x, norm1_g) * (1 + scale1) + shift1
    proj = h @ w_in
    widths = [ATTN_Q, ATTN_KV, ATTN_KV, GLA_K, GLA_K, GLA_V, GLA_RANK, GLA_V]
    offs = [int(o) for o in np.cumsum(widths)[:-1]]
    aq, ak, av, gq, gk, gv, glow, gout = jnp.split(proj, offs, axis=-1)
    attn_out = sliding_window_attention(
        aq.reshape(B, T, ATTN_HEADS, HEAD_DIM), ak.reshape(B, T, ATTN_KV_HEADS, HEAD_DIM),
        av.reshape(B, T, ATTN_KV_HEADS, HEAD_DIM), sinks, rel_bias)
    log_a = jax.nn.log_sigmoid((glow @ gk2_w + gk2_b).astype(jnp.float32)) / GLA_TAU
    gla_o = gated_linear_attention(
        gq.reshape(B, T, GLA_HEADS, GLA_DK), gk.reshape(B, T, GLA_HEADS, GLA_DK),
        gv.reshape(B, T, GLA_HEADS, GLA_DV), log_a.reshape(B, T, GLA_HEADS, GLA_DK))
    gla_o = rmsnorm(gla_o, gla_norm_g) * jax.nn.silu(gout.reshape(B, T, GLA_HEADS, GLA_DV).astype(jnp.float32))
    mixed = jnp.concatenate([attn_out, gla_o.reshape(B, T, GLA_V).astype(x.dtype)], axis=-1) @ w_out
    x = x + gate1 * mixed
    h = rmsnorm(x, norm2_g) * (1 + scale2) + shift2
    return x + gate2 * peer(h, peer_w_q, peer_keys, peer_u, peer_v)


def setup_inputs(seed: int = 0) -> dict:
    key = jax.random.key(seed)
    ks = jax.random.split(key, 20)
    f32 = jnp.float32
    D = D_MODEL

    def nrm(k, shape, scale):
        return jax.random.normal(k, shape, f32) * scale

    return {
        "x": nrm(ks[0], (BATCH, SEQ, D), 1.0),
        "c": nrm(ks[1], (BATCH, D), 1.0),
        "w_ada": nrm(ks[2], (DEPTH, D, N_MOD * D), 0.5 * D ** -0.5),
        "b_ada": nrm(ks[3], (DEPTH, N_MOD * D), 0.02),
        "norm1_g": 1.0 + nrm(ks[4], (DEPTH, D), 0.02),
        "norm2_g": 1.0 + nrm(ks[5], (DEPTH, D), 0.02),
        "w_in": nrm(ks[6], (DEPTH, D, IN_WIDTH), D ** -0.5),
        "attn_sinks": nrm(ks[7], (DEPTH, ATTN_HEADS), 0.5),
        "rel_bias": nrm(ks[8], (N_BUCKETS, ATTN_HEADS), 0.5),
        "gla_w_gk2": nrm(ks[9], (DEPTH, GLA_RANK, GLA_K), GLA_RANK ** -0.5),
        "gla_b_gk2": nrm(ks[10], (DEPTH, GLA_K), 0.02),
        "gla_norm_g": 1.0 + nrm(ks[11], (DEPTH, GLA_DV), 0.02),
        "w_out": nrm(ks[12], (DEPTH, MIX_WIDTH, D), MIX_WIDTH ** -0.5),
        "peer_w_q": nrm(ks[13], (DEPTH, D, PEER_HEADS * PEER_DKEY), D ** -0.5),
        "peer_keys": nrm(ks[14], (DEPTH, PEER_HEADS, 2, N_KEYS, PEER_DKEY // 2), (PEER_DKEY // 2) ** -0.5),
        "peer_u": nrm(ks[15], (DEPTH, N_EXPERTS, D), D ** -0.5),
        "peer_v": nrm(ks[16], (DEPTH, N_EXPERTS, D), PEER_HEADS ** -0.5),
        "final_g": 1.0 + nrm(ks[17], (D,), 0.02),
    }


def reference(x, c, w_ada, b_ada, norm1_g, norm2_g, w_in, attn_sinks, rel_bias,
              gla_w_gk2, gla_b_gk2, gla_norm_g, w_out, peer_w_q, peer_keys,
              peer_u, peer_v, final_g):
    c_act = jax.nn.silu(c)
    for l in range(DEPTH):
        mod = c_act @ w_ada[l] + b_ada[l]
        x = hybrid_layer(x, mod.astype(x.dtype), norm1_g[l], norm2_g[l], w_in[l],
                         attn_sinks[l], rel_bias, gla_w_gk2[l], gla_b_gk2[l],
                         gla_norm_g[l], w_out[l], peer_w_q[l], peer_keys[l],
                         peer_u[l], peer_v[l])
    return rmsnorm(x, final_g)
```

```python
from contextlib import ExitStack
import math
import numpy as np
import concourse.bass as bass
import concourse.mybir as mybir
from concourse.bass_utils import run_bass_kernel_spmd

F32 = mybir.dt.float32
BF16 = mybir.dt.bfloat16
AF = mybir.ActivationFunctionType
ALU = mybir.AluOpType

SAME_ENG_SYNC = True

D = 4096
KC = 32
TOK = 1024
MT = 512
NEXP = 16384
EPS = 1e-6
NEGM = -30000.0
IN_W = 8720
C_AQ, C_AK, C_AV, C_GQ, C_GK, C_GV, C_GLOW, C_GOUT = 0, 2048, 2304, 2560, 3584, 4608, 6656, 6672


class Prog:
    def __init__(self, nc):
        self.nc = nc
        self.ins = []
        self.last_w = {}
        self.readers = {}
        self.last_on = {}
        self.dmas_open = []

    maxops = None

    def op(self, eng, fn, reads=(), writes=(), dma=None, extra_deps=(), force=False):
        if self.maxops is not None and len(self.ins) >= self.maxops and not force:
            return None
        i = len(self.ins)
        deps = set(extra_deps)
        psk = [k for k in reads if k == 'ps0' or (isinstance(k, tuple) and k[0] == 'ps')]
        if psk:
            reads = [k for k in reads if k not in psk]
            writes = list(writes) + [k for k in psk if k not in writes]
        for k in reads:
            w = self.last_w.get(k)
            if w is not None:
                deps.add(w)
        for k in writes:
            w = self.last_w.get(k)
            if w is not None:
                deps.add(w)
            for r in self.readers.get(k, ()):
                deps.add(r)
        for k in reads:
            lst = self.readers.setdefault(k, [])
            if dma is None:
                for q in range(len(lst)):
                    J = self.ins[lst[q]]
                    if J['dma'] is None and J['eng'] == eng:
                        lst[q] = i
                        break
                else:
                    lst.append(i)
            else:
                lst.append(i)
        for k in writes:
            self.last_w[k] = i
            self.readers[k] = []
        deps.discard(i)
        self.ins.append(dict(eng=eng, fn=fn, deps=deps, dma=dma))
        if dma is None:
            self.last_on[eng] = i
        else:
            self.dmas_open.append(i)
        return i

    def pe(self, fn, reads=(), writes=()):
        return self.op('pe', fn, reads, writes)

    def act(self, fn, reads=(), writes=()):
        return self.op('act', fn, reads, writes)

    def dve(self, fn, reads=(), writes=()):
        return self.op('dve', fn, reads, writes)

    def pool(self, fn, reads=(), writes=()):
        return self.op('pool', fn, reads, writes)

    def dma(self, eng, key, fn, reads=(), writes=()):
        return self.op(eng, fn, reads, writes, dma=key)

    def barrier(self):
        deps = set(self.last_on.values()) | set(self.dmas_open)
        self.dmas_open = []
        for e in ['pe', 'act', 'dve', 'pool', 'sp']:
            self.op(e, lambda eng: eng.nop(), extra_deps=deps, force=True)
        self.last_w = {}
        self.readers = {}

    def emit(self, stack):
        nc = self.nc
        ins = self.ins
        n = len(ins)
        engs = ['pe', 'act', 'dve', 'pool', 'sp']

        def needs_wait(I, Dd):
            if Dd['dma'] is not None:
                return True
            if Dd['eng'] == I['eng'] and I['dma'] is None:
                if Dd['eng'] in ('pe', 'sp') or not SAME_ENG_SYNC:
                    return False
            return True

        need_sig = [False] * n
        for i, I in enumerate(ins):
            for d in I['deps']:
                Dd = ins[d]
                if Dd['dma'] is None and needs_wait(I, Dd):
                    need_sig[d] = True
        cnt = {e: 0 for e in engs}
        sigval = [0] * n
        dma_cnt = {}
        for i, I in enumerate(ins):
            if I['dma'] is not None:
                k = I['dma']
                dma_cnt[k] = dma_cnt.get(k, 0) + 16
                sigval[i] = dma_cnt[k]
            elif need_sig[i]:
                cnt[I['eng']] += 1
                sigval[i] = cnt[I['eng']]
        esem = {e: stack.enter_context(nc.semaphore('s_' + e)) for e in engs}
        dsem = {k: stack.enter_context(nc.semaphore('d_%d' % j)) for j, k in enumerate(dma_cnt)}
        self.stats = dict(n=n, cnt=dict(cnt), ndsem=len(dsem), maxdma=max(dma_cnt.values()) if dma_cnt else 0)
        per_eng = {e: [] for e in engs}
        for i, I in enumerate(ins):
            per_eng[I['eng']].append(i)

        def run(e, engobj):
            waited = {}
            for i in per_eng[e]:
                I = ins[i]
                need = {}
                for d in I['deps']:
                    Dd = ins[d]
                    if not needs_wait(I, Dd):
                        continue
                    if Dd['dma'] is not None:
                        key = ('d', Dd['dma'])
                    else:
                        key = ('e', Dd['eng'])
                    v = sigval[d]
                    if need.get(key, 0) < v:
                        need[key] = v
                for key, v in need.items():
                    if waited.get(key, 0) >= v:
                        continue
                    waited[key] = v
                    s = dsem[key[1]] if key[0] == 'd' else esem[key[1]]
                    engobj.wait_ge(s, v)
                r = I['fn'](engobj)
                if I['dma'] is not None:
                    r.then_inc(dsem[I['dma']], 16)
                elif need_sig[i]:
                    r.then_inc(esem[e], 1)

        block = stack.enter_context(nc.Block())
        block.sync(lambda eng: run('sp', eng))
        block.tensor(lambda eng: run('pe', eng))
        block.scalar(lambda eng: run('act', eng))
        block.vector(lambda eng: run('dve', eng))
        block.gpsimd(lambda eng: run('pool', eng))


class Arena:
    def __init__(self, nc, nbytes):
        self.nbytes = nbytes
        self.t = nc.alloc_sbuf_tensor("arena", [128, nbytes // 4], F32)

    def view(self, off, shape, dtype):
        esz = 4 if dtype == F32 else 2
        nel = int(np.prod(shape))
        nb = nel * esz
        assert off % 4 == 0 and nb % 4 == 0, (off, shape)
        assert off + nb <= self.nbytes, (off, nb, self.nbytes)
        ap = self.t[:, off // 4: (off + nb) // 4]
        if dtype != F32:
            ap = ap.bitcast(dtype)
        if len(shape) == 2:
            names = "a b"
            ap = ap.rearrange("p (a b) -> p a b", a=shape[0], b=shape[1])
        elif len(shape) == 3:
            ap = ap.rearrange("p (a b c) -> p a b c", a=shape[0], b=shape[1], c=shape[2])
        return ap


def build_program(dbg=None, upto=99, stop=None, fake_mod=False):
    nc = bass.Bass("TRN2", target_bir_lowering=False)
    dbg = dbg or {}

    def din(name, shape, dt=F32):
        return nc.dram_tensor(name, list(shape), dt, kind="ExternalInput").ap()

    x_main = din("x_main", [TOK, D])
    x_pre = din("x_pre", [TOK, D])
    flag_d = din("flag", [128, 1])
    c_d = din("c_t", [128, KC])
    wada_d = din("w_ada", [D, 6 * D]) if not fake_mod else None
    bada_d = din("b_ada_t", [128, 6 * KC])
    g1_d = din("g1_t", [128, KC])
    g2_d = din("g2_t", [128, KC])
    win_d = din("w_in", [D, IN_W])
    sinks_d = din("sinks", [1, 16])
    relb_d = din("rel_bias", [32, 16])
    oh_d = din("onehot", [32, 128])
    gk2w_d = din("gk2_w", [16, 1024])
    gk2b_d = din("gk2_b", [1, 1024])
    gng_d = din("gla_norm_g", [1, 512])
    wout_d = din("w_out", [D, D])
    if upto > 2:
        wq_d = din("w_q", [D, 2048])
        keyst_d = din("keys_t", [128, 16 * 128])
        ut_d = din("u_t", [D, NEXP])
        v_d = din("v", [NEXP, D])
        fg_d = din("final_g", [1, D])
        sel_d = din("sel", [24, 8 * 128])
    if fake_mod:
        modt_d = din("modT_dbg", [128, 192])
    out_d = nc.dram_tensor("out", [TOK, D], F32, kind="ExternalOutput").ap()
    x1s = nc.dram_tensor("x1s", [TOK, D], F32).ap()
    aws = nc.dram_tensor("aws", [128, 128, TOK], BF16).ap()
    gext = nc.dram_tensor("gext", [16, 384], F32).ap()
    dbg_out = {}
    for name, shape in dbg.items():
        dbg_out[name] = nc.dram_tensor("dbg_" + name, list(shape), F32, kind="ExternalOutput").ap()

    st = ExitStack()
    with st:
        A = Arena(nc, 200 * 1024)
        PS = nc.alloc_psum_tensor("ps", [128, 8, 512], F32)
        p = Prog(nc)
        import os
        if os.environ.get('K_MAXOPS'):
            p.maxops = int(os.environ['K_MAXOPS'])
        R_HT, R_CT, R_W, R_S, R_P = 0, 32768, 65536, 65536 + 49152, 65536 + 2 * 49152
        po = [R_P]

        def palloc(shape, dt):
            esz = 4 if dt == F32 else 2
            nb = int(np.prod(shape)) * esz
            nb = (nb + 31) // 32 * 32
            v = A.view(po[0], shape, dt)
            po[0] += nb
            return v

        ident_f = A.view(po[0], [128], F32); po[0] += 512
        ident_b = A.view(po[0], [128], BF16); po[0] += 256
        ones_b = A.view(po[0], [128], BF16); po[0] += 256
        ones_f = A.view(po[0], [128], F32); po[0] += 512
        Lm = A.view(po[0], [128], F32); po[0] += 512
        Um = A.view(po[0], [128], F32); po[0] += 512
        modT = A.view(po[0], [192], F32); po[0] += 768
        badat = A.view(po[0], [192], F32); po[0] += 768
        a1 = A.view(po[0], [32], F32); po[0] += 128
        a2 = A.view(po[0], [32], F32); po[0] += 128
        g1t = A.view(po[0], [32], F32); po[0] += 128
        g2t = A.view(po[0], [32], F32); po[0] += 128
        ct = A.view(po[0], [32], F32); po[0] += 128
        cact = A.view(po[0], [32], BF16); po[0] += 64
        flag = A.view(po[0], [8], F32); po[0] += 32
        small = A.view(po[0], [64], F32); po[0] += 256
        esink = A.view(po[0], [16], F32); po[0] += 64
        gnb = A.view(po[0], [512], F32); po[0] += 2048
        gk2w = A.view(po[0], [1024], BF16); po[0] += 2048
        gk2b = A.view(po[0], [1024], BF16); po[0] += 2048
        P_DYN = po[0]
        bcur = A.view(po[0], [16, 128], BF16); po[0] += 4096
        bprev = A.view(po[0], [16, 128], BF16); po[0] += 4096
        Sst = A.view(po[0], [4, 2, 512], F32); po[0] += 16384
        assert po[0] <= 200 * 1024, po[0]

        s1 = modT[:, 0:32]
        gate1 = modT[:, 64:96]
        s2 = modT[:, 96:128]
        gate2 = modT[:, 160:192]

        def bank(b, n=512):
            return PS[:, b, 0:n]

        p.pool(lambda e: e.memset(ident_f, 1.0), writes=['ident_f'])
        p.pool(lambda e: e.affine_select(out=ident_f, in_=ident_f, pattern=[[-1, 128]], compare_op=ALU.is_equal,
                                         fill=0.0, base=0, channel_multiplier=1), reads=['ident_f'], writes=['ident_f'])
        p.pool(lambda e: e.memset(ones_f, 1.0), writes=['ones_f'])
        p.pool(lambda e: e.memset(ones_b, 1.0), writes=['ones_b'])
        p.dve(lambda e: e.tensor_copy(out=ident_b, in_=ident_f), reads=['ident_f'], writes=['ident_b'])
        p.pool(lambda e: e.memset(Lm, 1.0), writes=['Lm'])
        p.pool(lambda e: e.affine_select(out=Lm, in_=Lm, pattern=[[1, 128]], compare_op=ALU.is_ge,
                                         fill=0.0, base=0, channel_multiplier=-1), reads=['Lm'], writes=['Lm'])
        p.pool(lambda e: e.memset(Lm[0:64, 64:128], 0.0), reads=['Lm'], writes=['Lm'])
        p.pool(lambda e: e.memset(Um, 1.0), writes=['Um'])
        p.pool(lambda e: e.affine_select(out=Um, in_=Um, pattern=[[-1, 128]], compare_op=ALU.is_gt,
                                         fill=0.0, base=0, channel_multiplier=1), reads=['Um'], writes=['Um'])
        p.pool(lambda e: e.memset(Um[64:128, 0:64], 0.0), reads=['Um'], writes=['Um'])
        p.pool(lambda e: e.memset(Sst, 0.0), writes=['Sst'])

        sm = 'small_ld'
        p.dma('sp', sm, lambda e: e.dma_start(out=ct, in_=c_d), writes=['ct'])
        p.dma('sp', sm, lambda e: e.dma_start(out=badat, in_=bada_d), writes=['badat'])
        p.dma('sp', sm, lambda e: e.dma_start(out=g1t, in_=g1_d), writes=['g1t'])
        p.dma('sp', sm, lambda e: e.dma_start(out=g2t, in_=g2_d), writes=['g2t'])
        p.dma('sp', sm, lambda e: e.dma_start(out=flag[:, 0:1], in_=flag_d), writes=['flag'])
        p.dma('sp', sm, lambda e: e.dma_start(out=esink, in_=sinks_d.partition_broadcast(128)[:, 0, :]), writes=['esink'])
        p.dma('sp', sm, lambda e: e.dma_start(out=gnb, in_=gng_d.partition_broadcast(128)[:, 0, :]), writes=['gnb'])
        p.dma('pool', 'small_ld2', lambda e: e.dma_start(out=gk2w[0:16, :], in_=gk2w_d), writes=['gk2w'])
        p.dma('pool', 'small_ld2', lambda e: e.dma_start(out=gk2b[0:1, :], in_=gk2b_d), writes=['gk2b'])
        relb = A.view(R_S, [16], F32)
        ohs = A.view(R_S + 64, [128], F32)
        gx = A.view(R_S + 1024, [384], F32)
        p.dma('sp', sm, lambda e: e.dma_start(out=relb[0:32, :], in_=relb_d), writes=['relb'])
        p.dma('sp', sm, lambda e: e.dma_start(out=ohs[0:32, :], in_=oh_d), writes=['ohs'])
        p.barrier()
        p.act(lambda e: e.activation(out=esink, in_=esink, func=AF.Exp), reads=['esink'], writes=['esink'])
        p.pe(lambda e: e.matmul(PS[0:16, 0, 0:128], lhsT=relb[0:32, :], rhs=ohs[0:32, :], start=True, stop=True),
             reads=['relb', 'ohs'], writes=['ps0'])
        p.pool(lambda e: e.memset(gx[0:16, :], NEGM), writes=['gx'])
        p.dve(lambda e: e.tensor_copy(out=gx[0:16, 128:256], in_=PS[0:16, 0, 0:128]), reads=['ps0', 'gx'], writes=['gx'])
        p.dma('sp', 'gx', lambda e: e.dma_start(out=gext, in_=gx[0:16, :]), reads=['gx'], writes=['gext'])
        p.barrier()
        bcf = A.view(R_S + 4096, [16, 128], F32)
        bpf = A.view(R_S + 4096 + 8192, [16, 128], F32)
        for k in range(128):
            p.dma('sp', 'bias_ld', (lambda k: lambda e: e.dma_start(out=bcf[k:k + 1, :, :], in_=gext[:, 128 - k:256 - k].unsqueeze(0)))(k),
                  writes=[('bcf', k)])
            p.dma('sp', 'bias_ld', (lambda k: lambda e: e.dma_start(out=bpf[k:k + 1, :, :], in_=gext[:, 256 - k:384 - k].unsqueeze(0)))(k),
                  writes=[('bpf', k)])
        p.barrier()
        p.dve(lambda e: e.tensor_copy(out=bcur, in_=bcf), writes=['bcur'])
        p.dve(lambda e: e.tensor_copy(out=bprev, in_=bpf), writes=['bprev'])
        if 'bcur' in dbg:
            p.dma('sp', 'dbg', lambda e: e.dma_start(out=dbg_out['bcur'], in_=bcf), reads=['bcur'], writes=['dbg_bcur'])
        p.barrier()

        p.act(lambda e: e.activation(out=cact, in_=ct, func=AF.Silu), writes=['cact'])
        wada_v = wada_d.rearrange("(k p) c -> p k c", p=128) if not fake_mod else None
        PS_fake = A.view(R_S, [192], F32)
        Wt = [A.view(R_W + i * 16384, [32, 256], BF16) for i in range(3)]
        wctr = [0]

        def load_w(view, c0, ncols):
            slot = wctr[0] % 3
            wctr[0] += 1
            t = Wt[slot]
            p.dma('pool', ('W', slot), lambda e: e.dma_start(out=t[:, :, 0:ncols], in_=view[:, :, c0:c0 + ncols]),
                  writes=[('W', slot)])
            return t, ('W', slot)

        if fake_mod:
            p.dma('sp', 'fm', lambda e: e.dma_start(out=PS_fake, in_=modt_d), writes=['ps0'])
        for tno in range(0 if fake_mod else 6 * D // 256):
            wt, wk = load_w(wada_v, tno * 256, 256)
            for cc in range(2):
                j = tno * 2 + cc
                for k in range(KC):
                    p.pe((lambda wt, cc, j, k: lambda e: e.matmul(PS[:, 0, j:j + 1], lhsT=wt[:, k, cc * 128:(cc + 1) * 128],
                                                                  rhs=cact[:, k:k + 1], start=(k == 0), stop=(k == KC - 1)))(wt, cc, j, k),
                         reads=[wk, 'cact'], writes=['ps0'])
        if fake_mod:
            p.dve(lambda e: e.tensor_copy(out=modT, in_=PS_fake), reads=['ps0'], writes=['modT'])
        else:
            p.dve(lambda e: e.tensor_tensor(out=modT, in0=PS[:, 0, 0:192], in1=badat, op=ALU.add), reads=['ps0'], writes=['modT'])
        p.dve(lambda e: e.scalar_tensor_tensor(out=a1, in0=modT[:, 32:64], scalar=1.0, in1=g1t, op0=ALU.add, op1=ALU.mult),
              reads=['modT'], writes=['a1'])
        p.dve(lambda e: e.scalar_tensor_tensor(out=a2, in0=modT[:, 128:160], scalar=1.0, in1=g2t, op0=ALU.add, op1=ALU.mult),
              reads=['modT'], writes=['a2'])
        if 'modT' in dbg:
            p.dma('sp', 'dbg', lambda e: e.dma_start(out=dbg_out['modT'], in_=modT), reads=['modT'], writes=['dbg_modT'])
        p.barrier()
        if upto <= 1:
            p.emit(st)
            return nc, p

        def rms_norm_to_hT(src, row0, ntiles, a_t, s_t, hT, xs_off, junk_off):
            xs = [A.view(xs_off + i * 16384, [4096], F32) for i in range(2)]
            junk = A.view(junk_off, [4096], BF16)
            for tt in range(ntiles):
                xt = xs[tt % 2]
                xk = ('xs', tt % 2)
                sc = small[:, (tt % 2) * 4:(tt % 2) * 4 + 4]
                sk = ('nsc', tt % 2)
                p.dma('sp', xk, (lambda xt, tt: lambda e: e.dma_start(out=xt, in_=src[row0 + tt * 128: row0 + (tt + 1) * 128, :]))(xt, tt),
                      writes=[xk])
                p.act((lambda xt, sc: lambda e: e.activation(out=junk, in_=xt, func=AF.Square, accum_out=sc[:, 0:1]))(xt, sc),
                      reads=[xk], writes=['junk', sk])
                p.dve((lambda sc: lambda e: e.tensor_scalar(out=sc[:, 1:2], in0=sc[:, 0:1], scalar1=1.0 / D, scalar2=EPS,
                                                            op0=ALU.mult, op1=ALU.add))(sc), reads=[sk], writes=[sk])
                p.act((lambda sc: lambda e: e.activation(out=sc[:, 2:3], in_=sc[:, 1:2], func=AF.Sqrt))(sc), reads=[sk], writes=[sk])
                p.dve((lambda sc: lambda e: e.reciprocal(out=sc[:, 3:4], in_=sc[:, 2:3]))(sc), reads=[sk], writes=[sk])
                p.act((lambda xt, sc: lambda e: e.activation(out=xt, in_=xt, func=AF.Copy, scale=sc[:, 3:4]))(xt, sc),
                      reads=[xk, sk], writes=[xk])
                for g4 in range(8):
                    b = 4 + (g4 % 2)
                    for q in range(4):
                        c = g4 * 4 + q
                        p.pe((lambda xt, b, q, c: lambda e: e.transpose(PS[:, b, q * 128:(q + 1) * 128], xt[:, c * 128:(c + 1) * 128], ident_f))(xt, b, q, c),
                             reads=[xk, 'ident_f'], writes=[('ps', b)])
                    for q in range(4):
                        c = g4 * 4 + q
                        if g4 % 2 == 0:
                            p.dve((lambda b, q, c, tt: lambda e: e.tensor_scalar(out=hT[:, c, tt * 128:(tt + 1) * 128], in0=PS[:, b, q * 128:(q + 1) * 128],
                                                                                 scalar1=a_t[:, c:c + 1], scalar2=s_t[:, c:c + 1], op0=ALU.mult, op1=ALU.add))(b, q, c, tt),
                                  reads=[('ps', b)], writes=[('hT', c)])
                        else:
                            p.act((lambda b, q, c, tt: lambda e: e.activation(out=hT[:, c, tt * 128:(tt + 1) * 128], in_=PS[:, b, q * 128:(q + 1) * 128],
                                                                              func=AF.Identity, scale=a_t[:, c:c + 1], bias=s_t[:, c:c + 1]))(b, q, c, tt),
                                  reads=[('ps', b)], writes=[('hT', c)])

        pbank = [0]

        def next_pbank():
            b = pbank[0] % 2
            pbank[0] += 1
            return b

        hT_keys = [('hT', c) for c in range(KC)]

        def gemm_fm(wview, c0, ncols, hT, T, evac, src_keys):
            done = 0
            while done < ncols:
                n = min(256, ncols - done)
                wt, wk = load_w(wview, c0 + done, n)
                for cc in range((n + 127) // 128):
                    m = min(128, n - cc * 128)
                    for h0 in range(0, T, 512):
                        b = next_pbank()
                        for k in range(KC):
                            p.pe((lambda wt, cc, m, b, k, h0: lambda e: e.matmul(PS[0:m, b, 0:min(512, T - h0)], lhsT=wt[:, k, cc * 128:cc * 128 + m],
                                                                                 rhs=hT[:, k, h0:h0 + min(512, T - h0)], start=(k == 0), stop=(k == KC - 1)))(wt, cc, m, b, k, h0),
                                 reads=[wk] + src_keys, writes=[('ps', b)])
                        evac((done // 128) + cc, b, h0)
                done += n

        def gemm_tm(wview, c0, ncols, hT, T, evac, src_keys, also_fm=None):
            done = 0
            while done < ncols:
                n = min(256, ncols - done)
                wt, wk = load_w(wview, c0 + done, n)
                for tt in range(T // 128):
                    b = next_pbank()
                    for k in range(KC):
                        p.pe((lambda wt, n, b, k, tt: lambda e: e.matmul(PS[:, b, 0:n], lhsT=hT[:, k, tt * 128:(tt + 1) * 128],
                                                                         rhs=wt[:, k, 0:n], start=(k == 0), stop=(k == KC - 1)))(wt, n, b, k, tt),
                             reads=[wk] + src_keys, writes=[('ps', b)])
                    evac(done, n, tt, b)
                if also_fm is not None:
                    for cc in range(n // 128):
                        b = next_pbank()
                        for k in range(KC):
                            p.pe((lambda wt, cc, b, k: lambda e: e.matmul(PS[:, b, 0:T], lhsT=wt[:, k, cc * 128:(cc + 1) * 128],
                                                                          rhs=hT[:, k, 0:T], start=(k == 0), stop=(k == KC - 1)))(wt, cc, b, k),
                                 reads=[wk] + src_keys, writes=[('ps', b)])
                        also_fm((done // 128) + cc, b)
                done += n

        win_v = win_d.rearrange("(k p) c -> p k c", p=128)
        wout_v = wout_d.rearrange("(k p) c -> p k c", p=128)
        hT = A.view(R_HT, [32, MT], BF16)
        cT = A.view(R_CT, [32, MT], BF16)
        so = [R_S]

        def salloc(shape, dt):
            esz = 4 if dt == F32 else 2
            nb = (int(np.prod(shape)) * esz + 31) // 32 * 32
            v = A.view(so[0], shape, dt)
            so[0] += nb
            assert so[0] <= R_S + 49152, so[0]
            return v

        KT = salloc([2, 5 * 128], BF16)
        Vt = salloc([5, 2, 128], BF16)
        glowT = salloc([MT], BF16)
        S_AFTER_KV = so[0]
        QTg = salloc([4, 8, 128], BF16)
        tmpS = [salloc([512], F32) for _ in range(2)]
        PT = [salloc([512], BF16) for _ in range(2)]
        den = salloc([512], F32)
        so[0] = S_AFTER_KV
        gqT = salloc([2, MT], BF16)
        gkT = salloc([2, MT], BF16)
        gkt = salloc([4, 256], BF16)
        gv = salloc([4, 512], BF16)
        SG = salloc([4, 512], BF16)
        la = salloc([4, 256], F32)
        eb = salloc([2, MT], F32)
        enb = salloc([2, MT], F32)
        qdT = salloc([2, MT], BF16)
        kinvT = salloc([2, MT], BF16)
        kdec = salloc([4, 256], BF16)
        dl = salloc([2, 8], F32)
        Sbf = salloc([2, 512], BF16)
        attn_sb = salloc([64], BF16)
        ybf = salloc([512], BF16)
        tmp256 = salloc([256], F32)
        GLA_END = so[0]
        so[0] = S_AFTER_KV
        G1B = salloc([4096], F32)
        xres = [salloc([4, 256], F32) for _ in range(2)]
        dtmp = salloc([128], F32)

        PSb = PS[:, :, :].bitcast(BF16) if False else None

        p.pool(lambda e: e.memset(KT, 0.0), writes=['KT'])
        p.pool(lambda e: e.memset(Vt, 0.0), writes=['Vt'])

        def build_rowbcast(dst, colvec, tag):
            for c in range(KC):
                b = 2 + (c // 4) % 2
                q = c % 4
                p.dve((lambda c: lambda e: e.tensor_scalar(out=dtmp, in0=ident_f, scalar1=colvec[:, c:c + 1], scalar2=None, op0=ALU.mult))(c),
                      reads=['ident_f', 'modT'], writes=['dtmp'])
                p.pe((lambda b, q: lambda e: e.matmul(PS[:, b, q * 128:(q + 1) * 128], lhsT=ones_f, rhs=dtmp, start=True, stop=True))(b, q),
                     reads=['dtmp', 'ones_f'], writes=[('ps', b)])
                if q == 3:
                    p.act((lambda b, c: lambda e: e.activation(out=dst[:, (c - 3) * 128:(c + 1) * 128], in_=PS[:, b, :], func=AF.Identity))(b, c),
                          reads=[('ps', b)], writes=[tag])

        macro = [('P', 0), ('P', 1), ('M', 0), ('M', 1)]
        for kind, mi in macro:
            main = kind == 'M'
            src = x_main if main else x_pre
            rms_norm_to_hT(src, mi * MT, MT // 128, a1, s1, hT, R_CT, R_W + 32768)
            p.barrier()
            if stop == 'norm%s%d' % (kind, mi):
                p.barrier()
                p.emit(st)
                return nc, p
            need_kv = main or mi == 1

            if need_kv:
                def ev_k(ci, b, h0):
                    p.act((lambda ci, b: lambda e: e.activation(out=KT[:, ci, 128:128 + MT], in_=PS[:, b, 0:MT], func=AF.Identity))(ci, b),
                          reads=[('ps', b)], writes=['KT'])
                gemm_fm(win_v, C_AK, 256, hT, MT, ev_k, hT_keys)

                def ev_v(c0, n, tt, b):
                    p.dve((lambda tt, b: lambda e: e.tensor_copy(out=Vt[:, 1 + tt, :, :], in_=PS[:, b, 0:256].rearrange("p (g d) -> p g d", g=2)))(tt, b),
                          reads=[('ps', b)], writes=['Vt'])
                gemm_tm(win_v, C_AV, 256, hT, MT, ev_v, hT_keys)
            if main:
                for g in range(2):
                    def ev_q(ci, b, h0, g=g):
                        p.act((lambda ci, b: lambda e: e.activation(out=QTg[:, :, ci, :], in_=PS[:, b, 0:MT].rearrange("p (n q) -> p n q", n=4),
                                                                    func=AF.Copy, scale=128 ** -0.5))(ci, b),
                              reads=[('ps', b)], writes=['QTg'])
                    gemm_fm(win_v, C_AQ + g * 1024, 1024, hT, MT, ev_q, hT_keys)
                    for n in range(4):
                        for hf in range(2):
                            hs = slice(hf * 4, hf * 4 + 4)
                            hg = slice(g * 8 + hf * 4, g * 8 + hf * 4 + 4)
                            rq = QTg[:, n, hs, :].rearrange("p j q -> p (j q)")
                            p.pe((lambda g, n, rq: lambda e: e.matmul(PS[:, 2, :], lhsT=KT[:, g, (n + 1) * 128:(n + 2) * 128], rhs=rq, start=True, stop=True))(g, n, rq),
                                 reads=['KT', 'QTg'], writes=[('ps', 2)])
                            p.pe((lambda g, n, rq: lambda e: e.matmul(PS[:, 3, :], lhsT=KT[:, g, n * 128:(n + 1) * 128], rhs=rq, start=True, stop=True))(g, n, rq),
                                 reads=['KT', 'QTg'], writes=[('ps', 3)])
                            for w_, (bk, bt) in enumerate([(2, bcur), (3, bprev)]):
                                p.dve((lambda w_, bk, bt, hg: lambda e: e.tensor_tensor(out=tmpS[w_], in0=PS[:, bk, :], in1=bt[:, hg, :].rearrange("p j q -> p (j q)"), op=ALU.add))(w_, bk, bt, hg),
                                      reads=[('ps', bk)], writes=[('tmpS', w_)])
                                p.act((lambda w_: lambda e: e.activation(out=PT[w_], in_=tmpS[w_], func=AF.Exp))(w_),
                                      reads=[('tmpS', w_)], writes=[('PT', w_)])
                            if mi == 0 and n == 0:
                                p.dve(lambda e: e.tensor_scalar(out=PT[1], in0=PT[1], scalar1=flag[:, 0:1], scalar2=None, op0=ALU.mult),
                                      reads=[('PT', 1), 'flag'], writes=[('PT', 1)])
                            p.pe(lambda e: e.matmul(PS[:, 6, :], lhsT=ones_b, rhs=PT[0], start=True, stop=False), reads=[('PT', 0), 'ones_b'], writes=[('ps', 6)])
                            p.pe(lambda e: e.matmul(PS[:, 6, :], lhsT=ones_b, rhs=PT[1], start=False, stop=True), reads=[('PT', 1), 'ones_b'], writes=[('ps', 6)])
                            p.pe((lambda g, n: lambda e: e.matmul(PS[:, 7, :], lhsT=Vt[:, n + 1, g, :], rhs=PT[0], start=True, stop=False))(g, n),
                                 reads=[('PT', 0), 'Vt'], writes=[('ps', 7)])
                            p.pe((lambda g, n: lambda e: e.matmul(PS[:, 7, :], lhsT=Vt[:, n, g, :], rhs=PT[1], start=False, stop=True))(g, n),
                                 reads=[('PT', 1), 'Vt'], writes=[('ps', 7)])
                            for j in range(4):
                                hh = g * 8 + hf * 4 + j
                                p.dve((lambda j, hh: lambda e: e.tensor_scalar(out=den[:, j * 128:(j + 1) * 128], in0=PS[:, 6, j * 128:(j + 1) * 128],
                                                                               scalar1=esink[:, hh:hh + 1], scalar2=None, op0=ALU.add))(j, hh),
                                      reads=[('ps', 6), 'esink'], writes=['den'])
                            p.dve(lambda e: e.reciprocal(out=den, in_=den), reads=['den'], writes=['den'])
                            p.dve((lambda g, n, hf: lambda e: e.tensor_tensor(out=cT[:, g * 8 + hf * 4:g * 8 + hf * 4 + 4, n * 128:(n + 1) * 128],
                                                                              in0=PS[:, 7, :].rearrange("p (j q) -> p j q", j=4),
                                                                              in1=den.rearrange("p (j q) -> p j q", j=4), op=ALU.mult))(g, n, hf),
                                  reads=[('ps', 7), 'den'], writes=[('cT', g * 8 + hf * 4 + jj) for jj in range(4)])
            if need_kv:
                p.pool(lambda e: e.tensor_copy(out=KT[:, :, 0:128], in_=KT[:, :, 512:640]), reads=['KT'], writes=['KT'])
                p.pool(lambda e: e.tensor_copy(out=Vt[:, 0, :, :], in_=Vt[:, 4, :, :]), reads=['Vt'], writes=['Vt'])
            if stop == 'swa%s%d' % (kind, mi):
                p.barrier()
                p.emit(st)
                return nc, p
            if 'cT_attn' in dbg and main and mi == 0:
                p.barrier()
                p.dve(lambda e: e.tensor_copy(out=eb[:, 0, :], in_=cT[:, 0, :]), writes=['dbgtmp'])
                p.dma('sp', 'dbg', lambda e: e.dma_start(out=dbg_out['cT_attn'], in_=eb[:, 0, :]), reads=['dbgtmp'], writes=['dbg_ct'])
            p.barrier()
            def ev_glow(ci, b, h0):
                p.act((lambda b: lambda e: e.activation(out=glowT[0:16, :], in_=PS[0:16, b, 0:MT], func=AF.Identity))(b),
                      reads=[('ps', b)], writes=['glowT'])
            gemm_fm(win_v, C_GLOW, 16, hT, MT, ev_glow, hT_keys)
            for h in range(4):
                if main:
                    def ev_gq(ci, b, h0):
                        p.act((lambda ci, b: lambda e: e.activation(out=gqT[:, ci, :], in_=PS[:, b, 0:MT], func=AF.Identity))(ci, b),
                              reads=[('ps', b)], writes=['gqT'])
                    gemm_fm(win_v, C_GQ + h * 256, 256, hT, MT, ev_gq, hT_keys)

                def ev_gk(c0, n, tt, b):
                    p.dve((lambda tt, b: lambda e: e.tensor_copy(out=gkt[:, tt, :], in_=PS[:, b, 0:256]))(tt, b), reads=[('ps', b)], writes=['gkt'])

                def ev_gkT(ci, b):
                    p.act((lambda ci, b: lambda e: e.activation(out=gkT[:, ci, :], in_=PS[:, b, 0:MT], func=AF.Identity))(ci, b),
                          reads=[('ps', b)], writes=['gkT'])
                gemm_tm(win_v, C_GK + h * 256, 256, hT, MT, ev_gk, hT_keys, also_fm=ev_gkT if main else None)

                def ev_gv(c0, n, tt, b):
                    p.act((lambda c0, tt, b: lambda e: e.activation(out=gv[:, tt, c0:c0 + 256], in_=PS[:, b, 0:256], func=AF.Identity))(c0, tt, b),
                          reads=[('ps', b)], writes=['gv'])
                gemm_tm(win_v, C_GV + h * 512, 512, hT, MT, ev_gv, hT_keys)
                if main:
                    def ev_go(c0, n, tt, b):
                        p.act((lambda b: lambda e: e.activation(out=tmp256, in_=PS[:, b, 0:256], func=AF.Silu))(b),
                              reads=[('ps', b)], writes=['tmp256'])
                        p.dve((lambda c0, tt: lambda e: e.tensor_tensor(out=SG[:, tt, c0:c0 + 256], in0=tmp256, in1=gnb[:, c0:c0 + 256], op=ALU.mult))(c0, tt),
                              reads=['tmp256', 'gnb'], writes=['SG'])
                    gemm_tm(win_v, C_GOUT + h * 512, 512, hT, MT, ev_go, hT_keys)
                for tt in range(4):
                    p.pe((lambda tt, h: lambda e: e.matmul(PS[:, 2, 0:256], lhsT=glowT[0:16, tt * 128:(tt + 1) * 128], rhs=gk2w[0:16, h * 256:(h + 1) * 256],
                                                           start=True, stop=False))(tt, h), reads=['glowT', 'gk2w'], writes=[('ps', 2)])
                    p.pe((lambda tt, h: lambda e: e.matmul(PS[:, 2, 0:256], lhsT=ones_b[0:1, :], rhs=gk2b[0:1, h * 256:(h + 1) * 256],
                                                           start=False, stop=True))(tt, h), reads=['gk2b', 'ones_b'], writes=[('ps', 2)])
                    p.act(lambda e: e.activation(out=tmp256, in_=PS[:, 2, 0:256], func=AF.Exp, scale=-1.0), reads=[('ps', 2)], writes=['tmp256'])
                    p.act(lambda e: e.activation(out=tmp256, in_=tmp256, func=AF.Ln, bias=1.0), reads=['tmp256'], writes=['tmp256'])
                    p.act((lambda tt: lambda e: e.activation(out=la[:, tt, :], in_=tmp256, func=AF.Copy, scale=-1.0 / 16.0))(tt),
                          reads=['tmp256'], writes=['la'])
                for dc in range(2):
                    for tt in range(4):
                        p.pe((lambda dc, tt: lambda e: e.matmul(PS[:, 2 + dc, tt * 128:(tt + 1) * 128], lhsT=la[:, tt, dc * 128:(dc + 1) * 128], rhs=Lm,
                                                                start=True, stop=True))(dc, tt), reads=['la', 'Lm'], writes=[('ps', 2 + dc)])
                p.act(lambda e: e.activation(out=eb, in_=PS[:, 2:4, :], func=AF.Exp), reads=[('ps', 2), ('ps', 3)], writes=['eb'])
                if main:
                    p.act(lambda e: e.activation(out=enb, in_=PS[:, 2:4, :], func=AF.Exp, scale=-1.0), reads=[('ps', 2), ('ps', 3)], writes=['enb'])
                    p.dve(lambda e: e.scalar_tensor_tensor(out=qdT, in0=gqT, scalar=1.0 / 16.0, in1=eb, op0=ALU.mult, op1=ALU.mult),
                          reads=['gqT', 'eb'], writes=['qdT'])
                    p.dve(lambda e: e.tensor_tensor(out=kinvT, in0=gkT, in1=enb, op=ALU.mult), reads=['gkT', 'enb'], writes=['kinvT'])
                p.dve(lambda e: e.tensor_copy(out=dl, in_=eb[:, :, 63::64]), reads=['eb'], writes=['dl'])
                for tt in range(4):
                    p.pe((lambda tt: lambda e: e.matmul(PS[:, 2, 0:256], lhsT=Um, rhs=la[:, tt, :], start=True, stop=True))(tt),
                         reads=['la', 'Um'], writes=[('ps', 2)])
                    p.act(lambda e: e.activation(out=tmp256, in_=PS[:, 2, 0:256], func=AF.Exp), reads=[('ps', 2)], writes=['tmp256'])
                    p.dve((lambda tt: lambda e: e.tensor_tensor(out=kdec[:, tt, :], in0=gkt[:, tt, :], in1=tmp256, op=ALU.mult))(tt),
                          reads=['gkt', 'tmp256'], writes=['kdec'])
                p.act((lambda h: lambda e: e.activation(out=Sbf, in_=Sst[:, h, :, :], func=AF.Identity))(h), reads=['Sst'], writes=['Sbf'])
                for c in range(8):
                    tt, par = c // 2, c % 2
                    r0 = 64 * par
                    t0 = c * 64
                    rs = slice(r0, r0 + 64)
                    if main:
                        for dc in range(2):
                            p.pe((lambda dc, t0, rs: lambda e: e.matmul(PS[rs, 3, 0:64], lhsT=kinvT[:, dc, t0:t0 + 64], rhs=qdT[:, dc, t0:t0 + 64],
                                                                        start=(dc == 0), stop=(dc == 1)))(dc, t0, rs),
                                 reads=['kinvT', 'qdT'], writes=[('ps', 3)])
                        p.dve((lambda rs: lambda e: e.tensor_tensor(out=attn_sb[rs, :], in0=PS[rs, 3, 0:64], in1=Lm[rs, rs], op=ALU.mult))(rs),
                              reads=[('ps', 3), 'Lm'], writes=['attn_sb'])
                        p.pe((lambda rs, tt: lambda e: e.matmul(PS[rs, 6, :], lhsT=attn_sb[rs, :], rhs=gv[rs, tt, :], start=True, stop=False))(rs, tt),
                             reads=['attn_sb', 'gv'], writes=[('ps', 6)])
                        for dc in range(2):
                            p.pe((lambda dc, t0, rs: lambda e: e.matmul(PS[rs, 6, :], lhsT=qdT[:, dc, t0:t0 + 64], rhs=Sbf[:, dc, :],
                                                                        start=False, stop=(dc == 1)))(dc, t0, rs),
                                 reads=['qdT', 'Sbf'], writes=[('ps', 6)])
                    for dc in range(2):
                        p.pe((lambda dc, rs, tt: lambda e: e.matmul(PS[:, 4 + dc, :], lhsT=kdec[rs, tt, dc * 128:(dc + 1) * 128], rhs=gv[rs, tt, :],
                                                                    start=True, stop=True))(dc, rs, tt),
                             reads=['kdec', 'gv'], writes=[('ps', 4 + dc)])
                        p.dve((lambda dc, c, h: lambda e: e.scalar_tensor_tensor(out=Sst[:, h, dc, :], in0=Sst[:, h, dc, :], scalar=dl[:, dc, c:c + 1],
                                                                                 in1=PS[:, 4 + dc, :], op0=ALU.mult, op1=ALU.add))(dc, c, h),
                              reads=['Sst', 'dl', ('ps', 4 + dc)], writes=['Sst'])
                    p.act((lambda h: lambda e: e.activation(out=Sbf, in_=Sst[:, h, :, :], func=AF.Identity))(h), reads=['Sst'], writes=['Sbf'])
                    if main and par == 1:
                        sc = small[:, 8:12]
                        p.act(lambda e: e.activation(out=ybf, in_=PS[:, 6, :], func=AF.Square, accum_out=sc[:, 0:1]), reads=[('ps', 6)], writes=['ybf', 'gsc'])
                        p.dve(lambda e: e.tensor_scalar(out=sc[:, 1:2], in0=sc[:, 0:1], scalar1=1.0 / 512, scalar2=EPS, op0=ALU.mult, op1=ALU.add),
                              reads=['gsc'], writes=['gsc'])
                        p.act(lambda e: e.activation(out=sc[:, 2:3], in_=sc[:, 1:2], func=AF.Sqrt), reads=['gsc'], writes=['gsc'])
                        p.dve(lambda e: e.reciprocal(out=sc[:, 3:4], in_=sc[:, 2:3]), reads=['gsc'], writes=['gsc'])
                        p.dve((lambda tt: lambda e: e.scalar_tensor_tensor(out=ybf, in0=PS[:, 6, :], scalar=sc[:, 3:4], in1=SG[:, tt, :],
                                                                           op0=ALU.mult, op1=ALU.mult))(tt), reads=[('ps', 6), 'gsc', 'SG'], writes=['ybf'])
                        pst = PS[:, 7, :].bitcast(BF16)
                        for j in range(4):
                            p.pe((lambda j: lambda e: e.transpose(pst[:, j * 128:(j + 1) * 128], ybf[:, j * 128:(j + 1) * 128], ident_b))(j),
                                 reads=['ybf', 'ident_b'], writes=[('ps', 7)])
                        p.act((lambda h, tt: lambda e: e.activation(out=cT[:, 16 + h * 4:16 + h * 4 + 4, tt * 128:(tt + 1) * 128],
                                                                    in_=pst[:, 0:512].rearrange("p (j q) -> p j q", j=4), func=AF.Identity))(h, tt),
                              reads=[('ps', 7)], writes=[('cT', 16 + h * 4 + jj) for jj in range(4)])
                if not main and mi == 1:
                    p.dve((lambda h: lambda e: e.tensor_scalar(out=Sst[:, h, :, :], in0=Sst[:, h, :, :], scalar1=flag[:, 0:1], scalar2=None, op0=ALU.mult))(h),
                          reads=['Sst', 'flag'], writes=['Sst'])
            p.barrier()
            if stop == 'gla%s%d' % (kind, mi):
                p.emit(st)
                return nc, p
            if 'cT_gla' in dbg and main and mi == 0:
                p.dve(lambda e: e.tensor_copy(out=eb[:, 0, :], in_=cT[:, 16, :]), writes=['dbgtmp'])
                p.dma('sp', 'dbg', lambda e: e.dma_start(out=dbg_out['cT_gla'], in_=eb[:, 0, :]), reads=['dbgtmp'], writes=['dbg_ct2'])
                p.barrier()
            if not main:
                continue
            build_rowbcast(G1B, gate1, 'G1B')
            cT_keys = [('cT', c) for c in range(KC)]
            xstate = {}

            def ev_o(c0, n, tt, b):
                cg = c0 // 256
                xr = xres[cg % 2]
                xk = ('xres', cg % 2)
                if tt == 0:
                    srcap = x_main[mi * MT:(mi + 1) * MT, c0:c0 + 256].rearrange("(t p) c -> p t c", p=128)
                    p.dma('sp', xk, (lambda xr, srcap: lambda e: e.dma_start(out=xr, in_=srcap))(xr, srcap), writes=[xk])
                p.dve((lambda c0, b: lambda e: e.tensor_tensor(out=tmp256, in0=PS[:, b, 0:256], in1=G1B[:, c0:c0 + 256], op=ALU.mult))(c0, b),
                      reads=[('ps', b), 'G1B'], writes=['tmp256'])
                p.dve((lambda xr, tt: lambda e: e.tensor_tensor(out=xr[:, tt, :], in0=xr[:, tt, :], in1=tmp256, op=ALU.add))(xr, tt),
                      reads=['tmp256', xk], writes=[xk])
                if tt == 3:
                    dstap = x1s[mi * MT:(mi + 1) * MT, c0:c0 + 256].rearrange("(t p) c -> p t c", p=128)
                    p.dma('sp', ('xst', cg % 2), (lambda xr, dstap: lambda e: e.dma_start(out=dstap, in_=xr))(xr, dstap),
                          reads=[xk], writes=[xk, 'x1s'])
            gemm_tm(wout_v, 0, D, cT, MT, ev_o, cT_keys)
            p.barrier()
        if upto <= 2:
            p.emit(st)
            return nc, p

        wq_v = wq_d.rearrange("(k p) c -> p k c", p=128)
        ut_v = ut_d.rearrange("(k p) e -> p k e", p=128)
        h2T = A.view(R_HT, [32, TOK], BF16)
        h2_keys = [('hT', c) for c in range(KC)]
        rms_norm_to_hT(x1s, 0, TOK // 128, a2, s2, h2T, R_W, R_W + 32768)
        p.barrier()
        qT = A.view(R_S, [16, TOK], BF16)
        RZB = A.view(R_S + 32768, [8, TOK], BF16)

        def ev_pq(ci, b, h0):
            p.act((lambda ci, b, h0: lambda e: e.activation(out=qT[:, ci, h0:h0 + 512], in_=PS[:, b, :], func=AF.Identity))(ci, b, h0),
                  reads=[('ps', b)], writes=['qT'])
        gemm_fm(wq_v, 0, 2048, h2T, TOK, ev_pq, h2_keys)
        p.barrier()
        wo = [R_W]

        def walloc(shape, dt):
            esz = 4 if dt == F32 else 2
            nb = (int(np.prod(shape)) * esz + 31) // 32 * 32
            v = A.view(wo[0], shape, dt)
            wo[0] += nb
            assert wo[0] <= R_W + 49152, wo[0]
            return v
        ut = [walloc([32, 256], BF16) for _ in range(2)]
        keysT = walloc([16, 128], BF16)
        THR = walloc([TOK], BF16)
        RZR = walloc([TOK], BF16)
        SEL = walloc([8, 128], BF16)
        scs = A.view(P_DYN + 16384, [16, 128], F32)
        qo = [P_DYN]

        def qalloc(shape, dt):
            esz = 4 if dt == F32 else 2
            nb = (int(np.prod(shape)) * esz + 31) // 32 * 32
            v = A.view(qo[0], shape, dt)
            qo[0] += nb
            assert qo[0] <= R_P + 36864, qo[0]
            return v
        t16 = qalloc([16, 16], F32)
        tmpk = qalloc([128], F32)
        cand = qalloc([16, 16], F32)
        tmpc = qalloc([256], F32)
        c16 = qalloc([8, 16], F32)
        tsc = qalloc([8, 8], F32)
        thrTM = qalloc([24], BF16)
        rzTM = qalloc([8], BF16)
        junk16 = qalloc([16], F32)
        p.dma('pool', 'peer_c', lambda e: e.dma_start(out=keysT, in_=keyst_d.rearrange("p (a k) -> p a k", a=16)), writes=['keysT'])
        p.dma('pool', 'peer_c', lambda e: e.dma_start(out=SEL[0:24, :, :], in_=sel_d.rearrange("p (a k) -> p a k", a=8)), writes=['SEL'])
        p.barrier()
        for tt in range(8):
            for hp in range(16):
                b = 4 + hp // 4
                p.pe((lambda hp, b, tt: lambda e: e.matmul(PS[:, b, (hp % 4) * 128:(hp % 4 + 1) * 128], lhsT=qT[:, hp, tt * 128:(tt + 1) * 128],
                                                           rhs=keysT[:, hp, :], start=True, stop=True))(hp, b, tt),
                     reads=['qT', 'keysT'], writes=[('ps', b)])
            p.act(lambda e: e.activation(out=scs, in_=PS[:, 4:8, :].rearrange("p b (c k) -> p (b c) k", c=4), func=AF.Identity),
                  reads=[('ps', 4), ('ps', 5), ('ps', 6), ('ps', 7)], writes=['scs'])
            for hp in range(16):
                p.dve((lambda hp: lambda e: e.max(out=t16[:, hp, 0:8], in_=scs[:, hp, :]))(hp), reads=['scs'], writes=['t16'])
                p.dve((lambda hp: lambda e: e.match_replace(out=tmpk, in_to_replace=t16[:, hp, 0:8], in_values=scs[:, hp, :], imm_value=-1e30))(hp),
                      reads=['scs', 't16'], writes=['tmpk'])
                p.dve((lambda hp: lambda e: e.max(out=t16[:, hp, 8:16], in_=tmpk))(hp), reads=['tmpk'], writes=['t16'])
            for h in range(8):
                p.dve((lambda h: lambda e: e.tensor_tensor(out=cand, in0=t16[:, 2 * h, :].unsqueeze(2).to_broadcast([128, 16, 16]),
                                                           in1=t16[:, 2 * h + 1, :].unsqueeze(1).to_broadcast([128, 16, 16]), op=ALU.add))(h),
                      reads=['t16'], writes=['cand'])
                cf = cand.rearrange("p a b -> p (a b)")
                p.dve((lambda h: lambda e: e.max(out=c16[:, h, 0:8], in_=cf))(h), reads=['cand'], writes=['c16'])
                p.dve((lambda h: lambda e: e.match_replace(out=tmpc, in_to_replace=c16[:, h, 0:8], in_values=cf, imm_value=-1e30))(h),
                      reads=['cand', 'c16'], writes=['tmpc'])
                p.dve((lambda h: lambda e: e.max(out=c16[:, h, 8:16], in_=tmpc))(h), reads=['tmpc'], writes=['c16'])
                p.dve((lambda h: lambda e: e.tensor_scalar(out=tsc[:, h, 0:1], in0=c16[:, h, 15:16], scalar1=-1.0, scalar2=3e-5, op0=ALU.mult, op1=ALU.add))(h),
                      reads=['c16'], writes=['tsc'])
                p.act((lambda h: lambda e: e.activation(out=junk16, in_=c16[:, h, :], func=AF.Exp, bias=tsc[:, h, 0:1], accum_out=tsc[:, h, 1:2]))(h),
                      reads=['c16', 'tsc'], writes=['tsc', 'junk16'])
                p.dve((lambda h: lambda e: e.reciprocal(out=tsc[:, h, 2:3], in_=tsc[:, h, 1:2]))(h), reads=['tsc'], writes=['tsc'])
                p.dve((lambda h: lambda e: e.tensor_copy(out=thrTM[:, h:h + 1], in_=tsc[:, h, 0:1]))(h), reads=['tsc'], writes=['thrTM'])
                p.dve((lambda h: lambda e: e.tensor_tensor(out=tsc[:, h, 3:4], in0=tsc[:, h, 0:1], in1=thrTM[:, h:h + 1], op=ALU.subtract))(h),
                      reads=['tsc', 'thrTM'], writes=['tsc'])
                p.dve((lambda h: lambda e: e.tensor_copy(out=thrTM[:, 8 + h:9 + h], in_=tsc[:, h, 3:4]))(h), reads=['tsc'], writes=['thrTM'])
                p.dve((lambda h: lambda e: e.tensor_tensor(out=tsc[:, h, 4:5], in0=tsc[:, h, 3:4], in1=thrTM[:, 8 + h:9 + h], op=ALU.subtract))(h),
                      reads=['tsc', 'thrTM'], writes=['tsc'])
                p.dve((lambda h: lambda e: e.tensor_copy(out=thrTM[:, 16 + h:17 + h], in_=tsc[:, h, 4:5]))(h), reads=['tsc'], writes=['thrTM'])
                p.dve((lambda h: lambda e: e.tensor_copy(out=rzTM[:, h:h + 1], in_=tsc[:, h, 2:3]))(h), reads=['tsc'], writes=['rzTM'])
            pst = PS[:, 2, :].bitcast(BF16)
            p.pe(lambda e: e.transpose(pst[0:24, 0:128], thrTM, ident_b), reads=['thrTM', 'ident_b'], writes=[('ps', 2)])
            p.pe(lambda e: e.transpose(pst[0:8, 128:256], rzTM, ident_b), reads=['rzTM', 'ident_b'], writes=[('ps', 2)])
            p.act((lambda tt: lambda e: e.activation(out=THR[0:24, tt * 128:(tt + 1) * 128], in_=pst[0:24, 0:128], func=AF.Identity))(tt),
                  reads=[('ps', 2)], writes=['THR'])
            p.act((lambda tt: lambda e: e.activation(out=RZR[0:8, tt * 128:(tt + 1) * 128], in_=pst[0:8, 128:256], func=AF.Identity))(tt),
                  reads=[('ps', 2)], writes=['RZR'])
        for h in range(8):
            for hf in range(2):
                b = hf
                p.pe((lambda h, hf, b: lambda e: e.matmul(PS[:, b, :], lhsT=SEL[0:8, h, :], rhs=RZR[0:8, hf * 512:(hf + 1) * 512], start=True, stop=True))(h, hf, b),
                     reads=['SEL', 'RZR'], writes=[('ps', b)])
                p.act((lambda h, hf, b: lambda e: e.activation(out=RZB[:, h, hf * 512:(hf + 1) * 512], in_=PS[:, b, :], func=AF.Identity))(h, hf, b),
                      reads=[('ps', b)], writes=['RZB'])
        if 'thr' in dbg:
            dbt = A.view(P_DYN + 12288, [TOK], F32)
            p.act(lambda e: e.activation(out=dbt[0:24, :], in_=THR[0:24, :], func=AF.Identity), reads=['THR'], writes=['dbt'])
            p.dma('sp', 'dbg', lambda e: e.dma_start(out=dbg_out['thr'], in_=dbt[0:24, :]), reads=['dbt'], writes=['dbg_thr'])
            p.barrier()
            dbt2 = A.view(P_DYN + 12288, [TOK], F32)
            p.act(lambda e: e.activation(out=dbt2[0:8, :], in_=RZR[0:8, :], func=AF.Identity), reads=['RZR'], writes=['dbt'])
            p.dma('sp', 'dbg', lambda e: e.dma_start(out=dbg_out['rzr'], in_=dbt2[0:8, :]), reads=['dbt'], writes=['dbg_rzr'])
        p.barrier()
        if upto <= 3:
            p.emit(st)
            return nc, p
        qo[0] = P_DYN
        Eb = [qalloc([TOK], BF16) for _ in range(2)]
        Wm = [qalloc([TOK], BF16) for _ in range(2)]
        Wn = [qalloc([TOK], BF16) for _ in range(2)]
        Gl = qalloc([TOK], BF16)
        AW = [qalloc([TOK], BF16) for _ in range(2)]
        NE = 128 if upto > 4 else 2
        for i1 in range(NE):
            if i1 % 2 == 0:
                us = (i1 // 2) % 2
                p.dma('pool', ('U', us), (lambda us, i1: lambda e: e.dma_start(out=ut[us], in_=ut_v[:, :, i1 * 128:i1 * 128 + 256]))(us, i1),
                      writes=[('U', us)])
            us = (i1 // 2) % 2
            uk = ('U', us)
            ec = (i1 % 2) * 128
            for hf in range(2):
                for k in range(KC):
                    p.pe((lambda us, ec, hf, k: lambda e: e.matmul(PS[:, hf, :], lhsT=ut[us][:, k, ec:ec + 128], rhs=h2T[:, k, hf * 512:(hf + 1) * 512],
                                                                   start=(k == 0), stop=(k == KC - 1)))(us, ec, hf, k),
                         reads=[uk] + h2_keys, writes=[('ps', hf)])
            p.act(lambda e: e.activation(out=Gl.rearrange("p (b t) -> p b t", b=2), in_=PS[:, 0:2, :], func=AF.Gelu), reads=[('ps', 0), ('ps', 1)], writes=['Gl'])
            for h in range(8):
                ub = 2 + 2 * (h % 2)
                s = h % 2
                for hf in range(2):
                    ts = slice(hf * 512, (hf + 1) * 512)
                    p.pe((lambda h, ub, hf, ts: lambda e: e.matmul(PS[:, ub + hf, :], lhsT=keysT[:, 2 * h + 1, :], rhs=qT[:, 2 * h + 1, ts], start=True, stop=False))(h, ub, hf, ts),
                         reads=['qT', 'keysT'], writes=[('ps', ub + hf)])
                    p.pe((lambda h, ub, hf, ts, i1: lambda e: e.matmul(PS[:, ub + hf, :], lhsT=keysT[:, 2 * h, i1:i1 + 1].to_broadcast([128, 128]), rhs=qT[:, 2 * h, ts],
                                                                       start=False, stop=False))(h, ub, hf, ts, i1),
                         reads=['qT', 'keysT'], writes=[('ps', ub + hf)])
                    p.pe((lambda h, ub, hf, ts: lambda e: e.matmul(PS[:, ub + hf, :], lhsT=SEL[0:24, h, :], rhs=THR[0:24, ts], start=False, stop=True))(h, ub, hf, ts),
                         reads=['SEL', 'THR'], writes=[('ps', ub + hf)])
                p.act((lambda ub, s: lambda e: e.activation(out=Eb[s].rearrange("p (b t) -> p b t", b=2), in_=PS[:, ub:ub + 2, :], func=AF.Exp))(ub, s),
                      reads=[('ps', ub), ('ps', ub + 1)], writes=[('Eb', s)])
                p.dve((lambda ub, s: lambda e: e.scalar_tensor_tensor(out=Wm[s].rearrange("p (b t) -> p b t", b=2), in0=PS[:, ub:ub + 2, :], scalar=0.0, in1=Eb[s].rearrange("p (b t) -> p b t", b=2),
                                                                      op0=ALU.is_ge, op1=ALU.mult))(ub, s),
                      reads=[('ps', ub), ('ps', ub + 1), ('Eb', s)], writes=[('Wm', s)])
                p.pool((lambda s, h: lambda e: e.tensor_tensor(out=Wn[s], in0=Wm[s], in1=RZB[:, h, :], op=ALU.mult))(s, h),
                       reads=[('Wm', s), 'RZB'], writes=[('Wn', s)])
                for hf in range(2):
                    p.pe((lambda s, hf, h: lambda e: e.matmul(PS[:, 6 + hf, :], lhsT=ident_b, rhs=Wn[s][:, hf * 512:(hf + 1) * 512], start=(h == 0), stop=(h == 7)))(s, hf, h),
                         reads=[('Wn', s), 'ident_b'], writes=[('ps', 6 + hf)])
            a = i1 % 2
            p.dve((lambda a: lambda e: e.tensor_tensor(out=AW[a].rearrange("p (b t) -> p b t", b=2), in0=PS[:, 6:8, :], in1=Gl.rearrange("p (b t) -> p b t", b=2), op=ALU.mult))(a),
                  reads=[('ps', 6), ('ps', 7), 'Gl'], writes=[('AW', a)])
            p.dma('sp', ('AWst', a), (lambda a, i1: lambda e: e.dma_start(out=aws[i1], in_=AW[a]))(a, i1), reads=[('AW', a)], writes=[('AW', a), 'aws'])
        if 'aw0' in dbg:
            p.barrier()
            dbt = A.view(R_HT, [TOK], F32)
            p.act(lambda e: e.activation(out=dbt, in_=AW[0], func=AF.Identity), writes=['dbt'])
            p.dma('sp', 'dbg', lambda e: e.dma_start(out=dbg_out['aw0'], in_=dbt), reads=['dbt'], writes=['dbg_aw0'])
        p.barrier()
        if upto <= 4:
            p.emit(st)
            return nc, p
        acc = A.view(0, [8, D], F32)
        vt = [A.view(131072 + i * 16384, [2, D], BF16) for i in range(2)]
        awt = [A.view(P_DYN + i * 4096, [2, TOK], BF16) for i in range(2)]
        v_v = v_d.rearrange("(g a p) d -> g p a d", a=2, p=128)
        aws_v = aws.rearrange("(g a) p t -> g p a t", a=2)
        nb_ = [0]
        for g in range(64):
            s = g % 2
            for a in range(2):
                p.dma('pool', ('V', s, a), (lambda s, g, a: lambda e: e.dma_start(out=vt[s][:, a, :], in_=v_v[g][:, a, :], max_dma_last_dim=8192))(s, g, a), writes=[('V', s, a)])
            p.dma('sp', ('AWld', s), (lambda s, g: lambda e: e.dma_start(out=awt[s], in_=aws_v[g]))(s, g), reads=['aws'], writes=[('AWl', s)])
            for tt in range(8):
                for dg in range(8):
                    b = nb_[0] % 8
                    nb_[0] += 1
                    for a in range(2):
                        p.pe((lambda s, a, tt, dg, b: lambda e: e.matmul(PS[:, b, :], lhsT=awt[s][:, a, tt * 128:(tt + 1) * 128], rhs=vt[s][:, a, dg * 512:(dg + 1) * 512],
                                                                         start=(a == 0), stop=(a == 1)))(s, a, tt, dg, b),
                             reads=[('V', s, a), ('AWl', s)], writes=[('ps', b)])
                    if g == 0:
                        p.dve((lambda tt, dg, b: lambda e: e.tensor_copy(out=acc[:, tt, dg * 512:(dg + 1) * 512], in_=PS[:, b, :]))(tt, dg, b),
                              reads=[('ps', b)], writes=[('acc', tt, dg)])
                    else:
                        p.dve((lambda tt, dg, b: lambda e: e.tensor_tensor(out=acc[:, tt, dg * 512:(dg + 1) * 512], in0=PS[:, b, :], in1=acc[:, tt, dg * 512:(dg + 1) * 512], op=ALU.add))(tt, dg, b),
                              reads=[('ps', b), ('acc', tt, dg)], writes=[('acc', tt, dg)])
        p.barrier()
        G2B = A.view(131072, [D], F32)
        FGB = A.view(131072 + 16384, [D], F32)
        x1t = A.view(P_DYN + 1024, [D], F32)
        dtmp2 = A.view(P_DYN, [128], F32)

        for c in range(KC):
            b = 2 + (c // 4) % 2
            q = c % 4
            p.dve((lambda c: lambda e: e.tensor_scalar(out=dtmp2, in0=ident_f, scalar1=gate2[:, c:c + 1], scalar2=None, op0=ALU.mult))(c),
                  reads=['ident_f', 'modT'], writes=['dtmp2'])
            p.pe((lambda b, q: lambda e: e.matmul(PS[:, b, q * 128:(q + 1) * 128], lhsT=ones_f, rhs=dtmp2, start=True, stop=True))(b, q),
                 reads=['dtmp2', 'ones_f'], writes=[('ps', b)])
            if q == 3:
                p.act((lambda b, c: lambda e: e.activation(out=G2B[:, (c - 3) * 128:(c + 1) * 128], in_=PS[:, b, :], func=AF.Identity))(b, c),
                      reads=[('ps', b)], writes=['G2B'])
        p.dma('sp', 'fgb', lambda e: e.dma_start(out=FGB, in_=fg_d.partition_broadcast(128)[:, 0, :]), writes=['FGB'])
        for tt in range(8):
            sc = small[:, 16:20]
            p.dma('sp', 'x1ld', (lambda tt: lambda e: e.dma_start(out=x1t, in_=x1s[tt * 128:(tt + 1) * 128, :]))(tt), reads=['x1s'], writes=['x1t'])
            p.dve((lambda tt: lambda e: e.tensor_tensor(out=acc[:, tt, :], in0=acc[:, tt, :], in1=G2B, op=ALU.mult))(tt), reads=['G2B'], writes=[('accf', tt)])
            p.pool((lambda tt: lambda e: e.tensor_tensor(out=x1t, in0=x1t, in1=acc[:, tt, :], op=ALU.add))(tt), reads=[('accf', tt), 'x1t'], writes=['x1t'])
            p.act(lambda e: e.activation(out=PS[:, :, :], in_=x1t.rearrange("p (b t) -> p b t", b=8), func=AF.Square, accum_out=sc[:, 0:1]), reads=['x1t'], writes=['junkf', 'fsc'])
            p.dve(lambda e: e.tensor_scalar(out=sc[:, 1:2], in0=sc[:, 0:1], scalar1=1.0 / D, scalar2=EPS, op0=ALU.mult, op1=ALU.add), reads=['fsc'], writes=['fsc'])
            p.act(lambda e: e.activation(out=sc[:, 2:3], in_=sc[:, 1:2], func=AF.Sqrt), reads=['fsc'], writes=['fsc'])
            p.dve(lambda e: e.reciprocal(out=sc[:, 3:4], in_=sc[:, 2:3]), reads=['fsc'], writes=['fsc'])
            p.dve(lambda e: e.scalar_tensor_tensor(out=x1t, in0=x1t, scalar=sc[:, 3:4], in1=FGB, op0=ALU.mult, op1=ALU.mult), reads=['x1t', 'fsc', 'FGB'], writes=['x1t'])
            p.dma('sp', 'ost', (lambda tt: lambda e: e.dma_start(out=out_d[tt * 128:(tt + 1) * 128, :], in_=x1t))(tt), reads=['x1t'], writes=['x1t', 'out'])
        p.barrier()
        p.emit(st)
    return nc, p


def t5_bucket_np(dist):
    max_exact = 16
    d = np.maximum(dist, 0)
    lr = np.log(np.maximum(d, 1).astype(np.float32) / max_exact) / math.log(128 / max_exact)
    large = max_exact + (lr * (32 - max_exact)).astype(np.int32)
    large = np.minimum(large, 31)
    return np.where(d < max_exact, d, large)


def make_in_maps(inputs, cores=range(8)):
    f = lambda a: np.ascontiguousarray(np.asarray(a, dtype=np.float32))
    x = f(inputs["x"]); c = f(inputs["c"])
    fm = lambda v, n: np.ascontiguousarray(v.reshape(n, 128).T)
    onehot = np.zeros((32, 128), np.float32)
    onehot[t5_bucket_np(np.arange(128)), np.arange(128)] = 1.0
    sel = np.zeros((24, 8, 128), np.float32)
    for h in range(8):
        for part in range(3):
            sel[part * 8 + h, h, :] = 1.0
    keys = f(inputs["peer_keys"])[0]
    keys_t = np.ascontiguousarray(keys.transpose(3, 0, 1, 2).reshape(128, 16 * 128))
    shared = {
        "w_ada": f(inputs["w_ada"])[0],
        "b_ada_t": fm(f(inputs["b_ada"])[0], 192),
        "g1_t": fm(f(inputs["norm1_g"])[0], 32),
        "g2_t": fm(f(inputs["norm2_g"])[0], 32),
        "w_in": f(inputs["w_in"])[0],
        "sinks": f(inputs["attn_sinks"])[0].reshape(1, 16),
        "rel_bias": f(inputs["rel_bias"]),
        "onehot": onehot,
        "gk2_w": f(inputs["gla_w_gk2"])[0],
        "gk2_b": f(inputs["gla_b_gk2"])[0].reshape(1, 1024),
        "gla_norm_g": f(inputs["gla_norm_g"])[0].reshape(1, 512),
        "w_out": f(inputs["w_out"])[0],
        "w_q": f(inputs["peer_w_q"])[0],
        "keys_t": keys_t,
        "u_t": np.ascontiguousarray(f(inputs["peer_u"])[0].T),
        "v": f(inputs["peer_v"])[0],
        "final_g": f(inputs["final_g"]).reshape(1, D),
        "sel": sel.reshape(24, 8 * 128),
    }
    maps = []
    for i in cores:
        b, half = i // 2, i % 2
        m = dict(shared)
        m["x_main"] = np.ascontiguousarray(x[b, half * TOK:(half + 1) * TOK])
        m["x_pre"] = np.ascontiguousarray(x[b, 0:TOK])
        m["flag"] = np.full((128, 1), float(half), np.float32)
        m["c_t"] = fm(c[b], 32)
        maps.append(m)
    return maps


_CACHE = {}


def kernel(**inputs):
    if "nc" not in _CACHE:
        _CACHE["nc"] = build_program()[0]
    nc = _CACHE["nc"]
    maps = make_in_maps(inputs)
    res = run_bass_kernel_spmd(nc, maps, core_ids=list(range(8)))
    out = np.empty((4, 2048, D), np.float32)
    for i in range(8):
        b, half = i // 2, i % 2
        out[b, half * TOK:(half + 1) * TOK] = res.results[i]["out"]
    return out
```

```python
from contextlib import ExitStack
import math
import numpy as np
import concourse.bass as bass
import concourse.mybir as mybir
from concourse.bass_utils import run_bass_kernel_spmd

F32 = mybir.dt.float32
BF16 = mybir.dt.bfloat16
AF = mybir.ActivationFunctionType
ALU = mybir.AluOpType

SAME_ENG_SYNC = True

D = 4096
KC = 32
TOK = 1024
MT = 512
NEXP = 16384
EPS = 1e-6
NEGM = -30000.0
IN_W = 8720
C_AQ, C_AK, C_AV, C_GQ, C_GK, C_GV, C_GLOW, C_GOUT = 0, 2048, 2304, 2560, 3584, 4608, 6656, 6672


class Prog:
    def __init__(self, nc):
        self.nc = nc
        self.ins = []
        self.last_w = {}
        self.readers = {}
        self.last_on = {}
        self.dmas_open = []

    maxops = None

    def op(self, eng, fn, reads=(), writes=(), dma=None, extra_deps=(), force=False):
        if self.maxops is not None and len(self.ins) >= self.maxops and not force:
            return None
        i = len(self.ins)
        deps = set(extra_deps)
        psk = [k for k in reads if k == 'ps0' or (isinstance(k, tuple) and k[0] == 'ps')]
        if psk:
            reads = [k for k in reads if k not in psk]
            writes = list(writes) + [k for k in psk if k not in writes]
        for k in reads:
            w = self.last_w.get(k)
            if w is not None:
                deps.add(w)
        for k in writes:
            w = self.last_w.get(k)
            if w is not None:
                deps.add(w)
            for r in self.readers.get(k, ()):
                deps.add(r)
        for k in reads:
            lst = self.readers.setdefault(k, [])
            if dma is None:
                for q in range(len(lst)):
                    J = self.ins[lst[q]]
                    if J['dma'] is None and J['eng'] == eng:
                        lst[q] = i
                        break
                else:
                    lst.append(i)
            else:
                lst.append(i)
        for k in writes:
            self.last_w[k] = i
            self.readers[k] = []
        deps.discard(i)
        self.ins.append(dict(eng=eng, fn=fn, deps=deps, dma=dma))
        if dma is None:
            self.last_on[eng] = i
        else:
            self.dmas_open.append(i)
        return i

    def pe(self, fn, reads=(), writes=()):
        return self.op('pe', fn, reads, writes)

    def act(self, fn, reads=(), writes=()):
        return self.op('act', fn, reads, writes)

    def dve(self, fn, reads=(), writes=()):
        return self.op('dve', fn, reads, writes)

    def pool(self, fn, reads=(), writes=()):
        return self.op('pool', fn, reads, writes)

    def dma(self, eng, key, fn, reads=(), writes=()):
        return self.op(eng, fn, reads, writes, dma=key)

    def barrier(self):
        deps = set(self.last_on.values()) | set(self.dmas_open)
        self.dmas_open = []
        for e in ['pe', 'act', 'dve', 'pool', 'sp']:
            self.op(e, lambda eng: eng.nop(), extra_deps=deps, force=True)
        self.last_w = {}
        self.readers = {}

    def emit(self, stack):
        nc = self.nc
        ins = self.ins
        n = len(ins)
        engs = ['pe', 'act', 'dve', 'pool', 'sp']

        def needs_wait(I, Dd):
            if Dd['dma'] is not None:
                return True
            if Dd['eng'] == I['eng'] and I['dma'] is None:
                if Dd['eng'] in ('pe', 'sp') or not SAME_ENG_SYNC:
                    return False
            return True

        need_sig = [False] * n
        for i, I in enumerate(ins):
            for d in I['deps']:
                Dd = ins[d]
                if Dd['dma'] is None and needs_wait(I, Dd):
                    need_sig[d] = True
        cnt = {e: 0 for e in engs}
        sigval = [0] * n
        dma_cnt = {}
        for i, I in enumerate(ins):
            if I['dma'] is not None:
                k = I['dma']
                dma_cnt[k] = dma_cnt.get(k, 0) + 16
                sigval[i] = dma_cnt[k]
            elif need_sig[i]:
                cnt[I['eng']] += 1
                sigval[i] = cnt[I['eng']]
        esem = {e: stack.enter_context(nc.semaphore('s_' + e)) for e in engs}
        dsem = {k: stack.enter_context(nc.semaphore('d_%d' % j)) for j, k in enumerate(dma_cnt)}
        self.stats = dict(n=n, cnt=dict(cnt), ndsem=len(dsem), maxdma=max(dma_cnt.values()) if dma_cnt else 0)
        per_eng = {e: [] for e in engs}
        for i, I in enumerate(ins):
            per_eng[I['eng']].append(i)

        def run(e, engobj):
            waited = {}
            for i in per_eng[e]:
                I = ins[i]
                need = {}
                for d in I['deps']:
                    Dd = ins[d]
                    if not needs_wait(I, Dd):
                        continue
                    if Dd['dma'] is not None:
                        key = ('d', Dd['dma'])
                    else:
                        key = ('e', Dd['eng'])
                    v = sigval[d]
                    if need.get(key, 0) < v:
                        need[key] = v
                for key, v in need.items():
                    if waited.get(key, 0) >= v:
                        continue
                    waited[key] = v
                    s = dsem[key[1]] if key[0] == 'd' else esem[key[1]]
                    engobj.wait_ge(s, v)
                r = I['fn'](engobj)
                if I['dma'] is not None:
                    r.then_inc(dsem[I['dma']], 16)
                elif need_sig[i]:
                    r.then_inc(esem[e], 1)

        block = stack.enter_context(nc.Block())
        block.sync(lambda eng: run('sp', eng))
        block.tensor(lambda eng: run('pe', eng))
        block.scalar(lambda eng: run('act', eng))
        block.vector(lambda eng: run('dve', eng))
        block.gpsimd(lambda eng: run('pool', eng))


class Arena:
    def __init__(self, nc, nbytes):
        self.nbytes = nbytes
        self.t = nc.alloc_sbuf_tensor("arena", [128, nbytes // 4], F32)

    def view(self, off, shape, dtype):
        esz = 4 if dtype == F32 else 2
        nel = int(np.prod(shape))
        nb = nel * esz
        assert off % 4 == 0 and nb % 4 == 0, (off, shape)
        assert off + nb <= self.nbytes, (off, nb, self.nbytes)
        ap = self.t[:, off // 4: (off + nb) // 4]
        if dtype != F32:
            ap = ap.bitcast(dtype)
        if len(shape) == 2:
            names = "a b"
            ap = ap.rearrange("p (a b) -> p a b", a=shape[0], b=shape[1])
        elif len(shape) == 3:
            ap = ap.rearrange("p (a b c) -> p a b c", a=shape[0], b=shape[1], c=shape[2])
        return ap


def build_program(dbg=None, upto=99, stop=None, fake_mod=False):
    nc = bass.Bass("TRN2", target_bir_lowering=False)
    dbg = dbg or {}

    def din(name, shape, dt=F32):
        return nc.dram_tensor(name, list(shape), dt, kind="ExternalInput").ap()

    x_main = din("x_main", [TOK, D])
    x_pre = din("x_pre", [TOK, D])
    flag_d = din("flag", [128, 1])
    c_d = din("c_t", [128, KC])
    wada_d = din("w_ada", [D, 6 * D]) if not fake_mod else None
    bada_d = din("b_ada_t", [128, 6 * KC])
    g1_d = din("g1_t", [128, KC])
    g2_d = din("g2_t", [128, KC])
    win_d = din("w_in", [D, IN_W])
    sinks_d = din("sinks", [1, 16])
    relb_d = din("rel_bias", [32, 16])
    oh_d = din("onehot", [32, 128])
    gk2w_d = din("gk2_w", [16, 1024])
    gk2b_d = din("gk2_b", [1, 1024])
    gng_d = din("gla_norm_g", [1, 512])
    wout_d = din("w_out", [D, D])
    if upto > 2:
        wq_d = din("w_q", [D, 2048])
        keyst_d = din("keys_t", [128, 16 * 128])
        ut_d = din("u_t", [D, NEXP])
        v_d = din("v", [NEXP, D])
        fg_d = din("final_g", [1, D])
        sel_d = din("sel", [24, 8 * 128])
    if fake_mod:
        modt_d = din("modT_dbg", [128, 192])
    out_d = nc.dram_tensor("out", [TOK, D], F32, kind="ExternalOutput").ap()
    x1s = nc.dram_tensor("x1s", [TOK, D], F32).ap()
    aws = nc.dram_tensor("aws", [128, 128, TOK], BF16).ap()
    gext = nc.dram_tensor("gext", [16, 384], F32).ap()
    dbg_out = {}
    for name, shape in dbg.items():
        dbg_out[name] = nc.dram_tensor("dbg_" + name, list(shape), F32, kind="ExternalOutput").ap()

    st = ExitStack()
    with st:
        A = Arena(nc, 200 * 1024)
        PS = nc.alloc_psum_tensor("ps", [128, 8, 512], F32)
        p = Prog(nc)
        import os
        if os.environ.get('K_MAXOPS'):
            p.maxops = int(os.environ['K_MAXOPS'])
        R_HT, R_CT, R_W, R_S, R_P = 0, 32768, 65536, 65536 + 49152, 65536 + 2 * 49152
        po = [R_P]

        def palloc(shape, dt):
            esz = 4 if dt == F32 else 2
            nb = int(np.prod(shape)) * esz
            nb = (nb + 31) // 32 * 32
            v = A.view(po[0], shape, dt)
            po[0] += nb
            return v

        ident_f = A.view(po[0], [128], F32); po[0] += 512
        ident_b = A.view(po[0], [128], BF16); po[0] += 256
        ones_b = A.view(po[0], [128], BF16); po[0] += 256
        ones_f = A.view(po[0], [128], F32); po[0] += 512
        Lm = A.view(po[0], [128], F32); po[0] += 512
        Um = A.view(po[0], [128], F32); po[0] += 512
        modT = A.view(po[0], [192], F32); po[0] += 768
        badat = A.view(po[0], [192], F32); po[0] += 768
        a1 = A.view(po[0], [32], F32); po[0] += 128
        a2 = A.view(po[0], [32], F32); po[0] += 128
        g1t = A.view(po[0], [32], F32); po[0] += 128
        g2t = A.view(po[0], [32], F32); po[0] += 128
        ct = A.view(po[0], [32], F32); po[0] += 128
        cact = A.view(po[0], [32], BF16); po[0] += 64
        flag = A.view(po[0], [8], F32); po[0] += 32
        small = A.view(po[0], [64], F32); po[0] += 256
        esink = A.view(po[0], [16], F32); po[0] += 64
        gnb = A.view(po[0], [512], F32); po[0] += 2048
        gk2w = A.view(po[0], [1024], BF16); po[0] += 2048
        gk2b = A.view(po[0], [1024], BF16); po[0] += 2048
        P_DYN = po[0]
        bcur = A.view(po[0], [16, 128], BF16); po[0] += 4096
        bprev = A.view(po[0], [16, 128], BF16); po[0] += 4096
        Sst = A.view(po[0], [4, 2, 512], F32); po[0] += 16384
        assert po[0] <= 200 * 1024, po[0]

        s1 = modT[:, 0:32]
        gate1 = modT[:, 64:96]
        s2 = modT[:, 96:128]
        gate2 = modT[:, 160:192]

        def bank(b, n=512):
            return PS[:, b, 0:n]

        p.pool(lambda e: e.memset(ident_f, 1.0), writes=['ident_f'])
        p.pool(lambda e: e.affine_select(out=ident_f, in_=ident_f, pattern=[[-1, 128]], compare_op=ALU.is_equal,
                                         fill=0.0, base=0, channel_multiplier=1), reads=['ident_f'], writes=['ident_f'])
        p.pool(lambda e: e.memset(ones_f, 1.0), writes=['ones_f'])
        p.pool(lambda e: e.memset(ones_b, 1.0), writes=['ones_b'])
        p.dve(lambda e: e.tensor_copy(out=ident_b, in_=ident_f), reads=['ident_f'], writes=['ident_b'])
        p.pool(lambda e: e.memset(Lm, 1.0), writes=['Lm'])
        p.pool(lambda e: e.affine_select(out=Lm, in_=Lm, pattern=[[1, 128]], compare_op=ALU.is_ge,
                                         fill=0.0, base=0, channel_multiplier=-1), reads=['Lm'], writes=['Lm'])
        p.pool(lambda e: e.memset(Lm[0:64, 64:128], 0.0), reads=['Lm'], writes=['Lm'])
        p.pool(lambda e: e.memset(Um, 1.0), writes=['Um'])
        p.pool(lambda e: e.affine_select(out=Um, in_=Um, pattern=[[-1, 128]], compare_op=ALU.is_gt,
                                         fill=0.0, base=0, channel_multiplier=1), reads=['Um'], writes=['Um'])
        p.pool(lambda e: e.memset(Um[64:128, 0:64], 0.0), reads=['Um'], writes=['Um'])
        p.pool(lambda e: e.memset(Sst, 0.0), writes=['Sst'])

        sm = 'small_ld'
        p.dma('sp', sm, lambda e: e.dma_start(out=ct, in_=c_d), writes=['ct'])
        p.dma('sp', sm, lambda e: e.dma_start(out=badat, in_=bada_d), writes=['badat'])
        p.dma('sp', sm, lambda e: e.dma_start(out=g1t, in_=g1_d), writes=['g1t'])
        p.dma('sp', sm, lambda e: e.dma_start(out=g2t, in_=g2_d), writes=['g2t'])
        p.dma('sp', sm, lambda e: e.dma_start(out=flag[:, 0:1], in_=flag_d), writes=['flag'])
        p.dma('sp', sm, lambda e: e.dma_start(out=esink, in_=sinks_d.partition_broadcast(128)[:, 0, :]), writes=['esink'])
        p.dma('sp', sm, lambda e: e.dma_start(out=gnb, in_=gng_d.partition_broadcast(128)[:, 0, :]), writes=['gnb'])
        p.dma('pool', 'small_ld2', lambda e: e.dma_start(out=gk2w[0:16, :], in_=gk2w_d), writes=['gk2w'])
        p.dma('pool', 'small_ld2', lambda e: e.dma_start(out=gk2b[0:1, :], in_=gk2b_d), writes=['gk2b'])
        relb = A.view(R_S, [16], F32)
        ohs = A.view(R_S + 64, [128], F32)
        gx = A.view(R_S + 1024, [384], F32)
        p.dma('sp', sm, lambda e: e.dma_start(out=relb[0:32, :], in_=relb_d), writes=['relb'])
        p.dma('sp', sm, lambda e: e.dma_start(out=ohs[0:32, :], in_=oh_d), writes=['ohs'])
        p.barrier()
        p.act(lambda e: e.activation(out=esink, in_=esink, func=AF.Exp), reads=['esink'], writes=['esink'])
        p.pe(lambda e: e.matmul(PS[0:16, 0, 0:128], lhsT=relb[0:32, :], rhs=ohs[0:32, :], start=True, stop=True),
             reads=['relb', 'ohs'], writes=['ps0'])
        p.pool(lambda e: e.memset(gx[0:16, :], NEGM), writes=['gx'])
        p.dve(lambda e: e.tensor_copy(out=gx[0:16, 128:256], in_=PS[0:16, 0, 0:128]), reads=['ps0', 'gx'], writes=['gx'])
        p.dma('sp', 'gx', lambda e: e.dma_start(out=gext, in_=gx[0:16, :]), reads=['gx'], writes=['gext'])
        p.barrier()
        bcf = A.view(R_S + 4096, [16, 128], F32)
        bpf = A.view(R_S + 4096 + 8192, [16, 128], F32)
        for k in range(128):
            p.dma('sp', 'bias_ld', (lambda k: lambda e: e.dma_start(out=bcf[k:k + 1, :, :], in_=gext[:, 128 - k:256 - k].unsqueeze(0)))(k),
                  writes=[('bcf', k)])
            p.dma('sp', 'bias_ld', (lambda k: lambda e: e.dma_start(out=bpf[k:k + 1, :, :], in_=gext[:, 256 - k:384 - k].unsqueeze(0)))(k),
                  writes=[('bpf', k)])
        p.barrier()
        p.dve(lambda e: e.tensor_copy(out=bcur, in_=bcf), writes=['bcur'])
        p.dve(lambda e: e.tensor_copy(out=bprev, in_=bpf), writes=['bprev'])
        if 'bcur' in dbg:
            p.dma('sp', 'dbg', lambda e: e.dma_start(out=dbg_out['bcur'], in_=bcf), reads=['bcur'], writes=['dbg_bcur'])
        p.barrier()

        p.act(lambda e: e.activation(out=cact, in_=ct, func=AF.Silu), writes=['cact'])
        wada_v = wada_d.rearrange("(k p) c -> p k c", p=128) if not fake_mod else None
        PS_fake = A.view(R_S, [192], F32)
        Wt = [A.view(R_W + i * 16384, [32, 256], BF16) for i in range(3)]
        wctr = [0]

        def load_w(view, c0, ncols):
            slot = wctr[0] % 3
            wctr[0] += 1
            t = Wt[slot]
            p.dma('pool', ('W', slot), lambda e: e.dma_start(out=t[:, :, 0:ncols], in_=view[:, :, c0:c0 + ncols]),
                  writes=[('W', slot)])
            return t, ('W', slot)

        if fake_mod:
            p.dma('sp', 'fm', lambda e: e.dma_start(out=PS_fake, in_=modt_d), writes=['ps0'])
        for tno in range(0 if fake_mod else 6 * D // 256):
            wt, wk = load_w(wada_v, tno * 256, 256)
            for cc in range(2):
                j = tno * 2 + cc
                for k in range(KC):
                    p.pe((lambda wt, cc, j, k: lambda e: e.matmul(PS[:, 0, j:j + 1], lhsT=wt[:, k, cc * 128:(cc + 1) * 128],
                                                                  rhs=cact[:, k:k + 1], start=(k == 0), stop=(k == KC - 1)))(wt, cc, j, k),
                         reads=[wk, 'cact'], writes=['ps0'])
        if fake_mod:
            p.dve(lambda e: e.tensor_copy(out=modT, in_=PS_fake), reads=['ps0'], writes=['modT'])
        else:
            p.dve(lambda e: e.tensor_tensor(out=modT, in0=PS[:, 0, 0:192], in1=badat, op=ALU.add), reads=['ps0'], writes=['modT'])
        p.dve(lambda e: e.scalar_tensor_tensor(out=a1, in0=modT[:, 32:64], scalar=1.0, in1=g1t, op0=ALU.add, op1=ALU.mult),
              reads=['modT'], writes=['a1'])
        p.dve(lambda e: e.scalar_tensor_tensor(out=a2, in0=modT[:, 128:160], scalar=1.0, in1=g2t, op0=ALU.add, op1=ALU.mult),
              reads=['modT'], writes=['a2'])
        if 'modT' in dbg:
            p.dma('sp', 'dbg', lambda e: e.dma_start(out=dbg_out['modT'], in_=modT), reads=['modT'], writes=['dbg_modT'])
        p.barrier()
        if upto <= 1:
            p.emit(st)
            return nc, p

        def rms_norm_to_hT(src, row0, ntiles, a_t, s_t, hT, xs_off, junk_off):
            xs = [A.view(xs_off + i * 16384, [4096], F32) for i in range(2)]
            junk = A.view(junk_off, [4096], BF16)
            for tt in range(ntiles):
                xt = xs[tt % 2]
                xk = ('xs', tt % 2)
                sc = small[:, (tt % 2) * 4:(tt % 2) * 4 + 4]
                sk = ('nsc', tt % 2)
                p.dma('sp', xk, (lambda xt, tt: lambda e: e.dma_start(out=xt, in_=src[row0 + tt * 128: row0 + (tt + 1) * 128, :]))(xt, tt),
                      writes=[xk])
                p.act((lambda xt, sc: lambda e: e.activation(out=junk, in_=xt, func=AF.Square, accum_out=sc[:, 0:1]))(xt, sc),
                      reads=[xk], writes=['junk', sk])
                p.dve((lambda sc: lambda e: e.tensor_scalar(out=sc[:, 1:2], in0=sc[:, 0:1], scalar1=1.0 / D, scalar2=EPS,
                                                            op0=ALU.mult, op1=ALU.add))(sc), reads=[sk], writes=[sk])
                p.act((lambda sc: lambda e: e.activation(out=sc[:, 2:3], in_=sc[:, 1:2], func=AF.Sqrt))(sc), reads=[sk], writes=[sk])
                p.dve((lambda sc: lambda e: e.reciprocal(out=sc[:, 3:4], in_=sc[:, 2:3]))(sc), reads=[sk], writes=[sk])
                p.act((lambda xt, sc: lambda e: e.activation(out=xt, in_=xt, func=AF.Copy, scale=sc[:, 3:4]))(xt, sc),
                      reads=[xk, sk], writes=[xk])
                for g4 in range(8):
                    b = 4 + (g4 % 2)
                    for q in range(4):
                        c = g4 * 4 + q
                        p.pe((lambda xt, b, q, c: lambda e: e.transpose(PS[:, b, q * 128:(q + 1) * 128], xt[:, c * 128:(c + 1) * 128], ident_f))(xt, b, q, c),
                             reads=[xk, 'ident_f'], writes=[('ps', b)])
                    for q in range(4):
                        c = g4 * 4 + q
                        if g4 % 2 == 0:
                            p.dve((lambda b, q, c, tt: lambda e: e.tensor_scalar(out=hT[:, c, tt * 128:(tt + 1) * 128], in0=PS[:, b, q * 128:(q + 1) * 128],
                                                                                 scalar1=a_t[:, c:c + 1], scalar2=s_t[:, c:c + 1], op0=ALU.mult, op1=ALU.add))(b, q, c, tt),
                                  reads=[('ps', b)], writes=[('hT', c)])
                        else:
                            p.act((lambda b, q, c, tt: lambda e: e.activation(out=hT[:, c, tt * 128:(tt + 1) * 128], in_=PS[:, b, q * 128:(q + 1) * 128],
                                                                              func=AF.Identity, scale=a_t[:, c:c + 1], bias=s_t[:, c:c + 1]))(b, q, c, tt),
                                  reads=[('ps', b)], writes=[('hT', c)])

        pbank = [0]

        def next_pbank():
            b = pbank[0] % 2
            pbank[0] += 1
            return b

        hT_keys = [('hT', c) for c in range(KC)]

        def gemm_fm(wview, c0, ncols, hT, T, evac, src_keys):
            done = 0
            while done < ncols:
                n = min(256, ncols - done)
                wt, wk = load_w(wview, c0 + done, n)
                for cc in range((n + 127) // 128):
                    m = min(128, n - cc * 128)
                    for h0 in range(0, T, 512):
                        b = next_pbank()
                        for k in range(KC):
                            p.pe((lambda wt, cc, m, b, k, h0: lambda e: e.matmul(PS[0:m, b, 0:min(512, T - h0)], lhsT=wt[:, k, cc * 128:cc * 128 + m],
                                                                                 rhs=hT[:, k, h0:h0 + min(512, T - h0)], start=(k == 0), stop=(k == KC - 1)))(wt, cc, m, b, k, h0),
                                 reads=[wk] + src_keys, writes=[('ps', b)])
                        evac((done // 128) + cc, b, h0)
                done += n

        def gemm_tm(wview, c0, ncols, hT, T, evac, src_keys, also_fm=None):
            done = 0
            while done < ncols:
                n = min(256, ncols - done)
                wt, wk = load_w(wview, c0 + done, n)
                for tt in range(T // 128):
                    b = next_pbank()
                    for k in range(KC):
                        p.pe((lambda wt, n, b, k, tt: lambda e: e.matmul(PS[:, b, 0:n], lhsT=hT[:, k, tt * 128:(tt + 1) * 128],
                                                                         rhs=wt[:, k, 0:n], start=(k == 0), stop=(k == KC - 1)))(wt, n, b, k, tt),
                             reads=[wk] + src_keys, writes=[('ps', b)])
                    evac(done, n, tt, b)
                if also_fm is not None:
                    for cc in range(n // 128):
                        b = next_pbank()
                        for k in range(KC):
                            p.pe((lambda wt, cc, b, k: lambda e: e.matmul(PS[:, b, 0:T], lhsT=wt[:, k, cc * 128:(cc + 1) * 128],
                                                                          rhs=hT[:, k, 0:T], start=(k == 0), stop=(k == KC - 1)))(wt, cc, b, k),
                                 reads=[wk] + src_keys, writes=[('ps', b)])
                        also_fm((done // 128) + cc, b)
                done += n

        win_v = win_d.rearrange("(k p) c -> p k c", p=128)
        wout_v = wout_d.rearrange("(k p) c -> p k c", p=128)
        hT = A.view(R_HT, [32, MT], BF16)
        cT = A.view(R_CT, [32, MT], BF16)
        so = [R_S]

        def salloc(shape, dt):
            esz = 4 if dt == F32 else 2
            nb = (int(np.prod(shape)) * esz + 31) // 32 * 32
            v = A.view(so[0], shape, dt)
            so[0] += nb
            assert so[0] <= R_S + 49152, so[0]
            return v

        KT = salloc([2, 5 * 128], BF16)
        Vt = salloc([5, 2, 128], BF16)
        glowT = salloc([MT], BF16)
        S_AFTER_KV = so[0]
        QTg = salloc([4, 8, 128], BF16)
        tmpS = [salloc([512], F32) for _ in range(2)]
        PT = [salloc([512], BF16) for _ in range(2)]
        den = salloc([512], F32)
        so[0] = S_AFTER_KV
        gqT = salloc([2, MT], BF16)
        gkT = salloc([2, MT], BF16)
        gkt = salloc([4, 256], BF16)
        gv = salloc([4, 512], BF16)
        SG = salloc([4, 512], BF16)
        la = salloc([4, 256], F32)
        eb = salloc([2, MT], F32)
        enb = salloc([2, MT], F32)
        qdT = salloc([2, MT], BF16)
        kinvT = salloc([2, MT], BF16)
        kdec = salloc([4, 256], BF16)
        dl = salloc([2, 8], F32)
        Sbf = salloc([2, 512], BF16)
        attn_sb = salloc([64], BF16)
        ybf = salloc([512], BF16)
        tmp256 = salloc([256], F32)
        GLA_END = so[0]
        so[0] = S_AFTER_KV
        G1B = salloc([4096], F32)
        xres = [salloc([4, 256], F32) for _ in range(2)]
        dtmp = salloc([128], F32)

        PSb = PS[:, :, :].bitcast(BF16) if False else None

        p.pool(lambda e: e.memset(KT, 0.0), writes=['KT'])
        p.pool(lambda e: e.memset(Vt, 0.0), writes=['Vt'])

        def build_rowbcast(dst, colvec, tag):
            for c in range(KC):
                b = 2 + (c // 4) % 2
                q = c % 4
                p.dve((lambda c: lambda e: e.tensor_scalar(out=dtmp, in0=ident_f, scalar1=colvec[:, c:c + 1], scalar2=None, op0=ALU.mult))(c),
                      reads=['ident_f', 'modT'], writes=['dtmp'])
                p.pe((lambda b, q: lambda e: e.matmul(PS[:, b, q * 128:(q + 1) * 128], lhsT=ones_f, rhs=dtmp, start=True, stop=True))(b, q),
                     reads=['dtmp', 'ones_f'], writes=[('ps', b)])
                if q == 3:
                    p.act((lambda b, c: lambda e: e.activation(out=dst[:, (c - 3) * 128:(c + 1) * 128], in_=PS[:, b, :], func=AF.Identity))(b, c),
                          reads=[('ps', b)], writes=[tag])

        macro = [('P', 0), ('P', 1), ('M', 0), ('M', 1)]
        for kind, mi in macro:
            main = kind == 'M'
            src = x_main if main else x_pre
            rms_norm_to_hT(src, mi * MT, MT // 128, a1, s1, hT, R_CT, R_W + 32768)
            p.barrier()
            if stop == 'norm%s%d' % (kind, mi):
                p.barrier()
                p.emit(st)
                return nc, p
            need_kv = main or mi == 1

            if need_kv:
                def ev_k(ci, b, h0):
                    p.act((lambda ci, b: lambda e: e.activation(out=KT[:, ci, 128:128 + MT], in_=PS[:, b, 0:MT], func=AF.Identity))(ci, b),
                          reads=[('ps', b)], writes=['KT'])
                gemm_fm(win_v, C_AK, 256, hT, MT, ev_k, hT_keys)

                def ev_v(c0, n, tt, b):
                    p.dve((lambda tt, b: lambda e: e.tensor_copy(out=Vt[:, 1 + tt, :, :], in_=PS[:, b, 0:256].rearrange("p (g d) -> p g d", g=2)))(tt, b),
                          reads=[('ps', b)], writes=['Vt'])
                gemm_tm(win_v, C_AV, 256, hT, MT, ev_v, hT_keys)
            if main:
                for g in range(2):
                    def ev_q(ci, b, h0, g=g):
                        p.act((lambda ci, b: lambda e: e.activation(out=QTg[:, :, ci, :], in_=PS[:, b, 0:MT].rearrange("p (n q) -> p n q", n=4),
                                                                    func=AF.Copy, scale=128 ** -0.5))(ci, b),
                              reads=[('ps', b)], writes=['QTg'])
                    gemm_fm(win_v, C_AQ + g * 1024, 1024, hT, MT, ev_q, hT_keys)
                    for n in range(4):
                        for hf in range(2):
                            hs = slice(hf * 4, hf * 4 + 4)
                            hg = slice(g * 8 + hf * 4, g * 8 + hf * 4 + 4)
                            rq = QTg[:, n, hs, :].rearrange("p j q -> p (j q)")
                            p.pe((lambda g, n, rq: lambda e: e.matmul(PS[:, 2, :], lhsT=KT[:, g, (n + 1) * 128:(n + 2) * 128], rhs=rq, start=True, stop=True))(g, n, rq),
                                 reads=['KT', 'QTg'], writes=[('ps', 2)])
                            p.pe((lambda g, n, rq: lambda e: e.matmul(PS[:, 3, :], lhsT=KT[:, g, n * 128:(n + 1) * 128], rhs=rq, start=True, stop=True))(g, n, rq),
                                 reads=['KT', 'QTg'], writes=[('ps', 3)])
                            for w_, (bk, bt) in enumerate([(2, bcur), (3, bprev)]):
                                p.dve((lambda w_, bk, bt, hg: lambda e: e.tensor_tensor(out=tmpS[w_], in0=PS[:, bk, :], in1=bt[:, hg, :].rearrange("p j q -> p (j q)"), op=ALU.add))(w_, bk, bt, hg),
                                      reads=[('ps', bk)], writes=[('tmpS', w_)])
                                p.act((lambda w_: lambda e: e.activation(out=PT[w_], in_=tmpS[w_], func=AF.Exp))(w_),
                                      reads=[('tmpS', w_)], writes=[('PT', w_)])
                            if mi == 0 and n == 0:
                                p.dve(lambda e: e.tensor_scalar(out=PT[1], in0=PT[1], scalar1=flag[:, 0:1], scalar2=None, op0=ALU.mult),
                                      reads=[('PT', 1), 'flag'], writes=[('PT', 1)])
                            p.pe(lambda e: e.matmul(PS[:, 6, :], lhsT=ones_b, rhs=PT[0], start=True, stop=False), reads=[('PT', 0), 'ones_b'], writes=[('ps', 6)])
                            p.pe(lambda e: e.matmul(PS[:, 6, :], lhsT=ones_b, rhs=PT[1], start=False, stop=True), reads=[('PT', 1), 'ones_b'], writes=[('ps', 6)])
                            p.pe((lambda g, n: lambda e: e.matmul(PS[:, 7, :], lhsT=Vt[:, n + 1, g, :], rhs=PT[0], start=True, stop=False))(g, n),
                                 reads=[('PT', 0), 'Vt'], writes=[('ps', 7)])
                            p.pe((lambda g, n: lambda e: e.matmul(PS[:, 7, :], lhsT=Vt[:, n, g, :], rhs=PT[1], start=False, stop=True))(g, n),
                                 reads=[('PT', 1), 'Vt'], writes=[('ps', 7)])
                            for j in range(4):
                                hh = g * 8 + hf * 4 + j
                                p.dve((lambda j, hh: lambda e: e.tensor_scalar(out=den[:, j * 128:(j + 1) * 128], in0=PS[:, 6, j * 128:(j + 1) * 128],
                                                                               scalar1=esink[:, hh:hh + 1], scalar2=None, op0=ALU.add))(j, hh),
                                      reads=[('ps', 6), 'esink'], writes=['den'])
                            p.dve(lambda e: e.reciprocal(out=den, in_=den), reads=['den'], writes=['den'])
                            p.dve((lambda g, n, hf: lambda e: e.tensor_tensor(out=cT[:, g * 8 + hf * 4:g * 8 + hf * 4 + 4, n * 128:(n + 1) * 128],
                                                                              in0=PS[:, 7, :].rearrange("p (j q) -> p j q", j=4),
                                                                              in1=den.rearrange("p (j q) -> p j q", j=4), op=ALU.mult))(g, n, hf),
                                  reads=[('ps', 7), 'den'], writes=[('cT', g * 8 + hf * 4 + jj) for jj in range(4)])
            if need_kv:
                p.pool(lambda e: e.tensor_copy(out=KT[:, :, 0:128], in_=KT[:, :, 512:640]), reads=['KT'], writes=['KT'])
                p.pool(lambda e: e.tensor_copy(out=Vt[:, 0, :, :], in_=Vt[:, 4, :, :]), reads=['Vt'], writes=['Vt'])
            if stop == 'swa%s%d' % (kind, mi):
                p.barrier()
                p.emit(st)
                return nc, p
            if 'cT_attn' in dbg and main and mi == 0:
                p.barrier()
                p.dve(lambda e: e.tensor_copy(out=eb[:, 0, :], in_=cT[:, 0, :]), writes=['dbgtmp'])
                p.dma('sp', 'dbg', lambda e: e.dma_start(out=dbg_out['cT_attn'], in_=eb[:, 0, :]), reads=['dbgtmp'], writes=['dbg_ct'])
            p.barrier()
            def ev_glow(ci, b, h0):
                p.act((lambda b: lambda e: e.activation(out=glowT[0:16, :], in_=PS[0:16, b, 0:MT], func=AF.Identity))(b),
                      reads=[('ps', b)], writes=['glowT'])
            gemm_fm(win_v, C_GLOW, 16, hT, MT, ev_glow, hT_keys)
            for h in range(4):
                if main:
                    def ev_gq(ci, b, h0):
                        p.act((lambda ci, b: lambda e: e.activation(out=gqT[:, ci, :], in_=PS[:, b, 0:MT], func=AF.Identity))(ci, b),
                              reads=[('ps', b)], writes=['gqT'])
                    gemm_fm(win_v, C_GQ + h * 256, 256, hT, MT, ev_gq, hT_keys)

                def ev_gk(c0, n, tt, b):
                    p.dve((lambda tt, b: lambda e: e.tensor_copy(out=gkt[:, tt, :], in_=PS[:, b, 0:256]))(tt, b), reads=[('ps', b)], writes=['gkt'])

                def ev_gkT(ci, b):
                    p.act((lambda ci, b: lambda e: e.activation(out=gkT[:, ci, :], in_=PS[:, b, 0:MT], func=AF.Identity))(ci, b),
                          reads=[('ps', b)], writes=['gkT'])
                gemm_tm(win_v, C_GK + h * 256, 256, hT, MT, ev_gk, hT_keys, also_fm=ev_gkT if main else None)

                def ev_gv(c0, n, tt, b):
                    p.act((lambda c0, tt, b: lambda e: e.activation(out=gv[:, tt, c0:c0 + 256], in_=PS[:, b, 0:256], func=AF.Identity))(c0, tt, b),
                          reads=[('ps', b)], writes=['gv'])
                gemm_tm(win_v, C_GV + h * 512, 512, hT, MT, ev_gv, hT_keys)
                if main:
                    def ev_go(c0, n, tt, b):
                        p.act((lambda b: lambda e: e.activation(out=tmp256, in_=PS[:, b, 0:256], func=AF.Silu))(b),
                              reads=[('ps', b)], writes=['tmp256'])
                        p.dve((lambda c0, tt: lambda e: e.tensor_tensor(out=SG[:, tt, c0:c0 + 256], in0=tmp256, in1=gnb[:, c0:c0 + 256], op=ALU.mult))(c0, tt),
                              reads=['tmp256', 'gnb'], writes=['SG'])
                    gemm_tm(win_v, C_GOUT + h * 512, 512, hT, MT, ev_go, hT_keys)
                for tt in range(4):
                    p.pe((lambda tt, h: lambda e: e.matmul(PS[:, 2, 0:256], lhsT=glowT[0:16, tt * 128:(tt + 1) * 128], rhs=gk2w[0:16, h * 256:(h + 1) * 256],
                                                           start=True, stop=False))(tt, h), reads=['glowT', 'gk2w'], writes=[('ps', 2)])
                    p.pe((lambda tt, h: lambda e: e.matmul(PS[:, 2, 0:256], lhsT=ones_b[0:1, :], rhs=gk2b[0:1, h * 256:(h + 1) * 256],
                                                           start=False, stop=True))(tt, h), reads=['gk2b', 'ones_b'], writes=[('ps', 2)])
                    p.act(lambda e: e.activation(out=tmp256, in_=PS[:, 2, 0:256], func=AF.Exp, scale=-1.0), reads=[('ps', 2)], writes=['tmp256'])
                    p.act(lambda e: e.activation(out=tmp256, in_=tmp256, func=AF.Ln, bias=1.0), reads=['tmp256'], writes=['tmp256'])
                    p.act((lambda tt: lambda e: e.activation(out=la[:, tt, :], in_=tmp256, func=AF.Copy, scale=-1.0 / 16.0))(tt),
                          reads=['tmp256'], writes=['la'])
                for dc in range(2):
                    for tt in range(4):
                        p.pe((lambda dc, tt: lambda e: e.matmul(PS[:, 2 + dc, tt * 128:(tt + 1) * 128], lhsT=la[:, tt, dc * 128:(dc + 1) * 128], rhs=Lm,
                                                                start=True, stop=True))(dc, tt), reads=['la', 'Lm'], writes=[('ps', 2 + dc)])
                p.act(lambda e: e.activation(out=eb, in_=PS[:, 2:4, :], func=AF.Exp), reads=[('ps', 2), ('ps', 3)], writes=['eb'])
                if main:
                    p.act(lambda e: e.activation(out=enb, in_=PS[:, 2:4, :], func=AF.Exp, scale=-1.0), reads=[('ps', 2), ('ps', 3)], writes=['enb'])
                    p.dve(lambda e: e.scalar_tensor_tensor(out=qdT, in0=gqT, scalar=1.0 / 16.0, in1=eb, op0=ALU.mult, op1=ALU.mult),
                          reads=['gqT', 'eb'], writes=['qdT'])
                    p.dve(lambda e: e.tensor_tensor(out=kinvT, in0=gkT, in1=enb, op=ALU.mult), reads=['gkT', 'enb'], writes=['kinvT'])
                p.dve(lambda e: e.tensor_copy(out=dl, in_=eb[:, :, 63::64]), reads=['eb'], writes=['dl'])
                for tt in range(4):
                    p.pe((lambda tt: lambda e: e.matmul(PS[:, 2, 0:256], lhsT=Um, rhs=la[:, tt, :], start=True, stop=True))(tt),
                         reads=['la', 'Um'], writes=[('ps', 2)])
                    p.act(lambda e: e.activation(out=tmp256, in_=PS[:, 2, 0:256], func=AF.Exp), reads=[('ps', 2)], writes=['tmp256'])
                    p.dve((lambda tt: lambda e: e.tensor_tensor(out=kdec[:, tt, :], in0=gkt[:, tt, :], in1=tmp256, op=ALU.mult))(tt),
                          reads=['gkt', 'tmp256'], writes=['kdec'])
                p.act((lambda h: lambda e: e.activation(out=Sbf, in_=Sst[:, h, :, :], func=AF.Identity))(h), reads=['Sst'], writes=['Sbf'])
                for c in range(8):
                    tt, par = c // 2, c % 2
                    r0 = 64 * par
                    t0 = c * 64
                    rs = slice(r0, r0 + 64)
                    if main:
                        for dc in range(2):
                            p.pe((lambda dc, t0, rs: lambda e: e.matmul(PS[rs, 3, 0:64], lhsT=kinvT[:, dc, t0:t0 + 64], rhs=qdT[:, dc, t0:t0 + 64],
                                                                        start=(dc == 0), stop=(dc == 1)))(dc, t0, rs),
                                 reads=['kinvT', 'qdT'], writes=[('ps', 3)])
                        p.dve((lambda rs: lambda e: e.tensor_tensor(out=attn_sb[rs, :], in0=PS[rs, 3, 0:64], in1=Lm[rs, rs], op=ALU.mult))(rs),
                              reads=[('ps', 3), 'Lm'], writes=['attn_sb'])
                        p.pe((lambda rs, tt: lambda e: e.matmul(PS[rs, 6, :], lhsT=attn_sb[rs, :], rhs=gv[rs, tt, :], start=True, stop=False))(rs, tt),
                             reads=['attn_sb', 'gv'], writes=[('ps', 6)])
                        for dc in range(2):
                            p.pe((lambda dc, t0, rs: lambda e: e.matmul(PS[rs, 6, :], lhsT=qdT[:, dc, t0:t0 + 64], rhs=Sbf[:, dc, :],
                                                                        start=False, stop=(dc == 1)))(dc, t0, rs),
                                 reads=['qdT', 'Sbf'], writes=[('ps', 6)])
                    for dc in range(2):
                        p.pe((lambda dc, rs, tt: lambda e: e.matmul(PS[:, 4 + dc, :], lhsT=kdec[rs, tt, dc * 128:(dc + 1) * 128], rhs=gv[rs, tt, :],
                                                                    start=True, stop=True))(dc, rs, tt),
                             reads=['kdec', 'gv'], writes=[('ps', 4 + dc)])
                        p.dve((lambda dc, c, h: lambda e: e.scalar_tensor_tensor(out=Sst[:, h, dc, :], in0=Sst[:, h, dc, :], scalar=dl[:, dc, c:c + 1],
                                                                                 in1=PS[:, 4 + dc, :], op0=ALU.mult, op1=ALU.add))(dc, c, h),
                              reads=['Sst', 'dl', ('ps', 4 + dc)], writes=['Sst'])
                    p.act((lambda h: lambda e: e.activation(out=Sbf, in_=Sst[:, h, :, :], func=AF.Identity))(h), reads=['Sst'], writes=['Sbf'])
                    if main and par == 1:
                        sc = small[:, 8:12]
                        p.act(lambda e: e.activation(out=ybf, in_=PS[:, 6, :], func=AF.Square, accum_out=sc[:, 0:1]), reads=[('ps', 6)], writes=['ybf', 'gsc'])
                        p.dve(lambda e: e.tensor_scalar(out=sc[:, 1:2], in0=sc[:, 0:1], scalar1=1.0 / 512, scalar2=EPS, op0=ALU.mult, op1=ALU.add),
                              reads=['gsc'], writes=['gsc'])
                        p.act(lambda e: e.activation(out=sc[:, 2:3], in_=sc[:, 1:2], func=AF.Sqrt), reads=['gsc'], writes=['gsc'])
                        p.dve(lambda e: e.reciprocal(out=sc[:, 3:4], in_=sc[:, 2:3]), reads=['gsc'], writes=['gsc'])
                        p.dve((lambda tt: lambda e: e.scalar_tensor_tensor(out=ybf, in0=PS[:, 6, :], scalar=sc[:, 3:4], in1=SG[:, tt, :],
                                                                           op0=ALU.mult, op1=ALU.mult))(tt), reads=[('ps', 6), 'gsc', 'SG'], writes=['ybf'])
                        pst = PS[:, 7, :].bitcast(BF16)
                        for j in range(4):
                            p.pe((lambda j: lambda e: e.transpose(pst[:, j * 128:(j + 1) * 128], ybf[:, j * 128:(j + 1) * 128], ident_b))(j),
                                 reads=['ybf', 'ident_b'], writes=[('ps', 7)])
                        p.act((lambda h, tt: lambda e: e.activation(out=cT[:, 16 + h * 4:16 + h * 4 + 4, tt * 128:(tt + 1) * 128],
                                                                    in_=pst[:, 0:512].rearrange("p (j q) -> p j q", j=4), func=AF.Identity))(h, tt),
                              reads=[('ps', 7)], writes=[('cT', 16 + h * 4 + jj) for jj in range(4)])
                if not main and mi == 1:
                    p.dve((lambda h: lambda e: e.tensor_scalar(out=Sst[:, h, :, :], in0=Sst[:, h, :, :], scalar1=flag[:, 0:1], scalar2=None, op0=ALU.mult))(h),
                          reads=['Sst', 'flag'], writes=['Sst'])
            p.barrier()
            if stop == 'gla%s%d' % (kind, mi):
                p.emit(st)
                return nc, p
            if 'cT_gla' in dbg and main and mi == 0:
                p.dve(lambda e: e.tensor_copy(out=eb[:, 0, :], in_=cT[:, 16, :]), writes=['dbgtmp'])
                p.dma('sp', 'dbg', lambda e: e.dma_start(out=dbg_out['cT_gla'], in_=eb[:, 0, :]), reads=['dbgtmp'], writes=['dbg_ct2'])
                p.barrier()
            if not main:
                continue
            build_rowbcast(G1B, gate1, 'G1B')
            cT_keys = [('cT', c) for c in range(KC)]
            xstate = {}

            def ev_o(c0, n, tt, b):
                cg = c0 // 256
                xr = xres[cg % 2]
                xk = ('xres', cg % 2)
                if tt == 0:
                    srcap = x_main[mi * MT:(mi + 1) * MT, c0:c0 + 256].rearrange("(t p) c -> p t c", p=128)
                    p.dma('sp', xk, (lambda xr, srcap: lambda e: e.dma_start(out=xr, in_=srcap))(xr, srcap), writes=[xk])
                p.dve((lambda c0, b: lambda e: e.tensor_tensor(out=tmp256, in0=PS[:, b, 0:256], in1=G1B[:, c0:c0 + 256], op=ALU.mult))(c0, b),
                      reads=[('ps', b), 'G1B'], writes=['tmp256'])
                p.dve((lambda xr, tt: lambda e: e.tensor_tensor(out=xr[:, tt, :], in0=xr[:, tt, :], in1=tmp256, op=ALU.add))(xr, tt),
                      reads=['tmp256', xk], writes=[xk])
                if tt == 3:
                    dstap = x1s[mi * MT:(mi + 1) * MT, c0:c0 + 256].rearrange("(t p) c -> p t c", p=128)
                    p.dma('sp', ('xst', cg % 2), (lambda xr, dstap: lambda e: e.dma_start(out=dstap, in_=xr))(xr, dstap),
                          reads=[xk], writes=[xk, 'x1s'])
            gemm_tm(wout_v, 0, D, cT, MT, ev_o, cT_keys)
            p.barrier()
        if upto <= 2:
            p.emit(st)
            return nc, p

        wq_v = wq_d.rearrange("(k p) c -> p k c", p=128)
        ut_v = ut_d.rearrange("(k p) e -> p k e", p=128)
        h2T = A.view(R_HT, [32, TOK], BF16)
        h2_keys = [('hT', c) for c in range(KC)]
        rms_norm_to_hT(x1s, 0, TOK // 128, a2, s2, h2T, R_W, R_W + 32768)
        p.barrier()
        qT = A.view(R_S, [16, TOK], BF16)
        RZB = A.view(R_S + 32768, [8, TOK], BF16)

        def ev_pq(ci, b, h0):
            p.act((lambda ci, b, h0: lambda e: e.activation(out=qT[:, ci, h0:h0 + 512], in_=PS[:, b, :], func=AF.Identity))(ci, b, h0),
                  reads=[('ps', b)], writes=['qT'])
        gemm_fm(wq_v, 0, 2048, h2T, TOK, ev_pq, h2_keys)
        p.barrier()
        wo = [R_W]

        def walloc(shape, dt):
            esz = 4 if dt == F32 else 2
            nb = (int(np.prod(shape)) * esz + 31) // 32 * 32
            v = A.view(wo[0], shape, dt)
            wo[0] += nb
            assert wo[0] <= R_W + 49152, wo[0]
            return v
        ut = [walloc([32, 256], BF16) for _ in range(2)]
        keysT = walloc([16, 128], BF16)
        THR = walloc([TOK], BF16)
        RZR = walloc([TOK], BF16)
        SEL = walloc([8, 128], BF16)
        scs = A.view(P_DYN + 16384, [16, 128], F32)
        qo = [P_DYN]

        def qalloc(shape, dt):
            esz = 4 if dt == F32 else 2
            nb = (int(np.prod(shape)) * esz + 31) // 32 * 32
            v = A.view(qo[0], shape, dt)
            qo[0] += nb
            assert qo[0] <= R_P + 36864, qo[0]
            return v
        t16 = qalloc([16, 16], F32)
        tmpk = qalloc([128], F32)
        cand = qalloc([16, 16], F32)
        tmpc = qalloc([256], F32)
        c16 = qalloc([8, 16], F32)
        tsc = qalloc([8, 8], F32)
        thrTM = qalloc([24], BF16)
        rzTM = qalloc([8], BF16)
        junk16 = qalloc([16], F32)
        p.dma('pool', 'peer_c', lambda e: e.dma_start(out=keysT, in_=keyst_d.rearrange("p (a k) -> p a k", a=16)), writes=['keysT'])
        p.dma('pool', 'peer_c', lambda e: e.dma_start(out=SEL[0:24, :, :], in_=sel_d.rearrange("p (a k) -> p a k", a=8)), writes=['SEL'])
        p.barrier()
        for tt in range(8):
            for hp in range(16):
                b = 4 + hp // 4
                p.pe((lambda hp, b, tt: lambda e: e.matmul(PS[:, b, (hp % 4) * 128:(hp % 4 + 1) * 128], lhsT=qT[:, hp, tt * 128:(tt + 1) * 128],
                                                           rhs=keysT[:, hp, :], start=True, stop=True))(hp, b, tt),
                     reads=['qT', 'keysT'], writes=[('ps', b)])
            p.act(lambda e: e.activation(out=scs, in_=PS[:, 4:8, :].rearrange("p b (c k) -> p (b c) k", c=4), func=AF.Identity),
                  reads=[('ps', 4), ('ps', 5), ('ps', 6), ('ps', 7)], writes=['scs'])
            for hp in range(16):
                p.dve((lambda hp: lambda e: e.max(out=t16[:, hp, 0:8], in_=scs[:, hp, :]))(hp), reads=['scs'], writes=['t16'])
                p.dve((lambda hp: lambda e: e.match_replace(out=tmpk, in_to_replace=t16[:, hp, 0:8], in_values=scs[:, hp, :], imm_value=-1e30))(hp),
                      reads=['scs', 't16'], writes=['tmpk'])
                p.dve((lambda hp: lambda e: e.max(out=t16[:, hp, 8:16], in_=tmpk))(hp), reads=['tmpk'], writes=['t16'])
            for h in range(8):
                p.dve((lambda h: lambda e: e.tensor_tensor(out=cand, in0=t16[:, 2 * h, :].unsqueeze(2).to_broadcast([128, 16, 16]),
                                                           in1=t16[:, 2 * h + 1, :].unsqueeze(1).to_broadcast([128, 16, 16]), op=ALU.add))(h),
                      reads=['t16'], writes=['cand'])
                cf = cand.rearrange("p a b -> p (a b)")
                p.dve((lambda h: lambda e: e.max(out=c16[:, h, 0:8], in_=cf))(h), reads=['cand'], writes=['c16'])
                p.dve((lambda h: lambda e: e.match_replace(out=tmpc, in_to_replace=c16[:, h, 0:8], in_values=cf, imm_value=-1e30))(h),
                      reads=['cand', 'c16'], writes=['tmpc'])
                p.dve((lambda h: lambda e: e.max(out=c16[:, h, 8:16], in_=tmpc))(h), reads=['tmpc'], writes=['c16'])
                p.dve((lambda h: lambda e: e.tensor_scalar(out=tsc[:, h, 0:1], in0=c16[:, h, 15:16], scalar1=-1.0, scalar2=3e-5, op0=ALU.mult, op1=ALU.add))(h),
                      reads=['c16'], writes=['tsc'])
                p.act((lambda h: lambda e: e.activation(out=junk16, in_=c16[:, h, :], func=AF.Exp, bias=tsc[:, h, 0:1], accum_out=tsc[:, h, 1:2]))(h),
                      reads=['c16', 'tsc'], writes=['tsc', 'junk16'])
                p.dve((lambda h: lambda e: e.reciprocal(out=tsc[:, h, 2:3], in_=tsc[:, h, 1:2]))(h), reads=['tsc'], writes=['tsc'])
                p.dve((lambda h: lambda e: e.tensor_copy(out=thrTM[:, h:h + 1], in_=tsc[:, h, 0:1]))(h), reads=['tsc'], writes=['thrTM'])
                p.dve((lambda h: lambda e: e.tensor_tensor(out=tsc[:, h, 3:4], in0=tsc[:, h, 0:1], in1=thrTM[:, h:h + 1], op=ALU.subtract))(h),
                      reads=['tsc', 'thrTM'], writes=['tsc'])
                p.dve((lambda h: lambda e: e.tensor_copy(out=thrTM[:, 8 + h:9 + h], in_=tsc[:, h, 3:4]))(h), reads=['tsc'], writes=['thrTM'])
                p.dve((lambda h: lambda e: e.tensor_tensor(out=tsc[:, h, 4:5], in0=tsc[:, h, 3:4], in1=thrTM[:, 8 + h:9 + h], op=ALU.subtract))(h),
                      reads=['tsc', 'thrTM'], writes=['tsc'])
                p.dve((lambda h: lambda e: e.tensor_copy(out=thrTM[:, 16 + h:17 + h], in_=tsc[:, h, 4:5]))(h), reads=['tsc'], writes=['thrTM'])
                p.dve((lambda h: lambda e: e.tensor_copy(out=rzTM[:, h:h + 1], in_=tsc[:, h, 2:3]))(h), reads=['tsc'], writes=['rzTM'])
            pst = PS[:, 2, :].bitcast(BF16)
            p.pe(lambda e: e.transpose(pst[0:24, 0:128], thrTM, ident_b), reads=['thrTM', 'ident_b'], writes=[('ps', 2)])
            p.pe(lambda e: e.transpose(pst[0:8, 128:256], rzTM, ident_b), reads=['rzTM', 'ident_b'], writes=[('ps', 2)])
            p.act((lambda tt: lambda e: e.activation(out=THR[0:24, tt * 128:(tt + 1) * 128], in_=pst[0:24, 0:128], func=AF.Identity))(tt),
                  reads=[('ps', 2)], writes=['THR'])
            p.act((lambda tt: lambda e: e.activation(out=RZR[0:8, tt * 128:(tt + 1) * 128], in_=pst[0:8, 128:256], func=AF.Identity))(tt),
                  reads=[('ps', 2)], writes=['RZR'])
        for h in range(8):
            for hf in range(2):
                b = hf
                p.pe((lambda h, hf, b: lambda e: e.matmul(PS[:, b, :], lhsT=SEL[0:8, h, :], rhs=RZR[0:8, hf * 512:(hf + 1) * 512], start=True, stop=True))(h, hf, b),
                     reads=['SEL', 'RZR'], writes=[('ps', b)])
                p.act((lambda h, hf, b: lambda e: e.activation(out=RZB[:, h, hf * 512:(hf + 1) * 512], in_=PS[:, b, :], func=AF.Identity))(h, hf, b),
                      reads=[('ps', b)], writes=['RZB'])
        if 'thr' in dbg:
            dbt = A.view(P_DYN + 12288, [TOK], F32)
            p.act(lambda e: e.activation(out=dbt[0:24, :], in_=THR[0:24, :], func=AF.Identity), reads=['THR'], writes=['dbt'])
            p.dma('sp', 'dbg', lambda e: e.dma_start(out=dbg_out['thr'], in_=dbt[0:24, :]), reads=['dbt'], writes=['dbg_thr'])
            p.barrier()
            dbt2 = A.view(P_DYN + 12288, [TOK], F32)
            p.act(lambda e: e.activation(out=dbt2[0:8, :], in_=RZR[0:8, :], func=AF.Identity), reads=['RZR'], writes=['dbt'])
            p.dma('sp', 'dbg', lambda e: e.dma_start(out=dbg_out['rzr'], in_=dbt2[0:8, :]), reads=['dbt'], writes=['dbg_rzr'])
        p.barrier()
        if upto <= 3:
            p.emit(st)
            return nc, p
        qo[0] = P_DYN
        HT_ = 512
        Eb = [qalloc([HT_], BF16) for _ in range(4)]
        Wm = [qalloc([HT_], BF16) for _ in range(4)]
        Wn = [qalloc([HT_], BF16) for _ in range(8)]
        Gl = qalloc([TOK], BF16)
        AW = [qalloc([TOK], BF16) for _ in range(2)]
        NE = 128 if upto > 4 else 2

        def emit_U(i1, sg):
            h, hf = sg // 2, sg % 2
            b = 2 + sg % 4
            ts = slice(hf * 512, (hf + 1) * 512)
            p.pe(lambda e: e.matmul(PS[:, b, :], lhsT=keysT[:, 2 * h + 1, :], rhs=qT[:, 2 * h + 1, ts], start=True, stop=False),
                 reads=['qT', 'keysT'], writes=[('ps', b)])
            p.pe(lambda e: e.matmul(PS[:, b, :], lhsT=keysT[:, 2 * h, i1:i1 + 1].to_broadcast([128, 128]), rhs=qT[:, 2 * h, ts], start=False, stop=False),
                 reads=['qT', 'keysT'], writes=[('ps', b)])
            p.pe(lambda e: e.matmul(PS[:, b, :], lhsT=SEL[0:24, h, :], rhs=THR[0:24, ts], start=False, stop=True),
                 reads=['SEL', 'THR'], writes=[('ps', b)])
            r = sg % 4
            p.act(lambda e: e.activation(out=Eb[r], in_=PS[:, b, :], func=AF.Exp), reads=[('ps', b)], writes=[('Eb', r)])
            p.dve(lambda e: e.scalar_tensor_tensor(out=Wm[r], in0=PS[:, b, :], scalar=0.0, in1=Eb[r], op0=ALU.is_ge, op1=ALU.mult),
                  reads=[('ps', b), ('Eb', r)], writes=[('Wm', r)])
            r8 = sg % 8
            p.pool(lambda e: e.tensor_tensor(out=Wn[r8], in0=Wm[r], in1=RZB[:, h, ts], op=ALU.mult), reads=[('Wm', r), 'RZB'], writes=[('Wn', r8)])

        def emit_acc(sg):
            h, hf = sg // 2, sg % 2
            r8 = sg % 8
            p.pe(lambda e: e.matmul(PS[:, 6 + hf, :], lhsT=ident_b, rhs=Wn[r8], start=(h == 0), stop=(h == 7)),
                 reads=[('Wn', r8), 'ident_b'], writes=[('ps', 6 + hf)])

        for i1 in range(NE):
            if i1 % 2 == 0:
                us = (i1 // 2) % 2
                p.dma('pool', ('U', us), (lambda us, i1: lambda e: e.dma_start(out=ut[us], in_=ut_v[:, :, i1 * 128:i1 * 128 + 256]))(us, i1),
                      writes=[('U', us)])
            us = (i1 // 2) % 2
            uk = ('U', us)
            ec = (i1 % 2) * 128
            for hf in range(2):
                for k in range(KC):
                    p.pe((lambda us, ec, hf, k: lambda e: e.matmul(PS[:, hf, :], lhsT=ut[us][:, k, ec:ec + 128], rhs=h2T[:, k, hf * 512:(hf + 1) * 512],
                                                                   start=(k == 0), stop=(k == KC - 1)))(us, ec, hf, k),
                         reads=[uk] + h2_keys, writes=[('ps', hf)])
            p.act(lambda e: e.activation(out=Gl.rearrange("p (b t) -> p b t", b=2), in_=PS[:, 0:2, :], func=AF.Gelu), reads=[('ps', 0), ('ps', 1)], writes=['Gl'])
            for sg in range(4):
                emit_U(i1, sg)
            for sg in range(16):
                if sg + 4 < 16:
                    emit_U(i1, sg + 4)
                emit_acc(sg)
            a = i1 % 2
            p.dve((lambda a: lambda e: e.tensor_tensor(out=AW[a].rearrange("p (b t) -> p b t", b=2), in0=PS[:, 6:8, :], in1=Gl.rearrange("p (b t) -> p b t", b=2), op=ALU.mult))(a),
                  reads=[('ps', 6), ('ps', 7), 'Gl'], writes=[('AW', a)])
            p.dma('sp', ('AWst', a), (lambda a, i1: lambda e: e.dma_start(out=aws[i1], in_=AW[a]))(a, i1), reads=[('AW', a)], writes=[('AW', a), 'aws'])
        if 'aw0' in dbg:
            p.barrier()
            dbt = A.view(R_HT, [TOK], F32)
            p.act(lambda e: e.activation(out=dbt, in_=AW[0], func=AF.Identity), writes=['dbt'])
            p.dma('sp', 'dbg', lambda e: e.dma_start(out=dbg_out['aw0'], in_=dbt), reads=['dbt'], writes=['dbg_aw0'])
        p.barrier()
        if upto <= 4:
            p.emit(st)
            return nc, p
        acc = A.view(0, [8, D], F32)
        vt = [A.view(131072 + i * 8192, [8, 512], BF16) for i in range(2)]
        awt = [A.view(131072 + 16384, [8, TOK], BF16), A.view(P_DYN, [8, TOK], BF16)]
        v_v = v_d.rearrange("(g a p) d -> g p a d", a=8, p=128)
        aws_v = aws.rearrange("(g a) p t -> g p a t", a=8)
        it = 0
        for dg in range(8):
            for eg in range(16):
                s_ = it % 2
                it += 1
                p.dma('pool', ('V', s_), (lambda s_, eg, dg: lambda e: e.dma_start(out=vt[s_], in_=v_v[eg][:, :, dg * 512:(dg + 1) * 512]))(s_, eg, dg), writes=[('V', s_)])
                p.dma('sp', ('AWld', s_), (lambda s_, eg: lambda e: e.dma_start(out=awt[s_], in_=aws_v[eg]))(s_, eg), reads=['aws'], writes=[('AWl', s_)])
                for a in range(8):
                    for tt in range(8):
                        p.pe((lambda s_, a, tt, eg: lambda e: e.matmul(PS[:, tt, :], lhsT=awt[s_][:, a, tt * 128:(tt + 1) * 128], rhs=vt[s_][:, a, :],
                                                                      start=(eg == 0 and a == 0), stop=(eg == 15 and a == 7)))(s_, a, tt, eg),
                             reads=[('V', s_), ('AWl', s_)], writes=[('ps', tt)])
            for tt in range(8):
                if tt % 2 == 0:
                    p.act((lambda tt, dg: lambda e: e.activation(out=acc[:, tt, dg * 512:(dg + 1) * 512], in_=PS[:, tt, :], func=AF.Identity))(tt, dg),
                          reads=[('ps', tt)], writes=[('acc', tt, dg)])
                else:
                    p.dve((lambda tt, dg: lambda e: e.tensor_copy(out=acc[:, tt, dg * 512:(dg + 1) * 512], in_=PS[:, tt, :]))(tt, dg),
                          reads=[('ps', tt)], writes=[('acc', tt, dg)])
        p.barrier()
        G2B = A.view(131072, [D], F32)
        FGB = A.view(131072 + 16384, [D], F32)
        x1t = A.view(P_DYN + 1024, [D], F32)
        dtmp2 = A.view(P_DYN, [128], F32)

        for c in range(KC):
            b = 2 + (c // 4) % 2
            q = c % 4
            p.dve((lambda c: lambda e: e.tensor_scalar(out=dtmp2, in0=ident_f, scalar1=gate2[:, c:c + 1], scalar2=None, op0=ALU.mult))(c),
                  reads=['ident_f', 'modT'], writes=['dtmp2'])
            p.pe((lambda b, q: lambda e: e.matmul(PS[:, b, q * 128:(q + 1) * 128], lhsT=ones_f, rhs=dtmp2, start=True, stop=True))(b, q),
                 reads=['dtmp2', 'ones_f'], writes=[('ps', b)])
            if q == 3:
                p.act((lambda b, c: lambda e: e.activation(out=G2B[:, (c - 3) * 128:(c + 1) * 128], in_=PS[:, b, :], func=AF.Identity))(b, c),
                      reads=[('ps', b)], writes=['G2B'])
        p.dma('sp', 'fgb', lambda e: e.dma_start(out=FGB, in_=fg_d.partition_broadcast(128)[:, 0, :]), writes=['FGB'])
        for tt in range(8):
            sc = small[:, 16:20]
            p.dma('sp', 'x1ld', (lambda tt: lambda e: e.dma_start(out=x1t, in_=x1s[tt * 128:(tt + 1) * 128, :]))(tt), reads=['x1s'], writes=['x1t'])
            p.dve((lambda tt: lambda e: e.tensor_tensor(out=acc[:, tt, :], in0=acc[:, tt, :], in1=G2B, op=ALU.mult))(tt), reads=['G2B'], writes=[('accf', tt)])
            p.pool((lambda tt: lambda e: e.tensor_tensor(out=x1t, in0=x1t, in1=acc[:, tt, :], op=ALU.add))(tt), reads=[('accf', tt), 'x1t'], writes=['x1t'])
            p.act(lambda e: e.activation(out=PS[:, :, :], in_=x1t.rearrange("p (b t) -> p b t", b=8), func=AF.Square, accum_out=sc[:, 0:1]), reads=['x1t'], writes=['junkf', 'fsc'])
            p.dve(lambda e: e.tensor_scalar(out=sc[:, 1:2], in0=sc[:, 0:1], scalar1=1.0 / D, scalar2=EPS, op0=ALU.mult, op1=ALU.add), reads=['fsc'], writes=['fsc'])
            p.act(lambda e: e.activation(out=sc[:, 2:3], in_=sc[:, 1:2], func=AF.Sqrt), reads=['fsc'], writes=['fsc'])
            p.dve(lambda e: e.reciprocal(out=sc[:, 3:4], in_=sc[:, 2:3]), reads=['fsc'], writes=['fsc'])
            p.dve(lambda e: e.scalar_tensor_tensor(out=x1t, in0=x1t, scalar=sc[:, 3:4], in1=FGB, op0=ALU.mult, op1=ALU.mult), reads=['x1t', 'fsc', 'FGB'], writes=['x1t'])
            p.dma('sp', 'ost', (lambda tt: lambda e: e.dma_start(out=out_d[tt * 128:(tt + 1) * 128, :], in_=x1t))(tt), reads=['x1t'], writes=['x1t', 'out'])
        p.barrier()
        p.emit(st)
    return nc, p


def t5_bucket_np(dist):
    max_exact = 16
    d = np.maximum(dist, 0)
    lr = np.log(np.maximum(d, 1).astype(np.float32) / max_exact) / math.log(128 / max_exact)
    large = max_exact + (lr * (32 - max_exact)).astype(np.int32)
    large = np.minimum(large, 31)
    return np.where(d < max_exact, d, large)


def make_in_maps(inputs, cores=range(8)):
    f = lambda a: np.ascontiguousarray(np.asarray(a, dtype=np.float32))
    x = f(inputs["x"]); c = f(inputs["c"])
    fm = lambda v, n: np.ascontiguousarray(v.reshape(n, 128).T)
    onehot = np.zeros((32, 128), np.float32)
    onehot[t5_bucket_np(np.arange(128)), np.arange(128)] = 1.0
    sel = np.zeros((24, 8, 128), np.float32)
    for h in range(8):
        for part in range(3):
            sel[part * 8 + h, h, :] = 1.0
    keys = f(inputs["peer_keys"])[0]
    keys_t = np.ascontiguousarray(keys.transpose(3, 0, 1, 2).reshape(128, 16 * 128))
    shared = {
        "w_ada": f(inputs["w_ada"])[0],
        "b_ada_t": fm(f(inputs["b_ada"])[0], 192),
        "g1_t": fm(f(inputs["norm1_g"])[0], 32),
        "g2_t": fm(f(inputs["norm2_g"])[0], 32),
        "w_in": f(inputs["w_in"])[0],
        "sinks": f(inputs["attn_sinks"])[0].reshape(1, 16),
        "rel_bias": f(inputs["rel_bias"]),
        "onehot": onehot,
        "gk2_w": f(inputs["gla_w_gk2"])[0],
        "gk2_b": f(inputs["gla_b_gk2"])[0].reshape(1, 1024),
        "gla_norm_g": f(inputs["gla_norm_g"])[0].reshape(1, 512),
        "w_out": f(inputs["w_out"])[0],
        "w_q": f(inputs["peer_w_q"])[0],
        "keys_t": keys_t,
        "u_t": np.ascontiguousarray(f(inputs["peer_u"])[0].T),
        "v": f(inputs["peer_v"])[0],
        "final_g": f(inputs["final_g"]).reshape(1, D),
        "sel": sel.reshape(24, 8 * 128),
    }
    maps = []
    for i in cores:
        b, half = i // 2, i % 2
        m = dict(shared)
        m["x_main"] = np.ascontiguousarray(x[b, half * TOK:(half + 1) * TOK])
        m["x_pre"] = np.ascontiguousarray(x[b, 0:TOK])
        m["flag"] = np.full((128, 1), float(half), np.float32)
        m["c_t"] = fm(c[b], 32)
        maps.append(m)
    return maps


_CACHE = {}


def kernel(**inputs):
    if "nc" not in _CACHE:
        _CACHE["nc"] = build_program()[0]
    nc = _CACHE["nc"]
    maps = make_in_maps(inputs)
    res = run_bass_kernel_spmd(nc, maps, core_ids=list(range(8)))
    out = np.empty((4, 2048, D), np.float32)
    for i in range(8):
        b, half = i // 2, i % 2
        out[b, half * TOK:(half + 1) * TOK] = res.results[i]["out"]
    return out
```

```python
from contextlib import ExitStack
import math
import numpy as np
import concourse.bass as bass
import concourse.mybir as mybir
from concourse.bass_utils import run_bass_kernel_spmd

F32 = mybir.dt.float32
BF16 = mybir.dt.bfloat16
AF = mybir.ActivationFunctionType
ALU = mybir.AluOpType

SAME_ENG_SYNC = True

D = 4096
KC = 32
TOK = 1024
MT = 512
NEXP = 16384
EPS = 1e-6
NEGM = -30000.0
IN_W = 8720
C_AQ, C_AK, C_AV, C_GQ, C_GK, C_GV, C_GLOW, C_GOUT = 0, 2048, 2304, 2560, 3584, 4608, 6656, 6672


class Prog:
    def __init__(self, nc):
        self.nc = nc
        self.ins = []
        self.last_w = {}
        self.readers = {}
        self.last_on = {}
        self.dmas_open = []

    maxops = None

    def op(self, eng, fn, reads=(), writes=(), dma=None, extra_deps=(), force=False):
        if self.maxops is not None and len(self.ins) >= self.maxops and not force:
            return None
        i = len(self.ins)
        deps = set(extra_deps)
        psk = [k for k in reads if k == 'ps0' or (isinstance(k, tuple) and k[0] == 'ps')]
        if psk:
            reads = [k for k in reads if k not in psk]
            writes = list(writes) + [k for k in psk if k not in writes]
        for k in reads:
            w = self.last_w.get(k)
            if w is not None:
                deps.add(w)
        for k in writes:
            w = self.last_w.get(k)
            if w is not None:
                deps.add(w)
            for r in self.readers.get(k, ()):
                deps.add(r)
        for k in reads:
            lst = self.readers.setdefault(k, [])
            if dma is None:
                for q in range(len(lst)):
                    J = self.ins[lst[q]]
                    if J['dma'] is None and J['eng'] == eng:
                        lst[q] = i
                        break
                else:
                    lst.append(i)
            else:
                lst.append(i)
        for k in writes:
            self.last_w[k] = i
            self.readers[k] = []
        deps.discard(i)
        self.ins.append(dict(eng=eng, fn=fn, deps=deps, dma=dma))
        if dma is None:
            self.last_on[eng] = i
        else:
            self.dmas_open.append(i)
        return i

    def pe(self, fn, reads=(), writes=()):
        return self.op('pe', fn, reads, writes)

    def act(self, fn, reads=(), writes=()):
        return self.op('act', fn, reads, writes)

    def dve(self, fn, reads=(), writes=()):
        return self.op('dve', fn, reads, writes)

    def pool(self, fn, reads=(), writes=()):
        return self.op('pool', fn, reads, writes)

    def dma(self, eng, key, fn, reads=(), writes=()):
        return self.op(eng, fn, reads, writes, dma=key)

    def barrier(self):
        deps = set(self.last_on.values()) | set(self.dmas_open)
        self.dmas_open = []
        for e in ['pe', 'act', 'dve', 'pool', 'sp']:
            self.op(e, lambda eng: eng.nop(), extra_deps=deps, force=True)
        self.last_w = {}
        self.readers = {}

    def emit(self, stack):
        nc = self.nc
        ins = self.ins
        n = len(ins)
        engs = ['pe', 'act', 'dve', 'pool', 'sp']

        def needs_wait(I, Dd):
            if Dd['dma'] is not None:
                return True
            if Dd['eng'] == I['eng'] and I['dma'] is None:
                if Dd['eng'] in ('pe', 'sp') or not SAME_ENG_SYNC:
                    return False
            return True

        need_sig = [False] * n
        for i, I in enumerate(ins):
            for d in I['deps']:
                Dd = ins[d]
                if Dd['dma'] is None and needs_wait(I, Dd):
                    need_sig[d] = True
        cnt = {e: 0 for e in engs}
        sigval = [0] * n
        dma_cnt = {}
        for i, I in enumerate(ins):
            if I['dma'] is not None:
                k = I['dma']
                dma_cnt[k] = dma_cnt.get(k, 0) + 16
                sigval[i] = dma_cnt[k]
            elif need_sig[i]:
                cnt[I['eng']] += 1
                sigval[i] = cnt[I['eng']]
        esem = {e: stack.enter_context(nc.semaphore('s_' + e)) for e in engs}
        dsem = {k: stack.enter_context(nc.semaphore('d_%d' % j)) for j, k in enumerate(dma_cnt)}
        self.stats = dict(n=n, cnt=dict(cnt), ndsem=len(dsem), maxdma=max(dma_cnt.values()) if dma_cnt else 0)
        per_eng = {e: [] for e in engs}
        for i, I in enumerate(ins):
            per_eng[I['eng']].append(i)

        def run(e, engobj):
            waited = {}
            for i in per_eng[e]:
                I = ins[i]
                need = {}
                for d in I['deps']:
                    Dd = ins[d]
                    if not needs_wait(I, Dd):
                        continue
                    if Dd['dma'] is not None:
                        key = ('d', Dd['dma'])
                    else:
                        key = ('e', Dd['eng'])
                    v = sigval[d]
                    if need.get(key, 0) < v:
                        need[key] = v
                for key, v in need.items():
                    if waited.get(key, 0) >= v:
                        continue
                    waited[key] = v
                    s = dsem[key[1]] if key[0] == 'd' else esem[key[1]]
                    engobj.wait_ge(s, v)
                r = I['fn'](engobj)
                if I['dma'] is not None:
                    r.then_inc(dsem[I['dma']], 16)
                elif need_sig[i]:
                    r.then_inc(esem[e], 1)

        block = stack.enter_context(nc.Block())
        block.sync(lambda eng: run('sp', eng))
        block.tensor(lambda eng: run('pe', eng))
        block.scalar(lambda eng: run('act', eng))
        block.vector(lambda eng: run('dve', eng))
        block.gpsimd(lambda eng: run('pool', eng))


class Arena:
    def __init__(self, nc, nbytes):
        self.nbytes = nbytes
        self.t = nc.alloc_sbuf_tensor("arena", [128, nbytes // 4], F32)

    def view(self, off, shape, dtype):
        esz = 4 if dtype == F32 else 2
        nel = int(np.prod(shape))
        nb = nel * esz
        assert off % 4 == 0 and nb % 4 == 0, (off, shape)
        assert off + nb <= self.nbytes, (off, nb, self.nbytes)
        ap = self.t[:, off // 4: (off + nb) // 4]
        if dtype != F32:
            ap = ap.bitcast(dtype)
        if len(shape) == 2:
            names = "a b"
            ap = ap.rearrange("p (a b) -> p a b", a=shape[0], b=shape[1])
        elif len(shape) == 3:
            ap = ap.rearrange("p (a b c) -> p a b c", a=shape[0], b=shape[1], c=shape[2])
        return ap


def build_program(dbg=None, upto=99, stop=None, fake_mod=False):
    nc = bass.Bass("TRN2", target_bir_lowering=False)
    dbg = dbg or {}

    def din(name, shape, dt=F32):
        return nc.dram_tensor(name, list(shape), dt, kind="ExternalInput").ap()

    x_main = din("x_main", [TOK, D])
    x_pre = din("x_pre", [TOK, D])
    flag_d = din("flag", [128, 1])
    c_d = din("c_t", [128, KC])
    wada_d = din("w_ada", [D, 6 * D]) if not fake_mod else None
    bada_d = din("b_ada_t", [128, 6 * KC])
    g1_d = din("g1_t", [128, KC])
    g2_d = din("g2_t", [128, KC])
    win_d = din("w_in", [D, IN_W])
    sinks_d = din("sinks", [1, 16])
    relb_d = din("rel_bias", [32, 16])
    oh_d = din("onehot", [32, 128])
    gk2w_d = din("gk2_w", [16, 1024])
    gk2b_d = din("gk2_b", [1, 1024])
    gng_d = din("gla_norm_g", [1, 512])
    wout_d = din("w_out", [D, D])
    if upto > 2:
        wq_d = din("w_q", [D, 2048])
        keyst_d = din("keys_t", [128, 16 * 128])
        ut_d = din("u_t", [D, NEXP])
        v_d = din("v", [NEXP, D])
        fg_d = din("final_g", [1, D])
        sel_d = din("sel", [24, 8 * 128])
    if fake_mod:
        modt_d = din("modT_dbg", [128, 192])
    out_d = nc.dram_tensor("out", [TOK, D], F32, kind="ExternalOutput").ap()
    x1s = nc.dram_tensor("x1s", [TOK, D], F32).ap()
    aws = nc.dram_tensor("aws", [128, 128, TOK], BF16).ap()
    gext = nc.dram_tensor("gext", [16, 384], F32).ap()
    dbg_out = {}
    for name, shape in dbg.items():
        dbg_out[name] = nc.dram_tensor("dbg_" + name, list(shape), F32, kind="ExternalOutput").ap()

    st = ExitStack()
    with st:
        A = Arena(nc, 200 * 1024)
        PS = nc.alloc_psum_tensor("ps", [128, 8, 512], F32)
        p = Prog(nc)
        import os
        if os.environ.get('K_MAXOPS'):
            p.maxops = int(os.environ['K_MAXOPS'])
        R_HT, R_CT, R_W, R_S, R_P = 0, 32768, 65536, 65536 + 49152, 65536 + 2 * 49152
        po = [R_P]

        def palloc(shape, dt):
            esz = 4 if dt == F32 else 2
            nb = int(np.prod(shape)) * esz
            nb = (nb + 31) // 32 * 32
            v = A.view(po[0], shape, dt)
            po[0] += nb
            return v

        ident_f = A.view(po[0], [128], F32); po[0] += 512
        ident_b = A.view(po[0], [128], BF16); po[0] += 256
        ones_b = A.view(po[0], [128], BF16); po[0] += 256
        ones_f = A.view(po[0], [128], F32); po[0] += 512
        Lm = A.view(po[0], [128], F32); po[0] += 512
        Um = A.view(po[0], [128], F32); po[0] += 512
        modT = A.view(po[0], [192], F32); po[0] += 768
        badat = A.view(po[0], [192], F32); po[0] += 768
        a1 = A.view(po[0], [32], F32); po[0] += 128
        a2 = A.view(po[0], [32], F32); po[0] += 128
        g1t = A.view(po[0], [32], F32); po[0] += 128
        g2t = A.view(po[0], [32], F32); po[0] += 128
        ct = A.view(po[0], [32], F32); po[0] += 128
        cact = A.view(po[0], [32], BF16); po[0] += 64
        flag = A.view(po[0], [8], F32); po[0] += 32
        small = A.view(po[0], [64], F32); po[0] += 256
        esink = A.view(po[0], [16], F32); po[0] += 64
        gnb = A.view(po[0], [512], F32); po[0] += 2048
        gk2w = A.view(po[0], [1024], BF16); po[0] += 2048
        gk2b = A.view(po[0], [1024], BF16); po[0] += 2048
        P_DYN = po[0]
        bcur = A.view(po[0], [16, 128], BF16); po[0] += 4096
        bprev = A.view(po[0], [16, 128], BF16); po[0] += 4096
        Sst = A.view(po[0], [4, 2, 512], F32); po[0] += 16384
        assert po[0] <= 200 * 1024, po[0]

        s1 = modT[:, 0:32]
        gate1 = modT[:, 64:96]
        s2 = modT[:, 96:128]
        gate2 = modT[:, 160:192]

        def bank(b, n=512):
            return PS[:, b, 0:n]

        p.pool(lambda e: e.memset(ident_f, 1.0), writes=['ident_f'])
        p.pool(lambda e: e.affine_select(out=ident_f, in_=ident_f, pattern=[[-1, 128]], compare_op=ALU.is_equal,
                                         fill=0.0, base=0, channel_multiplier=1), reads=['ident_f'], writes=['ident_f'])
        p.pool(lambda e: e.memset(ones_f, 1.0), writes=['ones_f'])
        p.pool(lambda e: e.memset(ones_b, 1.0), writes=['ones_b'])
        p.dve(lambda e: e.tensor_copy(out=ident_b, in_=ident_f), reads=['ident_f'], writes=['ident_b'])
        p.pool(lambda e: e.memset(Lm, 1.0), writes=['Lm'])
        p.pool(lambda e: e.affine_select(out=Lm, in_=Lm, pattern=[[1, 128]], compare_op=ALU.is_ge,
                                         fill=0.0, base=0, channel_multiplier=-1), reads=['Lm'], writes=['Lm'])
        p.pool(lambda e: e.memset(Lm[0:64, 64:128], 0.0), reads=['Lm'], writes=['Lm'])
        p.pool(lambda e: e.memset(Um, 1.0), writes=['Um'])
        p.pool(lambda e: e.affine_select(out=Um, in_=Um, pattern=[[-1, 128]], compare_op=ALU.is_gt,
                                         fill=0.0, base=0, channel_multiplier=1), reads=['Um'], writes=['Um'])
        p.pool(lambda e: e.memset(Um[64:128, 0:64], 0.0), reads=['Um'], writes=['Um'])
        p.pool(lambda e: e.memset(Sst, 0.0), writes=['Sst'])

        sm = 'small_ld'
        p.dma('sp', sm, lambda e: e.dma_start(out=ct, in_=c_d), writes=['ct'])
        p.dma('sp', sm, lambda e: e.dma_start(out=badat, in_=bada_d), writes=['badat'])
        p.dma('sp', sm, lambda e: e.dma_start(out=g1t, in_=g1_d), writes=['g1t'])
        p.dma('sp', sm, lambda e: e.dma_start(out=g2t, in_=g2_d), writes=['g2t'])
        p.dma('sp', sm, lambda e: e.dma_start(out=flag[:, 0:1], in_=flag_d), writes=['flag'])
        p.dma('sp', sm, lambda e: e.dma_start(out=esink, in_=sinks_d.partition_broadcast(128)[:, 0, :]), writes=['esink'])
        p.dma('sp', sm, lambda e: e.dma_start(out=gnb, in_=gng_d.partition_broadcast(128)[:, 0, :]), writes=['gnb'])
        p.dma('pool', 'small_ld2', lambda e: e.dma_start(out=gk2w[0:16, :], in_=gk2w_d), writes=['gk2w'])
        p.dma('pool', 'small_ld2', lambda e: e.dma_start(out=gk2b[0:1, :], in_=gk2b_d), writes=['gk2b'])
        relb = A.view(R_S, [16], F32)
        ohs = A.view(R_S + 64, [128], F32)
        gx = A.view(R_S + 1024, [384], F32)
        p.dma('sp', sm, lambda e: e.dma_start(out=relb[0:32, :], in_=relb_d), writes=['relb'])
        p.dma('sp', sm, lambda e: e.dma_start(out=ohs[0:32, :], in_=oh_d), writes=['ohs'])
        p.barrier()
        p.act(lambda e: e.activation(out=esink, in_=esink, func=AF.Exp), reads=['esink'], writes=['esink'])
        p.pe(lambda e: e.matmul(PS[0:16, 0, 0:128], lhsT=relb[0:32, :], rhs=ohs[0:32, :], start=True, stop=True),
             reads=['relb', 'ohs'], writes=['ps0'])
        p.pool(lambda e: e.memset(gx[0:16, :], NEGM), writes=['gx'])
        p.dve(lambda e: e.tensor_copy(out=gx[0:16, 128:256], in_=PS[0:16, 0, 0:128]), reads=['ps0', 'gx'], writes=['gx'])
        p.dma('sp', 'gx', lambda e: e.dma_start(out=gext, in_=gx[0:16, :]), reads=['gx'], writes=['gext'])
        p.barrier()
        bcf = A.view(R_S + 4096, [16, 128], F32)
        bpf = A.view(R_S + 4096 + 8192, [16, 128], F32)
        for k in range(128):
            p.dma('sp', 'bias_ld', (lambda k: lambda e: e.dma_start(out=bcf[k:k + 1, :, :], in_=gext[:, 128 - k:256 - k].unsqueeze(0)))(k),
                  writes=[('bcf', k)])
            p.dma('sp', 'bias_ld', (lambda k: lambda e: e.dma_start(out=bpf[k:k + 1, :, :], in_=gext[:, 256 - k:384 - k].unsqueeze(0)))(k),
                  writes=[('bpf', k)])
        p.barrier()
        p.dve(lambda e: e.tensor_copy(out=bcur, in_=bcf), writes=['bcur'])
        p.dve(lambda e: e.tensor_copy(out=bprev, in_=bpf), writes=['bprev'])
        if 'bcur' in dbg:
            p.dma('sp', 'dbg', lambda e: e.dma_start(out=dbg_out['bcur'], in_=bcf), reads=['bcur'], writes=['dbg_bcur'])
        p.barrier()

        p.act(lambda e: e.activation(out=cact, in_=ct, func=AF.Silu), writes=['cact'])
        wada_v = wada_d.rearrange("(k p) c -> p k c", p=128) if not fake_mod else None
        PS_fake = A.view(R_S, [192], F32)
        Wt = [A.view(R_W + i * 16384, [32, 256], BF16) for i in range(3)]
        wctr = [0]

        def load_w(view, c0, ncols):
            slot = wctr[0] % 3
            wctr[0] += 1
            t = Wt[slot]
            p.dma('pool', ('W', slot), lambda e: e.dma_start(out=t[:, :, 0:ncols], in_=view[:, :, c0:c0 + ncols]),
                  writes=[('W', slot)])
            return t, ('W', slot)

        if fake_mod:
            p.dma('sp', 'fm', lambda e: e.dma_start(out=PS_fake, in_=modt_d), writes=['ps0'])
        for tno in range(0 if fake_mod else 6 * D // 256):
            wt, wk = load_w(wada_v, tno * 256, 256)
            for cc in range(2):
                j = tno * 2 + cc
                for k in range(KC):
                    p.pe((lambda wt, cc, j, k: lambda e: e.matmul(PS[:, 0, j:j + 1], lhsT=wt[:, k, cc * 128:(cc + 1) * 128],
                                                                  rhs=cact[:, k:k + 1], start=(k == 0), stop=(k == KC - 1)))(wt, cc, j, k),
                         reads=[wk, 'cact'], writes=['ps0'])
        if fake_mod:
            p.dve(lambda e: e.tensor_copy(out=modT, in_=PS_fake), reads=['ps0'], writes=['modT'])
        else:
            p.dve(lambda e: e.tensor_tensor(out=modT, in0=PS[:, 0, 0:192], in1=badat, op=ALU.add), reads=['ps0'], writes=['modT'])
        p.dve(lambda e: e.scalar_tensor_tensor(out=a1, in0=modT[:, 32:64], scalar=1.0, in1=g1t, op0=ALU.add, op1=ALU.mult),
              reads=['modT'], writes=['a1'])
        p.dve(lambda e: e.scalar_tensor_tensor(out=a2, in0=modT[:, 128:160], scalar=1.0, in1=g2t, op0=ALU.add, op1=ALU.mult),
              reads=['modT'], writes=['a2'])
        if 'modT' in dbg:
            p.dma('sp', 'dbg', lambda e: e.dma_start(out=dbg_out['modT'], in_=modT), reads=['modT'], writes=['dbg_modT'])
        p.barrier()
        if upto <= 1:
            p.emit(st)
            return nc, p

        def rms_norm_to_hT(src, row0, ntiles, a_t, s_t, hT, xs_off, junk_off):
            xs = [A.view(xs_off + i * 16384, [4096], F32) for i in range(2)]
            junk = A.view(junk_off, [4096], BF16)
            for tt in range(ntiles):
                xt = xs[tt % 2]
                xk = ('xs', tt % 2)
                sc = small[:, (tt % 2) * 4:(tt % 2) * 4 + 4]
                sk = ('nsc', tt % 2)
                p.dma('sp', xk, (lambda xt, tt: lambda e: e.dma_start(out=xt, in_=src[row0 + tt * 128: row0 + (tt + 1) * 128, :]))(xt, tt),
                      writes=[xk])
                p.act((lambda xt, sc: lambda e: e.activation(out=junk, in_=xt, func=AF.Square, accum_out=sc[:, 0:1]))(xt, sc),
                      reads=[xk], writes=['junk', sk])
                p.dve((lambda sc: lambda e: e.tensor_scalar(out=sc[:, 1:2], in0=sc[:, 0:1], scalar1=1.0 / D, scalar2=EPS,
                                                            op0=ALU.mult, op1=ALU.add))(sc), reads=[sk], writes=[sk])
                p.act((lambda sc: lambda e: e.activation(out=sc[:, 2:3], in_=sc[:, 1:2], func=AF.Sqrt))(sc), reads=[sk], writes=[sk])
                p.dve((lambda sc: lambda e: e.reciprocal(out=sc[:, 3:4], in_=sc[:, 2:3]))(sc), reads=[sk], writes=[sk])
                p.act((lambda xt, sc: lambda e: e.activation(out=xt, in_=xt, func=AF.Copy, scale=sc[:, 3:4]))(xt, sc),
                      reads=[xk, sk], writes=[xk])
                for g4 in range(8):
                    b = 4 + (g4 % 2)
                    for q in range(4):
                        c = g4 * 4 + q
                        p.pe((lambda xt, b, q, c: lambda e: e.transpose(PS[:, b, q * 128:(q + 1) * 128], xt[:, c * 128:(c + 1) * 128], ident_f))(xt, b, q, c),
                             reads=[xk, 'ident_f'], writes=[('ps', b)])
                    for q in range(4):
                        c = g4 * 4 + q
                        if g4 % 2 == 0:
                            p.dve((lambda b, q, c, tt: lambda e: e.tensor_scalar(out=hT[:, c, tt * 128:(tt + 1) * 128], in0=PS[:, b, q * 128:(q + 1) * 128],
                                                                                 scalar1=a_t[:, c:c + 1], scalar2=s_t[:, c:c + 1], op0=ALU.mult, op1=ALU.add))(b, q, c, tt),
                                  reads=[('ps', b)], writes=[('hT', c)])
                        else:
                            p.act((lambda b, q, c, tt: lambda e: e.activation(out=hT[:, c, tt * 128:(tt + 1) * 128], in_=PS[:, b, q * 128:(q + 1) * 128],
                                                                              func=AF.Identity, scale=a_t[:, c:c + 1], bias=s_t[:, c:c + 1]))(b, q, c, tt),
                                  reads=[('ps', b)], writes=[('hT', c)])

        pbank = [0]

        def next_pbank():
            b = pbank[0] % 2
            pbank[0] += 1
            return b

        hT_keys = [('hT', c) for c in range(KC)]

        def gemm_fm(wview, c0, ncols, hT, T, evac, src_keys):
            done = 0
            while done < ncols:
                n = min(256, ncols - done)
                wt, wk = load_w(wview, c0 + done, n)
                for cc in range((n + 127) // 128):
                    m = min(128, n - cc * 128)
                    for h0 in range(0, T, 512):
                        b = next_pbank()
                        for k in range(KC):
                            p.pe((lambda wt, cc, m, b, k, h0: lambda e: e.matmul(PS[0:m, b, 0:min(512, T - h0)], lhsT=wt[:, k, cc * 128:cc * 128 + m],
                                                                                 rhs=hT[:, k, h0:h0 + min(512, T - h0)], start=(k == 0), stop=(k == KC - 1)))(wt, cc, m, b, k, h0),
                                 reads=[wk] + src_keys, writes=[('ps', b)])
                        evac((done // 128) + cc, b, h0)
                done += n

        def gemm_tm(wview, c0, ncols, hT, T, evac, src_keys, also_fm=None):
            done = 0
            while done < ncols:
                n = min(256, ncols - done)
                wt, wk = load_w(wview, c0 + done, n)
                for tt in range(T // 128):
                    b = next_pbank()
                    for k in range(KC):
                        p.pe((lambda wt, n, b, k, tt: lambda e: e.matmul(PS[:, b, 0:n], lhsT=hT[:, k, tt * 128:(tt + 1) * 128],
                                                                         rhs=wt[:, k, 0:n], start=(k == 0), stop=(k == KC - 1)))(wt, n, b, k, tt),
                             reads=[wk] + src_keys, writes=[('ps', b)])
                    evac(done, n, tt, b)
                if also_fm is not None:
                    for cc in range(n // 128):
                        b = next_pbank()
                        for k in range(KC):
                            p.pe((lambda wt, cc, b, k: lambda e: e.matmul(PS[:, b, 0:T], lhsT=wt[:, k, cc * 128:(cc + 1) * 128],
                                                                          rhs=hT[:, k, 0:T], start=(k == 0), stop=(k == KC - 1)))(wt, cc, b, k),
                                 reads=[wk] + src_keys, writes=[('ps', b)])
                        also_fm((done // 128) + cc, b)
                done += n

        win_v = win_d.rearrange("(k p) c -> p k c", p=128)
        wout_v = wout_d.rearrange("(k p) c -> p k c", p=128)
        hT = A.view(R_HT, [32, MT], BF16)
        cT = A.view(R_CT, [32, MT], BF16)
        so = [R_S]

        def salloc(shape, dt):
            esz = 4 if dt == F32 else 2
            nb = (int(np.prod(shape)) * esz + 31) // 32 * 32
            v = A.view(so[0], shape, dt)
            so[0] += nb
            assert so[0] <= R_S + 49152, so[0]
            return v

        KT = salloc([2, 5 * 128], BF16)
        Vt = salloc([5, 2, 128], BF16)
        glowT = salloc([MT], BF16)
        S_AFTER_KV = so[0]
        QTg = salloc([4, 8, 128], BF16)
        tmpS = [salloc([512], F32) for _ in range(2)]
        PT = [salloc([512], BF16) for _ in range(2)]
        den = salloc([512], F32)
        so[0] = S_AFTER_KV
        gqT = salloc([2, MT], BF16)
        gkT = salloc([2, MT], BF16)
        gkt = salloc([4, 256], BF16)
        gv = salloc([4, 512], BF16)
        SG = salloc([4, 512], BF16)
        la = salloc([4, 256], F32)
        eb = salloc([2, MT], F32)
        enb = salloc([2, MT], F32)
        qdT = salloc([2, MT], BF16)
        kinvT = salloc([2, MT], BF16)
        kdec = salloc([4, 256], BF16)
        dl = salloc([2, 8], F32)
        Sbf = salloc([2, 512], BF16)
        attn_sb = salloc([64], BF16)
        ybf = salloc([512], BF16)
        tmp256 = salloc([256], F32)
        GLA_END = so[0]
        so[0] = S_AFTER_KV
        G1B = salloc([4096], F32)
        xres = [salloc([4, 256], F32) for _ in range(2)]
        dtmp = salloc([128], F32)

        PSb = PS[:, :, :].bitcast(BF16) if False else None

        p.pool(lambda e: e.memset(KT, 0.0), writes=['KT'])
        p.pool(lambda e: e.memset(Vt, 0.0), writes=['Vt'])

        def build_rowbcast(dst, colvec, tag):
            for c in range(KC):
                b = 2 + (c // 4) % 2
                q = c % 4
                p.dve((lambda c: lambda e: e.tensor_scalar(out=dtmp, in0=ident_f, scalar1=colvec[:, c:c + 1], scalar2=None, op0=ALU.mult))(c),
                      reads=['ident_f', 'modT'], writes=['dtmp'])
                p.pe((lambda b, q: lambda e: e.matmul(PS[:, b, q * 128:(q + 1) * 128], lhsT=ones_f, rhs=dtmp, start=True, stop=True))(b, q),
                     reads=['dtmp', 'ones_f'], writes=[('ps', b)])
                if q == 3:
                    p.act((lambda b, c: lambda e: e.activation(out=dst[:, (c - 3) * 128:(c + 1) * 128], in_=PS[:, b, :], func=AF.Identity))(b, c),
                          reads=[('ps', b)], writes=[tag])

        macro = [('P', 0), ('P', 1), ('M', 0), ('M', 1)]
        for kind, mi in macro:
            main = kind == 'M'
            src = x_main if main else x_pre
            rms_norm_to_hT(src, mi * MT, MT // 128, a1, s1, hT, R_CT, R_W + 32768)
            p.barrier()
            if stop == 'norm%s%d' % (kind, mi):
                p.barrier()
                p.emit(st)
                return nc, p
            need_kv = main or mi == 1

            if need_kv:
                def ev_k(ci, b, h0):
                    p.act((lambda ci, b: lambda e: e.activation(out=KT[:, ci, 128:128 + MT], in_=PS[:, b, 0:MT], func=AF.Identity))(ci, b),
                          reads=[('ps', b)], writes=['KT'])
                gemm_fm(win_v, C_AK, 256, hT, MT, ev_k, hT_keys)

                def ev_v(c0, n, tt, b):
                    p.dve((lambda tt, b: lambda e: e.tensor_copy(out=Vt[:, 1 + tt, :, :], in_=PS[:, b, 0:256].rearrange("p (g d) -> p g d", g=2)))(tt, b),
                          reads=[('ps', b)], writes=['Vt'])
                gemm_tm(win_v, C_AV, 256, hT, MT, ev_v, hT_keys)
            if main:
                for g in range(2):
                    def ev_q(ci, b, h0, g=g):
                        p.act((lambda ci, b: lambda e: e.activation(out=QTg[:, :, ci, :], in_=PS[:, b, 0:MT].rearrange("p (n q) -> p n q", n=4),
                                                                    func=AF.Copy, scale=128 ** -0.5))(ci, b),
                              reads=[('ps', b)], writes=['QTg'])
                    gemm_fm(win_v, C_AQ + g * 1024, 1024, hT, MT, ev_q, hT_keys)
                    for n in range(4):
                        for hf in range(2):
                            hs = slice(hf * 4, hf * 4 + 4)
                            hg = slice(g * 8 + hf * 4, g * 8 + hf * 4 + 4)
                            rq = QTg[:, n, hs, :].rearrange("p j q -> p (j q)")
                            p.pe((lambda g, n, rq: lambda e: e.matmul(PS[:, 2, :], lhsT=KT[:, g, (n + 1) * 128:(n + 2) * 128], rhs=rq, start=True, stop=True))(g, n, rq),
                                 reads=['KT', 'QTg'], writes=[('ps', 2)])
                            p.pe((lambda g, n, rq: lambda e: e.matmul(PS[:, 3, :], lhsT=KT[:, g, n * 128:(n + 1) * 128], rhs=rq, start=True, stop=True))(g, n, rq),
                                 reads=['KT', 'QTg'], writes=[('ps', 3)])
                            for w_, (bk, bt) in enumerate([(2, bcur), (3, bprev)]):
                                p.dve((lambda w_, bk, bt, hg: lambda e: e.tensor_tensor(out=tmpS[w_], in0=PS[:, bk, :], in1=bt[:, hg, :].rearrange("p j q -> p (j q)"), op=ALU.add))(w_, bk, bt, hg),
                                      reads=[('ps', bk)], writes=[('tmpS', w_)])
                                p.act((lambda w_: lambda e: e.activation(out=PT[w_], in_=tmpS[w_], func=AF.Exp))(w_),
                                      reads=[('tmpS', w_)], writes=[('PT', w_)])
                            if mi == 0 and n == 0:
                                p.dve(lambda e: e.tensor_scalar(out=PT[1], in0=PT[1], scalar1=flag[:, 0:1], scalar2=None, op0=ALU.mult),
                                      reads=[('PT', 1), 'flag'], writes=[('PT', 1)])
                            p.pe(lambda e: e.matmul(PS[:, 6, :], lhsT=ones_b, rhs=PT[0], start=True, stop=False), reads=[('PT', 0), 'ones_b'], writes=[('ps', 6)])
                            p.pe(lambda e: e.matmul(PS[:, 6, :], lhsT=ones_b, rhs=PT[1], start=False, stop=True), reads=[('PT', 1), 'ones_b'], writes=[('ps', 6)])
                            p.pe((lambda g, n: lambda e: e.matmul(PS[:, 7, :], lhsT=Vt[:, n + 1, g, :], rhs=PT[0], start=True, stop=False))(g, n),
                                 reads=[('PT', 0), 'Vt'], writes=[('ps', 7)])
                            p.pe((lambda g, n: lambda e: e.matmul(PS[:, 7, :], lhsT=Vt[:, n, g, :], rhs=PT[1], start=False, stop=True))(g, n),
                                 reads=[('PT', 1), 'Vt'], writes=[('ps', 7)])
                            for j in range(4):
                                hh = g * 8 + hf * 4 + j
                                p.dve((lambda j, hh: lambda e: e.tensor_scalar(out=den[:, j * 128:(j + 1) * 128], in0=PS[:, 6, j * 128:(j + 1) * 128],
                                                                               scalar1=esink[:, hh:hh + 1], scalar2=None, op0=ALU.add))(j, hh),
                                      reads=[('ps', 6), 'esink'], writes=['den'])
                            p.dve(lambda e: e.reciprocal(out=den, in_=den), reads=['den'], writes=['den'])
                            p.dve((lambda g, n, hf: lambda e: e.tensor_tensor(out=cT[:, g * 8 + hf * 4:g * 8 + hf * 4 + 4, n * 128:(n + 1) * 128],
                                                                              in0=PS[:, 7, :].rearrange("p (j q) -> p j q", j=4),
                                                                              in1=den.rearrange("p (j q) -> p j q", j=4), op=ALU.mult))(g, n, hf),
                                  reads=[('ps', 7), 'den'], writes=[('cT', g * 8 + hf * 4 + jj) for jj in range(4)])
            if need_kv:
                p.pool(lambda e: e.tensor_copy(out=KT[:, :, 0:128], in_=KT[:, :, 512:640]), reads=['KT'], writes=['KT'])
                p.pool(lambda e: e.tensor_copy(out=Vt[:, 0, :, :], in_=Vt[:, 4, :, :]), reads=['Vt'], writes=['Vt'])
            if stop == 'swa%s%d' % (kind, mi):
                p.barrier()
                p.emit(st)
                return nc, p
            if 'cT_attn' in dbg and main and mi == 0:
                p.barrier()
                p.dve(lambda e: e.tensor_copy(out=eb[:, 0, :], in_=cT[:, 0, :]), writes=['dbgtmp'])
                p.dma('sp', 'dbg', lambda e: e.dma_start(out=dbg_out['cT_attn'], in_=eb[:, 0, :]), reads=['dbgtmp'], writes=['dbg_ct'])
            p.barrier()
            def ev_glow(ci, b, h0):
                p.act((lambda b: lambda e: e.activation(out=glowT[0:16, :], in_=PS[0:16, b, 0:MT], func=AF.Identity))(b),
                      reads=[('ps', b)], writes=['glowT'])
            gemm_fm(win_v, C_GLOW, 16, hT, MT, ev_glow, hT_keys)
            for h in range(4):
                if main:
                    def ev_gq(ci, b, h0):
                        p.act((lambda ci, b: lambda e: e.activation(out=gqT[:, ci, :], in_=PS[:, b, 0:MT], func=AF.Identity))(ci, b),
                              reads=[('ps', b)], writes=['gqT'])
                    gemm_fm(win_v, C_GQ + h * 256, 256, hT, MT, ev_gq, hT_keys)

                def ev_gk(c0, n, tt, b):
                    p.dve((lambda tt, b: lambda e: e.tensor_copy(out=gkt[:, tt, :], in_=PS[:, b, 0:256]))(tt, b), reads=[('ps', b)], writes=['gkt'])

                def ev_gkT(ci, b):
                    p.act((lambda ci, b: lambda e: e.activation(out=gkT[:, ci, :], in_=PS[:, b, 0:MT], func=AF.Identity))(ci, b),
                          reads=[('ps', b)], writes=['gkT'])
                gemm_tm(win_v, C_GK + h * 256, 256, hT, MT, ev_gk, hT_keys, also_fm=ev_gkT if main else None)

                def ev_gv(c0, n, tt, b):
                    p.act((lambda c0, tt, b: lambda e: e.activation(out=gv[:, tt, c0:c0 + 256], in_=PS[:, b, 0:256], func=AF.Identity))(c0, tt, b),
                          reads=[('ps', b)], writes=['gv'])
                gemm_tm(win_v, C_GV + h * 512, 512, hT, MT, ev_gv, hT_keys)
                if main:
                    def ev_go(c0, n, tt, b):
                        p.act((lambda b: lambda e: e.activation(out=tmp256, in_=PS[:, b, 0:256], func=AF.Silu))(b),
                              reads=[('ps', b)], writes=['tmp256'])
                        p.dve((lambda c0, tt: lambda e: e.tensor_tensor(out=SG[:, tt, c0:c0 + 256], in0=tmp256, in1=gnb[:, c0:c0 + 256], op=ALU.mult))(c0, tt),
                              reads=['tmp256', 'gnb'], writes=['SG'])
                    gemm_tm(win_v, C_GOUT + h * 512, 512, hT, MT, ev_go, hT_keys)
                for tt in range(4):
                    p.pe((lambda tt, h: lambda e: e.matmul(PS[:, 2, 0:256], lhsT=glowT[0:16, tt * 128:(tt + 1) * 128], rhs=gk2w[0:16, h * 256:(h + 1) * 256],
                                                           start=True, stop=False))(tt, h), reads=['glowT', 'gk2w'], writes=[('ps', 2)])
                    p.pe((lambda tt, h: lambda e: e.matmul(PS[:, 2, 0:256], lhsT=ones_b[0:1, :], rhs=gk2b[0:1, h * 256:(h + 1) * 256],
                                                           start=False, stop=True))(tt, h), reads=['gk2b', 'ones_b'], writes=[('ps', 2)])
                    p.act(lambda e: e.activation(out=tmp256, in_=PS[:, 2, 0:256], func=AF.Exp, scale=-1.0), reads=[('ps', 2)], writes=['tmp256'])
                    p.act(lambda e: e.activation(out=tmp256, in_=tmp256, func=AF.Ln, bias=1.0), reads=['tmp256'], writes=['tmp256'])
                    p.act((lambda tt: lambda e: e.activation(out=la[:, tt, :], in_=tmp256, func=AF.Copy, scale=-1.0 / 16.0))(tt),
                          reads=['tmp256'], writes=['la'])
                for dc in range(2):
                    for tt in range(4):
                        p.pe((lambda dc, tt: lambda e: e.matmul(PS[:, 2 + dc, tt * 128:(tt + 1) * 128], lhsT=la[:, tt, dc * 128:(dc + 1) * 128], rhs=Lm,
                                                                start=True, stop=True))(dc, tt), reads=['la', 'Lm'], writes=[('ps', 2 + dc)])
                p.act(lambda e: e.activation(out=eb, in_=PS[:, 2:4, :], func=AF.Exp), reads=[('ps', 2), ('ps', 3)], writes=['eb'])
                if main:
                    p.act(lambda e: e.activation(out=enb, in_=PS[:, 2:4, :], func=AF.Exp, scale=-1.0), reads=[('ps', 2), ('ps', 3)], writes=['enb'])
                    p.dve(lambda e: e.scalar_tensor_tensor(out=qdT, in0=gqT, scalar=1.0 / 16.0, in1=eb, op0=ALU.mult, op1=ALU.mult),
                          reads=['gqT', 'eb'], writes=['qdT'])
                    p.dve(lambda e: e.tensor_tensor(out=kinvT, in0=gkT, in1=enb, op=ALU.mult), reads=['gkT', 'enb'], writes=['kinvT'])
                p.dve(lambda e: e.tensor_copy(out=dl, in_=eb[:, :, 63::64]), reads=['eb'], writes=['dl'])
                for tt in range(4):
                    p.pe((lambda tt: lambda e: e.matmul(PS[:, 2, 0:256], lhsT=Um, rhs=la[:, tt, :], start=True, stop=True))(tt),
                         reads=['la', 'Um'], writes=[('ps', 2)])
                    p.act(lambda e: e.activation(out=tmp256, in_=PS[:, 2, 0:256], func=AF.Exp), reads=[('ps', 2)], writes=['tmp256'])
                    p.dve((lambda tt: lambda e: e.tensor_tensor(out=kdec[:, tt, :], in0=gkt[:, tt, :], in1=tmp256, op=ALU.mult))(tt),
                          reads=['gkt', 'tmp256'], writes=['kdec'])
                p.act((lambda h: lambda e: e.activation(out=Sbf, in_=Sst[:, h, :, :], func=AF.Identity))(h), reads=['Sst'], writes=['Sbf'])
                for c in range(8):
                    tt, par = c // 2, c % 2
                    r0 = 64 * par
                    t0 = c * 64
                    rs = slice(r0, r0 + 64)
                    if main:
                        for dc in range(2):
                            p.pe((lambda dc, t0, rs: lambda e: e.matmul(PS[rs, 3, 0:64], lhsT=kinvT[:, dc, t0:t0 + 64], rhs=qdT[:, dc, t0:t0 + 64],
                                                                        start=(dc == 0), stop=(dc == 1)))(dc, t0, rs),
                                 reads=['kinvT', 'qdT'], writes=[('ps', 3)])
                        p.dve((lambda rs: lambda e: e.tensor_tensor(out=attn_sb[rs, :], in0=PS[rs, 3, 0:64], in1=Lm[rs, rs], op=ALU.mult))(rs),
                              reads=[('ps', 3), 'Lm'], writes=['attn_sb'])
                        p.pe((lambda rs, tt: lambda e: e.matmul(PS[rs, 6, :], lhsT=attn_sb[rs, :], rhs=gv[rs, tt, :], start=True, stop=False))(rs, tt),
                             reads=['attn_sb', 'gv'], writes=[('ps', 6)])
                        for dc in range(2):
                            p.pe((lambda dc, t0, rs: lambda e: e.matmul(PS[rs, 6, :], lhsT=qdT[:, dc, t0:t0 + 64], rhs=Sbf[:, dc, :],
                                                                        start=False, stop=(dc == 1)))(dc, t0, rs),
                                 reads=['qdT', 'Sbf'], writes=[('ps', 6)])
                    for dc in range(2):
                        p.pe((lambda dc, rs, tt: lambda e: e.matmul(PS[:, 4 + dc, :], lhsT=kdec[rs, tt, dc * 128:(dc + 1) * 128], rhs=gv[rs, tt, :],
                                                                    start=True, stop=True))(dc, rs, tt),
                             reads=['kdec', 'gv'], writes=[('ps', 4 + dc)])
                        p.dve((lambda dc, c, h: lambda e: e.scalar_tensor_tensor(out=Sst[:, h, dc, :], in0=Sst[:, h, dc, :], scalar=dl[:, dc, c:c + 1],
                                                                                 in1=PS[:, 4 + dc, :], op0=ALU.mult, op1=ALU.add))(dc, c, h),
                              reads=['Sst', 'dl', ('ps', 4 + dc)], writes=['Sst'])
                    p.act((lambda h: lambda e: e.activation(out=Sbf, in_=Sst[:, h, :, :], func=AF.Identity))(h), reads=['Sst'], writes=['Sbf'])
                    if main and par == 1:
                        sc = small[:, 8:12]
                        p.act(lambda e: e.activation(out=ybf, in_=PS[:, 6, :], func=AF.Square, accum_out=sc[:, 0:1]), reads=[('ps', 6)], writes=['ybf', 'gsc'])
                        p.dve(lambda e: e.tensor_scalar(out=sc[:, 1:2], in0=sc[:, 0:1], scalar1=1.0 / 512, scalar2=EPS, op0=ALU.mult, op1=ALU.add),
                              reads=['gsc'], writes=['gsc'])
                        p.act(lambda e: e.activation(out=sc[:, 2:3], in_=sc[:, 1:2], func=AF.Sqrt), reads=['gsc'], writes=['gsc'])
                        p.dve(lambda e: e.reciprocal(out=sc[:, 3:4], in_=sc[:, 2:3]), reads=['gsc'], writes=['gsc'])
                        p.dve((lambda tt: lambda e: e.scalar_tensor_tensor(out=ybf, in0=PS[:, 6, :], scalar=sc[:, 3:4], in1=SG[:, tt, :],
                                                                           op0=ALU.mult, op1=ALU.mult))(tt), reads=[('ps', 6), 'gsc', 'SG'], writes=['ybf'])
                        pst = PS[:, 7, :].bitcast(BF16)
                        for j in range(4):
                            p.pe((lambda j: lambda e: e.transpose(pst[:, j * 128:(j + 1) * 128], ybf[:, j * 128:(j + 1) * 128], ident_b))(j),
                                 reads=['ybf', 'ident_b'], writes=[('ps', 7)])
                        p.act((lambda h, tt: lambda e: e.activation(out=cT[:, 16 + h * 4:16 + h * 4 + 4, tt * 128:(tt + 1) * 128],
                                                                    in_=pst[:, 0:512].rearrange("p (j q) -> p j q", j=4), func=AF.Identity))(h, tt),
                              reads=[('ps', 7)], writes=[('cT', 16 + h * 4 + jj) for jj in range(4)])
                if not main and mi == 1:
                    p.dve((lambda h: lambda e: e.tensor_scalar(out=Sst[:, h, :, :], in0=Sst[:, h, :, :], scalar1=flag[:, 0:1], scalar2=None, op0=ALU.mult))(h),
                          reads=['Sst', 'flag'], writes=['Sst'])
            p.barrier()
            if stop == 'gla%s%d' % (kind, mi):
                p.emit(st)
                return nc, p
            if 'cT_gla' in dbg and main and mi == 0:
                p.dve(lambda e: e.tensor_copy(out=eb[:, 0, :], in_=cT[:, 16, :]), writes=['dbgtmp'])
                p.dma('sp', 'dbg', lambda e: e.dma_start(out=dbg_out['cT_gla'], in_=eb[:, 0, :]), reads=['dbgtmp'], writes=['dbg_ct2'])
                p.barrier()
            if not main:
                continue
            build_rowbcast(G1B, gate1, 'G1B')
            cT_keys = [('cT', c) for c in range(KC)]
            xstate = {}

            def ev_o(c0, n, tt, b):
                cg = c0 // 256
                xr = xres[cg % 2]
                xk = ('xres', cg % 2)
                if tt == 0:
                    srcap = x_main[mi * MT:(mi + 1) * MT, c0:c0 + 256].rearrange("(t p) c -> p t c", p=128)
                    p.dma('sp', xk, (lambda xr, srcap: lambda e: e.dma_start(out=xr, in_=srcap))(xr, srcap), writes=[xk])
                p.dve((lambda c0, b: lambda e: e.tensor_tensor(out=tmp256, in0=PS[:, b, 0:256], in1=G1B[:, c0:c0 + 256], op=ALU.mult))(c0, b),
                      reads=[('ps', b), 'G1B'], writes=['tmp256'])
                p.dve((lambda xr, tt: lambda e: e.tensor_tensor(out=xr[:, tt, :], in0=xr[:, tt, :], in1=tmp256, op=ALU.add))(xr, tt),
                      reads=['tmp256', xk], writes=[xk])
                if tt == 3:
                    dstap = x1s[mi * MT:(mi + 1) * MT, c0:c0 + 256].rearrange("(t p) c -> p t c", p=128)
                    p.dma('sp', ('xst', cg % 2), (lambda xr, dstap: lambda e: e.dma_start(out=dstap, in_=xr))(xr, dstap),
                          reads=[xk], writes=[xk, 'x1s'])
            gemm_tm(wout_v, 0, D, cT, MT, ev_o, cT_keys)
            p.barrier()
        if upto <= 2:
            p.emit(st)
            return nc, p

        wq_v = wq_d.rearrange("(k p) c -> p k c", p=128)
        ut_v = ut_d.rearrange("(k p) e -> p k e", p=128)
        h2T = A.view(R_HT, [32, TOK], BF16)
        h2_keys = [('hT', c) for c in range(KC)]
        rms_norm_to_hT(x1s, 0, TOK // 128, a2, s2, h2T, R_W, R_W + 32768)
        p.barrier()
        qT = A.view(R_S, [16, TOK], BF16)
        RZB = A.view(R_S + 32768, [8, TOK], BF16)

        def ev_pq(ci, b, h0):
            p.act((lambda ci, b, h0: lambda e: e.activation(out=qT[:, ci, h0:h0 + 512], in_=PS[:, b, :], func=AF.Identity))(ci, b, h0),
                  reads=[('ps', b)], writes=['qT'])
        gemm_fm(wq_v, 0, 2048, h2T, TOK, ev_pq, h2_keys)
        p.barrier()
        wo = [R_W]

        def walloc(shape, dt):
            esz = 4 if dt == F32 else 2
            nb = (int(np.prod(shape)) * esz + 31) // 32 * 32
            v = A.view(wo[0], shape, dt)
            wo[0] += nb
            assert wo[0] <= R_W + 49152, wo[0]
            return v
        ut = [walloc([32, 256], BF16) for _ in range(2)]
        keysT = walloc([16, 128], BF16)
        THR = walloc([TOK], BF16)
        RZR = walloc([TOK], BF16)
        SEL = walloc([8, 128], BF16)
        scs = A.view(P_DYN + 16384, [16, 128], F32)
        qo = [P_DYN]

        def qalloc(shape, dt):
            esz = 4 if dt == F32 else 2
            nb = (int(np.prod(shape)) * esz + 31) // 32 * 32
            v = A.view(qo[0], shape, dt)
            qo[0] += nb
            assert qo[0] <= R_P + 36864, qo[0]
            return v
        t16 = qalloc([16, 16], F32)
        tmpk = qalloc([128], F32)
        cand = qalloc([16, 16], F32)
        tmpc = qalloc([256], F32)
        c16 = qalloc([8, 16], F32)
        tsc = qalloc([8, 8], F32)
        thrTM = qalloc([24], BF16)
        rzTM = qalloc([8], BF16)
        junk16 = qalloc([16], F32)
        p.dma('pool', 'peer_c', lambda e: e.dma_start(out=keysT, in_=keyst_d.rearrange("p (a k) -> p a k", a=16)), writes=['keysT'])
        p.dma('pool', 'peer_c', lambda e: e.dma_start(out=SEL[0:24, :, :], in_=sel_d.rearrange("p (a k) -> p a k", a=8)), writes=['SEL'])
        p.barrier()
        for tt in range(8):
            for hp in range(16):
                b = 4 + hp // 4
                p.pe((lambda hp, b, tt: lambda e: e.matmul(PS[:, b, (hp % 4) * 128:(hp % 4 + 1) * 128], lhsT=qT[:, hp, tt * 128:(tt + 1) * 128],
                                                           rhs=keysT[:, hp, :], start=True, stop=True))(hp, b, tt),
                     reads=['qT', 'keysT'], writes=[('ps', b)])
            p.act(lambda e: e.activation(out=scs, in_=PS[:, 4:8, :].rearrange("p b (c k) -> p (b c) k", c=4), func=AF.Identity),
                  reads=[('ps', 4), ('ps', 5), ('ps', 6), ('ps', 7)], writes=['scs'])
            for hp in range(16):
                p.dve((lambda hp: lambda e: e.max(out=t16[:, hp, 0:8], in_=scs[:, hp, :]))(hp), reads=['scs'], writes=['t16'])
                p.dve((lambda hp: lambda e: e.match_replace(out=tmpk, in_to_replace=t16[:, hp, 0:8], in_values=scs[:, hp, :], imm_value=-1e30))(hp),
                      reads=['scs', 't16'], writes=['tmpk'])
                p.dve((lambda hp: lambda e: e.max(out=t16[:, hp, 8:16], in_=tmpk))(hp), reads=['tmpk'], writes=['t16'])
            for h in range(8):
                p.dve((lambda h: lambda e: e.tensor_tensor(out=cand, in0=t16[:, 2 * h, :].unsqueeze(2).to_broadcast([128, 16, 16]),
                                                           in1=t16[:, 2 * h + 1, :].unsqueeze(1).to_broadcast([128, 16, 16]), op=ALU.add))(h),
                      reads=['t16'], writes=['cand'])
                cf = cand.rearrange("p a b -> p (a b)")
                p.dve((lambda h: lambda e: e.max(out=c16[:, h, 0:8], in_=cf))(h), reads=['cand'], writes=['c16'])
                p.dve((lambda h: lambda e: e.match_replace(out=tmpc, in_to_replace=c16[:, h, 0:8], in_values=cf, imm_value=-1e30))(h),
                      reads=['cand', 'c16'], writes=['tmpc'])
                p.dve((lambda h: lambda e: e.max(out=c16[:, h, 8:16], in_=tmpc))(h), reads=['tmpc'], writes=['c16'])
                p.dve((lambda h: lambda e: e.tensor_scalar(out=tsc[:, h, 0:1], in0=c16[:, h, 15:16], scalar1=-1.0, scalar2=3e-5, op0=ALU.mult, op1=ALU.add))(h),
                      reads=['c16'], writes=['tsc'])
                p.act((lambda h: lambda e: e.activation(out=junk16, in_=c16[:, h, :], func=AF.Exp, bias=tsc[:, h, 0:1], accum_out=tsc[:, h, 1:2]))(h),
                      reads=['c16', 'tsc'], writes=['tsc', 'junk16'])
                p.dve((lambda h: lambda e: e.reciprocal(out=tsc[:, h, 2:3], in_=tsc[:, h, 1:2]))(h), reads=['tsc'], writes=['tsc'])
                p.dve((lambda h: lambda e: e.tensor_copy(out=thrTM[:, h:h + 1], in_=tsc[:, h, 0:1]))(h), reads=['tsc'], writes=['thrTM'])
                p.dve((lambda h: lambda e: e.tensor_tensor(out=tsc[:, h, 3:4], in0=tsc[:, h, 0:1], in1=thrTM[:, h:h + 1], op=ALU.subtract))(h),
                      reads=['tsc', 'thrTM'], writes=['tsc'])
                p.dve((lambda h: lambda e: e.tensor_copy(out=thrTM[:, 8 + h:9 + h], in_=tsc[:, h, 3:4]))(h), reads=['tsc'], writes=['thrTM'])
                p.dve((lambda h: lambda e: e.tensor_tensor(out=tsc[:, h, 4:5], in0=tsc[:, h, 3:4], in1=thrTM[:, 8 + h:9 + h], op=ALU.subtract))(h),
                      reads=['tsc', 'thrTM'], writes=['tsc'])
                p.dve((lambda h: lambda e: e.tensor_copy(out=thrTM[:, 16 + h:17 + h], in_=tsc[:, h, 4:5]))(h), reads=['tsc'], writes=['thrTM'])
                p.dve((lambda h: lambda e: e.tensor_copy(out=rzTM[:, h:h + 1], in_=tsc[:, h, 2:3]))(h), reads=['tsc'], writes=['rzTM'])
            pst = PS[:, 2, :].bitcast(BF16)
            p.pe(lambda e: e.transpose(pst[0:24, 0:128], thrTM, ident_b), reads=['thrTM', 'ident_b'], writes=[('ps', 2)])
            p.pe(lambda e: e.transpose(pst[0:8, 128:256], rzTM, ident_b), reads=['rzTM', 'ident_b'], writes=[('ps', 2)])
            p.act((lambda tt: lambda e: e.activation(out=THR[0:24, tt * 128:(tt + 1) * 128], in_=pst[0:24, 0:128], func=AF.Identity))(tt),
                  reads=[('ps', 2)], writes=['THR'])
            p.act((lambda tt: lambda e: e.activation(out=RZR[0:8, tt * 128:(tt + 1) * 128], in_=pst[0:8, 128:256], func=AF.Identity))(tt),
                  reads=[('ps', 2)], writes=['RZR'])
        for h in range(8):
            for hf in range(2):
                b = hf
                p.pe((lambda h, hf, b: lambda e: e.matmul(PS[:, b, :], lhsT=SEL[0:8, h, :], rhs=RZR[0:8, hf * 512:(hf + 1) * 512], start=True, stop=True))(h, hf, b),
                     reads=['SEL', 'RZR'], writes=[('ps', b)])
                p.act((lambda h, hf, b: lambda e: e.activation(out=RZB[:, h, hf * 512:(hf + 1) * 512], in_=PS[:, b, :], func=AF.Identity))(h, hf, b),
                      reads=[('ps', b)], writes=['RZB'])
        if 'thr' in dbg:
            dbt = A.view(P_DYN + 12288, [TOK], F32)
            p.act(lambda e: e.activation(out=dbt[0:24, :], in_=THR[0:24, :], func=AF.Identity), reads=['THR'], writes=['dbt'])
            p.dma('sp', 'dbg', lambda e: e.dma_start(out=dbg_out['thr'], in_=dbt[0:24, :]), reads=['dbt'], writes=['dbg_thr'])
            p.barrier()
            dbt2 = A.view(P_DYN + 12288, [TOK], F32)
            p.act(lambda e: e.activation(out=dbt2[0:8, :], in_=RZR[0:8, :], func=AF.Identity), reads=['RZR'], writes=['dbt'])
            p.dma('sp', 'dbg', lambda e: e.dma_start(out=dbg_out['rzr'], in_=dbt2[0:8, :]), reads=['dbt'], writes=['dbg_rzr'])
        p.barrier()
        if upto <= 3:
            p.emit(st)
            return nc, p
        qo[0] = P_DYN
        HT_ = 512
        Eb = [qalloc([HT_], BF16) for _ in range(4)]
        Wm = [qalloc([HT_], BF16) for _ in range(4)]
        Wn = [qalloc([HT_], BF16) for _ in range(8)]
        Gl = qalloc([TOK], BF16)
        AW = [qalloc([TOK], BF16) for _ in range(2)]
        NE = 128 if upto > 4 else 2

        def emit_U(i1, sg):
            h, hf = sg // 2, sg % 2
            b = 2 + sg % 4
            ts = slice(hf * 512, (hf + 1) * 512)
            p.pe(lambda e: e.matmul(PS[:, b, :], lhsT=keysT[:, 2 * h + 1, :], rhs=qT[:, 2 * h + 1, ts], start=True, stop=False),
                 reads=['qT', 'keysT'], writes=[('ps', b)])
            p.pe(lambda e: e.matmul(PS[:, b, :], lhsT=keysT[:, 2 * h, i1:i1 + 1].to_broadcast([128, 128]), rhs=qT[:, 2 * h, ts], start=False, stop=False),
                 reads=['qT', 'keysT'], writes=[('ps', b)])
            p.pe(lambda e: e.matmul(PS[:, b, :], lhsT=SEL[0:24, h, :], rhs=THR[0:24, ts], start=False, stop=True),
                 reads=['SEL', 'THR'], writes=[('ps', b)])
            r = sg % 4
            p.act(lambda e: e.activation(out=Eb[r], in_=PS[:, b, :], func=AF.Exp), reads=[('ps', b)], writes=[('Eb', r)])
            p.dve(lambda e: e.scalar_tensor_tensor(out=Wm[r], in0=PS[:, b, :], scalar=0.0, in1=Eb[r], op0=ALU.is_ge, op1=ALU.mult),
                  reads=[('ps', b), ('Eb', r)], writes=[('Wm', r)])
            r8 = sg % 8
            p.pool(lambda e: e.tensor_tensor(out=Wn[r8], in0=Wm[r], in1=RZB[:, h, ts], op=ALU.mult), reads=[('Wm', r), 'RZB'], writes=[('Wn', r8)])

        def emit_acc(sg):
            h, hf = sg // 2, sg % 2
            r8 = sg % 8
            p.pe(lambda e: e.matmul(PS[:, 6 + hf, :], lhsT=ident_b, rhs=Wn[r8], start=(h == 0), stop=(h == 7)),
                 reads=[('Wn', r8), 'ident_b'], writes=[('ps', 6 + hf)])

        Gl2 = [Gl, qalloc([TOK], BF16)]

        def load_u(pi):
            us = pi % 2
            p.dma('pool', ('U', us), lambda e: e.dma_start(out=ut[us], in_=ut_v[:, :, pi * 256:pi * 256 + 256]), writes=[('U', us)])

        def actT_mms(i1):
            us = (i1 // 2) % 2
            uk = ('U', us)
            ec = (i1 % 2) * 128
            lst = []
            for hf in range(2):
                for k in range(KC):
                    lst.append((lambda hf, k: lambda: p.pe(lambda e: e.matmul(PS[:, hf, :], lhsT=ut[us][:, k, ec:ec + 128], rhs=h2T[:, k, hf * 512:(hf + 1) * 512],
                                                                             start=(k == 0), stop=(k == KC - 1)),
                                                           reads=[uk] + h2_keys, writes=[('ps', hf)]))(hf, k))
            return lst

        def emit_gelu(i1):
            g = Gl2[i1 % 2]
            p.act(lambda e: e.activation(out=g.rearrange("p (b t) -> p b t", b=2), in_=PS[:, 0:2, :], func=AF.Gelu), reads=[('ps', 0), ('ps', 1)], writes=[('Gl', i1 % 2)])

        load_u(0)
        for f in actT_mms(0):
            f()
        emit_gelu(0)
        for i1 in range(NE):
            if i1 % 2 == 0 and (i1 // 2 + 1) * 2 < NE:
                load_u(i1 // 2 + 1)
            nxt = actT_mms(i1 + 1) if i1 + 1 < NE else []
            for sg in range(4):
                emit_U(i1, sg)
            for sg in range(16):
                if sg + 4 < 16:
                    emit_U(i1, sg + 4)
                emit_acc(sg)
                for f in nxt[sg * 4:(sg + 1) * 4]:
                    f()
            a = i1 % 2
            g = Gl2[i1 % 2]
            p.dve((lambda a, g: lambda e: e.tensor_tensor(out=AW[a].rearrange("p (b t) -> p b t", b=2), in0=PS[:, 6:8, :], in1=g.rearrange("p (b t) -> p b t", b=2), op=ALU.mult))(a, g),
                  reads=[('ps', 6), ('ps', 7), ('Gl', i1 % 2)], writes=[('AW', a)])
            p.dma('sp', ('AWst', a), (lambda a, i1: lambda e: e.dma_start(out=aws[i1], in_=AW[a]))(a, i1), reads=[('AW', a)], writes=[('AW', a), 'aws'])
            if i1 + 1 < NE:
                emit_gelu(i1 + 1)
        if 'aw0' in dbg:
            p.barrier()
            dbt = A.view(R_HT, [TOK], F32)
            p.act(lambda e: e.activation(out=dbt, in_=AW[0], func=AF.Identity), writes=['dbt'])
            p.dma('sp', 'dbg', lambda e: e.dma_start(out=dbg_out['aw0'], in_=dbt), reads=['dbt'], writes=['dbg_aw0'])
        p.barrier()
        if upto <= 4:
            p.emit(st)
            return nc, p
        acc = A.view(0, [8, D], F32)
        vt = [A.view(131072 + i * 8192, [8, 512], BF16) for i in range(2)]
        awt = [A.view(131072 + 16384, [8, TOK], BF16), A.view(P_DYN, [8, TOK], BF16)]
        v_v = v_d.rearrange("(g a p) d -> g p a d", a=8, p=128)
        aws_v = aws.rearrange("(g a) p t -> g p a t", a=8)
        it = 0
        for dg in range(8):
            for eg in range(16):
                s_ = it % 2
                it += 1
                p.dma('pool', ('V', s_), (lambda s_, eg, dg: lambda e: e.dma_start(out=vt[s_], in_=v_v[eg][:, :, dg * 512:(dg + 1) * 512]))(s_, eg, dg), writes=[('V', s_)])
                p.dma('sp', ('AWld', s_), (lambda s_, eg: lambda e: e.dma_start(out=awt[s_], in_=aws_v[eg]))(s_, eg), reads=['aws'], writes=[('AWl', s_)])
                for a in range(8):
                    for tt in range(8):
                        p.pe((lambda s_, a, tt, eg: lambda e: e.matmul(PS[:, tt, :], lhsT=awt[s_][:, a, tt * 128:(tt + 1) * 128], rhs=vt[s_][:, a, :],
                                                                      start=(eg == 0 and a == 0), stop=(eg == 15 and a == 7)))(s_, a, tt, eg),
                             reads=[('V', s_), ('AWl', s_)], writes=[('ps', tt)])
            for tt in range(8):
                if tt % 2 == 0:
                    p.act((lambda tt, dg: lambda e: e.activation(out=acc[:, tt, dg * 512:(dg + 1) * 512], in_=PS[:, tt, :], func=AF.Identity))(tt, dg),
                          reads=[('ps', tt)], writes=[('acc', tt, dg)])
                else:
                    p.dve((lambda tt, dg: lambda e: e.tensor_copy(out=acc[:, tt, dg * 512:(dg + 1) * 512], in_=PS[:, tt, :]))(tt, dg),
                          reads=[('ps', tt)], writes=[('acc', tt, dg)])
        p.barrier()
        G2B = A.view(131072, [D], F32)
        FGB = A.view(131072 + 16384, [D], F32)
        x1t = A.view(P_DYN + 1024, [D], F32)
        dtmp2 = A.view(P_DYN, [128], F32)

        for c in range(KC):
            b = 2 + (c // 4) % 2
            q = c % 4
            p.dve((lambda c: lambda e: e.tensor_scalar(out=dtmp2, in0=ident_f, scalar1=gate2[:, c:c + 1], scalar2=None, op0=ALU.mult))(c),
                  reads=['ident_f', 'modT'], writes=['dtmp2'])
            p.pe((lambda b, q: lambda e: e.matmul(PS[:, b, q * 128:(q + 1) * 128], lhsT=ones_f, rhs=dtmp2, start=True, stop=True))(b, q),
                 reads=['dtmp2', 'ones_f'], writes=[('ps', b)])
            if q == 3:
                p.act((lambda b, c: lambda e: e.activation(out=G2B[:, (c - 3) * 128:(c + 1) * 128], in_=PS[:, b, :], func=AF.Identity))(b, c),
                      reads=[('ps', b)], writes=['G2B'])
        p.dma('sp', 'fgb', lambda e: e.dma_start(out=FGB, in_=fg_d.partition_broadcast(128)[:, 0, :]), writes=['FGB'])
        for tt in range(8):
            sc = small[:, 16:20]
            p.dma('sp', 'x1ld', (lambda tt: lambda e: e.dma_start(out=x1t, in_=x1s[tt * 128:(tt + 1) * 128, :]))(tt), reads=['x1s'], writes=['x1t'])
            p.dve((lambda tt: lambda e: e.tensor_tensor(out=acc[:, tt, :], in0=acc[:, tt, :], in1=G2B, op=ALU.mult))(tt), reads=['G2B'], writes=[('accf', tt)])
            p.pool((lambda tt: lambda e: e.tensor_tensor(out=x1t, in0=x1t, in1=acc[:, tt, :], op=ALU.add))(tt), reads=[('accf', tt), 'x1t'], writes=['x1t'])
            p.act(lambda e: e.activation(out=PS[:, :, :], in_=x1t.rearrange("p (b t) -> p b t", b=8), func=AF.Square, accum_out=sc[:, 0:1]), reads=['x1t'], writes=['junkf', 'fsc'])
            p.dve(lambda e: e.tensor_scalar(out=sc[:, 1:2], in0=sc[:, 0:1], scalar1=1.0 / D, scalar2=EPS, op0=ALU.mult, op1=ALU.add), reads=['fsc'], writes=['fsc'])
            p.act(lambda e: e.activation(out=sc[:, 2:3], in_=sc[:, 1:2], func=AF.Sqrt), reads=['fsc'], writes=['fsc'])
            p.dve(lambda e: e.reciprocal(out=sc[:, 3:4], in_=sc[:, 2:3]), reads=['fsc'], writes=['fsc'])
            p.dve(lambda e: e.scalar_tensor_tensor(out=x1t, in0=x1t, scalar=sc[:, 3:4], in1=FGB, op0=ALU.mult, op1=ALU.mult), reads=['x1t', 'fsc', 'FGB'], writes=['x1t'])
            p.dma('sp', 'ost', (lambda tt: lambda e: e.dma_start(out=out_d[tt * 128:(tt + 1) * 128, :], in_=x1t))(tt), reads=['x1t'], writes=['x1t', 'out'])
        p.barrier()
        p.emit(st)
    return nc, p


def t5_bucket_np(dist):
    max_exact = 16
    d = np.maximum(dist, 0)
    lr = np.log(np.maximum(d, 1).astype(np.float32) / max_exact) / math.log(128 / max_exact)
    large = max_exact + (lr * (32 - max_exact)).astype(np.int32)
    large = np.minimum(large, 31)
    return np.where(d < max_exact, d, large)


def make_in_maps(inputs, cores=range(8)):
    f = lambda a: np.ascontiguousarray(np.asarray(a, dtype=np.float32))
    x = f(inputs["x"]); c = f(inputs["c"])
    fm = lambda v, n: np.ascontiguousarray(v.reshape(n, 128).T)
    onehot = np.zeros((32, 128), np.float32)
    onehot[t5_bucket_np(np.arange(128)), np.arange(128)] = 1.0
    sel = np.zeros((24, 8, 128), np.float32)
    for h in range(8):
        for part in range(3):
            sel[part * 8 + h, h, :] = 1.0
    keys = f(inputs["peer_keys"])[0]
    keys_t = np.ascontiguousarray(keys.transpose(3, 0, 1, 2).reshape(128, 16 * 128))
    shared = {
        "w_ada": f(inputs["w_ada"])[0],
        "b_ada_t": fm(f(inputs["b_ada"])[0], 192),
        "g1_t": fm(f(inputs["norm1_g"])[0], 32),
        "g2_t": fm(f(inputs["norm2_g"])[0], 32),
        "w_in": f(inputs["w_in"])[0],
        "sinks": f(inputs["attn_sinks"])[0].reshape(1, 16),
        "rel_bias": f(inputs["rel_bias"]),
        "onehot": onehot,
        "gk2_w": f(inputs["gla_w_gk2"])[0],
        "gk2_b": f(inputs["gla_b_gk2"])[0].reshape(1, 1024),
        "gla_norm_g": f(inputs["gla_norm_g"])[0].reshape(1, 512),
        "w_out": f(inputs["w_out"])[0],
        "w_q": f(inputs["peer_w_q"])[0],
        "keys_t": keys_t,
        "u_t": np.ascontiguousarray(f(inputs["peer_u"])[0].T),
        "v": f(inputs["peer_v"])[0],
        "final_g": f(inputs["final_g"]).reshape(1, D),
        "sel": sel.reshape(24, 8 * 128),
    }
    maps = []
    for i in cores:
        b, half = i // 2, i % 2
        m = dict(shared)
        m["x_main"] = np.ascontiguousarray(x[b, half * TOK:(half + 1) * TOK])
        m["x_pre"] = np.ascontiguousarray(x[b, 0:TOK])
        m["flag"] = np.full((128, 1), float(half), np.float32)
        m["c_t"] = fm(c[b], 32)
        maps.append(m)
    return maps


_CACHE = {}


def kernel(**inputs):
    if "nc" not in _CACHE:
        _CACHE["nc"] = build_program()[0]
    nc = _CACHE["nc"]
    maps = make_in_maps(inputs)
    res = run_bass_kernel_spmd(nc, maps, core_ids=list(range(8)))
    out = np.empty((4, 2048, D), np.float32)
    for i in range(8):
        b, half = i // 2, i % 2
        out[b, half * TOK:(half + 1) * TOK] = res.results[i]["out"]
    return out
```

```python
from contextlib import ExitStack
import math
import numpy as np
import concourse.bass as bass
import concourse.mybir as mybir
from concourse.bass_utils import run_bass_kernel_spmd

F32 = mybir.dt.float32
BF16 = mybir.dt.bfloat16
AF = mybir.ActivationFunctionType
ALU = mybir.AluOpType

SAME_ENG_SYNC = True

D = 4096
KC = 32
TOK = 1024
MT = 512
NEXP = 16384
EPS = 1e-6
NEGM = -30000.0
IN_W = 8720
C_AQ, C_AK, C_AV, C_GQ, C_GK, C_GV, C_GLOW, C_GOUT = 0, 2048, 2304, 2560, 3584, 4608, 6656, 6672


class Prog:
    def __init__(self, nc):
        self.nc = nc
        self.ins = []
        self.last_w = {}
        self.readers = {}
        self.last_on = {}
        self.dmas_open = []

    maxops = None

    def op(self, eng, fn, reads=(), writes=(), dma=None, extra_deps=(), force=False):
        if self.maxops is not None and len(self.ins) >= self.maxops and not force:
            return None
        i = len(self.ins)
        deps = set(extra_deps)
        psk = [k for k in reads if k == 'ps0' or (isinstance(k, tuple) and k[0] == 'ps')]
        if psk:
            reads = [k for k in reads if k not in psk]
            writes = list(writes) + [k for k in psk if k not in writes]
        for k in reads:
            w = self.last_w.get(k)
            if w is not None:
                deps.add(w)
        for k in writes:
            w = self.last_w.get(k)
            if w is not None:
                deps.add(w)
            for r in self.readers.get(k, ()):
                deps.add(r)
        for k in reads:
            lst = self.readers.setdefault(k, [])
            if dma is None:
                for q in range(len(lst)):
                    J = self.ins[lst[q]]
                    if J['dma'] is None and J['eng'] == eng:
                        lst[q] = i
                        break
                else:
                    lst.append(i)
            else:
                lst.append(i)
        for k in writes:
            self.last_w[k] = i
            self.readers[k] = []
        deps.discard(i)
        self.ins.append(dict(eng=eng, fn=fn, deps=deps, dma=dma))
        if dma is None:
            self.last_on[eng] = i
        else:
            self.dmas_open.append(i)
        return i

    def pe(self, fn, reads=(), writes=()):
        return self.op('pe', fn, reads, writes)

    def act(self, fn, reads=(), writes=()):
        return self.op('act', fn, reads, writes)

    def dve(self, fn, reads=(), writes=()):
        return self.op('dve', fn, reads, writes)

    def pool(self, fn, reads=(), writes=()):
        return self.op('pool', fn, reads, writes)

    def dma(self, eng, key, fn, reads=(), writes=()):
        return self.op(eng, fn, reads, writes, dma=key)

    def barrier(self):
        deps = set(self.last_on.values()) | set(self.dmas_open)
        self.dmas_open = []
        for e in ['pe', 'act', 'dve', 'pool', 'sp']:
            self.op(e, lambda eng: eng.nop(), extra_deps=deps, force=True)
        self.last_w = {}
        self.readers = {}

    def emit(self, stack):
        nc = self.nc
        ins = self.ins
        n = len(ins)
        engs = ['pe', 'act', 'dve', 'pool', 'sp']

        def needs_wait(I, Dd):
            if Dd['dma'] is not None:
                return True
            if Dd['eng'] == I['eng'] and I['dma'] is None:
                if Dd['eng'] in ('pe', 'sp') or not SAME_ENG_SYNC:
                    return False
            return True

        need_sig = [False] * n
        for i, I in enumerate(ins):
            for d in I['deps']:
                Dd = ins[d]
                if Dd['dma'] is None and needs_wait(I, Dd):
                    need_sig[d] = True
        cnt = {e: 0 for e in engs}
        sigval = [0] * n
        dma_cnt = {}
        for i, I in enumerate(ins):
            if I['dma'] is not None:
                k = I['dma']
                dma_cnt[k] = dma_cnt.get(k, 0) + 16
                sigval[i] = dma_cnt[k]
            elif need_sig[i]:
                cnt[I['eng']] += 1
                sigval[i] = cnt[I['eng']]
        esem = {e: stack.enter_context(nc.semaphore('s_' + e)) for e in engs}
        dsem = {k: stack.enter_context(nc.semaphore('d_%d' % j)) for j, k in enumerate(dma_cnt)}
        self.stats = dict(n=n, cnt=dict(cnt), ndsem=len(dsem), maxdma=max(dma_cnt.values()) if dma_cnt else 0)
        per_eng = {e: [] for e in engs}
        for i, I in enumerate(ins):
            per_eng[I['eng']].append(i)

        def run(e, engobj):
            waited = {}
            for i in per_eng[e]:
                I = ins[i]
                need = {}
                for d in I['deps']:
                    Dd = ins[d]
                    if not needs_wait(I, Dd):
                        continue
                    if Dd['dma'] is not None:
                        key = ('d', Dd['dma'])
                    else:
                        key = ('e', Dd['eng'])
                    v = sigval[d]
                    if need.get(key, 0) < v:
                        need[key] = v
                for key, v in need.items():
                    if waited.get(key, 0) >= v:
                        continue
                    waited[key] = v
                    s = dsem[key[1]] if key[0] == 'd' else esem[key[1]]
                    engobj.wait_ge(s, v)
                r = I['fn'](engobj)
                if I['dma'] is not None:
                    r.then_inc(dsem[I['dma']], 16)
                elif need_sig[i]:
                    r.then_inc(esem[e], 1)

        block = stack.enter_context(nc.Block())
        block.sync(lambda eng: run('sp', eng))
        block.tensor(lambda eng: run('pe', eng))
        block.scalar(lambda eng: run('act', eng))
        block.vector(lambda eng: run('dve', eng))
        block.gpsimd(lambda eng: run('pool', eng))


class Arena:
    def __init__(self, nc, nbytes):
        self.nbytes = nbytes
        self.t = nc.alloc_sbuf_tensor("arena", [128, nbytes // 4], F32)

    def view(self, off, shape, dtype):
        esz = 4 if dtype == F32 else 2
        nel = int(np.prod(shape))
        nb = nel * esz
        assert off % 4 == 0 and nb % 4 == 0, (off, shape)
        assert off + nb <= self.nbytes, (off, nb, self.nbytes)
        ap = self.t[:, off // 4: (off + nb) // 4]
        if dtype != F32:
            ap = ap.bitcast(dtype)
        if len(shape) == 2:
            names = "a b"
            ap = ap.rearrange("p (a b) -> p a b", a=shape[0], b=shape[1])
        elif len(shape) == 3:
            ap = ap.rearrange("p (a b c) -> p a b c", a=shape[0], b=shape[1], c=shape[2])
        return ap


def build_program(dbg=None, upto=99, stop=None, fake_mod=False):
    nc = bass.Bass("TRN2", target_bir_lowering=False)
    dbg = dbg or {}

    def din(name, shape, dt=F32):
        return nc.dram_tensor(name, list(shape), dt, kind="ExternalInput").ap()

    x_main = din("x_main", [TOK, D])
    x_pre = din("x_pre", [TOK, D])
    flag_d = din("flag", [128, 1])
    c_d = din("c_t", [128, KC])
    wada_d = din("w_ada", [D, 6 * D]) if not fake_mod else None
    bada_d = din("b_ada_t", [128, 6 * KC])
    g1_d = din("g1_t", [128, KC])
    g2_d = din("g2_t", [128, KC])
    win_d = din("w_in", [D, IN_W])
    sinks_d = din("sinks", [1, 16])
    relb_d = din("rel_bias", [32, 16])
    oh_d = din("onehot", [32, 128])
    gk2w_d = din("gk2_w", [16, 1024])
    gk2b_d = din("gk2_b", [1, 1024])
    gng_d = din("gla_norm_g", [1, 512])
    wout_d = din("w_out", [D, D])
    if upto > 2:
        wq_d = din("w_q", [D, 2048])
        keyst_d = din("keys_t", [128, 16 * 128])
        ut_d = din("u_t", [D, NEXP])
        v_d = din("v", [NEXP, D])
        fg_d = din("final_g", [1, D])
        sel_d = din("sel", [24, 8 * 128])
    if fake_mod:
        modt_d = din("modT_dbg", [128, 192])
    out_d = nc.dram_tensor("out", [TOK, D], F32, kind="ExternalOutput").ap()
    x1s = nc.dram_tensor("x1s", [TOK, D], F32).ap()
    aws = nc.dram_tensor("aws", [128, 128, TOK], BF16).ap()
    gext = nc.dram_tensor("gext", [16, 384], F32).ap()
    dbg_out = {}
    for name, shape in dbg.items():
        dbg_out[name] = nc.dram_tensor("dbg_" + name, list(shape), F32, kind="ExternalOutput").ap()

    st = ExitStack()
    with st:
        A = Arena(nc, 200 * 1024)
        PS = nc.alloc_psum_tensor("ps", [128, 8, 512], F32)
        p = Prog(nc)
        import os
        if os.environ.get('K_MAXOPS'):
            p.maxops = int(os.environ['K_MAXOPS'])
        R_HT, R_CT, R_W, R_S, R_P = 0, 32768, 65536, 65536 + 49152, 65536 + 2 * 49152
        po = [R_P]

        def palloc(shape, dt):
            esz = 4 if dt == F32 else 2
            nb = int(np.prod(shape)) * esz
            nb = (nb + 31) // 32 * 32
            v = A.view(po[0], shape, dt)
            po[0] += nb
            return v

        ident_f = A.view(po[0], [128], F32); po[0] += 512
        ident_b = A.view(po[0], [128], BF16); po[0] += 256
        ones_b = A.view(po[0], [128], BF16); po[0] += 256
        ones_f = A.view(po[0], [128], F32); po[0] += 512
        Lm = A.view(po[0], [128], F32); po[0] += 512
        Um = A.view(po[0], [128], F32); po[0] += 512
        modT = A.view(po[0], [192], F32); po[0] += 768
        badat = A.view(po[0], [192], F32); po[0] += 768
        a1 = A.view(po[0], [32], F32); po[0] += 128
        a2 = A.view(po[0], [32], F32); po[0] += 128
        g1t = A.view(po[0], [32], F32); po[0] += 128
        g2t = A.view(po[0], [32], F32); po[0] += 128
        ct = A.view(po[0], [32], F32); po[0] += 128
        cact = A.view(po[0], [32], BF16); po[0] += 64
        flag = A.view(po[0], [8], F32); po[0] += 32
        small = A.view(po[0], [64], F32); po[0] += 256
        esink = A.view(po[0], [16], F32); po[0] += 64
        gnb = A.view(po[0], [512], F32); po[0] += 2048
        gk2w = A.view(po[0], [1024], BF16); po[0] += 2048
        gk2b = A.view(po[0], [1024], BF16); po[0] += 2048
        P_DYN = po[0]
        bcur = A.view(po[0], [16, 128], BF16); po[0] += 4096
        bprev = A.view(po[0], [16, 128], BF16); po[0] += 4096
        Sst = A.view(po[0], [4, 2, 512], F32); po[0] += 16384
        assert po[0] <= 200 * 1024, po[0]

        s1 = modT[:, 0:32]
        gate1 = modT[:, 64:96]
        s2 = modT[:, 96:128]
        gate2 = modT[:, 160:192]

        def bank(b, n=512):
            return PS[:, b, 0:n]

        p.pool(lambda e: e.memset(ident_f, 1.0), writes=['ident_f'])
        p.pool(lambda e: e.affine_select(out=ident_f, in_=ident_f, pattern=[[-1, 128]], compare_op=ALU.is_equal,
                                         fill=0.0, base=0, channel_multiplier=1), reads=['ident_f'], writes=['ident_f'])
        p.pool(lambda e: e.memset(ones_f, 1.0), writes=['ones_f'])
        p.pool(lambda e: e.memset(ones_b, 1.0), writes=['ones_b'])
        p.dve(lambda e: e.tensor_copy(out=ident_b, in_=ident_f), reads=['ident_f'], writes=['ident_b'])
        p.pool(lambda e: e.memset(Lm, 1.0), writes=['Lm'])
        p.pool(lambda e: e.affine_select(out=Lm, in_=Lm, pattern=[[1, 128]], compare_op=ALU.is_ge,
                                         fill=0.0, base=0, channel_multiplier=-1), reads=['Lm'], writes=['Lm'])
        p.pool(lambda e: e.memset(Lm[0:64, 64:128], 0.0), reads=['Lm'], writes=['Lm'])
        p.pool(lambda e: e.memset(Um, 1.0), writes=['Um'])
        p.pool(lambda e: e.affine_select(out=Um, in_=Um, pattern=[[-1, 128]], compare_op=ALU.is_gt,
                                         fill=0.0, base=0, channel_multiplier=1), reads=['Um'], writes=['Um'])
        p.pool(lambda e: e.memset(Um[64:128, 0:64], 0.0), reads=['Um'], writes=['Um'])
        p.pool(lambda e: e.memset(Sst, 0.0), writes=['Sst'])

        sm = 'small_ld'
        p.dma('sp', sm, lambda e: e.dma_start(out=ct, in_=c_d), writes=['ct'])
        p.dma('sp', sm, lambda e: e.dma_start(out=badat, in_=bada_d), writes=['badat'])
        p.dma('sp', sm, lambda e: e.dma_start(out=g1t, in_=g1_d), writes=['g1t'])
        p.dma('sp', sm, lambda e: e.dma_start(out=g2t, in_=g2_d), writes=['g2t'])
        p.dma('sp', sm, lambda e: e.dma_start(out=flag[:, 0:1], in_=flag_d), writes=['flag'])
        p.dma('sp', sm, lambda e: e.dma_start(out=esink, in_=sinks_d.partition_broadcast(128)[:, 0, :]), writes=['esink'])
        p.dma('sp', sm, lambda e: e.dma_start(out=gnb, in_=gng_d.partition_broadcast(128)[:, 0, :]), writes=['gnb'])
        p.dma('pool', 'small_ld2', lambda e: e.dma_start(out=gk2w[0:16, :], in_=gk2w_d), writes=['gk2w'])
        p.dma('pool', 'small_ld2', lambda e: e.dma_start(out=gk2b[0:1, :], in_=gk2b_d), writes=['gk2b'])
        relb = A.view(R_S, [16], F32)
        ohs = A.view(R_S + 64, [128], F32)
        gx = A.view(R_S + 1024, [384], F32)
        p.dma('sp', sm, lambda e: e.dma_start(out=relb[0:32, :], in_=relb_d), writes=['relb'])
        p.dma('sp', sm, lambda e: e.dma_start(out=ohs[0:32, :], in_=oh_d), writes=['ohs'])
        p.barrier()
        p.act(lambda e: e.activation(out=esink, in_=esink, func=AF.Exp), reads=['esink'], writes=['esink'])
        p.pe(lambda e: e.matmul(PS[0:16, 0, 0:128], lhsT=relb[0:32, :], rhs=ohs[0:32, :], start=True, stop=True),
             reads=['relb', 'ohs'], writes=['ps0'])
        p.pool(lambda e: e.memset(gx[0:16, :], NEGM), writes=['gx'])
        p.dve(lambda e: e.tensor_copy(out=gx[0:16, 128:256], in_=PS[0:16, 0, 0:128]), reads=['ps0', 'gx'], writes=['gx'])
        p.dma('sp', 'gx', lambda e: e.dma_start(out=gext, in_=gx[0:16, :]), reads=['gx'], writes=['gext'])
        p.barrier()
        bcf = A.view(R_S + 4096, [16, 128], F32)
        bpf = A.view(R_S + 4096 + 8192, [16, 128], F32)
        for k in range(128):
            p.dma('sp', 'bias_ld', (lambda k: lambda e: e.dma_start(out=bcf[k:k + 1, :, :], in_=gext[:, 128 - k:256 - k].unsqueeze(0)))(k),
                  writes=[('bcf', k)])
            p.dma('sp', 'bias_ld', (lambda k: lambda e: e.dma_start(out=bpf[k:k + 1, :, :], in_=gext[:, 256 - k:384 - k].unsqueeze(0)))(k),
                  writes=[('bpf', k)])
        p.barrier()
        p.dve(lambda e: e.tensor_copy(out=bcur, in_=bcf), writes=['bcur'])
        p.dve(lambda e: e.tensor_copy(out=bprev, in_=bpf), writes=['bprev'])
        if 'bcur' in dbg:
            p.dma('sp', 'dbg', lambda e: e.dma_start(out=dbg_out['bcur'], in_=bcf), reads=['bcur'], writes=['dbg_bcur'])
        p.barrier()

        p.act(lambda e: e.activation(out=cact, in_=ct, func=AF.Silu), writes=['cact'])
        wada_v = wada_d.rearrange("(k p) c -> p k c", p=128) if not fake_mod else None
        PS_fake = A.view(R_S, [192], F32)
        Wt = [A.view(R_W + i * 16384, [32, 256], BF16) for i in range(3)]
        wctr = [0]

        def load_w(view, c0, ncols):
            slot = wctr[0] % 3
            wctr[0] += 1
            t = Wt[slot]
            p.dma('pool', ('W', slot), lambda e: e.dma_start(out=t[:, :, 0:ncols], in_=view[:, :, c0:c0 + ncols]),
                  writes=[('W', slot)])
            return t, ('W', slot)

        if fake_mod:
            p.dma('sp', 'fm', lambda e: e.dma_start(out=PS_fake, in_=modt_d), writes=['ps0'])
        for tno in range(0 if fake_mod else 6 * D // 256):
            wt, wk = load_w(wada_v, tno * 256, 256)
            for cc in range(2):
                j = tno * 2 + cc
                for k in range(KC):
                    p.pe((lambda wt, cc, j, k: lambda e: e.matmul(PS[:, 0, j:j + 1], lhsT=wt[:, k, cc * 128:(cc + 1) * 128],
                                                                  rhs=cact[:, k:k + 1], start=(k == 0), stop=(k == KC - 1)))(wt, cc, j, k),
                         reads=[wk, 'cact'], writes=['ps0'])
        if fake_mod:
            p.dve(lambda e: e.tensor_copy(out=modT, in_=PS_fake), reads=['ps0'], writes=['modT'])
        else:
            p.dve(lambda e: e.tensor_tensor(out=modT, in0=PS[:, 0, 0:192], in1=badat, op=ALU.add), reads=['ps0'], writes=['modT'])
        p.dve(lambda e: e.scalar_tensor_tensor(out=a1, in0=modT[:, 32:64], scalar=1.0, in1=g1t, op0=ALU.add, op1=ALU.mult),
              reads=['modT'], writes=['a1'])
        p.dve(lambda e: e.scalar_tensor_tensor(out=a2, in0=modT[:, 128:160], scalar=1.0, in1=g2t, op0=ALU.add, op1=ALU.mult),
              reads=['modT'], writes=['a2'])
        if 'modT' in dbg:
            p.dma('sp', 'dbg', lambda e: e.dma_start(out=dbg_out['modT'], in_=modT), reads=['modT'], writes=['dbg_modT'])
        p.barrier()
        if upto <= 1:
            p.emit(st)
            return nc, p

        def rms_norm_to_hT(src, row0, ntiles, a_t, s_t, hT, xs_off, junk_off):
            xs = [A.view(xs_off + i * 16384, [4096], F32) for i in range(2)]
            junk = A.view(junk_off, [4096], BF16)
            for tt in range(ntiles):
                xt = xs[tt % 2]
                xk = ('xs', tt % 2)
                sc = small[:, (tt % 2) * 4:(tt % 2) * 4 + 4]
                sk = ('nsc', tt % 2)
                p.dma('sp', xk, (lambda xt, tt: lambda e: e.dma_start(out=xt, in_=src[row0 + tt * 128: row0 + (tt + 1) * 128, :]))(xt, tt),
                      writes=[xk])
                p.act((lambda xt, sc: lambda e: e.activation(out=junk, in_=xt, func=AF.Square, accum_out=sc[:, 0:1]))(xt, sc),
                      reads=[xk], writes=['junk', sk])
                p.dve((lambda sc: lambda e: e.tensor_scalar(out=sc[:, 1:2], in0=sc[:, 0:1], scalar1=1.0 / D, scalar2=EPS,
                                                            op0=ALU.mult, op1=ALU.add))(sc), reads=[sk], writes=[sk])
                p.act((lambda sc: lambda e: e.activation(out=sc[:, 2:3], in_=sc[:, 1:2], func=AF.Sqrt))(sc), reads=[sk], writes=[sk])
                p.dve((lambda sc: lambda e: e.reciprocal(out=sc[:, 3:4], in_=sc[:, 2:3]))(sc), reads=[sk], writes=[sk])
                p.act((lambda xt, sc: lambda e: e.activation(out=xt, in_=xt, func=AF.Copy, scale=sc[:, 3:4]))(xt, sc),
                      reads=[xk, sk], writes=[xk])
                for g4 in range(8):
                    b = 4 + (g4 % 2)
                    for q in range(4):
                        c = g4 * 4 + q
                        p.pe((lambda xt, b, q, c: lambda e: e.transpose(PS[:, b, q * 128:(q + 1) * 128], xt[:, c * 128:(c + 1) * 128], ident_f))(xt, b, q, c),
                             reads=[xk, 'ident_f'], writes=[('ps', b)])
                    for q in range(4):
                        c = g4 * 4 + q
                        if g4 % 2 == 0:
                            p.dve((lambda b, q, c, tt: lambda e: e.tensor_scalar(out=hT[:, c, tt * 128:(tt + 1) * 128], in0=PS[:, b, q * 128:(q + 1) * 128],
                                                                                 scalar1=a_t[:, c:c + 1], scalar2=s_t[:, c:c + 1], op0=ALU.mult, op1=ALU.add))(b, q, c, tt),
                                  reads=[('ps', b)], writes=[('hT', c)])
                        else:
                            p.act((lambda b, q, c, tt: lambda e: e.activation(out=hT[:, c, tt * 128:(tt + 1) * 128], in_=PS[:, b, q * 128:(q + 1) * 128],
                                                                              func=AF.Identity, scale=a_t[:, c:c + 1], bias=s_t[:, c:c + 1]))(b, q, c, tt),
                                  reads=[('ps', b)], writes=[('hT', c)])

        pbank = [0]

        def next_pbank():
            b = pbank[0] % 2
            pbank[0] += 1
            return b

        hT_keys = [('hT', c) for c in range(KC)]

        def gemm_fm(wview, c0, ncols, hT, T, evac, src_keys):
            done = 0
            while done < ncols:
                n = min(256, ncols - done)
                wt, wk = load_w(wview, c0 + done, n)
                for cc in range((n + 127) // 128):
                    m = min(128, n - cc * 128)
                    for h0 in range(0, T, 512):
                        b = next_pbank()
                        for k in range(KC):
                            p.pe((lambda wt, cc, m, b, k, h0: lambda e: e.matmul(PS[0:m, b, 0:min(512, T - h0)], lhsT=wt[:, k, cc * 128:cc * 128 + m],
                                                                                 rhs=hT[:, k, h0:h0 + min(512, T - h0)], start=(k == 0), stop=(k == KC - 1)))(wt, cc, m, b, k, h0),
                                 reads=[wk] + src_keys, writes=[('ps', b)])
                        evac((done // 128) + cc, b, h0)
                done += n

        def gemm_tm(wview, c0, ncols, hT, T, evac, src_keys, also_fm=None):
            done = 0
            while done < ncols:
                n = min(256, ncols - done)
                wt, wk = load_w(wview, c0 + done, n)
                for tt in range(T // 128):
                    b = next_pbank()
                    for k in range(KC):
                        p.pe((lambda wt, n, b, k, tt: lambda e: e.matmul(PS[:, b, 0:n], lhsT=hT[:, k, tt * 128:(tt + 1) * 128],
                                                                         rhs=wt[:, k, 0:n], start=(k == 0), stop=(k == KC - 1)))(wt, n, b, k, tt),
                             reads=[wk] + src_keys, writes=[('ps', b)])
                    evac(done, n, tt, b)
                if also_fm is not None:
                    for cc in range(n // 128):
                        b = next_pbank()
                        for k in range(KC):
                            p.pe((lambda wt, cc, b, k: lambda e: e.matmul(PS[:, b, 0:T], lhsT=wt[:, k, cc * 128:(cc + 1) * 128],
                                                                          rhs=hT[:, k, 0:T], start=(k == 0), stop=(k == KC - 1)))(wt, cc, b, k),
                                 reads=[wk] + src_keys, writes=[('ps', b)])
                        also_fm((done // 128) + cc, b)
                done += n

        win_v = win_d.rearrange("(k p) c -> p k c", p=128)
        wout_v = wout_d.rearrange("(k p) c -> p k c", p=128)
        hT = A.view(R_HT, [32, MT], BF16)
        cT = A.view(R_CT, [32, MT], BF16)
        so = [R_S]

        def salloc(shape, dt):
            esz = 4 if dt == F32 else 2
            nb = (int(np.prod(shape)) * esz + 31) // 32 * 32
            v = A.view(so[0], shape, dt)
            so[0] += nb
            assert so[0] <= R_S + 49152, so[0]
            return v

        KT = salloc([2, 5 * 128], BF16)
        Vt = salloc([5, 2, 128], BF16)
        glowT = salloc([MT], BF16)
        S_AFTER_KV = so[0]
        QTg = salloc([4, 8, 128], BF16)
        tmpS = [salloc([512], F32) for _ in range(2)]
        PT = [salloc([512], BF16) for _ in range(2)]
        den = salloc([512], F32)
        so[0] = S_AFTER_KV
        gqT = salloc([2, MT], BF16)
        gkT = salloc([2, MT], BF16)
        gkt = salloc([4, 256], BF16)
        gv = salloc([4, 512], BF16)
        SG = salloc([4, 512], BF16)
        la = salloc([4, 256], F32)
        eb = salloc([2, MT], F32)
        enb = salloc([2, MT], F32)
        qdT = salloc([2, MT], BF16)
        kinvT = salloc([2, MT], BF16)
        kdec = salloc([4, 256], BF16)
        dl = salloc([2, 8], F32)
        Sbf = salloc([2, 512], BF16)
        attn_sb = salloc([64], BF16)
        ybf = salloc([512], BF16)
        tmp256 = salloc([256], F32)
        GLA_END = so[0]
        so[0] = S_AFTER_KV
        G1B = salloc([4096], F32)
        xres = [salloc([4, 256], F32) for _ in range(2)]
        dtmp = salloc([128], F32)

        PSb = PS[:, :, :].bitcast(BF16) if False else None

        p.pool(lambda e: e.memset(KT, 0.0), writes=['KT'])
        p.pool(lambda e: e.memset(Vt, 0.0), writes=['Vt'])

        def build_rowbcast(dst, colvec, tag):
            for c in range(KC):
                b = 2 + (c // 4) % 2
                q = c % 4
                p.dve((lambda c: lambda e: e.tensor_scalar(out=dtmp, in0=ident_f, scalar1=colvec[:, c:c + 1], scalar2=None, op0=ALU.mult))(c),
                      reads=['ident_f', 'modT'], writes=['dtmp'])
                p.pe((lambda b, q: lambda e: e.matmul(PS[:, b, q * 128:(q + 1) * 128], lhsT=ones_f, rhs=dtmp, start=True, stop=True))(b, q),
                     reads=['dtmp', 'ones_f'], writes=[('ps', b)])
                if q == 3:
                    p.act((lambda b, c: lambda e: e.activation(out=dst[:, (c - 3) * 128:(c + 1) * 128], in_=PS[:, b, :], func=AF.Identity))(b, c),
                          reads=[('ps', b)], writes=[tag])

        macro = [('P', 0), ('P', 1), ('M', 0), ('M', 1)]
        for kind, mi in macro:
            main = kind == 'M'
            src = x_main if main else x_pre
            rms_norm_to_hT(src, mi * MT, MT // 128, a1, s1, hT, R_CT, R_W + 32768)
            p.barrier()
            if stop == 'norm%s%d' % (kind, mi):
                p.barrier()
                p.emit(st)
                return nc, p
            need_kv = main or mi == 1

            if need_kv:
                def ev_k(ci, b, h0):
                    p.act((lambda ci, b: lambda e: e.activation(out=KT[:, ci, 128:128 + MT], in_=PS[:, b, 0:MT], func=AF.Identity))(ci, b),
                          reads=[('ps', b)], writes=['KT'])
                gemm_fm(win_v, C_AK, 256, hT, MT, ev_k, hT_keys)

                def ev_v(c0, n, tt, b):
                    p.dve((lambda tt, b: lambda e: e.tensor_copy(out=Vt[:, 1 + tt, :, :], in_=PS[:, b, 0:256].rearrange("p (g d) -> p g d", g=2)))(tt, b),
                          reads=[('ps', b)], writes=['Vt'])
                gemm_tm(win_v, C_AV, 256, hT, MT, ev_v, hT_keys)
            if main:
                for g in range(2):
                    def ev_q(ci, b, h0, g=g):
                        p.act((lambda ci, b: lambda e: e.activation(out=QTg[:, :, ci, :], in_=PS[:, b, 0:MT].rearrange("p (n q) -> p n q", n=4),
                                                                    func=AF.Copy, scale=128 ** -0.5))(ci, b),
                              reads=[('ps', b)], writes=['QTg'])
                    gemm_fm(win_v, C_AQ + g * 1024, 1024, hT, MT, ev_q, hT_keys)
                    for n in range(4):
                        for hf in range(2):
                            hs = slice(hf * 4, hf * 4 + 4)
                            hg = slice(g * 8 + hf * 4, g * 8 + hf * 4 + 4)
                            rq = QTg[:, n, hs, :].rearrange("p j q -> p (j q)")
                            p.pe((lambda g, n, rq: lambda e: e.matmul(PS[:, 2, :], lhsT=KT[:, g, (n + 1) * 128:(n + 2) * 128], rhs=rq, start=True, stop=True))(g, n, rq),
                                 reads=['KT', 'QTg'], writes=[('ps', 2)])
                            p.pe((lambda g, n, rq: lambda e: e.matmul(PS[:, 3, :], lhsT=KT[:, g, n * 128:(n + 1) * 128], rhs=rq, start=True, stop=True))(g, n, rq),
                                 reads=['KT', 'QTg'], writes=[('ps', 3)])
                            for w_, (bk, bt) in enumerate([(2, bcur), (3, bprev)]):
                                p.dve((lambda w_, bk, bt, hg: lambda e: e.tensor_tensor(out=tmpS[w_], in0=PS[:, bk, :], in1=bt[:, hg, :].rearrange("p j q -> p (j q)"), op=ALU.add))(w_, bk, bt, hg),
                                      reads=[('ps', bk)], writes=[('tmpS', w_)])
                                p.act((lambda w_: lambda e: e.activation(out=PT[w_], in_=tmpS[w_], func=AF.Exp))(w_),
                                      reads=[('tmpS', w_)], writes=[('PT', w_)])
                            if mi == 0 and n == 0:
                                p.dve(lambda e: e.tensor_scalar(out=PT[1], in0=PT[1], scalar1=flag[:, 0:1], scalar2=None, op0=ALU.mult),
                                      reads=[('PT', 1), 'flag'], writes=[('PT', 1)])
                            p.pe(lambda e: e.matmul(PS[:, 6, :], lhsT=ones_b, rhs=PT[0], start=True, stop=False), reads=[('PT', 0), 'ones_b'], writes=[('ps', 6)])
                            p.pe(lambda e: e.matmul(PS[:, 6, :], lhsT=ones_b, rhs=PT[1], start=False, stop=True), reads=[('PT', 1), 'ones_b'], writes=[('ps', 6)])
                            p.pe((lambda g, n: lambda e: e.matmul(PS[:, 7, :], lhsT=Vt[:, n + 1, g, :], rhs=PT[0], start=True, stop=False))(g, n),
                                 reads=[('PT', 0), 'Vt'], writes=[('ps', 7)])
                            p.pe((lambda g, n: lambda e: e.matmul(PS[:, 7, :], lhsT=Vt[:, n, g, :], rhs=PT[1], start=False, stop=True))(g, n),
                                 reads=[('PT', 1), 'Vt'], writes=[('ps', 7)])
                            for j in range(4):
                                hh = g * 8 + hf * 4 + j
                                p.dve((lambda j, hh: lambda e: e.tensor_scalar(out=den[:, j * 128:(j + 1) * 128], in0=PS[:, 6, j * 128:(j + 1) * 128],
                                                                               scalar1=esink[:, hh:hh + 1], scalar2=None, op0=ALU.add))(j, hh),
                                      reads=[('ps', 6), 'esink'], writes=['den'])
                            p.dve(lambda e: e.reciprocal(out=den, in_=den), reads=['den'], writes=['den'])
                            p.dve((lambda g, n, hf: lambda e: e.tensor_tensor(out=cT[:, g * 8 + hf * 4:g * 8 + hf * 4 + 4, n * 128:(n + 1) * 128],
                                                                              in0=PS[:, 7, :].rearrange("p (j q) -> p j q", j=4),
                                                                              in1=den.rearrange("p (j q) -> p j q", j=4), op=ALU.mult))(g, n, hf),
                                  reads=[('ps', 7), 'den'], writes=[('cT', g * 8 + hf * 4 + jj) for jj in range(4)])
            if need_kv:
                p.pool(lambda e: e.tensor_copy(out=KT[:, :, 0:128], in_=KT[:, :, 512:640]), reads=['KT'], writes=['KT'])
                p.pool(lambda e: e.tensor_copy(out=Vt[:, 0, :, :], in_=Vt[:, 4, :, :]), reads=['Vt'], writes=['Vt'])
            if stop == 'swa%s%d' % (kind, mi):
                p.barrier()
                p.emit(st)
                return nc, p
            if 'cT_attn' in dbg and main and mi == 0:
                p.barrier()
                p.dve(lambda e: e.tensor_copy(out=eb[:, 0, :], in_=cT[:, 0, :]), writes=['dbgtmp'])
                p.dma('sp', 'dbg', lambda e: e.dma_start(out=dbg_out['cT_attn'], in_=eb[:, 0, :]), reads=['dbgtmp'], writes=['dbg_ct'])
            p.barrier()
            def ev_glow(ci, b, h0):
                p.act((lambda b: lambda e: e.activation(out=glowT[0:16, :], in_=PS[0:16, b, 0:MT], func=AF.Identity))(b),
                      reads=[('ps', b)], writes=['glowT'])
            gemm_fm(win_v, C_GLOW, 16, hT, MT, ev_glow, hT_keys)
            for h in range(4):
                if main:
                    def ev_gq(ci, b, h0):
                        p.act((lambda ci, b: lambda e: e.activation(out=gqT[:, ci, :], in_=PS[:, b, 0:MT], func=AF.Identity))(ci, b),
                              reads=[('ps', b)], writes=['gqT'])
                    gemm_fm(win_v, C_GQ + h * 256, 256, hT, MT, ev_gq, hT_keys)

                def ev_gk(c0, n, tt, b):
                    p.dve((lambda tt, b: lambda e: e.tensor_copy(out=gkt[:, tt, :], in_=PS[:, b, 0:256]))(tt, b), reads=[('ps', b)], writes=['gkt'])

                def ev_gkT(ci, b):
                    p.act((lambda ci, b: lambda e: e.activation(out=gkT[:, ci, :], in_=PS[:, b, 0:MT], func=AF.Identity))(ci, b),
                          reads=[('ps', b)], writes=['gkT'])
                gemm_tm(win_v, C_GK + h * 256, 256, hT, MT, ev_gk, hT_keys, also_fm=ev_gkT if main else None)

                def ev_gv(c0, n, tt, b):
                    p.act((lambda c0, tt, b: lambda e: e.activation(out=gv[:, tt, c0:c0 + 256], in_=PS[:, b, 0:256], func=AF.Identity))(c0, tt, b),
                          reads=[('ps', b)], writes=['gv'])
                gemm_tm(win_v, C_GV + h * 512, 512, hT, MT, ev_gv, hT_keys)
                if main:
                    def ev_go(c0, n, tt, b):
                        p.act((lambda b: lambda e: e.activation(out=tmp256, in_=PS[:, b, 0:256], func=AF.Silu))(b),
                              reads=[('ps', b)], writes=['tmp256'])
                        p.dve((lambda c0, tt: lambda e: e.tensor_tensor(out=SG[:, tt, c0:c0 + 256], in0=tmp256, in1=gnb[:, c0:c0 + 256], op=ALU.mult))(c0, tt),
                              reads=['tmp256', 'gnb'], writes=['SG'])
                    gemm_tm(win_v, C_GOUT + h * 512, 512, hT, MT, ev_go, hT_keys)
                for tt in range(4):
                    p.pe((lambda tt, h: lambda e: e.matmul(PS[:, 2, 0:256], lhsT=glowT[0:16, tt * 128:(tt + 1) * 128], rhs=gk2w[0:16, h * 256:(h + 1) * 256],
                                                           start=True, stop=False))(tt, h), reads=['glowT', 'gk2w'], writes=[('ps', 2)])
                    p.pe((lambda tt, h: lambda e: e.matmul(PS[:, 2, 0:256], lhsT=ones_b[0:1, :], rhs=gk2b[0:1, h * 256:(h + 1) * 256],
                                                           start=False, stop=True))(tt, h), reads=['gk2b', 'ones_b'], writes=[('ps', 2)])
                    p.act(lambda e: e.activation(out=tmp256, in_=PS[:, 2, 0:256], func=AF.Exp, scale=-1.0), reads=[('ps', 2)], writes=['tmp256'])
                    p.act(lambda e: e.activation(out=tmp256, in_=tmp256, func=AF.Ln, bias=1.0), reads=['tmp256'], writes=['tmp256'])
                    p.act((lambda tt: lambda e: e.activation(out=la[:, tt, :], in_=tmp256, func=AF.Copy, scale=-1.0 / 16.0))(tt),
                          reads=['tmp256'], writes=['la'])
                for dc in range(2):
                    for tt in range(4):
                        p.pe((lambda dc, tt: lambda e: e.matmul(PS[:, 2 + dc, tt * 128:(tt + 1) * 128], lhsT=la[:, tt, dc * 128:(dc + 1) * 128], rhs=Lm,
                                                                start=True, stop=True))(dc, tt), reads=['la', 'Lm'], writes=[('ps', 2 + dc)])
                p.act(lambda e: e.activation(out=eb, in_=PS[:, 2:4, :], func=AF.Exp), reads=[('ps', 2), ('ps', 3)], writes=['eb'])
                if main:
                    p.act(lambda e: e.activation(out=enb, in_=PS[:, 2:4, :], func=AF.Exp, scale=-1.0), reads=[('ps', 2), ('ps', 3)], writes=['enb'])
                    p.dve(lambda e: e.scalar_tensor_tensor(out=qdT, in0=gqT, scalar=1.0 / 16.0, in1=eb, op0=ALU.mult, op1=ALU.mult),
                          reads=['gqT', 'eb'], writes=['qdT'])
                    p.dve(lambda e: e.tensor_tensor(out=kinvT, in0=gkT, in1=enb, op=ALU.mult), reads=['gkT', 'enb'], writes=['kinvT'])
                p.dve(lambda e: e.tensor_copy(out=dl, in_=eb[:, :, 63::64]), reads=['eb'], writes=['dl'])
                for tt in range(4):
                    p.pe((lambda tt: lambda e: e.matmul(PS[:, 2, 0:256], lhsT=Um, rhs=la[:, tt, :], start=True, stop=True))(tt),
                         reads=['la', 'Um'], writes=[('ps', 2)])
                    p.act(lambda e: e.activation(out=tmp256, in_=PS[:, 2, 0:256], func=AF.Exp), reads=[('ps', 2)], writes=['tmp256'])
                    p.dve((lambda tt: lambda e: e.tensor_tensor(out=kdec[:, tt, :], in0=gkt[:, tt, :], in1=tmp256, op=ALU.mult))(tt),
                          reads=['gkt', 'tmp256'], writes=['kdec'])
                p.act((lambda h: lambda e: e.activation(out=Sbf, in_=Sst[:, h, :, :], func=AF.Identity))(h), reads=['Sst'], writes=['Sbf'])
                for c in range(8):
                    tt, par = c // 2, c % 2
                    r0 = 64 * par
                    t0 = c * 64
                    rs = slice(r0, r0 + 64)
                    if main:
                        for dc in range(2):
                            p.pe((lambda dc, t0, rs: lambda e: e.matmul(PS[rs, 3, 0:64], lhsT=kinvT[:, dc, t0:t0 + 64], rhs=qdT[:, dc, t0:t0 + 64],
                                                                        start=(dc == 0), stop=(dc == 1)))(dc, t0, rs),
                                 reads=['kinvT', 'qdT'], writes=[('ps', 3)])
                        p.dve((lambda rs: lambda e: e.tensor_tensor(out=attn_sb[rs, :], in0=PS[rs, 3, 0:64], in1=Lm[rs, rs], op=ALU.mult))(rs),
                              reads=[('ps', 3), 'Lm'], writes=['attn_sb'])
                        p.pe((lambda rs, tt: lambda e: e.matmul(PS[rs, 6, :], lhsT=attn_sb[rs, :], rhs=gv[rs, tt, :], start=True, stop=False))(rs, tt),
                             reads=['attn_sb', 'gv'], writes=[('ps', 6)])
                        for dc in range(2):
                            p.pe((lambda dc, t0, rs: lambda e: e.matmul(PS[rs, 6, :], lhsT=qdT[:, dc, t0:t0 + 64], rhs=Sbf[:, dc, :],
                                                                        start=False, stop=(dc == 1)))(dc, t0, rs),
                                 reads=['qdT', 'Sbf'], writes=[('ps', 6)])
                    for dc in range(2):
                        p.pe((lambda dc, rs, tt: lambda e: e.matmul(PS[:, 4 + dc, :], lhsT=kdec[rs, tt, dc * 128:(dc + 1) * 128], rhs=gv[rs, tt, :],
                                                                    start=True, stop=True))(dc, rs, tt),
                             reads=['kdec', 'gv'], writes=[('ps', 4 + dc)])
                        p.dve((lambda dc, c, h: lambda e: e.scalar_tensor_tensor(out=Sst[:, h, dc, :], in0=Sst[:, h, dc, :], scalar=dl[:, dc, c:c + 1],
                                                                                 in1=PS[:, 4 + dc, :], op0=ALU.mult, op1=ALU.add))(dc, c, h),
                              reads=['Sst', 'dl', ('ps', 4 + dc)], writes=['Sst'])
                    p.act((lambda h: lambda e: e.activation(out=Sbf, in_=Sst[:, h, :, :], func=AF.Identity))(h), reads=['Sst'], writes=['Sbf'])
                    if main and par == 1:
                        sc = small[:, 8:12]
                        p.act(lambda e: e.activation(out=ybf, in_=PS[:, 6, :], func=AF.Square, accum_out=sc[:, 0:1]), reads=[('ps', 6)], writes=['ybf', 'gsc'])
                        p.dve(lambda e: e.tensor_scalar(out=sc[:, 1:2], in0=sc[:, 0:1], scalar1=1.0 / 512, scalar2=EPS, op0=ALU.mult, op1=ALU.add),
                              reads=['gsc'], writes=['gsc'])
                        p.act(lambda e: e.activation(out=sc[:, 2:3], in_=sc[:, 1:2], func=AF.Sqrt), reads=['gsc'], writes=['gsc'])
                        p.dve(lambda e: e.reciprocal(out=sc[:, 3:4], in_=sc[:, 2:3]), reads=['gsc'], writes=['gsc'])
                        p.dve((lambda tt: lambda e: e.scalar_tensor_tensor(out=ybf, in0=PS[:, 6, :], scalar=sc[:, 3:4], in1=SG[:, tt, :],
                                                                           op0=ALU.mult, op1=ALU.mult))(tt), reads=[('ps', 6), 'gsc', 'SG'], writes=['ybf'])
                        pst = PS[:, 7, :].bitcast(BF16)
                        for j in range(4):
                            p.pe((lambda j: lambda e: e.transpose(pst[:, j * 128:(j + 1) * 128], ybf[:, j * 128:(j + 1) * 128], ident_b))(j),
                                 reads=['ybf', 'ident_b'], writes=[('ps', 7)])
                        p.act((lambda h, tt: lambda e: e.activation(out=cT[:, 16 + h * 4:16 + h * 4 + 4, tt * 128:(tt + 1) * 128],
                                                                    in_=pst[:, 0:512].rearrange("p (j q) -> p j q", j=4), func=AF.Identity))(h, tt),
                              reads=[('ps', 7)], writes=[('cT', 16 + h * 4 + jj) for jj in range(4)])
                if not main and mi == 1:
                    p.dve((lambda h: lambda e: e.tensor_scalar(out=Sst[:, h, :, :], in0=Sst[:, h, :, :], scalar1=flag[:, 0:1], scalar2=None, op0=ALU.mult))(h),
                          reads=['Sst', 'flag'], writes=['Sst'])
            p.barrier()
            if stop == 'gla%s%d' % (kind, mi):
                p.emit(st)
                return nc, p
            if 'cT_gla' in dbg and main and mi == 0:
                p.dve(lambda e: e.tensor_copy(out=eb[:, 0, :], in_=cT[:, 16, :]), writes=['dbgtmp'])
                p.dma('sp', 'dbg', lambda e: e.dma_start(out=dbg_out['cT_gla'], in_=eb[:, 0, :]), reads=['dbgtmp'], writes=['dbg_ct2'])
                p.barrier()
            if not main:
                continue
            build_rowbcast(G1B, gate1, 'G1B')
            cT_keys = [('cT', c) for c in range(KC)]
            xstate = {}

            def ev_o(c0, n, tt, b):
                cg = c0 // 256
                xr = xres[cg % 2]
                xk = ('xres', cg % 2)
                if tt == 0:
                    srcap = x_main[mi * MT:(mi + 1) * MT, c0:c0 + 256].rearrange("(t p) c -> p t c", p=128)
                    p.dma('sp', xk, (lambda xr, srcap: lambda e: e.dma_start(out=xr, in_=srcap))(xr, srcap), writes=[xk])
                p.dve((lambda c0, b: lambda e: e.tensor_tensor(out=tmp256, in0=PS[:, b, 0:256], in1=G1B[:, c0:c0 + 256], op=ALU.mult))(c0, b),
                      reads=[('ps', b), 'G1B'], writes=['tmp256'])
                p.dve((lambda xr, tt: lambda e: e.tensor_tensor(out=xr[:, tt, :], in0=xr[:, tt, :], in1=tmp256, op=ALU.add))(xr, tt),
                      reads=['tmp256', xk], writes=[xk])
                if tt == 3:
                    dstap = x1s[mi * MT:(mi + 1) * MT, c0:c0 + 256].rearrange("(t p) c -> p t c", p=128)
                    p.dma('sp', ('xst', cg % 2), (lambda xr, dstap: lambda e: e.dma_start(out=dstap, in_=xr))(xr, dstap),
                          reads=[xk], writes=[xk, 'x1s'])
            gemm_tm(wout_v, 0, D, cT, MT, ev_o, cT_keys)
            p.barrier()
        if upto <= 2:
            p.emit(st)
            return nc, p

        wq_v = wq_d.rearrange("(k p) c -> p k c", p=128)
        ut_v = ut_d.rearrange("(k p) e -> p k e", p=128)
        h2T = A.view(R_HT, [32, TOK], BF16)
        h2_keys = [('hT', c) for c in range(KC)]
        rms_norm_to_hT(x1s, 0, TOK // 128, a2, s2, h2T, R_W, R_W + 32768)
        p.barrier()
        qT = A.view(R_S, [16, TOK], BF16)

        def ev_pq(ci, b, h0):
            p.act((lambda ci, b, h0: lambda e: e.activation(out=qT[:, ci, h0:h0 + 512], in_=PS[:, b, :], func=AF.Identity))(ci, b, h0),
                  reads=[('ps', b)], writes=['qT'])
        gemm_fm(wq_v, 0, 2048, h2T, TOK, ev_pq, h2_keys)
        p.barrier()
        s2_all = A.view(R_W, [8, 8, 128], F32)
        sig = [A.view(R_W + 32768 + tt * 4096, [8, 128], F32) for tt in range(4)] + \
              [A.view(R_S + 32768 + tt * 4096, [8, 128], F32) for tt in range(4)]
        qo = [P_DYN]

        def qalloc(shape, dt):
            esz = 4 if dt == F32 else 2
            nb = (int(np.prod(shape)) * esz + 31) // 32 * 32
            v = A.view(qo[0], shape, dt)
            qo[0] += nb
            assert qo[0] <= R_P + 36864, qo[0]
            return v
        rz_all = qalloc([64], F32)
        Q_KEEP = qo[0]
        keysT = qalloc([16, 128], BF16)
        t16 = qalloc([16, 16], F32)
        tmpk = qalloc([128], F32)
        cand = qalloc([16, 16], F32)
        tmpc = qalloc([256], F32)
        c16 = qalloc([8, 16], F32)
        tsc = qalloc([8, 8], F32)
        junk16 = qalloc([16], F32)
        scs = qalloc([16, 128], F32)
        p.dma('pool', 'peer_c', lambda e: e.dma_start(out=keysT, in_=keyst_d.rearrange("p (a k) -> p a k", a=16)), writes=['keysT'])
        p.barrier()
        for tt in range(8):
            for hp in range(16):
                b = 4 + hp // 4
                p.pe((lambda hp, b, tt: lambda e: e.matmul(PS[:, b, (hp % 4) * 128:(hp % 4 + 1) * 128], lhsT=qT[:, hp, tt * 128:(tt + 1) * 128],
                                                           rhs=keysT[:, hp, :], start=True, stop=True))(hp, b, tt),
                     reads=['qT', 'keysT'], writes=[('ps', b)])
            p.act(lambda e: e.activation(out=scs, in_=PS[:, 4:8, :].rearrange("p b (c k) -> p (b c) k", c=4), func=AF.Identity),
                  reads=[('ps', 4), ('ps', 5), ('ps', 6), ('ps', 7)], writes=['scs'])
            p.pool((lambda tt: lambda e: e.tensor_copy(out=s2_all[:, tt, :, :], in_=scs.rearrange("p (h two) k -> p h two k", two=2)[:, :, 1, :]))(tt),
                   reads=['scs'], writes=[('s2', tt)])
            for hp in range(16):
                p.dve((lambda hp: lambda e: e.max(out=t16[:, hp, 0:8], in_=scs[:, hp, :]))(hp), reads=['scs'], writes=['t16'])
                p.dve((lambda hp: lambda e: e.match_replace(out=tmpk, in_to_replace=t16[:, hp, 0:8], in_values=scs[:, hp, :], imm_value=-1e30))(hp),
                      reads=['scs', 't16'], writes=['tmpk'])
                p.dve((lambda hp: lambda e: e.max(out=t16[:, hp, 8:16], in_=tmpk))(hp), reads=['tmpk'], writes=['t16'])
            for h in range(8):
                p.dve((lambda h: lambda e: e.tensor_tensor(out=cand, in0=t16[:, 2 * h, :].unsqueeze(2).to_broadcast([128, 16, 16]),
                                                           in1=t16[:, 2 * h + 1, :].unsqueeze(1).to_broadcast([128, 16, 16]), op=ALU.add))(h),
                      reads=['t16'], writes=['cand'])
                cf = cand.rearrange("p a b -> p (a b)")
                p.dve((lambda h: lambda e: e.max(out=c16[:, h, 0:8], in_=cf))(h), reads=['cand'], writes=['c16'])
                p.dve((lambda h: lambda e: e.match_replace(out=tmpc, in_to_replace=c16[:, h, 0:8], in_values=cf, imm_value=-1e30))(h),
                      reads=['cand', 'c16'], writes=['tmpc'])
                p.dve((lambda h: lambda e: e.max(out=c16[:, h, 8:16], in_=tmpc))(h), reads=['tmpc'], writes=['c16'])
                p.dve((lambda h: lambda e: e.tensor_scalar(out=tsc[:, h, 0:1], in0=c16[:, h, 15:16], scalar1=-1.0, scalar2=1e-3, op0=ALU.mult, op1=ALU.add))(h),
                      reads=['c16'], writes=['tsc'])
                p.act((lambda h: lambda e: e.activation(out=junk16, in_=c16[:, h, :], func=AF.Exp, bias=tsc[:, h, 0:1], accum_out=tsc[:, h, 1:2]))(h),
                      reads=['c16', 'tsc'], writes=['tsc', 'junk16'])
                p.dve((lambda h, tt: lambda e: e.reciprocal(out=rz_all[:, tt * 8 + h:tt * 8 + h + 1], in_=tsc[:, h, 1:2]))(h, tt), reads=['tsc'], writes=['rz_all'])
                p.act((lambda h: lambda e: e.activation(out=tsc[:, h, 3:4], in_=tsc[:, h, 1:2], func=AF.Ln))(h), reads=['tsc'], writes=['tsc'])
                p.dve((lambda h: lambda e: e.tensor_tensor(out=tsc[:, h, 4:5], in0=tsc[:, h, 0:1], in1=tsc[:, h, 3:4], op=ALU.subtract))(h),
                      reads=['tsc'], writes=['tsc'])
                p.dve((lambda h, tt: lambda e: e.tensor_scalar(out=sig[tt][:, h, :], in0=scs[:, 2 * h, :], scalar1=tsc[:, h, 4:5], scalar2=None, op0=ALU.add))(h, tt),
                      reads=['scs', 'tsc'], writes=[('sig', tt)])
        if 'thr' in dbg:
            p.barrier()
            p.dma('sp', 'dbg', lambda e: e.dma_start(out=dbg_out['thr'], in_=sig[0][:, :, 0]), writes=['dbg_thr'])
            p.dma('sp', 'dbg2', lambda e: e.dma_start(out=dbg_out['rzr'], in_=rz_all), writes=['dbg_rzr'])
        p.barrier()
        if upto <= 3:
            p.emit(st)
            return nc, p
        ut = [A.view(R_S + i * 16384, [32, 256], BF16) for i in range(2)]
        qo[0] = Q_KEEP
        Ep = [qalloc([128], F32) for _ in range(4)]
        Wm = [qalloc([128], BF16) for _ in range(8)]
        Gl2 = [qalloc([TOK], BF16) for _ in range(2)]
        AW = [qalloc([TOK], BF16) for _ in range(2)]
        NE = 128 if upto > 4 else 2

        def load_u(pi):
            us = pi % 2
            p.dma('pool', ('U', us), lambda e: e.dma_start(out=ut[us], in_=ut_v[:, :, pi * 256:pi * 256 + 256]), writes=[('U', us)])

        def actT_mms(i1):
            us = (i1 // 2) % 2
            uk = ('U', us)
            ec = (i1 % 2) * 128
            lst = []
            for hf in range(2):
                for k in range(KC):
                    lst.append((lambda hf, k: lambda: p.pe(lambda e: e.matmul(PS[:, hf, :], lhsT=ut[us][:, k, ec:ec + 128], rhs=h2T[:, k, hf * 512:(hf + 1) * 512],
                                                                             start=(k == 0), stop=(k == KC - 1)),
                                                           reads=[uk] + h2_keys, writes=[('ps', hf)]))(hf, k))
            return lst

        def emit_gelu(i1):
            g = Gl2[i1 % 2]
            p.act(lambda e: e.activation(out=g.rearrange("p (b t) -> p b t", b=2), in_=PS[:, 0:2, :], func=AF.Gelu), reads=[('ps', 0), ('ps', 1)], writes=[('Gl', i1 % 2)])

        def emit_gate(i1, k):
            tt, h = k // 8, k % 8
            r, r8 = k % 4, k % 8
            p.act(lambda e: e.activation(out=Ep[r], in_=s2_all[:, tt, h, :], func=AF.Exp, bias=sig[tt][:, h, i1:i1 + 1]), writes=[('Ep', r)])
            p.dve(lambda e: e.scalar_tensor_tensor(out=Wm[r8], in0=Ep[r], scalar=rz_all[:, tt * 8 + h:tt * 8 + h + 1], in1=Ep[r], op0=ALU.is_ge, op1=ALU.mult),
                  reads=[('Ep', r)], writes=[('Wm', r8)])

        def emit_acc(k):
            tt, h = k // 8, k % 8
            r8 = k % 8
            p.pe(lambda e: e.matmul(PS[:, 6 + tt // 4, (tt % 4) * 128:(tt % 4 + 1) * 128], lhsT=Wm[r8], rhs=ident_b, start=(h == 0), stop=(h == 7)),
                 reads=[('Wm', r8), 'ident_b'], writes=[('ps', 6 + tt // 4)])

        LAG = 4
        load_u(0)
        for f in actT_mms(0):
            f()
        emit_gelu(0)
        for i1 in range(NE):
            if i1 % 2 == 0 and (i1 // 2 + 1) * 2 < NE:
                load_u(i1 // 2 + 1)
            nxt = actT_mms(i1 + 1) if i1 + 1 < NE else []
            for k in range(64 + LAG):
                if k < 64:
                    emit_gate(i1, k)
                    if k < len(nxt):
                        nxt[k]()
                if k - LAG >= 0:
                    emit_acc(k - LAG)
            a = i1 % 2
            g = Gl2[i1 % 2]
            p.dve((lambda a, g: lambda e: e.tensor_tensor(out=AW[a].rearrange("p (b t) -> p b t", b=2), in0=PS[:, 6:8, :], in1=g.rearrange("p (b t) -> p b t", b=2), op=ALU.mult))(a, g),
                  reads=[('ps', 6), ('ps', 7), ('Gl', i1 % 2)], writes=[('AW', a)])
            p.dma('sp', ('AWst', a), (lambda a, i1: lambda e: e.dma_start(out=aws[i1], in_=AW[a]))(a, i1), reads=[('AW', a)], writes=[('AW', a), 'aws'])
            if i1 + 1 < NE:
                emit_gelu(i1 + 1)
        if 'aw0' in dbg:
            p.barrier()
            dbt = A.view(R_HT, [TOK], F32)
            p.act(lambda e: e.activation(out=dbt, in_=AW[0], func=AF.Identity), writes=['dbt'])
            p.dma('sp', 'dbg', lambda e: e.dma_start(out=dbg_out['aw0'], in_=dbt), reads=['dbt'], writes=['dbg_aw0'])
        p.barrier()
        if upto <= 4:
            p.emit(st)
            return nc, p
        acc = A.view(0, [8, D], F32)
        vt = [A.view(131072 + i * 8192, [8, 512], BF16) for i in range(2)]
        awt = [A.view(131072 + 16384, [8, TOK], BF16), A.view(P_DYN, [8, TOK], BF16)]
        v_v = v_d.rearrange("(g a p) d -> g p a d", a=8, p=128)
        aws_v = aws.rearrange("(g a) p t -> g p a t", a=8)
        it = 0
        for dg in range(8):
            for eg in range(16):
                s_ = it % 2
                it += 1
                p.dma('pool', ('V', s_), (lambda s_, eg, dg: lambda e: e.dma_start(out=vt[s_], in_=v_v[eg][:, :, dg * 512:(dg + 1) * 512]))(s_, eg, dg), writes=[('V', s_)])
                p.dma('sp', ('AWld', s_), (lambda s_, eg: lambda e: e.dma_start(out=awt[s_], in_=aws_v[eg]))(s_, eg), reads=['aws'], writes=[('AWl', s_)])
                for a in range(8):
                    for tt in range(8):
                        p.pe((lambda s_, a, tt, eg: lambda e: e.matmul(PS[:, tt, :], lhsT=awt[s_][:, a, tt * 128:(tt + 1) * 128], rhs=vt[s_][:, a, :],
                                                                      start=(eg == 0 and a == 0), stop=(eg == 15 and a == 7)))(s_, a, tt, eg),
                             reads=[('V', s_), ('AWl', s_)], writes=[('ps', tt)])
            for tt in range(8):
                if tt % 2 == 0:
                    p.act((lambda tt, dg: lambda e: e.activation(out=acc[:, tt, dg * 512:(dg + 1) * 512], in_=PS[:, tt, :], func=AF.Identity))(tt, dg),
                          reads=[('ps', tt)], writes=[('acc', tt, dg)])
                else:
                    p.dve((lambda tt, dg: lambda e: e.tensor_copy(out=acc[:, tt, dg * 512:(dg + 1) * 512], in_=PS[:, tt, :]))(tt, dg),
                          reads=[('ps', tt)], writes=[('acc', tt, dg)])
        p.barrier()
        G2B = A.view(131072, [D], F32)
        FGB = A.view(131072 + 16384, [D], F32)
        x1t = A.view(P_DYN + 1024, [D], F32)
        dtmp2 = A.view(P_DYN, [128], F32)

        for c in range(KC):
            b = 2 + (c // 4) % 2
            q = c % 4
            p.dve((lambda c: lambda e: e.tensor_scalar(out=dtmp2, in0=ident_f, scalar1=gate2[:, c:c + 1], scalar2=None, op0=ALU.mult))(c),
                  reads=['ident_f', 'modT'], writes=['dtmp2'])
            p.pe((lambda b, q: lambda e: e.matmul(PS[:, b, q * 128:(q + 1) * 128], lhsT=ones_f, rhs=dtmp2, start=True, stop=True))(b, q),
                 reads=['dtmp2', 'ones_f'], writes=[('ps', b)])
            if q == 3:
                p.act((lambda b, c: lambda e: e.activation(out=G2B[:, (c - 3) * 128:(c + 1) * 128], in_=PS[:, b, :], func=AF.Identity))(b, c),
                      reads=[('ps', b)], writes=['G2B'])
        p.dma('sp', 'fgb', lambda e: e.dma_start(out=FGB, in_=fg_d.partition_broadcast(128)[:, 0, :]), writes=['FGB'])
        for tt in range(8):
            sc = small[:, 16:20]
            p.dma('sp', 'x1ld', (lambda tt: lambda e: e.dma_start(out=x1t, in_=x1s[tt * 128:(tt + 1) * 128, :]))(tt), reads=['x1s'], writes=['x1t'])
            p.dve((lambda tt: lambda e: e.tensor_tensor(out=acc[:, tt, :], in0=acc[:, tt, :], in1=G2B, op=ALU.mult))(tt), reads=['G2B'], writes=[('accf', tt)])
            p.pool((lambda tt: lambda e: e.tensor_tensor(out=x1t, in0=x1t, in1=acc[:, tt, :], op=ALU.add))(tt), reads=[('accf', tt), 'x1t'], writes=['x1t'])
            p.act(lambda e: e.activation(out=PS[:, :, :], in_=x1t.rearrange("p (b t) -> p b t", b=8), func=AF.Square, accum_out=sc[:, 0:1]), reads=['x1t'], writes=['junkf', 'fsc'])
            p.dve(lambda e: e.tensor_scalar(out=sc[:, 1:2], in0=sc[:, 0:1], scalar1=1.0 / D, scalar2=EPS, op0=ALU.mult, op1=ALU.add), reads=['fsc'], writes=['fsc'])
            p.act(lambda e: e.activation(out=sc[:, 2:3], in_=sc[:, 1:2], func=AF.Sqrt), reads=['fsc'], writes=['fsc'])
            p.dve(lambda e: e.reciprocal(out=sc[:, 3:4], in_=sc[:, 2:3]), reads=['fsc'], writes=['fsc'])
            p.dve(lambda e: e.scalar_tensor_tensor(out=x1t, in0=x1t, scalar=sc[:, 3:4], in1=FGB, op0=ALU.mult, op1=ALU.mult), reads=['x1t', 'fsc', 'FGB'], writes=['x1t'])
            p.dma('sp', 'ost', (lambda tt: lambda e: e.dma_start(out=out_d[tt * 128:(tt + 1) * 128, :], in_=x1t))(tt), reads=['x1t'], writes=['x1t', 'out'])
        p.barrier()
        p.emit(st)
    return nc, p


def t5_bucket_np(dist):
    max_exact = 16
    d = np.maximum(dist, 0)
    lr = np.log(np.maximum(d, 1).astype(np.float32) / max_exact) / math.log(128 / max_exact)
    large = max_exact + (lr * (32 - max_exact)).astype(np.int32)
    large = np.minimum(large, 31)
    return np.where(d < max_exact, d, large)


def make_in_maps(inputs, cores=range(8)):
    f = lambda a: np.ascontiguousarray(np.asarray(a, dtype=np.float32))
    x = f(inputs["x"]); c = f(inputs["c"])
    fm = lambda v, n: np.ascontiguousarray(v.reshape(n, 128).T)
    onehot = np.zeros((32, 128), np.float32)
    onehot[t5_bucket_np(np.arange(128)), np.arange(128)] = 1.0
    sel = np.zeros((24, 8, 128), np.float32)
    for h in range(8):
        for part in range(3):
            sel[part * 8 + h, h, :] = 1.0
    keys = f(inputs["peer_keys"])[0]
    keys_t = np.ascontiguousarray(keys.transpose(3, 0, 1, 2).reshape(128, 16 * 128))
    shared = {
        "w_ada": f(inputs["w_ada"])[0],
        "b_ada_t": fm(f(inputs["b_ada"])[0], 192),
        "g1_t": fm(f(inputs["norm1_g"])[0], 32),
        "g2_t": fm(f(inputs["norm2_g"])[0], 32),
        "w_in": f(inputs["w_in"])[0],
        "sinks": f(inputs["attn_sinks"])[0].reshape(1, 16),
        "rel_bias": f(inputs["rel_bias"]),
        "onehot": onehot,
        "gk2_w": f(inputs["gla_w_gk2"])[0],
        "gk2_b": f(inputs["gla_b_gk2"])[0].reshape(1, 1024),
        "gla_norm_g": f(inputs["gla_norm_g"])[0].reshape(1, 512),
        "w_out": f(inputs["w_out"])[0],
        "w_q": f(inputs["peer_w_q"])[0],
        "keys_t": keys_t,
        "u_t": np.ascontiguousarray(f(inputs["peer_u"])[0].T),
        "v": f(inputs["peer_v"])[0],
        "final_g": f(inputs["final_g"]).reshape(1, D),
        "sel": sel.reshape(24, 8 * 128),
    }
    maps = []
    for i in cores:
        b, half = i // 2, i % 2
        m = dict(shared)
        m["x_main"] = np.ascontiguousarray(x[b, half * TOK:(half + 1) * TOK])
        m["x_pre"] = np.ascontiguousarray(x[b, 0:TOK])
        m["flag"] = np.full((128, 1), float(half), np.float32)
        m["c_t"] = fm(c[b], 32)
        maps.append(m)
    return maps


_CACHE = {}


def kernel(**inputs):
    if "nc" not in _CACHE:
        _CACHE["nc"] = build_program()[0]
    nc = _CACHE["nc"]
    maps = make_in_maps(inputs)
    res = run_bass_kernel_spmd(nc, maps, core_ids=list(range(8)))
    out = np.empty((4, 2048, D), np.float32)
    for i in range(8):
        b, half = i // 2, i % 2
        out[b, half * TOK:(half + 1) * TOK] = res.results[i]["out"]
    return out
```

```python
from contextlib import ExitStack
import math
import numpy as np
import concourse.bass as bass
import concourse.mybir as mybir
from concourse.bass_utils import run_bass_kernel_spmd

F32 = mybir.dt.float32
BF16 = mybir.dt.bfloat16
AF = mybir.ActivationFunctionType
ALU = mybir.AluOpType

SAME_ENG_SYNC = True

D = 4096
KC = 32
TOK = 1024
MT = 512
NEXP = 16384
EPS = 1e-6
NEGM = -30000.0
IN_W = 8720
C_AQ, C_AK, C_AV, C_GQ, C_GK, C_GV, C_GLOW, C_GOUT = 0, 2048, 2304, 2560, 3584, 4608, 6656, 6672


class Prog:
    def __init__(self, nc):
        self.nc = nc
        self.ins = []
        self.last_w = {}
        self.readers = {}
        self.last_on = {}
        self.dmas_open = []

    maxops = None

    def op(self, eng, fn, reads=(), writes=(), dma=None, extra_deps=(), force=False):
        if self.maxops is not None and len(self.ins) >= self.maxops and not force:
            return None
        i = len(self.ins)
        deps = set(extra_deps)
        psk = [k for k in reads if k == 'ps0' or (isinstance(k, tuple) and k[0] == 'ps')]
        if psk:
            reads = [k for k in reads if k not in psk]
            writes = list(writes) + [k for k in psk if k not in writes]
        for k in reads:
            w = self.last_w.get(k)
            if w is not None:
                deps.add(w)
        for k in writes:
            w = self.last_w.get(k)
            if w is not None:
                deps.add(w)
            for r in self.readers.get(k, ()):
                deps.add(r)
        for k in reads:
            lst = self.readers.setdefault(k, [])
            if dma is None:
                for q in range(len(lst)):
                    J = self.ins[lst[q]]
                    if J['dma'] is None and J['eng'] == eng:
                        lst[q] = i
                        break
                else:
                    lst.append(i)
            else:
                lst.append(i)
        for k in writes:
            self.last_w[k] = i
            self.readers[k] = []
        deps.discard(i)
        self.ins.append(dict(eng=eng, fn=fn, deps=deps, dma=dma))
        if dma is None:
            self.last_on[eng] = i
        else:
            self.dmas_open.append(i)
        return i

    def pe(self, fn, reads=(), writes=()):
        return self.op('pe', fn, reads, writes)

    def act(self, fn, reads=(), writes=()):
        return self.op('act', fn, reads, writes)

    def dve(self, fn, reads=(), writes=()):
        return self.op('dve', fn, reads, writes)

    def pool(self, fn, reads=(), writes=()):
        return self.op('pool', fn, reads, writes)

    def dma(self, eng, key, fn, reads=(), writes=()):
        return self.op(eng, fn, reads, writes, dma=key)

    def barrier(self):
        deps = set(self.last_on.values()) | set(self.dmas_open)
        self.dmas_open = []
        for e in ['pe', 'act', 'dve', 'pool', 'sp']:
            self.op(e, lambda eng: eng.nop(), extra_deps=deps, force=True)
        self.last_w = {}
        self.readers = {}

    def emit(self, stack):
        nc = self.nc
        ins = self.ins
        n = len(ins)
        engs = ['pe', 'act', 'dve', 'pool', 'sp']

        def needs_wait(I, Dd):
            if Dd['dma'] is not None:
                return True
            if Dd['eng'] == I['eng'] and I['dma'] is None:
                if Dd['eng'] in ('pe', 'sp') or not SAME_ENG_SYNC:
                    return False
            return True

        need_sig = [False] * n
        for i, I in enumerate(ins):
            for d in I['deps']:
                Dd = ins[d]
                if Dd['dma'] is None and needs_wait(I, Dd):
                    need_sig[d] = True
        cnt = {e: 0 for e in engs}
        sigval = [0] * n
        dma_cnt = {}
        for i, I in enumerate(ins):
            if I['dma'] is not None:
                k = I['dma']
                dma_cnt[k] = dma_cnt.get(k, 0) + 16
                sigval[i] = dma_cnt[k]
            elif need_sig[i]:
                cnt[I['eng']] += 1
                sigval[i] = cnt[I['eng']]
        esem = {e: stack.enter_context(nc.semaphore('s_' + e)) for e in engs}
        dsem = {k: stack.enter_context(nc.semaphore('d_%d' % j)) for j, k in enumerate(dma_cnt)}
        self.stats = dict(n=n, cnt=dict(cnt), ndsem=len(dsem), maxdma=max(dma_cnt.values()) if dma_cnt else 0)
        per_eng = {e: [] for e in engs}
        for i, I in enumerate(ins):
            per_eng[I['eng']].append(i)

        def run(e, engobj):
            waited = {}
            for i in per_eng[e]:
                I = ins[i]
                need = {}
                for d in I['deps']:
                    Dd = ins[d]
                    if not needs_wait(I, Dd):
                        continue
                    if Dd['dma'] is not None:
                        key = ('d', Dd['dma'])
                    else:
                        key = ('e', Dd['eng'])
                    v = sigval[d]
                    if need.get(key, 0) < v:
                        need[key] = v
                for key, v in need.items():
                    if waited.get(key, 0) >= v:
                        continue
                    waited[key] = v
                    s = dsem[key[1]] if key[0] == 'd' else esem[key[1]]
                    engobj.wait_ge(s, v)
                r = I['fn'](engobj)
                if I['dma'] is not None:
                    r.then_inc(dsem[I['dma']], 16)
                elif need_sig[i]:
                    r.then_inc(esem[e], 1)

        block = stack.enter_context(nc.Block())
        block.sync(lambda eng: run('sp', eng))
        block.tensor(lambda eng: run('pe', eng))
        block.scalar(lambda eng: run('act', eng))
        block.vector(lambda eng: run('dve', eng))
        block.gpsimd(lambda eng: run('pool', eng))


class Arena:
    def __init__(self, nc, nbytes):
        self.nbytes = nbytes
        self.t = nc.alloc_sbuf_tensor("arena", [128, nbytes // 4], F32)

    def view(self, off, shape, dtype):
        esz = 4 if dtype == F32 else 2
        nel = int(np.prod(shape))
        nb = nel * esz
        assert off % 4 == 0 and nb % 4 == 0, (off, shape)
        assert off + nb <= self.nbytes, (off, nb, self.nbytes)
        ap = self.t[:, off // 4: (off + nb) // 4]
        if dtype != F32:
            ap = ap.bitcast(dtype)
        if len(shape) == 2:
            names = "a b"
            ap = ap.rearrange("p (a b) -> p a b", a=shape[0], b=shape[1])
        elif len(shape) == 3:
            ap = ap.rearrange("p (a b c) -> p a b c", a=shape[0], b=shape[1], c=shape[2])
        return ap


def build_program(dbg=None, upto=99, stop=None, fake_mod=False):
    nc = bass.Bass("TRN2", target_bir_lowering=False)
    dbg = dbg or {}

    def din(name, shape, dt=F32):
        return nc.dram_tensor(name, list(shape), dt, kind="ExternalInput").ap()

    x_main = din("x_main", [TOK, D])
    x_pre = din("x_pre", [TOK, D])
    flag_d = din("flag", [128, 1])
    c_d = din("c_t", [128, KC])
    wada_d = din("w_ada", [D, 6 * D]) if not fake_mod else None
    bada_d = din("b_ada_t", [128, 6 * KC])
    g1_d = din("g1_t", [128, KC])
    g2_d = din("g2_t", [128, KC])
    win_d = din("w_in", [D, IN_W])
    sinks_d = din("sinks", [1, 16])
    relb_d = din("rel_bias", [32, 16])
    oh_d = din("onehot", [32, 128])
    gk2w_d = din("gk2_w", [16, 1024])
    gk2b_d = din("gk2_b", [1, 1024])
    gng_d = din("gla_norm_g", [1, 512])
    wout_d = din("w_out", [D, D])
    if upto > 2:
        wq_d = din("w_q", [D, 2048])
        keyst_d = din("keys_t", [128, 16 * 128])
        ut_d = din("u_t", [D, NEXP])
        v_d = din("v", [NEXP, D])
        fg_d = din("final_g", [1, D])
        sel_d = din("sel", [24, 8 * 128])
    if fake_mod:
        modt_d = din("modT_dbg", [128, 192])
    out_d = nc.dram_tensor("out", [TOK, D], F32, kind="ExternalOutput").ap()
    x1s = nc.dram_tensor("x1s", [TOK, D], F32).ap()
    aws = nc.dram_tensor("aws", [128, 128, TOK], BF16).ap()
    gext = nc.dram_tensor("gext", [16, 384], F32).ap()
    dbg_out = {}
    for name, shape in dbg.items():
        dbg_out[name] = nc.dram_tensor("dbg_" + name, list(shape), F32, kind="ExternalOutput").ap()

    st = ExitStack()
    with st:
        A = Arena(nc, 200 * 1024)
        PS = nc.alloc_psum_tensor("ps", [128, 8, 512], F32)
        p = Prog(nc)
        import os
        if os.environ.get('K_MAXOPS'):
            p.maxops = int(os.environ['K_MAXOPS'])
        R_HT, R_CT, R_W, R_S, R_P = 0, 32768, 65536, 65536 + 49152, 65536 + 2 * 49152
        po = [R_P]

        def palloc(shape, dt):
            esz = 4 if dt == F32 else 2
            nb = int(np.prod(shape)) * esz
            nb = (nb + 31) // 32 * 32
            v = A.view(po[0], shape, dt)
            po[0] += nb
            return v

        ident_f = A.view(po[0], [128], F32); po[0] += 512
        ident_b = A.view(po[0], [128], BF16); po[0] += 256
        ones_b = A.view(po[0], [128], BF16); po[0] += 256
        ones_f = A.view(po[0], [128], F32); po[0] += 512
        Lm = A.view(po[0], [128], F32); po[0] += 512
        Um = A.view(po[0], [128], F32); po[0] += 512
        modT = A.view(po[0], [192], F32); po[0] += 768
        badat = A.view(po[0], [192], F32); po[0] += 768
        a1 = A.view(po[0], [32], F32); po[0] += 128
        a2 = A.view(po[0], [32], F32); po[0] += 128
        g1t = A.view(po[0], [32], F32); po[0] += 128
        g2t = A.view(po[0], [32], F32); po[0] += 128
        ct = A.view(po[0], [32], F32); po[0] += 128
        cact = A.view(po[0], [32], BF16); po[0] += 64
        flag = A.view(po[0], [8], F32); po[0] += 32
        small = A.view(po[0], [64], F32); po[0] += 256
        esink = A.view(po[0], [16], F32); po[0] += 64
        gnb = A.view(po[0], [512], F32); po[0] += 2048
        gk2w = A.view(po[0], [1024], BF16); po[0] += 2048
        gk2b = A.view(po[0], [1024], BF16); po[0] += 2048
        P_DYN = po[0]
        bcur = A.view(po[0], [16, 128], BF16); po[0] += 4096
        bprev = A.view(po[0], [16, 128], BF16); po[0] += 4096
        Sst = A.view(po[0], [4, 2, 512], F32); po[0] += 16384
        assert po[0] <= 200 * 1024, po[0]

        s1 = modT[:, 0:32]
        gate1 = modT[:, 64:96]
        s2 = modT[:, 96:128]
        gate2 = modT[:, 160:192]

        def bank(b, n=512):
            return PS[:, b, 0:n]

        p.pool(lambda e: e.memset(ident_f, 1.0), writes=['ident_f'])
        p.pool(lambda e: e.affine_select(out=ident_f, in_=ident_f, pattern=[[-1, 128]], compare_op=ALU.is_equal,
                                         fill=0.0, base=0, channel_multiplier=1), reads=['ident_f'], writes=['ident_f'])
        p.pool(lambda e: e.memset(ones_f, 1.0), writes=['ones_f'])
        p.pool(lambda e: e.memset(ones_b, 1.0), writes=['ones_b'])
        p.dve(lambda e: e.tensor_copy(out=ident_b, in_=ident_f), reads=['ident_f'], writes=['ident_b'])
        p.pool(lambda e: e.memset(Lm, 1.0), writes=['Lm'])
        p.pool(lambda e: e.affine_select(out=Lm, in_=Lm, pattern=[[1, 128]], compare_op=ALU.is_ge,
                                         fill=0.0, base=0, channel_multiplier=-1), reads=['Lm'], writes=['Lm'])
        p.pool(lambda e: e.memset(Lm[0:64, 64:128], 0.0), reads=['Lm'], writes=['Lm'])
        p.pool(lambda e: e.memset(Um, 1.0), writes=['Um'])
        p.pool(lambda e: e.affine_select(out=Um, in_=Um, pattern=[[-1, 128]], compare_op=ALU.is_gt,
                                         fill=0.0, base=0, channel_multiplier=1), reads=['Um'], writes=['Um'])
        p.pool(lambda e: e.memset(Um[64:128, 0:64], 0.0), reads=['Um'], writes=['Um'])
        p.pool(lambda e: e.memset(Sst, 0.0), writes=['Sst'])

        sm = 'small_ld'
        p.dma('sp', sm, lambda e: e.dma_start(out=ct, in_=c_d), writes=['ct'])
        p.dma('sp', sm, lambda e: e.dma_start(out=badat, in_=bada_d), writes=['badat'])
        p.dma('sp', sm, lambda e: e.dma_start(out=g1t, in_=g1_d), writes=['g1t'])
        p.dma('sp', sm, lambda e: e.dma_start(out=g2t, in_=g2_d), writes=['g2t'])
        p.dma('sp', sm, lambda e: e.dma_start(out=flag[:, 0:1], in_=flag_d), writes=['flag'])
        p.dma('sp', sm, lambda e: e.dma_start(out=esink, in_=sinks_d.partition_broadcast(128)[:, 0, :]), writes=['esink'])
        p.dma('sp', sm, lambda e: e.dma_start(out=gnb, in_=gng_d.partition_broadcast(128)[:, 0, :]), writes=['gnb'])
        p.dma('pool', 'small_ld2', lambda e: e.dma_start(out=gk2w[0:16, :], in_=gk2w_d), writes=['gk2w'])
        p.dma('pool', 'small_ld2', lambda e: e.dma_start(out=gk2b[0:1, :], in_=gk2b_d), writes=['gk2b'])
        relb = A.view(R_S, [16], F32)
        ohs = A.view(R_S + 64, [128], F32)
        gx = A.view(R_S + 1024, [384], F32)
        p.dma('sp', sm, lambda e: e.dma_start(out=relb[0:32, :], in_=relb_d), writes=['relb'])
        p.dma('sp', sm, lambda e: e.dma_start(out=ohs[0:32, :], in_=oh_d), writes=['ohs'])
        p.barrier()
        p.act(lambda e: e.activation(out=esink, in_=esink, func=AF.Exp), reads=['esink'], writes=['esink'])
        p.pe(lambda e: e.matmul(PS[0:16, 0, 0:128], lhsT=relb[0:32, :], rhs=ohs[0:32, :], start=True, stop=True),
             reads=['relb', 'ohs'], writes=['ps0'])
        p.pool(lambda e: e.memset(gx[0:16, :], NEGM), writes=['gx'])
        p.dve(lambda e: e.tensor_copy(out=gx[0:16, 128:256], in_=PS[0:16, 0, 0:128]), reads=['ps0', 'gx'], writes=['gx'])
        p.dma('sp', 'gx', lambda e: e.dma_start(out=gext, in_=gx[0:16, :]), reads=['gx'], writes=['gext'])
        p.barrier()
        bcf = A.view(R_S + 4096, [16, 128], F32)
        bpf = A.view(R_S + 4096 + 8192, [16, 128], F32)
        for k in range(128):
            p.dma('sp', 'bias_ld', (lambda k: lambda e: e.dma_start(out=bcf[k:k + 1, :, :], in_=gext[:, 128 - k:256 - k].unsqueeze(0)))(k),
                  writes=[('bcf', k)])
            p.dma('sp', 'bias_ld', (lambda k: lambda e: e.dma_start(out=bpf[k:k + 1, :, :], in_=gext[:, 256 - k:384 - k].unsqueeze(0)))(k),
                  writes=[('bpf', k)])
        p.barrier()
        p.dve(lambda e: e.tensor_copy(out=bcur, in_=bcf), writes=['bcur'])
        p.dve(lambda e: e.tensor_copy(out=bprev, in_=bpf), writes=['bprev'])
        if 'bcur' in dbg:
            p.dma('sp', 'dbg', lambda e: e.dma_start(out=dbg_out['bcur'], in_=bcf), reads=['bcur'], writes=['dbg_bcur'])
        p.barrier()

        p.act(lambda e: e.activation(out=cact, in_=ct, func=AF.Silu), writes=['cact'])
        wada_v = wada_d.rearrange("(k p) c -> p k c", p=128) if not fake_mod else None
        PS_fake = A.view(R_S, [192], F32)
        Wt = [A.view(R_W + i * 16384, [32, 256], BF16) for i in range(3)]
        wctr = [0]

        def load_w(view, c0, ncols):
            slot = wctr[0] % 3
            wctr[0] += 1
            t = Wt[slot]
            p.dma('pool', ('W', slot), lambda e: e.dma_start(out=t[:, :, 0:ncols], in_=view[:, :, c0:c0 + ncols]),
                  writes=[('W', slot)])
            return t, ('W', slot)

        if fake_mod:
            p.dma('sp', 'fm', lambda e: e.dma_start(out=PS_fake, in_=modt_d), writes=['ps0'])
        Wa = [A.view(i * 32768, [32, 512], BF16) for i in range(3)]
        for tno in range(0 if fake_mod else 6 * D // 512):
            slot = tno % 3
            wt, wk = Wa[slot], ('Wa', slot)
            p.dma('pool', wk, (lambda wt, tno: lambda e: e.dma_start(out=wt, in_=wada_v[:, :, tno * 512:(tno + 1) * 512]))(wt, tno), writes=[wk])
            for cc in range(4):
                j = tno * 4 + cc
                for k in range(KC):
                    p.pe((lambda wt, cc, j, k: lambda e: e.matmul(PS[:, 0, j:j + 1], lhsT=wt[:, k, cc * 128:(cc + 1) * 128],
                                                                  rhs=cact[:, k:k + 1], start=(k == 0), stop=(k == KC - 1)))(wt, cc, j, k),
                         reads=[wk, 'cact'], writes=['ps0'])
        if fake_mod:
            p.dve(lambda e: e.tensor_copy(out=modT, in_=PS_fake), reads=['ps0'], writes=['modT'])
        else:
            p.dve(lambda e: e.tensor_tensor(out=modT, in0=PS[:, 0, 0:192], in1=badat, op=ALU.add), reads=['ps0'], writes=['modT'])
        p.dve(lambda e: e.scalar_tensor_tensor(out=a1, in0=modT[:, 32:64], scalar=1.0, in1=g1t, op0=ALU.add, op1=ALU.mult),
              reads=['modT'], writes=['a1'])
        p.dve(lambda e: e.scalar_tensor_tensor(out=a2, in0=modT[:, 128:160], scalar=1.0, in1=g2t, op0=ALU.add, op1=ALU.mult),
              reads=['modT'], writes=['a2'])
        if 'modT' in dbg:
            p.dma('sp', 'dbg', lambda e: e.dma_start(out=dbg_out['modT'], in_=modT), reads=['modT'], writes=['dbg_modT'])
        p.barrier()
        if upto <= 1:
            p.emit(st)
            return nc, p

        def rms_norm_to_hT(src, row0, ntiles, a_t, s_t, hT, xs_off, junk_off):
            xs = [A.view(xs_off + i * 16384, [4096], F32) for i in range(2)]
            junk = A.view(junk_off, [4096], BF16)
            for tt in range(ntiles):
                xt = xs[tt % 2]
                xk = ('xs', tt % 2)
                sc = small[:, (tt % 2) * 4:(tt % 2) * 4 + 4]
                sk = ('nsc', tt % 2)
                p.dma('sp', xk, (lambda xt, tt: lambda e: e.dma_start(out=xt, in_=src[row0 + tt * 128: row0 + (tt + 1) * 128, :]))(xt, tt),
                      writes=[xk])
                p.act((lambda xt, sc: lambda e: e.activation(out=junk, in_=xt, func=AF.Square, accum_out=sc[:, 0:1]))(xt, sc),
                      reads=[xk], writes=['junk', sk])
                p.dve((lambda sc: lambda e: e.tensor_scalar(out=sc[:, 1:2], in0=sc[:, 0:1], scalar1=1.0 / D, scalar2=EPS,
                                                            op0=ALU.mult, op1=ALU.add))(sc), reads=[sk], writes=[sk])
                p.act((lambda sc: lambda e: e.activation(out=sc[:, 2:3], in_=sc[:, 1:2], func=AF.Sqrt))(sc), reads=[sk], writes=[sk])
                p.dve((lambda sc: lambda e: e.reciprocal(out=sc[:, 3:4], in_=sc[:, 2:3]))(sc), reads=[sk], writes=[sk])
                p.act((lambda xt, sc: lambda e: e.activation(out=xt, in_=xt, func=AF.Copy, scale=sc[:, 3:4]))(xt, sc),
                      reads=[xk, sk], writes=[xk])
                for g4 in range(8):
                    b = 4 + (g4 % 2)
                    for q in range(4):
                        c = g4 * 4 + q
                        p.pe((lambda xt, b, q, c: lambda e: e.transpose(PS[:, b, q * 128:(q + 1) * 128], xt[:, c * 128:(c + 1) * 128], ident_f))(xt, b, q, c),
                             reads=[xk, 'ident_f'], writes=[('ps', b)])
                    for q in range(4):
                        c = g4 * 4 + q
                        if g4 % 2 == 0:
                            p.dve((lambda b, q, c, tt: lambda e: e.tensor_scalar(out=hT[:, c, tt * 128:(tt + 1) * 128], in0=PS[:, b, q * 128:(q + 1) * 128],
                                                                                 scalar1=a_t[:, c:c + 1], scalar2=s_t[:, c:c + 1], op0=ALU.mult, op1=ALU.add))(b, q, c, tt),
                                  reads=[('ps', b)], writes=[('hT', c)])
                        else:
                            p.act((lambda b, q, c, tt: lambda e: e.activation(out=hT[:, c, tt * 128:(tt + 1) * 128], in_=PS[:, b, q * 128:(q + 1) * 128],
                                                                              func=AF.Identity, scale=a_t[:, c:c + 1], bias=s_t[:, c:c + 1]))(b, q, c, tt),
                                  reads=[('ps', b)], writes=[('hT', c)])

        pbank = [0]

        def next_pbank():
            b = pbank[0] % 2
            pbank[0] += 1
            return b

        hT_keys = [('hT', c) for c in range(KC)]

        def gemm_fm(wview, c0, ncols, hT, T, evac, src_keys):
            done = 0
            while done < ncols:
                n = min(256, ncols - done)
                wt, wk = load_w(wview, c0 + done, n)
                for cc in range((n + 127) // 128):
                    m = min(128, n - cc * 128)
                    for h0 in range(0, T, 512):
                        b = next_pbank()
                        for k in range(KC):
                            p.pe((lambda wt, cc, m, b, k, h0: lambda e: e.matmul(PS[0:m, b, 0:min(512, T - h0)], lhsT=wt[:, k, cc * 128:cc * 128 + m],
                                                                                 rhs=hT[:, k, h0:h0 + min(512, T - h0)], start=(k == 0), stop=(k == KC - 1)))(wt, cc, m, b, k, h0),
                                 reads=[wk] + src_keys, writes=[('ps', b)])
                        evac((done // 128) + cc, b, h0)
                done += n

        def gemm_tm(wview, c0, ncols, hT, T, evac, src_keys, also_fm=None):
            done = 0
            while done < ncols:
                n = min(256, ncols - done)
                wt, wk = load_w(wview, c0 + done, n)
                for tt in range(T // 128):
                    b = next_pbank()
                    for k in range(KC):
                        p.pe((lambda wt, n, b, k, tt: lambda e: e.matmul(PS[:, b, 0:n], lhsT=hT[:, k, tt * 128:(tt + 1) * 128],
                                                                         rhs=wt[:, k, 0:n], start=(k == 0), stop=(k == KC - 1)))(wt, n, b, k, tt),
                             reads=[wk] + src_keys, writes=[('ps', b)])
                    evac(done, n, tt, b)
                if also_fm is not None:
                    for cc in range(n // 128):
                        b = next_pbank()
                        for k in range(KC):
                            p.pe((lambda wt, cc, b, k: lambda e: e.matmul(PS[:, b, 0:T], lhsT=wt[:, k, cc * 128:(cc + 1) * 128],
                                                                          rhs=hT[:, k, 0:T], start=(k == 0), stop=(k == KC - 1)))(wt, cc, b, k),
                                 reads=[wk] + src_keys, writes=[('ps', b)])
                        also_fm((done // 128) + cc, b)
                done += n

        win_v = win_d.rearrange("(k p) c -> p k c", p=128)
        wout_v = wout_d.rearrange("(k p) c -> p k c", p=128)
        hT = A.view(R_HT, [32, MT], BF16)
        cT = A.view(R_CT, [32, MT], BF16)
        so = [R_S]

        def salloc(shape, dt):
            esz = 4 if dt == F32 else 2
            nb = (int(np.prod(shape)) * esz + 31) // 32 * 32
            v = A.view(so[0], shape, dt)
            so[0] += nb
            assert so[0] <= R_S + 49152, so[0]
            return v

        KT = salloc([2, 5 * 128], BF16)
        Vt = salloc([5, 2, 128], BF16)
        glowT = salloc([MT], BF16)
        S_AFTER_KV = so[0]
        QTg = salloc([4, 8, 128], BF16)
        tmpS = [salloc([512], F32) for _ in range(2)]
        PT = [salloc([512], BF16) for _ in range(2)]
        den = salloc([512], F32)
        so[0] = S_AFTER_KV
        gqT = salloc([2, MT], BF16)
        gkT = salloc([2, MT], BF16)
        gkt = salloc([4, 256], BF16)
        gv = salloc([4, 512], BF16)
        SG = salloc([4, 512], BF16)
        la = salloc([4, 256], F32)
        eb = salloc([2, MT], F32)
        enb = salloc([2, MT], F32)
        qdT = salloc([2, MT], BF16)
        kinvT = salloc([2, MT], BF16)
        kdec = salloc([4, 256], BF16)
        dl = salloc([2, 8], F32)
        Sbf = salloc([2, 512], BF16)
        attn_sb = salloc([64], BF16)
        ybf = salloc([512], BF16)
        tmp256 = salloc([256], F32)
        GLA_END = so[0]
        so[0] = S_AFTER_KV
        G1B = salloc([4096], F32)
        xres = [salloc([4, 256], F32) for _ in range(2)]
        dtmp = salloc([128], F32)

        PSb = PS[:, :, :].bitcast(BF16) if False else None

        p.pool(lambda e: e.memset(KT, 0.0), writes=['KT'])
        p.pool(lambda e: e.memset(Vt, 0.0), writes=['Vt'])

        def build_rowbcast(dst, colvec, tag):
            for c in range(KC):
                b = 2 + (c // 4) % 2
                q = c % 4
                p.dve((lambda c: lambda e: e.tensor_scalar(out=dtmp, in0=ident_f, scalar1=colvec[:, c:c + 1], scalar2=None, op0=ALU.mult))(c),
                      reads=['ident_f', 'modT'], writes=['dtmp'])
                p.pe((lambda b, q: lambda e: e.matmul(PS[:, b, q * 128:(q + 1) * 128], lhsT=ones_f, rhs=dtmp, start=True, stop=True))(b, q),
                     reads=['dtmp', 'ones_f'], writes=[('ps', b)])
                if q == 3:
                    p.act((lambda b, c: lambda e: e.activation(out=dst[:, (c - 3) * 128:(c + 1) * 128], in_=PS[:, b, :], func=AF.Identity))(b, c),
                          reads=[('ps', b)], writes=[tag])

        macro = [('P', 0), ('P', 1), ('M', 0), ('M', 1)]
        for kind, mi in macro:
            main = kind == 'M'
            src = x_main if main else x_pre
            rms_norm_to_hT(src, mi * MT, MT // 128, a1, s1, hT, R_CT, R_W + 32768)
            p.barrier()
            if stop == 'norm%s%d' % (kind, mi):
                p.barrier()
                p.emit(st)
                return nc, p
            need_kv = main or mi == 1

            if need_kv:
                def ev_k(ci, b, h0):
                    p.act((lambda ci, b: lambda e: e.activation(out=KT[:, ci, 128:128 + MT], in_=PS[:, b, 0:MT], func=AF.Identity))(ci, b),
                          reads=[('ps', b)], writes=['KT'])
                gemm_fm(win_v, C_AK, 256, hT, MT, ev_k, hT_keys)

                def ev_v(c0, n, tt, b):
                    p.dve((lambda tt, b: lambda e: e.tensor_copy(out=Vt[:, 1 + tt, :, :], in_=PS[:, b, 0:256].rearrange("p (g d) -> p g d", g=2)))(tt, b),
                          reads=[('ps', b)], writes=['Vt'])
                gemm_tm(win_v, C_AV, 256, hT, MT, ev_v, hT_keys)
            if main:
                for g in range(2):
                    def ev_q(ci, b, h0, g=g):
                        p.act((lambda ci, b: lambda e: e.activation(out=QTg[:, :, ci, :], in_=PS[:, b, 0:MT].rearrange("p (n q) -> p n q", n=4),
                                                                    func=AF.Copy, scale=128 ** -0.5))(ci, b),
                              reads=[('ps', b)], writes=['QTg'])
                    gemm_fm(win_v, C_AQ + g * 1024, 1024, hT, MT, ev_q, hT_keys)
                    for n in range(4):
                        for hf in range(2):
                            hs = slice(hf * 4, hf * 4 + 4)
                            hg = slice(g * 8 + hf * 4, g * 8 + hf * 4 + 4)
                            rq = QTg[:, n, hs, :].rearrange("p j q -> p (j q)")
                            p.pe((lambda g, n, rq: lambda e: e.matmul(PS[:, 2, :], lhsT=KT[:, g, (n + 1) * 128:(n + 2) * 128], rhs=rq, start=True, stop=True))(g, n, rq),
                                 reads=['KT', 'QTg'], writes=[('ps', 2)])
                            p.pe((lambda g, n, rq: lambda e: e.matmul(PS[:, 3, :], lhsT=KT[:, g, n * 128:(n + 1) * 128], rhs=rq, start=True, stop=True))(g, n, rq),
                                 reads=['KT', 'QTg'], writes=[('ps', 3)])
                            for w_, (bk, bt) in enumerate([(2, bcur), (3, bprev)]):
                                p.dve((lambda w_, bk, bt, hg: lambda e: e.tensor_tensor(out=tmpS[w_], in0=PS[:, bk, :], in1=bt[:, hg, :].rearrange("p j q -> p (j q)"), op=ALU.add))(w_, bk, bt, hg),
                                      reads=[('ps', bk)], writes=[('tmpS', w_)])
                                p.act((lambda w_: lambda e: e.activation(out=PT[w_], in_=tmpS[w_], func=AF.Exp))(w_),
                                      reads=[('tmpS', w_)], writes=[('PT', w_)])
                            if mi == 0 and n == 0:
                                p.dve(lambda e: e.tensor_scalar(out=PT[1], in0=PT[1], scalar1=flag[:, 0:1], scalar2=None, op0=ALU.mult),
                                      reads=[('PT', 1), 'flag'], writes=[('PT', 1)])
                            p.pe(lambda e: e.matmul(PS[:, 6, :], lhsT=ones_b, rhs=PT[0], start=True, stop=False), reads=[('PT', 0), 'ones_b'], writes=[('ps', 6)])
                            p.pe(lambda e: e.matmul(PS[:, 6, :], lhsT=ones_b, rhs=PT[1], start=False, stop=True), reads=[('PT', 1), 'ones_b'], writes=[('ps', 6)])
                            p.pe((lambda g, n: lambda e: e.matmul(PS[:, 7, :], lhsT=Vt[:, n + 1, g, :], rhs=PT[0], start=True, stop=False))(g, n),
                                 reads=[('PT', 0), 'Vt'], writes=[('ps', 7)])
                            p.pe((lambda g, n: lambda e: e.matmul(PS[:, 7, :], lhsT=Vt[:, n, g, :], rhs=PT[1], start=False, stop=True))(g, n),
                                 reads=[('PT', 1), 'Vt'], writes=[('ps', 7)])
                            for j in range(4):
                                hh = g * 8 + hf * 4 + j
                                p.dve((lambda j, hh: lambda e: e.tensor_scalar(out=den[:, j * 128:(j + 1) * 128], in0=PS[:, 6, j * 128:(j + 1) * 128],
                                                                               scalar1=esink[:, hh:hh + 1], scalar2=None, op0=ALU.add))(j, hh),
                                      reads=[('ps', 6), 'esink'], writes=['den'])
                            p.dve(lambda e: e.reciprocal(out=den, in_=den), reads=['den'], writes=['den'])
                            p.dve((lambda g, n, hf: lambda e: e.tensor_tensor(out=cT[:, g * 8 + hf * 4:g * 8 + hf * 4 + 4, n * 128:(n + 1) * 128],
                                                                              in0=PS[:, 7, :].rearrange("p (j q) -> p j q", j=4),
                                                                              in1=den.rearrange("p (j q) -> p j q", j=4), op=ALU.mult))(g, n, hf),
                                  reads=[('ps', 7), 'den'], writes=[('cT', g * 8 + hf * 4 + jj) for jj in range(4)])
            if need_kv:
                p.pool(lambda e: e.tensor_copy(out=KT[:, :, 0:128], in_=KT[:, :, 512:640]), reads=['KT'], writes=['KT'])
                p.pool(lambda e: e.tensor_copy(out=Vt[:, 0, :, :], in_=Vt[:, 4, :, :]), reads=['Vt'], writes=['Vt'])
            if stop == 'swa%s%d' % (kind, mi):
                p.barrier()
                p.emit(st)
                return nc, p
            if 'cT_attn' in dbg and main and mi == 0:
                p.barrier()
                p.dve(lambda e: e.tensor_copy(out=eb[:, 0, :], in_=cT[:, 0, :]), writes=['dbgtmp'])
                p.dma('sp', 'dbg', lambda e: e.dma_start(out=dbg_out['cT_attn'], in_=eb[:, 0, :]), reads=['dbgtmp'], writes=['dbg_ct'])
            p.barrier()
            def ev_glow(ci, b, h0):
                p.act((lambda b: lambda e: e.activation(out=glowT[0:16, :], in_=PS[0:16, b, 0:MT], func=AF.Identity))(b),
                      reads=[('ps', b)], writes=['glowT'])
            gemm_fm(win_v, C_GLOW, 16, hT, MT, ev_glow, hT_keys)
            for h in range(4):
                if main:
                    def ev_gq(ci, b, h0):
                        p.act((lambda ci, b: lambda e: e.activation(out=gqT[:, ci, :], in_=PS[:, b, 0:MT], func=AF.Identity))(ci, b),
                              reads=[('ps', b)], writes=['gqT'])
                    gemm_fm(win_v, C_GQ + h * 256, 256, hT, MT, ev_gq, hT_keys)

                def ev_gk(c0, n, tt, b):
                    p.dve((lambda tt, b: lambda e: e.tensor_copy(out=gkt[:, tt, :], in_=PS[:, b, 0:256]))(tt, b), reads=[('ps', b)], writes=['gkt'])

                def ev_gkT(ci, b):
                    p.act((lambda ci, b: lambda e: e.activation(out=gkT[:, ci, :], in_=PS[:, b, 0:MT], func=AF.Identity))(ci, b),
                          reads=[('ps', b)], writes=['gkT'])
                gemm_tm(win_v, C_GK + h * 256, 256, hT, MT, ev_gk, hT_keys, also_fm=ev_gkT if main else None)

                def ev_gv(c0, n, tt, b):
                    p.act((lambda c0, tt, b: lambda e: e.activation(out=gv[:, tt, c0:c0 + 256], in_=PS[:, b, 0:256], func=AF.Identity))(c0, tt, b),
                          reads=[('ps', b)], writes=['gv'])
                gemm_tm(win_v, C_GV + h * 512, 512, hT, MT, ev_gv, hT_keys)
                if main:
                    def ev_go(c0, n, tt, b):
                        p.act((lambda b: lambda e: e.activation(out=tmp256, in_=PS[:, b, 0:256], func=AF.Silu))(b),
                              reads=[('ps', b)], writes=['tmp256'])
                        p.dve((lambda c0, tt: lambda e: e.tensor_tensor(out=SG[:, tt, c0:c0 + 256], in0=tmp256, in1=gnb[:, c0:c0 + 256], op=ALU.mult))(c0, tt),
                              reads=['tmp256', 'gnb'], writes=['SG'])
                    gemm_tm(win_v, C_GOUT + h * 512, 512, hT, MT, ev_go, hT_keys)
                for tt in range(4):
                    p.pe((lambda tt, h: lambda e: e.matmul(PS[:, 2, 0:256], lhsT=glowT[0:16, tt * 128:(tt + 1) * 128], rhs=gk2w[0:16, h * 256:(h + 1) * 256],
                                                           start=True, stop=False))(tt, h), reads=['glowT', 'gk2w'], writes=[('ps', 2)])
                    p.pe((lambda tt, h: lambda e: e.matmul(PS[:, 2, 0:256], lhsT=ones_b[0:1, :], rhs=gk2b[0:1, h * 256:(h + 1) * 256],
                                                           start=False, stop=True))(tt, h), reads=['gk2b', 'ones_b'], writes=[('ps', 2)])
                    p.act(lambda e: e.activation(out=tmp256, in_=PS[:, 2, 0:256], func=AF.Exp, scale=-1.0), reads=[('ps', 2)], writes=['tmp256'])
                    p.act(lambda e: e.activation(out=tmp256, in_=tmp256, func=AF.Ln, bias=1.0), reads=['tmp256'], writes=['tmp256'])
                    p.act((lambda tt: lambda e: e.activation(out=la[:, tt, :], in_=tmp256, func=AF.Copy, scale=-1.0 / 16.0))(tt),
                          reads=['tmp256'], writes=['la'])
                for dc in range(2):
                    for tt in range(4):
                        p.pe((lambda dc, tt: lambda e: e.matmul(PS[:, 2 + dc, tt * 128:(tt + 1) * 128], lhsT=la[:, tt, dc * 128:(dc + 1) * 128], rhs=Lm,
                                                                start=True, stop=True))(dc, tt), reads=['la', 'Lm'], writes=[('ps', 2 + dc)])
                p.act(lambda e: e.activation(out=eb, in_=PS[:, 2:4, :], func=AF.Exp), reads=[('ps', 2), ('ps', 3)], writes=['eb'])
                if main:
                    p.act(lambda e: e.activation(out=enb, in_=PS[:, 2:4, :], func=AF.Exp, scale=-1.0), reads=[('ps', 2), ('ps', 3)], writes=['enb'])
                    p.dve(lambda e: e.scalar_tensor_tensor(out=qdT, in0=gqT, scalar=1.0 / 16.0, in1=eb, op0=ALU.mult, op1=ALU.mult),
                          reads=['gqT', 'eb'], writes=['qdT'])
                    p.dve(lambda e: e.tensor_tensor(out=kinvT, in0=gkT, in1=enb, op=ALU.mult), reads=['gkT', 'enb'], writes=['kinvT'])
                p.dve(lambda e: e.tensor_copy(out=dl, in_=eb[:, :, 63::64]), reads=['eb'], writes=['dl'])
                for tt in range(4):
                    p.pe((lambda tt: lambda e: e.matmul(PS[:, 2, 0:256], lhsT=Um, rhs=la[:, tt, :], start=True, stop=True))(tt),
                         reads=['la', 'Um'], writes=[('ps', 2)])
                    p.act(lambda e: e.activation(out=tmp256, in_=PS[:, 2, 0:256], func=AF.Exp), reads=[('ps', 2)], writes=['tmp256'])
                    p.dve((lambda tt: lambda e: e.tensor_tensor(out=kdec[:, tt, :], in0=gkt[:, tt, :], in1=tmp256, op=ALU.mult))(tt),
                          reads=['gkt', 'tmp256'], writes=['kdec'])
                p.act((lambda h: lambda e: e.activation(out=Sbf, in_=Sst[:, h, :, :], func=AF.Identity))(h), reads=['Sst'], writes=['Sbf'])
                for c in range(8):
                    tt, par = c // 2, c % 2
                    r0 = 64 * par
                    t0 = c * 64
                    rs = slice(r0, r0 + 64)
                    if main:
                        for dc in range(2):
                            p.pe((lambda dc, t0, rs: lambda e: e.matmul(PS[rs, 3, 0:64], lhsT=kinvT[:, dc, t0:t0 + 64], rhs=qdT[:, dc, t0:t0 + 64],
                                                                        start=(dc == 0), stop=(dc == 1)))(dc, t0, rs),
                                 reads=['kinvT', 'qdT'], writes=[('ps', 3)])
                        p.dve((lambda rs: lambda e: e.tensor_tensor(out=attn_sb[rs, :], in0=PS[rs, 3, 0:64], in1=Lm[rs, rs], op=ALU.mult))(rs),
                              reads=[('ps', 3), 'Lm'], writes=['attn_sb'])
                        p.pe((lambda rs, tt: lambda e: e.matmul(PS[rs, 6, :], lhsT=attn_sb[rs, :], rhs=gv[rs, tt, :], start=True, stop=False))(rs, tt),
                             reads=['attn_sb', 'gv'], writes=[('ps', 6)])
                        for dc in range(2):
                            p.pe((lambda dc, t0, rs: lambda e: e.matmul(PS[rs, 6, :], lhsT=qdT[:, dc, t0:t0 + 64], rhs=Sbf[:, dc, :],
                                                                        start=False, stop=(dc == 1)))(dc, t0, rs),
                                 reads=['qdT', 'Sbf'], writes=[('ps', 6)])
                    for dc in range(2):
                        p.pe((lambda dc, rs, tt: lambda e: e.matmul(PS[:, 4 + dc, :], lhsT=kdec[rs, tt, dc * 128:(dc + 1) * 128], rhs=gv[rs, tt, :],
                                                                    start=True, stop=True))(dc, rs, tt),
                             reads=['kdec', 'gv'], writes=[('ps', 4 + dc)])
                        p.dve((lambda dc, c, h: lambda e: e.scalar_tensor_tensor(out=Sst[:, h, dc, :], in0=Sst[:, h, dc, :], scalar=dl[:, dc, c:c + 1],
                                                                                 in1=PS[:, 4 + dc, :], op0=ALU.mult, op1=ALU.add))(dc, c, h),
                              reads=['Sst', 'dl', ('ps', 4 + dc)], writes=['Sst'])
                    p.act((lambda h: lambda e: e.activation(out=Sbf, in_=Sst[:, h, :, :], func=AF.Identity))(h), reads=['Sst'], writes=['Sbf'])
                    if main and par == 1:
                        sc = small[:, 8:12]
                        p.act(lambda e: e.activation(out=ybf, in_=PS[:, 6, :], func=AF.Square, accum_out=sc[:, 0:1]), reads=[('ps', 6)], writes=['ybf', 'gsc'])
                        p.dve(lambda e: e.tensor_scalar(out=sc[:, 1:2], in0=sc[:, 0:1], scalar1=1.0 / 512, scalar2=EPS, op0=ALU.mult, op1=ALU.add),
                              reads=['gsc'], writes=['gsc'])
                        p.act(lambda e: e.activation(out=sc[:, 2:3], in_=sc[:, 1:2], func=AF.Sqrt), reads=['gsc'], writes=['gsc'])
                        p.dve(lambda e: e.reciprocal(out=sc[:, 3:4], in_=sc[:, 2:3]), reads=['gsc'], writes=['gsc'])
                        p.dve((lambda tt: lambda e: e.scalar_tensor_tensor(out=ybf, in0=PS[:, 6, :], scalar=sc[:, 3:4], in1=SG[:, tt, :],
                                                                           op0=ALU.mult, op1=ALU.mult))(tt), reads=[('ps', 6), 'gsc', 'SG'], writes=['ybf'])
                        pst = PS[:, 7, :].bitcast(BF16)
                        for j in range(4):
                            p.pe((lambda j: lambda e: e.transpose(pst[:, j * 128:(j + 1) * 128], ybf[:, j * 128:(j + 1) * 128], ident_b))(j),
                                 reads=['ybf', 'ident_b'], writes=[('ps', 7)])
                        p.act((lambda h, tt: lambda e: e.activation(out=cT[:, 16 + h * 4:16 + h * 4 + 4, tt * 128:(tt + 1) * 128],
                                                                    in_=pst[:, 0:512].rearrange("p (j q) -> p j q", j=4), func=AF.Identity))(h, tt),
                              reads=[('ps', 7)], writes=[('cT', 16 + h * 4 + jj) for jj in range(4)])
                if not main and mi == 1:
                    p.dve((lambda h: lambda e: e.tensor_scalar(out=Sst[:, h, :, :], in0=Sst[:, h, :, :], scalar1=flag[:, 0:1], scalar2=None, op0=ALU.mult))(h),
                          reads=['Sst', 'flag'], writes=['Sst'])
            p.barrier()
            if stop == 'gla%s%d' % (kind, mi):
                p.emit(st)
                return nc, p
            if 'cT_gla' in dbg and main and mi == 0:
                p.dve(lambda e: e.tensor_copy(out=eb[:, 0, :], in_=cT[:, 16, :]), writes=['dbgtmp'])
                p.dma('sp', 'dbg', lambda e: e.dma_start(out=dbg_out['cT_gla'], in_=eb[:, 0, :]), reads=['dbgtmp'], writes=['dbg_ct2'])
                p.barrier()
            if not main:
                continue
            build_rowbcast(G1B, gate1, 'G1B')
            cT_keys = [('cT', c) for c in range(KC)]
            xstate = {}

            def ev_o(c0, n, tt, b):
                cg = c0 // 256
                xr = xres[cg % 2]
                xk = ('xres', cg % 2)
                if tt == 0:
                    srcap = x_main[mi * MT:(mi + 1) * MT, c0:c0 + 256].rearrange("(t p) c -> p t c", p=128)
                    p.dma('sp', xk, (lambda xr, srcap: lambda e: e.dma_start(out=xr, in_=srcap))(xr, srcap), writes=[xk])
                p.dve((lambda c0, b: lambda e: e.tensor_tensor(out=tmp256, in0=PS[:, b, 0:256], in1=G1B[:, c0:c0 + 256], op=ALU.mult))(c0, b),
                      reads=[('ps', b), 'G1B'], writes=['tmp256'])
                p.dve((lambda xr, tt: lambda e: e.tensor_tensor(out=xr[:, tt, :], in0=xr[:, tt, :], in1=tmp256, op=ALU.add))(xr, tt),
                      reads=['tmp256', xk], writes=[xk])
                if tt == 3:
                    dstap = x1s[mi * MT:(mi + 1) * MT, c0:c0 + 256].rearrange("(t p) c -> p t c", p=128)
                    p.dma('sp', ('xst', cg % 2), (lambda xr, dstap: lambda e: e.dma_start(out=dstap, in_=xr))(xr, dstap),
                          reads=[xk], writes=[xk, 'x1s'])
            gemm_tm(wout_v, 0, D, cT, MT, ev_o, cT_keys)
            p.barrier()
        if upto <= 2:
            p.emit(st)
            return nc, p

        wq_v = wq_d.rearrange("(k p) c -> p k c", p=128)
        ut_v = ut_d.rearrange("(k p) e -> p k e", p=128)
        h2T = A.view(R_HT, [32, TOK], BF16)
        h2_keys = [('hT', c) for c in range(KC)]
        rms_norm_to_hT(x1s, 0, TOK // 128, a2, s2, h2T, R_W, R_W + 32768)
        p.barrier()
        qT = A.view(R_S, [16, TOK], BF16)

        def ev_pq(ci, b, h0):
            p.act((lambda ci, b, h0: lambda e: e.activation(out=qT[:, ci, h0:h0 + 512], in_=PS[:, b, :], func=AF.Identity))(ci, b, h0),
                  reads=[('ps', b)], writes=['qT'])
        gemm_fm(wq_v, 0, 2048, h2T, TOK, ev_pq, h2_keys)
        p.barrier()
        s2_all = A.view(R_W, [8, 8, 128], F32)
        sig = [A.view(R_W + 32768 + tt * 4096, [8, 128], F32) for tt in range(4)] + \
              [A.view(R_S + 32768 + tt * 4096, [8, 128], F32) for tt in range(4)]
        qo = [P_DYN]

        def qalloc(shape, dt):
            esz = 4 if dt == F32 else 2
            nb = (int(np.prod(shape)) * esz + 31) // 32 * 32
            v = A.view(qo[0], shape, dt)
            qo[0] += nb
            assert qo[0] <= R_P + 36864, qo[0]
            return v
        rz_all = qalloc([64], F32)
        Q_KEEP = qo[0]
        keysT = qalloc([16, 128], BF16)
        t16 = qalloc([16, 16], F32)
        tmpk = qalloc([128], F32)
        cand = qalloc([16, 16], F32)
        tmpc = qalloc([256], F32)
        c16 = qalloc([8, 16], F32)
        tsc = qalloc([8, 8], F32)
        junk16 = qalloc([16], F32)
        scs = qalloc([16, 128], F32)
        p.dma('pool', 'peer_c', lambda e: e.dma_start(out=keysT, in_=keyst_d.rearrange("p (a k) -> p a k", a=16)), writes=['keysT'])
        p.barrier()
        for tt in range(8):
            for hp in range(16):
                b = 4 + hp // 4
                p.pe((lambda hp, b, tt: lambda e: e.matmul(PS[:, b, (hp % 4) * 128:(hp % 4 + 1) * 128], lhsT=qT[:, hp, tt * 128:(tt + 1) * 128],
                                                           rhs=keysT[:, hp, :], start=True, stop=True))(hp, b, tt),
                     reads=['qT', 'keysT'], writes=[('ps', b)])
            p.act(lambda e: e.activation(out=scs, in_=PS[:, 4:8, :].rearrange("p b (c k) -> p (b c) k", c=4), func=AF.Identity),
                  reads=[('ps', 4), ('ps', 5), ('ps', 6), ('ps', 7)], writes=['scs'])
            p.pool((lambda tt: lambda e: e.tensor_copy(out=s2_all[:, tt, :, :], in_=scs.rearrange("p (h two) k -> p h two k", two=2)[:, :, 1, :]))(tt),
                   reads=['scs'], writes=[('s2', tt)])
            for hp in range(16):
                p.dve((lambda hp: lambda e: e.max(out=t16[:, hp, 0:8], in_=scs[:, hp, :]))(hp), reads=['scs'], writes=['t16'])
                p.dve((lambda hp: lambda e: e.match_replace(out=tmpk, in_to_replace=t16[:, hp, 0:8], in_values=scs[:, hp, :], imm_value=-1e30))(hp),
                      reads=['scs', 't16'], writes=['tmpk'])
                p.dve((lambda hp: lambda e: e.max(out=t16[:, hp, 8:16], in_=tmpk))(hp), reads=['tmpk'], writes=['t16'])
            for h in range(8):
                p.dve((lambda h: lambda e: e.tensor_tensor(out=cand, in0=t16[:, 2 * h, :].unsqueeze(2).to_broadcast([128, 16, 16]),
                                                           in1=t16[:, 2 * h + 1, :].unsqueeze(1).to_broadcast([128, 16, 16]), op=ALU.add))(h),
                      reads=['t16'], writes=['cand'])
                cf = cand.rearrange("p a b -> p (a b)")
                p.dve((lambda h: lambda e: e.max(out=c16[:, h, 0:8], in_=cf))(h), reads=['cand'], writes=['c16'])
                p.dve((lambda h: lambda e: e.match_replace(out=tmpc, in_to_replace=c16[:, h, 0:8], in_values=cf, imm_value=-1e30))(h),
                      reads=['cand', 'c16'], writes=['tmpc'])
                p.dve((lambda h: lambda e: e.max(out=c16[:, h, 8:16], in_=tmpc))(h), reads=['tmpc'], writes=['c16'])
                p.dve((lambda h: lambda e: e.tensor_scalar(out=tsc[:, h, 0:1], in0=c16[:, h, 15:16], scalar1=-1.0, scalar2=1e-3, op0=ALU.mult, op1=ALU.add))(h),
                      reads=['c16'], writes=['tsc'])
                p.act((lambda h: lambda e: e.activation(out=junk16, in_=c16[:, h, :], func=AF.Exp, bias=tsc[:, h, 0:1], accum_out=tsc[:, h, 1:2]))(h),
                      reads=['c16', 'tsc'], writes=['tsc', 'junk16'])
                p.dve((lambda h, tt: lambda e: e.reciprocal(out=rz_all[:, tt * 8 + h:tt * 8 + h + 1], in_=tsc[:, h, 1:2]))(h, tt), reads=['tsc'], writes=['rz_all'])
                p.act((lambda h: lambda e: e.activation(out=tsc[:, h, 3:4], in_=tsc[:, h, 1:2], func=AF.Ln))(h), reads=['tsc'], writes=['tsc'])
                p.dve((lambda h: lambda e: e.tensor_tensor(out=tsc[:, h, 4:5], in0=tsc[:, h, 0:1], in1=tsc[:, h, 3:4], op=ALU.subtract))(h),
                      reads=['tsc'], writes=['tsc'])
                p.dve((lambda h, tt: lambda e: e.tensor_scalar(out=sig[tt][:, h, :], in0=scs[:, 2 * h, :], scalar1=tsc[:, h, 4:5], scalar2=None, op0=ALU.add))(h, tt),
                      reads=['scs', 'tsc'], writes=[('sig', tt)])
        if 'thr' in dbg:
            p.barrier()
            p.dma('sp', 'dbg', lambda e: e.dma_start(out=dbg_out['thr'], in_=sig[0][:, :, 0]), writes=['dbg_thr'])
            p.dma('sp', 'dbg2', lambda e: e.dma_start(out=dbg_out['rzr'], in_=rz_all), writes=['dbg_rzr'])
        p.barrier()
        if upto <= 3:
            p.emit(st)
            return nc, p
        ut = [A.view(R_S + i * 16384, [32, 256], BF16) for i in range(2)]
        qo[0] = Q_KEEP
        Ep = [qalloc([128], F32) for _ in range(4)]
        Wm = [qalloc([128], BF16) for _ in range(8)]
        Gl2 = [qalloc([TOK], BF16) for _ in range(2)]
        AW = [qalloc([TOK], BF16) for _ in range(2)]
        NE = 128 if upto > 4 else 2

        def load_u(pi):
            us = pi % 2
            p.dma('pool', ('U', us), lambda e: e.dma_start(out=ut[us], in_=ut_v[:, :, pi * 256:pi * 256 + 256]), writes=[('U', us)])

        def actT_mms(i1):
            us = (i1 // 2) % 2
            uk = ('U', us)
            ec = (i1 % 2) * 128
            lst = []
            for hf in range(2):
                for k in range(KC):
                    lst.append((lambda hf, k: lambda: p.pe(lambda e: e.matmul(PS[:, hf, :], lhsT=ut[us][:, k, ec:ec + 128], rhs=h2T[:, k, hf * 512:(hf + 1) * 512],
                                                                             start=(k == 0), stop=(k == KC - 1)),
                                                           reads=[uk] + h2_keys, writes=[('ps', hf)]))(hf, k))
            return lst

        def emit_gelu(i1):
            g = Gl2[i1 % 2]
            p.act(lambda e: e.activation(out=g.rearrange("p (b t) -> p b t", b=2), in_=PS[:, 0:2, :], func=AF.Gelu), reads=[('ps', 0), ('ps', 1)], writes=[('Gl', i1 % 2)])

        def emit_gate(i1, k):
            tt, h = k // 8, k % 8
            r, r8 = k % 4, k % 8
            p.act(lambda e: e.activation(out=Ep[r], in_=s2_all[:, tt, h, :], func=AF.Exp, bias=sig[tt][:, h, i1:i1 + 1]), writes=[('Ep', r)])
            p.dve(lambda e: e.scalar_tensor_tensor(out=Wm[r8], in0=Ep[r], scalar=rz_all[:, tt * 8 + h:tt * 8 + h + 1], in1=Ep[r], op0=ALU.is_ge, op1=ALU.mult),
                  reads=[('Ep', r)], writes=[('Wm', r8)])

        def emit_acc(k):
            tt, h = k // 8, k % 8
            r8 = k % 8
            p.pe(lambda e: e.matmul(PS[:, 6 + tt // 4, (tt % 4) * 128:(tt % 4 + 1) * 128], lhsT=Wm[r8], rhs=ident_b, start=(h == 0), stop=(h == 7)),
                 reads=[('Wm', r8), 'ident_b'], writes=[('ps', 6 + tt // 4)])

        LAG = 4
        load_u(0)
        for f in actT_mms(0):
            f()
        emit_gelu(0)
        for i1 in range(NE):
            if i1 % 2 == 0 and (i1 // 2 + 1) * 2 < NE:
                load_u(i1 // 2 + 1)
            nxt = actT_mms(i1 + 1) if i1 + 1 < NE else []
            for k in range(64 + LAG):
                if k < 64:
                    emit_gate(i1, k)
                    if k < len(nxt):
                        nxt[k]()
                if k - LAG >= 0:
                    emit_acc(k - LAG)
            a = i1 % 2
            g = Gl2[i1 % 2]
            p.dve((lambda a, g: lambda e: e.tensor_tensor(out=AW[a].rearrange("p (b t) -> p b t", b=2), in0=PS[:, 6:8, :], in1=g.rearrange("p (b t) -> p b t", b=2), op=ALU.mult))(a, g),
                  reads=[('ps', 6), ('ps', 7), ('Gl', i1 % 2)], writes=[('AW', a)])
            p.dma('sp', ('AWst', a), (lambda a, i1: lambda e: e.dma_start(out=aws[i1], in_=AW[a]))(a, i1), reads=[('AW', a)], writes=[('AW', a), 'aws'])
            if i1 + 1 < NE:
                emit_gelu(i1 + 1)
        if 'aw0' in dbg:
            p.barrier()
            dbt = A.view(R_HT, [TOK], F32)
            p.act(lambda e: e.activation(out=dbt, in_=AW[0], func=AF.Identity), writes=['dbt'])
            p.dma('sp', 'dbg', lambda e: e.dma_start(out=dbg_out['aw0'], in_=dbt), reads=['dbt'], writes=['dbg_aw0'])
        p.barrier()
        if upto <= 4:
            p.emit(st)
            return nc, p
        acc = A.view(0, [8, D], F32)
        vt = [A.view(131072 + i * 8192, [8, 512], BF16) for i in range(2)]
        awt = [A.view(131072 + 16384, [8, TOK], BF16), A.view(P_DYN, [8, TOK], BF16)]
        v_v = v_d.rearrange("(g a p) d -> g p a d", a=8, p=128)
        aws_v = aws.rearrange("(g a) p t -> g p a t", a=8)
        it = 0
        for dg in range(8):
            for eg in range(16):
                s_ = it % 2
                it += 1
                p.dma('pool', ('V', s_), (lambda s_, eg, dg: lambda e: e.dma_start(out=vt[s_], in_=v_v[eg][:, :, dg * 512:(dg + 1) * 512]))(s_, eg, dg), writes=[('V', s_)])
                p.dma('sp', ('AWld', s_), (lambda s_, eg: lambda e: e.dma_start(out=awt[s_], in_=aws_v[eg]))(s_, eg), reads=['aws'], writes=[('AWl', s_)])
                for a in range(8):
                    for tt in range(8):
                        p.pe((lambda s_, a, tt, eg: lambda e: e.matmul(PS[:, tt, :], lhsT=awt[s_][:, a, tt * 128:(tt + 1) * 128], rhs=vt[s_][:, a, :],
                                                                      start=(eg == 0 and a == 0), stop=(eg == 15 and a == 7)))(s_, a, tt, eg),
                             reads=[('V', s_), ('AWl', s_)], writes=[('ps', tt)])
            for tt in range(8):
                if tt % 2 == 0:
                    p.act((lambda tt, dg: lambda e: e.activation(out=acc[:, tt, dg * 512:(dg + 1) * 512], in_=PS[:, tt, :], func=AF.Identity))(tt, dg),
                          reads=[('ps', tt)], writes=[('acc', tt, dg)])
                else:
                    p.dve((lambda tt, dg: lambda e: e.tensor_copy(out=acc[:, tt, dg * 512:(dg + 1) * 512], in_=PS[:, tt, :]))(tt, dg),
                          reads=[('ps', tt)], writes=[('acc', tt, dg)])
        p.barrier()
        G2B = A.view(131072, [D], F32)
        FGB = A.view(131072 + 16384, [D], F32)
        x1t = A.view(P_DYN + 1024, [D], F32)
        dtmp2 = A.view(P_DYN, [128], F32)

        for c in range(KC):
            b = 2 + (c // 4) % 2
            q = c % 4
            p.dve((lambda c: lambda e: e.tensor_scalar(out=dtmp2, in0=ident_f, scalar1=gate2[:, c:c + 1], scalar2=None, op0=ALU.mult))(c),
                  reads=['ident_f', 'modT'], writes=['dtmp2'])
            p.pe((lambda b, q: lambda e: e.matmul(PS[:, b, q * 128:(q + 1) * 128], lhsT=ones_f, rhs=dtmp2, start=True, stop=True))(b, q),
                 reads=['dtmp2', 'ones_f'], writes=[('ps', b)])
            if q == 3:
                p.act((lambda b, c: lambda e: e.activation(out=G2B[:, (c - 3) * 128:(c + 1) * 128], in_=PS[:, b, :], func=AF.Identity))(b, c),
                      reads=[('ps', b)], writes=['G2B'])
        p.dma('sp', 'fgb', lambda e: e.dma_start(out=FGB, in_=fg_d.partition_broadcast(128)[:, 0, :]), writes=['FGB'])
        for tt in range(8):
            sc = small[:, 16 + 4 * (tt % 2):20 + 4 * (tt % 2)]
            sk = ('fsc', tt % 2)
            at = acc[:, tt, :]
            ak = ('accf', tt)
            p.dma('sp', 'x1ld', (lambda tt: lambda e: e.dma_start(out=x1t, in_=x1s[tt * 128:(tt + 1) * 128, :]))(tt), reads=['x1s'], writes=['x1t'])
            p.dve((lambda at: lambda e: e.tensor_tensor(out=at, in0=at, in1=G2B, op=ALU.mult))(at), reads=['G2B'], writes=[ak])
            p.pool((lambda at: lambda e: e.tensor_tensor(out=at, in0=at, in1=x1t, op=ALU.add))(at), reads=['x1t', ak], writes=[ak])
            p.act((lambda at, sc: lambda e: e.activation(out=PS[:, :, :], in_=at.rearrange("p (b t) -> p b t", b=8), func=AF.Square, accum_out=sc[:, 0:1]))(at, sc),
                  reads=[ak], writes=['junkf', sk])
            p.dve((lambda sc: lambda e: e.tensor_scalar(out=sc[:, 1:2], in0=sc[:, 0:1], scalar1=1.0 / D, scalar2=EPS, op0=ALU.mult, op1=ALU.add))(sc), reads=[sk], writes=[sk])
            p.act((lambda sc: lambda e: e.activation(out=sc[:, 2:3], in_=sc[:, 1:2], func=AF.Sqrt))(sc), reads=[sk], writes=[sk])
            p.dve((lambda sc: lambda e: e.reciprocal(out=sc[:, 3:4], in_=sc[:, 2:3]))(sc), reads=[sk], writes=[sk])
            p.dve((lambda at, sc: lambda e: e.scalar_tensor_tensor(out=at, in0=at, scalar=sc[:, 3:4], in1=FGB, op0=ALU.mult, op1=ALU.mult))(at, sc),
                  reads=[ak, sk, 'FGB'], writes=[ak])
            p.dma('sp', ('ost', tt % 2), (lambda tt, at: lambda e: e.dma_start(out=out_d[tt * 128:(tt + 1) * 128, :], in_=at))(tt, at), reads=[ak], writes=[ak, 'out'])
        p.barrier()
        p.emit(st)
    return nc, p


def t5_bucket_np(dist):
    max_exact = 16
    d = np.maximum(dist, 0)
    lr = np.log(np.maximum(d, 1).astype(np.float32) / max_exact) / math.log(128 / max_exact)
    large = max_exact + (lr * (32 - max_exact)).astype(np.int32)
    large = np.minimum(large, 31)
    return np.where(d < max_exact, d, large)


def make_in_maps(inputs, cores=range(8)):
    f = lambda a: np.ascontiguousarray(np.asarray(a, dtype=np.float32))
    x = f(inputs["x"]); c = f(inputs["c"])
    fm = lambda v, n: np.ascontiguousarray(v.reshape(n, 128).T)
    onehot = np.zeros((32, 128), np.float32)
    onehot[t5_bucket_np(np.arange(128)), np.arange(128)] = 1.0
    sel = np.zeros((24, 8, 128), np.float32)
    for h in range(8):
        for part in range(3):
            sel[part * 8 + h, h, :] = 1.0
    keys = f(inputs["peer_keys"])[0]
    keys_t = np.ascontiguousarray(keys.transpose(3, 0, 1, 2).reshape(128, 16 * 128))
    shared = {
        "w_ada": f(inputs["w_ada"])[0],
        "b_ada_t": fm(f(inputs["b_ada"])[0], 192),
        "g1_t": fm(f(inputs["norm1_g"])[0], 32),
        "g2_t": fm(f(inputs["norm2_g"])[0], 32),
        "w_in": f(inputs["w_in"])[0],
        "sinks": f(inputs["attn_sinks"])[0].reshape(1, 16),
        "rel_bias": f(inputs["rel_bias"]),
        "onehot": onehot,
        "gk2_w": f(inputs["gla_w_gk2"])[0],
        "gk2_b": f(inputs["gla_b_gk2"])[0].reshape(1, 1024),
        "gla_norm_g": f(inputs["gla_norm_g"])[0].reshape(1, 512),
        "w_out": f(inputs["w_out"])[0],
        "w_q": f(inputs["peer_w_q"])[0],
        "keys_t": keys_t,
        "u_t": np.ascontiguousarray(f(inputs["peer_u"])[0].T),
        "v": f(inputs["peer_v"])[0],
        "final_g": f(inputs["final_g"]).reshape(1, D),
        "sel": sel.reshape(24, 8 * 128),
    }
    maps = []
    for i in cores:
        b, half = i // 2, i % 2
        m = dict(shared)
        m["x_main"] = np.ascontiguousarray(x[b, half * TOK:(half + 1) * TOK])
        m["x_pre"] = np.ascontiguousarray(x[b, 0:TOK])
        m["flag"] = np.full((128, 1), float(half), np.float32)
        m["c_t"] = fm(c[b], 32)
        maps.append(m)
    return maps


_CACHE = {}


def kernel(**inputs):
    if "nc" not in _CACHE:
        _CACHE["nc"] = build_program()[0]
    nc = _CACHE["nc"]
    maps = make_in_maps(inputs)
    res = run_bass_kernel_spmd(nc, maps, core_ids=list(range(8)))
    out = np.empty((4, 2048, D), np.float32)
    for i in range(8):
        b, half = i // 2, i % 2
        out[b, half * TOK:(half + 1) * TOK] = res.results[i]["out"]
    return out
```

```python
from contextlib import ExitStack
import math
import numpy as np
import concourse.bass as bass
import concourse.mybir as mybir
from concourse.bass_utils import run_bass_kernel_spmd

F32 = mybir.dt.float32
BF16 = mybir.dt.bfloat16
AF = mybir.ActivationFunctionType
ALU = mybir.AluOpType

SAME_ENG_SYNC = True

D = 4096
KC = 32
TOK = 1024
MT = 512
NEXP = 16384
EPS = 1e-6
NEGM = -30000.0
IN_W = 8720
C_AQ, C_AK, C_AV, C_GQ, C_GK, C_GV, C_GLOW, C_GOUT = 0, 2048, 2304, 2560, 3584, 4608, 6656, 6672


class Prog:
    def __init__(self, nc):
        self.nc = nc
        self.ins = []
        self.last_w = {}
        self.readers = {}
        self.last_on = {}
        self.dmas_open = []

    maxops = None

    def op(self, eng, fn, reads=(), writes=(), dma=None, extra_deps=(), force=False):
        if self.maxops is not None and len(self.ins) >= self.maxops and not force:
            return None
        i = len(self.ins)
        deps = set(extra_deps)
        psk = [k for k in reads if k == 'ps0' or (isinstance(k, tuple) and k[0] == 'ps')]
        if psk:
            reads = [k for k in reads if k not in psk]
            writes = list(writes) + [k for k in psk if k not in writes]
        for k in reads:
            w = self.last_w.get(k)
            if w is not None:
                deps.add(w)
        for k in writes:
            w = self.last_w.get(k)
            if w is not None:
                deps.add(w)
            for r in self.readers.get(k, ()):
                deps.add(r)
        for k in reads:
            lst = self.readers.setdefault(k, [])
            if dma is None:
                for q in range(len(lst)):
                    J = self.ins[lst[q]]
                    if J['dma'] is None and J['eng'] == eng:
                        lst[q] = i
                        break
                else:
                    lst.append(i)
            else:
                lst.append(i)
        for k in writes:
            self.last_w[k] = i
            self.readers[k] = []
        deps.discard(i)
        self.ins.append(dict(eng=eng, fn=fn, deps=deps, dma=dma))
        if dma is None:
            self.last_on[eng] = i
        else:
            self.dmas_open.append(i)
        return i

    def pe(self, fn, reads=(), writes=()):
        return self.op('pe', fn, reads, writes)

    def act(self, fn, reads=(), writes=()):
        return self.op('act', fn, reads, writes)

    def dve(self, fn, reads=(), writes=()):
        return self.op('dve', fn, reads, writes)

    def pool(self, fn, reads=(), writes=()):
        return self.op('pool', fn, reads, writes)

    def dma(self, eng, key, fn, reads=(), writes=()):
        return self.op(eng, fn, reads, writes, dma=key)

    def barrier(self):
        deps = set(self.last_on.values()) | set(self.dmas_open)
        self.dmas_open = []
        for e in ['pe', 'act', 'dve', 'pool', 'sp']:
            self.op(e, lambda eng: eng.nop(), extra_deps=deps, force=True)
        self.last_w = {}
        self.readers = {}

    def emit(self, stack):
        nc = self.nc
        ins = self.ins
        n = len(ins)
        engs = ['pe', 'act', 'dve', 'pool', 'sp']

        def needs_wait(I, Dd):
            if Dd['dma'] is not None:
                return True
            if Dd['eng'] == I['eng'] and I['dma'] is None:
                if Dd['eng'] in ('pe', 'sp') or not SAME_ENG_SYNC:
                    return False
            return True

        need_sig = [False] * n
        for i, I in enumerate(ins):
            for d in I['deps']:
                Dd = ins[d]
                if Dd['dma'] is None and needs_wait(I, Dd):
                    need_sig[d] = True
        cnt = {e: 0 for e in engs}
        sigval = [0] * n
        dma_cnt = {}
        for i, I in enumerate(ins):
            if I['dma'] is not None:
                k = I['dma']
                dma_cnt[k] = dma_cnt.get(k, 0) + 16
                sigval[i] = dma_cnt[k]
            elif need_sig[i]:
                cnt[I['eng']] += 1
                sigval[i] = cnt[I['eng']]
        esem = {e: stack.enter_context(nc.semaphore('s_' + e)) for e in engs}
        dsem = {k: stack.enter_context(nc.semaphore('d_%d' % j)) for j, k in enumerate(dma_cnt)}
        self.stats = dict(n=n, cnt=dict(cnt), ndsem=len(dsem), maxdma=max(dma_cnt.values()) if dma_cnt else 0)
        per_eng = {e: [] for e in engs}
        for i, I in enumerate(ins):
            per_eng[I['eng']].append(i)

        def run(e, engobj):
            waited = {}
            for i in per_eng[e]:
                I = ins[i]
                need = {}
                for d in I['deps']:
                    Dd = ins[d]
                    if not needs_wait(I, Dd):
                        continue
                    if Dd['dma'] is not None:
                        key = ('d', Dd['dma'])
                    else:
                        key = ('e', Dd['eng'])
                    v = sigval[d]
                    if need.get(key, 0) < v:
                        need[key] = v
                for key, v in need.items():
                    if waited.get(key, 0) >= v:
                        continue
                    waited[key] = v
                    s = dsem[key[1]] if key[0] == 'd' else esem[key[1]]
                    engobj.wait_ge(s, v)
                r = I['fn'](engobj)
                if I['dma'] is not None:
                    r.then_inc(dsem[I['dma']], 16)
                elif need_sig[i]:
                    r.then_inc(esem[e], 1)

        block = stack.enter_context(nc.Block())
        block.sync(lambda eng: run('sp', eng))
        block.tensor(lambda eng: run('pe', eng))
        block.scalar(lambda eng: run('act', eng))
        block.vector(lambda eng: run('dve', eng))
        block.gpsimd(lambda eng: run('pool', eng))


class Arena:
    def __init__(self, nc, nbytes):
        self.nbytes = nbytes
        self.t = nc.alloc_sbuf_tensor("arena", [128, nbytes // 4], F32)

    def view(self, off, shape, dtype):
        esz = 4 if dtype == F32 else 2
        nel = int(np.prod(shape))
        nb = nel * esz
        assert off % 4 == 0 and nb % 4 == 0, (off, shape)
        assert off + nb <= self.nbytes, (off, nb, self.nbytes)
        ap = self.t[:, off // 4: (off + nb) // 4]
        if dtype != F32:
            ap = ap.bitcast(dtype)
        if len(shape) == 2:
            names = "a b"
            ap = ap.rearrange("p (a b) -> p a b", a=shape[0], b=shape[1])
        elif len(shape) == 3:
            ap = ap.rearrange("p (a b c) -> p a b c", a=shape[0], b=shape[1], c=shape[2])
        return ap


def build_program(dbg=None, upto=99, stop=None, fake_mod=False):
    nc = bass.Bass("TRN2", target_bir_lowering=False)
    dbg = dbg or {}

    def din(name, shape, dt=F32):
        return nc.dram_tensor(name, list(shape), dt, kind="ExternalInput").ap()

    x_main = din("x_main", [TOK, D])
    x_pre = din("x_pre", [TOK, D])
    flag_d = din("flag", [128, 1])
    c_d = din("c_t", [128, KC])
    wada_d = din("w_ada", [D, 6 * D]) if not fake_mod else None
    bada_d = din("b_ada_t", [128, 6 * KC])
    g1_d = din("g1_t", [128, KC])
    g2_d = din("g2_t", [128, KC])
    win_d = din("w_in", [D, IN_W])
    sinks_d = din("sinks", [1, 16])
    relb_d = din("rel_bias", [32, 16])
    oh_d = din("onehot", [32, 128])
    gk2w_d = din("gk2_w", [16, 1024])
    gk2b_d = din("gk2_b", [1, 1024])
    gng_d = din("gla_norm_g", [1, 512])
    wout_d = din("w_out", [D, D])
    if upto > 2:
        wq_d = din("w_q", [D, 2048])
        keyst_d = din("keys_t", [128, 16 * 128])
        ut_d = din("u_t", [D, NEXP])
        v_d = din("v", [NEXP, D])
        fg_d = din("final_g", [1, D])
        sel_d = din("sel", [24, 8 * 128])
    if fake_mod:
        modt_d = din("modT_dbg", [128, 192])
    out_d = nc.dram_tensor("out", [TOK, D], F32, kind="ExternalOutput").ap()
    x1s = nc.dram_tensor("x1s", [TOK, D], F32).ap()
    aws = nc.dram_tensor("aws", [128, 128, TOK], BF16).ap()
    gext = nc.dram_tensor("gext", [16, 384], F32).ap()
    dbg_out = {}
    for name, shape in dbg.items():
        dbg_out[name] = nc.dram_tensor("dbg_" + name, list(shape), F32, kind="ExternalOutput").ap()

    st = ExitStack()
    with st:
        A = Arena(nc, 200 * 1024)
        PS = nc.alloc_psum_tensor("ps", [128, 8, 512], F32)
        p = Prog(nc)
        import os
        if os.environ.get('K_MAXOPS'):
            p.maxops = int(os.environ['K_MAXOPS'])
        R_HT, R_CT, R_W, R_S, R_P = 0, 32768, 65536, 65536 + 49152, 65536 + 2 * 49152
        po = [R_P]

        def palloc(shape, dt):
            esz = 4 if dt == F32 else 2
            nb = int(np.prod(shape)) * esz
            nb = (nb + 31) // 32 * 32
            v = A.view(po[0], shape, dt)
            po[0] += nb
            return v

        ident_f = A.view(po[0], [128], F32); po[0] += 512
        ident_b = A.view(po[0], [128], BF16); po[0] += 256
        ones_b = A.view(po[0], [128], BF16); po[0] += 256
        ones_f = A.view(po[0], [128], F32); po[0] += 512
        Lm = A.view(po[0], [128], F32); po[0] += 512
        Um = A.view(po[0], [128], F32); po[0] += 512
        modT = A.view(po[0], [192], F32); po[0] += 768
        badat = A.view(po[0], [192], F32); po[0] += 768
        a1 = A.view(po[0], [32], F32); po[0] += 128
        a2 = A.view(po[0], [32], F32); po[0] += 128
        g1t = A.view(po[0], [32], F32); po[0] += 128
        g2t = A.view(po[0], [32], F32); po[0] += 128
        ct = A.view(po[0], [32], F32); po[0] += 128
        cact = A.view(po[0], [32], BF16); po[0] += 64
        flag = A.view(po[0], [8], F32); po[0] += 32
        small = A.view(po[0], [64], F32); po[0] += 256
        esink = A.view(po[0], [16], F32); po[0] += 64
        gnb = A.view(po[0], [512], F32); po[0] += 2048
        gk2w = A.view(po[0], [1024], BF16); po[0] += 2048
        gk2b = A.view(po[0], [1024], BF16); po[0] += 2048
        P_DYN = po[0]
        bcur = A.view(po[0], [16, 128], BF16); po[0] += 4096
        bprev = A.view(po[0], [16, 128], BF16); po[0] += 4096
        Sst = A.view(po[0], [4, 2, 512], F32); po[0] += 16384
        assert po[0] <= 200 * 1024, po[0]

        s1 = modT[:, 0:32]
        gate1 = modT[:, 64:96]
        s2 = modT[:, 96:128]
        gate2 = modT[:, 160:192]

        def bank(b, n=512):
            return PS[:, b, 0:n]

        p.pool(lambda e: e.memset(ident_f, 1.0), writes=['ident_f'])
        p.pool(lambda e: e.affine_select(out=ident_f, in_=ident_f, pattern=[[-1, 128]], compare_op=ALU.is_equal,
                                         fill=0.0, base=0, channel_multiplier=1), reads=['ident_f'], writes=['ident_f'])
        p.pool(lambda e: e.memset(ones_f, 1.0), writes=['ones_f'])
        p.pool(lambda e: e.memset(ones_b, 1.0), writes=['ones_b'])
        p.dve(lambda e: e.tensor_copy(out=ident_b, in_=ident_f), reads=['ident_f'], writes=['ident_b'])
        p.pool(lambda e: e.memset(Lm, 1.0), writes=['Lm'])
        p.pool(lambda e: e.affine_select(out=Lm, in_=Lm, pattern=[[1, 128]], compare_op=ALU.is_ge,
                                         fill=0.0, base=0, channel_multiplier=-1), reads=['Lm'], writes=['Lm'])
        p.pool(lambda e: e.memset(Lm[0:64, 64:128], 0.0), reads=['Lm'], writes=['Lm'])
        p.pool(lambda e: e.memset(Um, 1.0), writes=['Um'])
        p.pool(lambda e: e.affine_select(out=Um, in_=Um, pattern=[[-1, 128]], compare_op=ALU.is_gt,
                                         fill=0.0, base=0, channel_multiplier=1), reads=['Um'], writes=['Um'])
        p.pool(lambda e: e.memset(Um[64:128, 0:64], 0.0), reads=['Um'], writes=['Um'])
        p.pool(lambda e: e.memset(Sst, 0.0), writes=['Sst'])

        sm = 'small_ld'
        p.dma('sp', sm, lambda e: e.dma_start(out=ct, in_=c_d), writes=['ct'])
        p.dma('sp', sm, lambda e: e.dma_start(out=badat, in_=bada_d), writes=['badat'])
        p.dma('sp', sm, lambda e: e.dma_start(out=g1t, in_=g1_d), writes=['g1t'])
        p.dma('sp', sm, lambda e: e.dma_start(out=g2t, in_=g2_d), writes=['g2t'])
        p.dma('sp', sm, lambda e: e.dma_start(out=flag[:, 0:1], in_=flag_d), writes=['flag'])
        p.dma('sp', sm, lambda e: e.dma_start(out=esink, in_=sinks_d.partition_broadcast(128)[:, 0, :]), writes=['esink'])
        p.dma('sp', sm, lambda e: e.dma_start(out=gnb, in_=gng_d.partition_broadcast(128)[:, 0, :]), writes=['gnb'])
        p.dma('pool', 'small_ld2', lambda e: e.dma_start(out=gk2w[0:16, :], in_=gk2w_d), writes=['gk2w'])
        p.dma('pool', 'small_ld2', lambda e: e.dma_start(out=gk2b[0:1, :], in_=gk2b_d), writes=['gk2b'])
        relb = A.view(R_S, [16], F32)
        ohs = A.view(R_S + 64, [128], F32)
        gx = A.view(R_S + 1024, [384], F32)
        p.dma('sp', sm, lambda e: e.dma_start(out=relb[0:32, :], in_=relb_d), writes=['relb'])
        p.dma('sp', sm, lambda e: e.dma_start(out=ohs[0:32, :], in_=oh_d), writes=['ohs'])
        p.barrier()
        p.act(lambda e: e.activation(out=esink, in_=esink, func=AF.Exp), reads=['esink'], writes=['esink'])
        p.pe(lambda e: e.matmul(PS[0:16, 0, 0:128], lhsT=relb[0:32, :], rhs=ohs[0:32, :], start=True, stop=True),
             reads=['relb', 'ohs'], writes=['ps0'])
        p.pool(lambda e: e.memset(gx[0:16, :], NEGM), writes=['gx'])
        p.dve(lambda e: e.tensor_copy(out=gx[0:16, 128:256], in_=PS[0:16, 0, 0:128]), reads=['ps0', 'gx'], writes=['gx'])
        p.dma('sp', 'gx', lambda e: e.dma_start(out=gext, in_=gx[0:16, :]), reads=['gx'], writes=['gext'])
        p.barrier()
        bcf = A.view(R_S + 4096, [16, 128], F32)
        bpf = A.view(R_S + 4096 + 8192, [16, 128], F32)
        for k in range(128):
            p.dma('sp', 'bias_ld', (lambda k: lambda e: e.dma_start(out=bcf[k:k + 1, :, :], in_=gext[:, 128 - k:256 - k].unsqueeze(0)))(k),
                  writes=[('bcf', k)])
            p.dma('sp', 'bias_ld', (lambda k: lambda e: e.dma_start(out=bpf[k:k + 1, :, :], in_=gext[:, 256 - k:384 - k].unsqueeze(0)))(k),
                  writes=[('bpf', k)])
        p.barrier()
        p.dve(lambda e: e.tensor_copy(out=bcur, in_=bcf), writes=['bcur'])
        p.dve(lambda e: e.tensor_copy(out=bprev, in_=bpf), writes=['bprev'])
        if 'bcur' in dbg:
            p.dma('sp', 'dbg', lambda e: e.dma_start(out=dbg_out['bcur'], in_=bcf), reads=['bcur'], writes=['dbg_bcur'])
        p.barrier()

        p.act(lambda e: e.activation(out=cact, in_=ct, func=AF.Silu), writes=['cact'])
        wada_v = wada_d.rearrange("(k p) c -> p k c", p=128) if not fake_mod else None
        PS_fake = A.view(R_S, [192], F32)
        Wt = [A.view(R_W + i * 16384, [32, 256], BF16) for i in range(3)]
        wctr = [0]

        def load_w(view, c0, ncols):
            slot = wctr[0] % 3
            wctr[0] += 1
            t = Wt[slot]
            p.dma('pool', ('W', slot), lambda e: e.dma_start(out=t[:, :, 0:ncols], in_=view[:, :, c0:c0 + ncols]),
                  writes=[('W', slot)])
            return t, ('W', slot)

        if fake_mod:
            p.dma('sp', 'fm', lambda e: e.dma_start(out=PS_fake, in_=modt_d), writes=['ps0'])
        Wa = [A.view(i * 32768, [32, 512], BF16) for i in range(3)]
        for tno in range(0 if fake_mod else 6 * D // 512):
            slot = tno % 3
            wt, wk = Wa[slot], ('Wa', slot)
            p.dma('pool', wk, (lambda wt, tno: lambda e: e.dma_start(out=wt, in_=wada_v[:, :, tno * 512:(tno + 1) * 512]))(wt, tno), writes=[wk])
            for cc in range(4):
                j = tno * 4 + cc
                for k in range(KC):
                    p.pe((lambda wt, cc, j, k: lambda e: e.matmul(PS[:, 0, j:j + 1], lhsT=wt[:, k, cc * 128:(cc + 1) * 128],
                                                                  rhs=cact[:, k:k + 1], start=(k == 0), stop=(k == KC - 1)))(wt, cc, j, k),
                         reads=[wk, 'cact'], writes=['ps0'])
        if fake_mod:
            p.dve(lambda e: e.tensor_copy(out=modT, in_=PS_fake), reads=['ps0'], writes=['modT'])
        else:
            p.dve(lambda e: e.tensor_tensor(out=modT, in0=PS[:, 0, 0:192], in1=badat, op=ALU.add), reads=['ps0'], writes=['modT'])
        p.dve(lambda e: e.scalar_tensor_tensor(out=a1, in0=modT[:, 32:64], scalar=1.0, in1=g1t, op0=ALU.add, op1=ALU.mult),
              reads=['modT'], writes=['a1'])
        p.dve(lambda e: e.scalar_tensor_tensor(out=a2, in0=modT[:, 128:160], scalar=1.0, in1=g2t, op0=ALU.add, op1=ALU.mult),
              reads=['modT'], writes=['a2'])
        if 'modT' in dbg:
            p.dma('sp', 'dbg', lambda e: e.dma_start(out=dbg_out['modT'], in_=modT), reads=['modT'], writes=['dbg_modT'])
        p.barrier()
        if upto <= 1:
            p.emit(st)
            return nc, p

        def rms_norm_to_hT(src, row0, ntiles, a_t, s_t, hT, xs_off, junk_off):
            xs = [A.view(xs_off + i * 16384, [4096], F32) for i in range(2)]
            junk = A.view(junk_off, [4096], BF16)
            for tt in range(ntiles):
                xt = xs[tt % 2]
                xk = ('xs', tt % 2)
                sc = small[:, (tt % 2) * 4:(tt % 2) * 4 + 4]
                sk = ('nsc', tt % 2)
                p.dma('sp', xk, (lambda xt, tt: lambda e: e.dma_start(out=xt, in_=src[row0 + tt * 128: row0 + (tt + 1) * 128, :]))(xt, tt),
                      writes=[xk])
                p.act((lambda xt, sc: lambda e: e.activation(out=junk, in_=xt, func=AF.Square, accum_out=sc[:, 0:1]))(xt, sc),
                      reads=[xk], writes=['junk', sk])
                p.dve((lambda sc: lambda e: e.tensor_scalar(out=sc[:, 1:2], in0=sc[:, 0:1], scalar1=1.0 / D, scalar2=EPS,
                                                            op0=ALU.mult, op1=ALU.add))(sc), reads=[sk], writes=[sk])
                p.act((lambda sc: lambda e: e.activation(out=sc[:, 2:3], in_=sc[:, 1:2], func=AF.Sqrt))(sc), reads=[sk], writes=[sk])
                p.dve((lambda sc: lambda e: e.reciprocal(out=sc[:, 3:4], in_=sc[:, 2:3]))(sc), reads=[sk], writes=[sk])
                p.act((lambda xt, sc: lambda e: e.activation(out=xt, in_=xt, func=AF.Copy, scale=sc[:, 3:4]))(xt, sc),
                      reads=[xk, sk], writes=[xk])
                for g4 in range(8):
                    b = 4 + (g4 % 2)
                    for q in range(4):
                        c = g4 * 4 + q
                        p.pe((lambda xt, b, q, c: lambda e: e.transpose(PS[:, b, q * 128:(q + 1) * 128], xt[:, c * 128:(c + 1) * 128], ident_f))(xt, b, q, c),
                             reads=[xk, 'ident_f'], writes=[('ps', b)])
                    for q in range(4):
                        c = g4 * 4 + q
                        if g4 % 2 == 0:
                            p.dve((lambda b, q, c, tt: lambda e: e.tensor_scalar(out=hT[:, c, tt * 128:(tt + 1) * 128], in0=PS[:, b, q * 128:(q + 1) * 128],
                                                                                 scalar1=a_t[:, c:c + 1], scalar2=s_t[:, c:c + 1], op0=ALU.mult, op1=ALU.add))(b, q, c, tt),
                                  reads=[('ps', b)], writes=[('hT', c)])
                        else:
                            p.act((lambda b, q, c, tt: lambda e: e.activation(out=hT[:, c, tt * 128:(tt + 1) * 128], in_=PS[:, b, q * 128:(q + 1) * 128],
                                                                              func=AF.Identity, scale=a_t[:, c:c + 1], bias=s_t[:, c:c + 1]))(b, q, c, tt),
                                  reads=[('ps', b)], writes=[('hT', c)])

        pbank = [0]

        def next_pbank():
            b = pbank[0] % 2
            pbank[0] += 1
            return b

        hT_keys = [('hT', c) for c in range(KC)]

        def gemm_fm(wview, c0, ncols, hT, T, evac, src_keys):
            done = 0
            while done < ncols:
                n = min(256, ncols - done)
                wt, wk = load_w(wview, c0 + done, n)
                for cc in range((n + 127) // 128):
                    m = min(128, n - cc * 128)
                    for h0 in range(0, T, 512):
                        b = next_pbank()
                        for k in range(KC):
                            p.pe((lambda wt, cc, m, b, k, h0: lambda e: e.matmul(PS[0:m, b, 0:min(512, T - h0)], lhsT=wt[:, k, cc * 128:cc * 128 + m],
                                                                                 rhs=hT[:, k, h0:h0 + min(512, T - h0)], start=(k == 0), stop=(k == KC - 1)))(wt, cc, m, b, k, h0),
                                 reads=[wk] + src_keys, writes=[('ps', b)])
                        evac((done // 128) + cc, b, h0)
                done += n

        def gemm_tm(wview, c0, ncols, hT, T, evac, src_keys, also_fm=None):
            done = 0
            while done < ncols:
                n = min(256, ncols - done)
                wt, wk = load_w(wview, c0 + done, n)
                for tt in range(T // 128):
                    b = next_pbank()
                    for k in range(KC):
                        p.pe((lambda wt, n, b, k, tt: lambda e: e.matmul(PS[:, b, 0:n], lhsT=hT[:, k, tt * 128:(tt + 1) * 128],
                                                                         rhs=wt[:, k, 0:n], start=(k == 0), stop=(k == KC - 1)))(wt, n, b, k, tt),
                             reads=[wk] + src_keys, writes=[('ps', b)])
                    evac(done, n, tt, b)
                if also_fm is not None:
                    for cc in range(n // 128):
                        b = next_pbank()
                        for k in range(KC):
                            p.pe((lambda wt, cc, b, k: lambda e: e.matmul(PS[:, b, 0:T], lhsT=wt[:, k, cc * 128:(cc + 1) * 128],
                                                                          rhs=hT[:, k, 0:T], start=(k == 0), stop=(k == KC - 1)))(wt, cc, b, k),
                                 reads=[wk] + src_keys, writes=[('ps', b)])
                        also_fm((done // 128) + cc, b)
                done += n

        win_v = win_d.rearrange("(k p) c -> p k c", p=128)
        wout_v = wout_d.rearrange("(k p) c -> p k c", p=128)
        hT = A.view(R_HT, [32, MT], BF16)
        cT = A.view(R_CT, [32, MT], BF16)
        so = [R_S]

        def salloc(shape, dt):
            esz = 4 if dt == F32 else 2
            nb = (int(np.prod(shape)) * esz + 31) // 32 * 32
            v = A.view(so[0], shape, dt)
            so[0] += nb
            assert so[0] <= R_S + 49152, so[0]
            return v

        KT = salloc([2, 5 * 128], BF16)
        Vt = salloc([5, 2, 128], BF16)
        glowT = salloc([MT], BF16)
        S_AFTER_KV = so[0]
        QTg = salloc([4, 8, 128], BF16)
        tmpS = [salloc([512], F32) for _ in range(2)]
        PT = [salloc([512], BF16) for _ in range(2)]
        den = salloc([512], F32)
        so[0] = S_AFTER_KV
        gqT = salloc([2, MT], BF16)
        gkT = salloc([2, MT], BF16)
        gkt = salloc([4, 256], BF16)
        gv = salloc([4, 512], BF16)
        SG = salloc([4, 512], BF16)
        la = salloc([4, 256], F32)
        eb = salloc([2, MT], F32)
        enb = salloc([2, MT], F32)
        qdT = salloc([2, MT], BF16)
        kinvT = salloc([2, MT], BF16)
        kdec = salloc([4, 256], BF16)
        dl = salloc([2, 8], F32)
        Sbf = salloc([2, 512], BF16)
        attn_sb = salloc([64], BF16)
        ybf = salloc([512], BF16)
        tmp256 = salloc([256], F32)
        GLA_END = so[0]
        so[0] = S_AFTER_KV
        G1B = salloc([4096], F32)
        xres = [salloc([4, 256], F32) for _ in range(2)]
        dtmp = salloc([128], F32)

        PSb = PS[:, :, :].bitcast(BF16) if False else None

        p.pool(lambda e: e.memset(KT, 0.0), writes=['KT'])
        p.pool(lambda e: e.memset(Vt, 0.0), writes=['Vt'])

        def build_rowbcast(dst, colvec, tag):
            for c in range(KC):
                b = 2 + (c // 4) % 2
                q = c % 4
                p.dve((lambda c: lambda e: e.tensor_scalar(out=dtmp, in0=ident_f, scalar1=colvec[:, c:c + 1], scalar2=None, op0=ALU.mult))(c),
                      reads=['ident_f', 'modT'], writes=['dtmp'])
                p.pe((lambda b, q: lambda e: e.matmul(PS[:, b, q * 128:(q + 1) * 128], lhsT=ones_f, rhs=dtmp, start=True, stop=True))(b, q),
                     reads=['dtmp', 'ones_f'], writes=[('ps', b)])
                if q == 3:
                    p.act((lambda b, c: lambda e: e.activation(out=dst[:, (c - 3) * 128:(c + 1) * 128], in_=PS[:, b, :], func=AF.Identity))(b, c),
                          reads=[('ps', b)], writes=[tag])

        macro = [('P', 0), ('P', 1), ('M', 0), ('M', 1)]
        for kind, mi in macro:
            main = kind == 'M'
            src = x_main if main else x_pre
            rms_norm_to_hT(src, mi * MT, MT // 128, a1, s1, hT, R_CT, R_W + 32768)
            p.barrier()
            if stop == 'norm%s%d' % (kind, mi):
                p.barrier()
                p.emit(st)
                return nc, p
            need_kv = main or mi == 1

            if need_kv:
                def ev_k(ci, b, h0):
                    p.act((lambda ci, b: lambda e: e.activation(out=KT[:, ci, 128:128 + MT], in_=PS[:, b, 0:MT], func=AF.Identity))(ci, b),
                          reads=[('ps', b)], writes=['KT'])
                gemm_fm(win_v, C_AK, 256, hT, MT, ev_k, hT_keys)

                def ev_v(c0, n, tt, b):
                    p.dve((lambda tt, b: lambda e: e.tensor_copy(out=Vt[:, 1 + tt, :, :], in_=PS[:, b, 0:256].rearrange("p (g d) -> p g d", g=2)))(tt, b),
                          reads=[('ps', b)], writes=['Vt'])
                gemm_tm(win_v, C_AV, 256, hT, MT, ev_v, hT_keys)
            if main:
                for g in range(2):
                    def ev_q(ci, b, h0, g=g):
                        p.act((lambda ci, b: lambda e: e.activation(out=QTg[:, :, ci, :], in_=PS[:, b, 0:MT].rearrange("p (n q) -> p n q", n=4),
                                                                    func=AF.Copy, scale=128 ** -0.5))(ci, b),
                              reads=[('ps', b)], writes=['QTg'])
                    gemm_fm(win_v, C_AQ + g * 1024, 1024, hT, MT, ev_q, hT_keys)
                    for n in range(4):
                        for hf in range(2):
                            hs = slice(hf * 4, hf * 4 + 4)
                            hg = slice(g * 8 + hf * 4, g * 8 + hf * 4 + 4)
                            rq = QTg[:, n, hs, :].rearrange("p j q -> p (j q)")
                            p.pe((lambda g, n, rq: lambda e: e.matmul(PS[:, 2, :], lhsT=KT[:, g, (n + 1) * 128:(n + 2) * 128], rhs=rq, start=True, stop=True))(g, n, rq),
                                 reads=['KT', 'QTg'], writes=[('ps', 2)])
                            p.pe((lambda g, n, rq: lambda e: e.matmul(PS[:, 3, :], lhsT=KT[:, g, n * 128:(n + 1) * 128], rhs=rq, start=True, stop=True))(g, n, rq),
                                 reads=['KT', 'QTg'], writes=[('ps', 3)])
                            for w_, (bk, bt) in enumerate([(2, bcur), (3, bprev)]):
                                p.dve((lambda w_, bk, bt, hg: lambda e: e.tensor_tensor(out=tmpS[w_], in0=PS[:, bk, :], in1=bt[:, hg, :].rearrange("p j q -> p (j q)"), op=ALU.add))(w_, bk, bt, hg),
                                      reads=[('ps', bk)], writes=[('tmpS', w_)])
                                p.act((lambda w_: lambda e: e.activation(out=PT[w_], in_=tmpS[w_], func=AF.Exp))(w_),
                                      reads=[('tmpS', w_)], writes=[('PT', w_)])
                            if mi == 0 and n == 0:
                                p.dve(lambda e: e.tensor_scalar(out=PT[1], in0=PT[1], scalar1=flag[:, 0:1], scalar2=None, op0=ALU.mult),
                                      reads=[('PT', 1), 'flag'], writes=[('PT', 1)])
                            p.pe(lambda e: e.matmul(PS[:, 6, :], lhsT=ones_b, rhs=PT[0], start=True, stop=False), reads=[('PT', 0), 'ones_b'], writes=[('ps', 6)])
                            p.pe(lambda e: e.matmul(PS[:, 6, :], lhsT=ones_b, rhs=PT[1], start=False, stop=True), reads=[('PT', 1), 'ones_b'], writes=[('ps', 6)])
                            p.pe((lambda g, n: lambda e: e.matmul(PS[:, 7, :], lhsT=Vt[:, n + 1, g, :], rhs=PT[0], start=True, stop=False))(g, n),
                                 reads=[('PT', 0), 'Vt'], writes=[('ps', 7)])
                            p.pe((lambda g, n: lambda e: e.matmul(PS[:, 7, :], lhsT=Vt[:, n, g, :], rhs=PT[1], start=False, stop=True))(g, n),
                                 reads=[('PT', 1), 'Vt'], writes=[('ps', 7)])
                            for j in range(4):
                                hh = g * 8 + hf * 4 + j
                                p.dve((lambda j, hh: lambda e: e.tensor_scalar(out=den[:, j * 128:(j + 1) * 128], in0=PS[:, 6, j * 128:(j + 1) * 128],
                                                                               scalar1=esink[:, hh:hh + 1], scalar2=None, op0=ALU.add))(j, hh),
                                      reads=[('ps', 6), 'esink'], writes=['den'])
                            p.dve(lambda e: e.reciprocal(out=den, in_=den), reads=['den'], writes=['den'])
                            p.dve((lambda g, n, hf: lambda e: e.tensor_tensor(out=cT[:, g * 8 + hf * 4:g * 8 + hf * 4 + 4, n * 128:(n + 1) * 128],
                                                                              in0=PS[:, 7, :].rearrange("p (j q) -> p j q", j=4),
                                                                              in1=den.rearrange("p (j q) -> p j q", j=4), op=ALU.mult))(g, n, hf),
                                  reads=[('ps', 7), 'den'], writes=[('cT', g * 8 + hf * 4 + jj) for jj in range(4)])
            if need_kv:
                p.pool(lambda e: e.tensor_copy(out=KT[:, :, 0:128], in_=KT[:, :, 512:640]), reads=['KT'], writes=['KT'])
                p.pool(lambda e: e.tensor_copy(out=Vt[:, 0, :, :], in_=Vt[:, 4, :, :]), reads=['Vt'], writes=['Vt'])
            if stop == 'swa%s%d' % (kind, mi):
                p.barrier()
                p.emit(st)
                return nc, p
            if 'cT_attn' in dbg and main and mi == 0:
                p.barrier()
                p.dve(lambda e: e.tensor_copy(out=eb[:, 0, :], in_=cT[:, 0, :]), writes=['dbgtmp'])
                p.dma('sp', 'dbg', lambda e: e.dma_start(out=dbg_out['cT_attn'], in_=eb[:, 0, :]), reads=['dbgtmp'], writes=['dbg_ct'])
            p.barrier()
            def ev_glow(ci, b, h0):
                p.act((lambda b: lambda e: e.activation(out=glowT[0:16, :], in_=PS[0:16, b, 0:MT], func=AF.Identity))(b),
                      reads=[('ps', b)], writes=['glowT'])
            gemm_fm(win_v, C_GLOW, 16, hT, MT, ev_glow, hT_keys)
            for h in range(4):
                if main:
                    def ev_gq(ci, b, h0):
                        p.act((lambda ci, b: lambda e: e.activation(out=gqT[:, ci, :], in_=PS[:, b, 0:MT], func=AF.Identity))(ci, b),
                              reads=[('ps', b)], writes=['gqT'])
                    gemm_fm(win_v, C_GQ + h * 256, 256, hT, MT, ev_gq, hT_keys)

                def ev_gk(c0, n, tt, b):
                    p.dve((lambda tt, b: lambda e: e.tensor_copy(out=gkt[:, tt, :], in_=PS[:, b, 0:256]))(tt, b), reads=[('ps', b)], writes=['gkt'])

                def ev_gkT(ci, b):
                    p.act((lambda ci, b: lambda e: e.activation(out=gkT[:, ci, :], in_=PS[:, b, 0:MT], func=AF.Identity))(ci, b),
                          reads=[('ps', b)], writes=['gkT'])
                gemm_tm(win_v, C_GK + h * 256, 256, hT, MT, ev_gk, hT_keys, also_fm=ev_gkT if main else None)

                def ev_gv(c0, n, tt, b):
                    p.act((lambda c0, tt, b: lambda e: e.activation(out=gv[:, tt, c0:c0 + 256], in_=PS[:, b, 0:256], func=AF.Identity))(c0, tt, b),
                          reads=[('ps', b)], writes=['gv'])
                gemm_tm(win_v, C_GV + h * 512, 512, hT, MT, ev_gv, hT_keys)
                if main:
                    def ev_go(c0, n, tt, b):
                        p.act((lambda b: lambda e: e.activation(out=tmp256, in_=PS[:, b, 0:256], func=AF.Silu))(b),
                              reads=[('ps', b)], writes=['tmp256'])
                        p.dve((lambda c0, tt: lambda e: e.tensor_tensor(out=SG[:, tt, c0:c0 + 256], in0=tmp256, in1=gnb[:, c0:c0 + 256], op=ALU.mult))(c0, tt),
                              reads=['tmp256', 'gnb'], writes=['SG'])
                    gemm_tm(win_v, C_GOUT + h * 512, 512, hT, MT, ev_go, hT_keys)
                for tt in range(4):
                    p.pe((lambda tt, h: lambda e: e.matmul(PS[:, 2, 0:256], lhsT=glowT[0:16, tt * 128:(tt + 1) * 128], rhs=gk2w[0:16, h * 256:(h + 1) * 256],
                                                           start=True, stop=False))(tt, h), reads=['glowT', 'gk2w'], writes=[('ps', 2)])
                    p.pe((lambda tt, h: lambda e: e.matmul(PS[:, 2, 0:256], lhsT=ones_b[0:1, :], rhs=gk2b[0:1, h * 256:(h + 1) * 256],
                                                           start=False, stop=True))(tt, h), reads=['gk2b', 'ones_b'], writes=[('ps', 2)])
                    p.act(lambda e: e.activation(out=tmp256, in_=PS[:, 2, 0:256], func=AF.Exp, scale=-1.0), reads=[('ps', 2)], writes=['tmp256'])
                    p.act(lambda e: e.activation(out=tmp256, in_=tmp256, func=AF.Ln, bias=1.0), reads=['tmp256'], writes=['tmp256'])
                    p.act((lambda tt: lambda e: e.activation(out=la[:, tt, :], in_=tmp256, func=AF.Copy, scale=-1.0 / 16.0))(tt),
                          reads=['tmp256'], writes=['la'])
                for dc in range(2):
                    for tt in range(4):
                        p.pe((lambda dc, tt: lambda e: e.matmul(PS[:, 2 + dc, tt * 128:(tt + 1) * 128], lhsT=la[:, tt, dc * 128:(dc + 1) * 128], rhs=Lm,
                                                                start=True, stop=True))(dc, tt), reads=['la', 'Lm'], writes=[('ps', 2 + dc)])
                p.act(lambda e: e.activation(out=eb, in_=PS[:, 2:4, :], func=AF.Exp), reads=[('ps', 2), ('ps', 3)], writes=['eb'])
                if main:
                    p.act(lambda e: e.activation(out=enb, in_=PS[:, 2:4, :], func=AF.Exp, scale=-1.0), reads=[('ps', 2), ('ps', 3)], writes=['enb'])
                    p.dve(lambda e: e.scalar_tensor_tensor(out=qdT, in0=gqT, scalar=1.0 / 16.0, in1=eb, op0=ALU.mult, op1=ALU.mult),
                          reads=['gqT', 'eb'], writes=['qdT'])
                    p.dve(lambda e: e.tensor_tensor(out=kinvT, in0=gkT, in1=enb, op=ALU.mult), reads=['gkT', 'enb'], writes=['kinvT'])
                p.dve(lambda e: e.tensor_copy(out=dl, in_=eb[:, :, 63::64]), reads=['eb'], writes=['dl'])
                for tt in range(4):
                    p.pe((lambda tt: lambda e: e.matmul(PS[:, 2, 0:256], lhsT=Um, rhs=la[:, tt, :], start=True, stop=True))(tt),
                         reads=['la', 'Um'], writes=[('ps', 2)])
                    p.act(lambda e: e.activation(out=tmp256, in_=PS[:, 2, 0:256], func=AF.Exp), reads=[('ps', 2)], writes=['tmp256'])
                    p.dve((lambda tt: lambda e: e.tensor_tensor(out=kdec[:, tt, :], in0=gkt[:, tt, :], in1=tmp256, op=ALU.mult))(tt),
                          reads=['gkt', 'tmp256'], writes=['kdec'])
                p.act((lambda h: lambda e: e.activation(out=Sbf, in_=Sst[:, h, :, :], func=AF.Identity))(h), reads=['Sst'], writes=['Sbf'])
                for c in range(8):
                    tt, par = c // 2, c % 2
                    r0 = 64 * par
                    t0 = c * 64
                    rs = slice(r0, r0 + 64)
                    if main:
                        for dc in range(2):
                            p.pe((lambda dc, t0, rs: lambda e: e.matmul(PS[rs, 3, 0:64], lhsT=kinvT[:, dc, t0:t0 + 64], rhs=qdT[:, dc, t0:t0 + 64],
                                                                        start=(dc == 0), stop=(dc == 1)))(dc, t0, rs),
                                 reads=['kinvT', 'qdT'], writes=[('ps', 3)])
                        p.dve((lambda rs: lambda e: e.tensor_tensor(out=attn_sb[rs, :], in0=PS[rs, 3, 0:64], in1=Lm[rs, rs], op=ALU.mult))(rs),
                              reads=[('ps', 3), 'Lm'], writes=['attn_sb'])
                        p.pe((lambda rs, tt: lambda e: e.matmul(PS[rs, 6, :], lhsT=attn_sb[rs, :], rhs=gv[rs, tt, :], start=True, stop=False))(rs, tt),
                             reads=['attn_sb', 'gv'], writes=[('ps', 6)])
                        for dc in range(2):
                            p.pe((lambda dc, t0, rs: lambda e: e.matmul(PS[rs, 6, :], lhsT=qdT[:, dc, t0:t0 + 64], rhs=Sbf[:, dc, :],
                                                                        start=False, stop=(dc == 1)))(dc, t0, rs),
                                 reads=['qdT', 'Sbf'], writes=[('ps', 6)])
                    for dc in range(2):
                        p.pe((lambda dc, rs, tt: lambda e: e.matmul(PS[:, 4 + dc, :], lhsT=kdec[rs, tt, dc * 128:(dc + 1) * 128], rhs=gv[rs, tt, :],
                                                                    start=True, stop=True))(dc, rs, tt),
                             reads=['kdec', 'gv'], writes=[('ps', 4 + dc)])
                        p.dve((lambda dc, c, h: lambda e: e.scalar_tensor_tensor(out=Sst[:, h, dc, :], in0=Sst[:, h, dc, :], scalar=dl[:, dc, c:c + 1],
                                                                                 in1=PS[:, 4 + dc, :], op0=ALU.mult, op1=ALU.add))(dc, c, h),
                              reads=['Sst', 'dl', ('ps', 4 + dc)], writes=['Sst'])
                    p.act((lambda h: lambda e: e.activation(out=Sbf, in_=Sst[:, h, :, :], func=AF.Identity))(h), reads=['Sst'], writes=['Sbf'])
                    if main and par == 1:
                        sc = small[:, 8:12]
                        p.act(lambda e: e.activation(out=ybf, in_=PS[:, 6, :], func=AF.Square, accum_out=sc[:, 0:1]), reads=[('ps', 6)], writes=['ybf', 'gsc'])
                        p.dve(lambda e: e.tensor_scalar(out=sc[:, 1:2], in0=sc[:, 0:1], scalar1=1.0 / 512, scalar2=EPS, op0=ALU.mult, op1=ALU.add),
                              reads=['gsc'], writes=['gsc'])
                        p.act(lambda e: e.activation(out=sc[:, 2:3], in_=sc[:, 1:2], func=AF.Sqrt), reads=['gsc'], writes=['gsc'])
                        p.dve(lambda e: e.reciprocal(out=sc[:, 3:4], in_=sc[:, 2:3]), reads=['gsc'], writes=['gsc'])
                        p.dve((lambda tt: lambda e: e.scalar_tensor_tensor(out=ybf, in0=PS[:, 6, :], scalar=sc[:, 3:4], in1=SG[:, tt, :],
                                                                           op0=ALU.mult, op1=ALU.mult))(tt), reads=[('ps', 6), 'gsc', 'SG'], writes=['ybf'])
                        pst = PS[:, 7, :].bitcast(BF16)
                        for j in range(4):
                            p.pe((lambda j: lambda e: e.transpose(pst[:, j * 128:(j + 1) * 128], ybf[:, j * 128:(j + 1) * 128], ident_b))(j),
                                 reads=['ybf', 'ident_b'], writes=[('ps', 7)])
                        p.act((lambda h, tt: lambda e: e.activation(out=cT[:, 16 + h * 4:16 + h * 4 + 4, tt * 128:(tt + 1) * 128],
                                                                    in_=pst[:, 0:512].rearrange("p (j q) -> p j q", j=4), func=AF.Identity))(h, tt),
                              reads=[('ps', 7)], writes=[('cT', 16 + h * 4 + jj) for jj in range(4)])
                if not main and mi == 1:
                    p.dve((lambda h: lambda e: e.tensor_scalar(out=Sst[:, h, :, :], in0=Sst[:, h, :, :], scalar1=flag[:, 0:1], scalar2=None, op0=ALU.mult))(h),
                          reads=['Sst', 'flag'], writes=['Sst'])
            p.barrier()
            if stop == 'gla%s%d' % (kind, mi):
                p.emit(st)
                return nc, p
            if 'cT_gla' in dbg and main and mi == 0:
                p.dve(lambda e: e.tensor_copy(out=eb[:, 0, :], in_=cT[:, 16, :]), writes=['dbgtmp'])
                p.dma('sp', 'dbg', lambda e: e.dma_start(out=dbg_out['cT_gla'], in_=eb[:, 0, :]), reads=['dbgtmp'], writes=['dbg_ct2'])
                p.barrier()
            if not main:
                continue
            build_rowbcast(G1B, gate1, 'G1B')
            cT_keys = [('cT', c) for c in range(KC)]
            xstate = {}

            def ev_o(c0, n, tt, b):
                cg = c0 // 256
                xr = xres[cg % 2]
                xk = ('xres', cg % 2)
                if tt == 0:
                    srcap = x_main[mi * MT:(mi + 1) * MT, c0:c0 + 256].rearrange("(t p) c -> p t c", p=128)
                    p.dma('sp', xk, (lambda xr, srcap: lambda e: e.dma_start(out=xr, in_=srcap))(xr, srcap), writes=[xk])
                p.dve((lambda c0, b: lambda e: e.tensor_tensor(out=tmp256, in0=PS[:, b, 0:256], in1=G1B[:, c0:c0 + 256], op=ALU.mult))(c0, b),
                      reads=[('ps', b), 'G1B'], writes=['tmp256'])
                p.dve((lambda xr, tt: lambda e: e.tensor_tensor(out=xr[:, tt, :], in0=xr[:, tt, :], in1=tmp256, op=ALU.add))(xr, tt),
                      reads=['tmp256', xk], writes=[xk])
                if tt == 3:
                    dstap = x1s[mi * MT:(mi + 1) * MT, c0:c0 + 256].rearrange("(t p) c -> p t c", p=128)
                    p.dma('sp', ('xst', cg % 2), (lambda xr, dstap: lambda e: e.dma_start(out=dstap, in_=xr))(xr, dstap),
                          reads=[xk], writes=[xk, 'x1s'])
            gemm_tm(wout_v, 0, D, cT, MT, ev_o, cT_keys)
            p.barrier()
        if upto <= 2:
            p.emit(st)
            return nc, p

        wq_v = wq_d.rearrange("(k p) c -> p k c", p=128)
        ut_v = ut_d.rearrange("(k p) e -> p k e", p=128)
        h2T = A.view(R_HT, [32, TOK], BF16)
        h2_keys = [('hT', c) for c in range(KC)]
        rms_norm_to_hT(x1s, 0, TOK // 128, a2, s2, h2T, R_W, R_W + 32768)
        p.barrier()
        qT = A.view(R_S, [16, TOK], BF16)

        def ev_pq(ci, b, h0):
            p.act((lambda ci, b, h0: lambda e: e.activation(out=qT[:, ci, h0:h0 + 512], in_=PS[:, b, :], func=AF.Identity))(ci, b, h0),
                  reads=[('ps', b)], writes=['qT'])
        gemm_fm(wq_v, 0, 2048, h2T, TOK, ev_pq, h2_keys)
        p.barrier()
        s2_all = A.view(R_W, [8, 8, 128], F32)
        sig = [A.view(R_W + 32768 + tt * 4096, [8, 128], F32) for tt in range(4)] + \
              [A.view(R_S + 32768 + tt * 4096, [8, 128], F32) for tt in range(4)]
        qo = [P_DYN]

        def qalloc(shape, dt):
            esz = 4 if dt == F32 else 2
            nb = (int(np.prod(shape)) * esz + 31) // 32 * 32
            v = A.view(qo[0], shape, dt)
            qo[0] += nb
            assert qo[0] <= R_P + 36864, qo[0]
            return v
        rz_all = qalloc([64], F32)
        Q_KEEP = qo[0]
        keysT = qalloc([16, 128], BF16)
        t16 = qalloc([16, 16], F32)
        tmpk = qalloc([128], F32)
        cand = qalloc([16, 16], F32)
        tmpc = qalloc([256], F32)
        c16 = qalloc([8, 16], F32)
        tsc = qalloc([8, 8], F32)
        junk16 = qalloc([16], F32)
        scs = qalloc([16, 128], F32)
        p.dma('pool', 'peer_c', lambda e: e.dma_start(out=keysT, in_=keyst_d.rearrange("p (a k) -> p a k", a=16)), writes=['keysT'])
        p.barrier()
        for tt in range(8):
            for hp in range(16):
                b = 4 + hp // 4
                p.pe((lambda hp, b, tt: lambda e: e.matmul(PS[:, b, (hp % 4) * 128:(hp % 4 + 1) * 128], lhsT=qT[:, hp, tt * 128:(tt + 1) * 128],
                                                           rhs=keysT[:, hp, :], start=True, stop=True))(hp, b, tt),
                     reads=['qT', 'keysT'], writes=[('ps', b)])
            p.act(lambda e: e.activation(out=scs, in_=PS[:, 4:8, :].rearrange("p b (c k) -> p (b c) k", c=4), func=AF.Identity),
                  reads=[('ps', 4), ('ps', 5), ('ps', 6), ('ps', 7)], writes=['scs'])
            p.pool((lambda tt: lambda e: e.tensor_copy(out=s2_all[:, tt, :, :], in_=scs.rearrange("p (h two) k -> p h two k", two=2)[:, :, 1, :]))(tt),
                   reads=['scs'], writes=[('s2', tt)])
            for hp in range(16):
                p.dve((lambda hp: lambda e: e.max(out=t16[:, hp, 0:8], in_=scs[:, hp, :]))(hp), reads=['scs'], writes=['t16'])
                p.dve((lambda hp: lambda e: e.match_replace(out=tmpk, in_to_replace=t16[:, hp, 0:8], in_values=scs[:, hp, :], imm_value=-1e30))(hp),
                      reads=['scs', 't16'], writes=['tmpk'])
                p.dve((lambda hp: lambda e: e.max(out=t16[:, hp, 8:16], in_=tmpk))(hp), reads=['tmpk'], writes=['t16'])
            for h in range(8):
                p.dve((lambda h: lambda e: e.tensor_tensor(out=cand, in0=t16[:, 2 * h, :].unsqueeze(2).to_broadcast([128, 16, 16]),
                                                           in1=t16[:, 2 * h + 1, :].unsqueeze(1).to_broadcast([128, 16, 16]), op=ALU.add))(h),
                      reads=['t16'], writes=['cand'])
                cf = cand.rearrange("p a b -> p (a b)")
                p.dve((lambda h: lambda e: e.max(out=c16[:, h, 0:8], in_=cf))(h), reads=['cand'], writes=['c16'])
                p.dve((lambda h: lambda e: e.match_replace(out=tmpc, in_to_replace=c16[:, h, 0:8], in_values=cf, imm_value=-1e30))(h),
                      reads=['cand', 'c16'], writes=['tmpc'])
                p.dve((lambda h: lambda e: e.max(out=c16[:, h, 8:16], in_=tmpc))(h), reads=['tmpc'], writes=['c16'])
                p.dve((lambda h: lambda e: e.tensor_scalar(out=tsc[:, h, 0:1], in0=c16[:, h, 15:16], scalar1=-1.0, scalar2=1e-3, op0=ALU.mult, op1=ALU.add))(h),
                      reads=['c16'], writes=['tsc'])
                p.act((lambda h: lambda e: e.activation(out=junk16, in_=c16[:, h, :], func=AF.Exp, bias=tsc[:, h, 0:1], accum_out=tsc[:, h, 1:2]))(h),
                      reads=['c16', 'tsc'], writes=['tsc', 'junk16'])
                p.dve((lambda h, tt: lambda e: e.reciprocal(out=rz_all[:, tt * 8 + h:tt * 8 + h + 1], in_=tsc[:, h, 1:2]))(h, tt), reads=['tsc'], writes=['rz_all'])
                p.act((lambda h: lambda e: e.activation(out=tsc[:, h, 3:4], in_=tsc[:, h, 1:2], func=AF.Ln))(h), reads=['tsc'], writes=['tsc'])
                p.dve((lambda h: lambda e: e.tensor_tensor(out=tsc[:, h, 4:5], in0=tsc[:, h, 0:1], in1=tsc[:, h, 3:4], op=ALU.subtract))(h),
                      reads=['tsc'], writes=['tsc'])
                p.dve((lambda h, tt: lambda e: e.tensor_scalar(out=sig[tt][:, h, :], in0=scs[:, 2 * h, :], scalar1=tsc[:, h, 4:5], scalar2=None, op0=ALU.add))(h, tt),
                      reads=['scs', 'tsc'], writes=[('sig', tt)])
            p.act((lambda tt: lambda e: e.activation(out=sig[tt][:, 6:8, :], in_=sig[tt][:, 6:8, :], func=AF.Exp))(tt), reads=[('sig', tt)], writes=[('sig', tt)])
            p.act((lambda tt: lambda e: e.activation(out=s2_all[:, tt, 6:8, :], in_=s2_all[:, tt, 6:8, :], func=AF.Exp))(tt), reads=[('s2', tt)], writes=[('s2', tt)])
        if 'thr' in dbg:
            p.barrier()
            p.dma('sp', 'dbg', lambda e: e.dma_start(out=dbg_out['thr'], in_=sig[0][:, :, 0]), writes=['dbg_thr'])
            p.dma('sp', 'dbg2', lambda e: e.dma_start(out=dbg_out['rzr'], in_=rz_all), writes=['dbg_rzr'])
        p.barrier()
        if upto <= 3:
            p.emit(st)
            return nc, p
        ut = [A.view(R_S + i * 16384, [32, 256], BF16) for i in range(2)]
        qo[0] = Q_KEEP
        Ep = [qalloc([128], F32) for _ in range(4)]
        Wm = [qalloc([128], BF16) for _ in range(8)]
        Gl2 = [qalloc([TOK], BF16) for _ in range(2)]
        AW = [qalloc([TOK], BF16) for _ in range(2)]
        NE = 128 if upto > 4 else 2

        def load_u(pi):
            us = pi % 2
            p.dma('pool', ('U', us), lambda e: e.dma_start(out=ut[us], in_=ut_v[:, :, pi * 256:pi * 256 + 256]), writes=[('U', us)])

        def actT_mms(i1):
            us = (i1 // 2) % 2
            uk = ('U', us)
            ec = (i1 % 2) * 128
            lst = []
            for hf in range(2):
                for k in range(KC):
                    lst.append((lambda hf, k: lambda: p.pe(lambda e: e.matmul(PS[:, hf, :], lhsT=ut[us][:, k, ec:ec + 128], rhs=h2T[:, k, hf * 512:(hf + 1) * 512],
                                                                             start=(k == 0), stop=(k == KC - 1)),
                                                           reads=[uk] + h2_keys, writes=[('ps', hf)]))(hf, k))
            return lst

        def emit_gelu(i1):
            g = Gl2[i1 % 2]
            p.act(lambda e: e.activation(out=g.rearrange("p (b t) -> p b t", b=2), in_=PS[:, 0:2, :], func=AF.Gelu), reads=[('ps', 0), ('ps', 1)], writes=[('Gl', i1 % 2)])

        def emit_gate(i1, k):
            tt, h = k // 8, k % 8
            r, r8 = k % 4, k % 8
            if h >= 6:
                p.pool(lambda e: e.tensor_scalar(out=Ep[r], in0=s2_all[:, tt, h, :], scalar1=sig[tt][:, h, i1:i1 + 1], scalar2=1.0, op0=ALU.mult, op1=ALU.mult),
                       writes=[('Ep', r)])
            else:
                p.act(lambda e: e.activation(out=Ep[r], in_=s2_all[:, tt, h, :], func=AF.Exp, bias=sig[tt][:, h, i1:i1 + 1]), writes=[('Ep', r)])
            p.dve(lambda e: e.scalar_tensor_tensor(out=Wm[r8], in0=Ep[r], scalar=rz_all[:, tt * 8 + h:tt * 8 + h + 1], in1=Ep[r], op0=ALU.is_ge, op1=ALU.mult),
                  reads=[('Ep', r)], writes=[('Wm', r8)])

        def emit_acc(k):
            tt, h = k // 8, k % 8
            r8 = k % 8
            p.pe(lambda e: e.matmul(PS[:, 6 + tt // 4, (tt % 4) * 128:(tt % 4 + 1) * 128], lhsT=Wm[r8], rhs=ident_b, start=(h == 0), stop=(h == 7)),
                 reads=[('Wm', r8), 'ident_b'], writes=[('ps', 6 + tt // 4)])

        LAG = 4
        load_u(0)
        for f in actT_mms(0):
            f()
        emit_gelu(0)
        for i1 in range(NE):
            if i1 % 2 == 0 and (i1 // 2 + 1) * 2 < NE:
                load_u(i1 // 2 + 1)
            nxt = actT_mms(i1 + 1) if i1 + 1 < NE else []
            for k in range(64 + LAG):
                if k < 64:
                    emit_gate(i1, k)
                    if k < len(nxt):
                        nxt[k]()
                if k - LAG >= 0:
                    emit_acc(k - LAG)
            a = i1 % 2
            g = Gl2[i1 % 2]
            p.dve((lambda a, g: lambda e: e.tensor_tensor(out=AW[a].rearrange("p (b t) -> p b t", b=2), in0=PS[:, 6:8, :], in1=g.rearrange("p (b t) -> p b t", b=2), op=ALU.mult))(a, g),
                  reads=[('ps', 6), ('ps', 7), ('Gl', i1 % 2)], writes=[('AW', a)])
            p.dma('sp', ('AWst', a), (lambda a, i1: lambda e: e.dma_start(out=aws[i1], in_=AW[a]))(a, i1), reads=[('AW', a)], writes=[('AW', a), 'aws'])
            if i1 + 1 < NE:
                emit_gelu(i1 + 1)
        if 'aw0' in dbg:
            p.barrier()
            dbt = A.view(R_HT, [TOK], F32)
            p.act(lambda e: e.activation(out=dbt, in_=AW[0], func=AF.Identity), writes=['dbt'])
            p.dma('sp', 'dbg', lambda e: e.dma_start(out=dbg_out['aw0'], in_=dbt), reads=['dbt'], writes=['dbg_aw0'])
        p.barrier()
        if upto <= 4:
            p.emit(st)
            return nc, p
        acc = A.view(0, [8, D], F32)
        vt = [A.view(131072 + i * 8192, [8, 512], BF16) for i in range(2)]
        awt = [A.view(131072 + 16384, [8, TOK], BF16), A.view(P_DYN, [8, TOK], BF16)]
        v_v = v_d.rearrange("(g a p) d -> g p a d", a=8, p=128)
        aws_v = aws.rearrange("(g a) p t -> g p a t", a=8)
        it = 0
        for dg in range(8):
            for eg in range(16):
                s_ = it % 2
                it += 1
                p.dma('pool', ('V', s_), (lambda s_, eg, dg: lambda e: e.dma_start(out=vt[s_], in_=v_v[eg][:, :, dg * 512:(dg + 1) * 512]))(s_, eg, dg), writes=[('V', s_)])
                p.dma('sp', ('AWld', s_), (lambda s_, eg: lambda e: e.dma_start(out=awt[s_], in_=aws_v[eg]))(s_, eg), reads=['aws'], writes=[('AWl', s_)])
                for a in range(8):
                    for tt in range(8):
                        p.pe((lambda s_, a, tt, eg: lambda e: e.matmul(PS[:, tt, :], lhsT=awt[s_][:, a, tt * 128:(tt + 1) * 128], rhs=vt[s_][:, a, :],
                                                                      start=(eg == 0 and a == 0), stop=(eg == 15 and a == 7)))(s_, a, tt, eg),
                             reads=[('V', s_), ('AWl', s_)], writes=[('ps', tt)])
            for tt in range(8):
                if tt % 2 == 0:
                    p.act((lambda tt, dg: lambda e: e.activation(out=acc[:, tt, dg * 512:(dg + 1) * 512], in_=PS[:, tt, :], func=AF.Identity))(tt, dg),
                          reads=[('ps', tt)], writes=[('acc', tt, dg)])
                else:
                    p.dve((lambda tt, dg: lambda e: e.tensor_copy(out=acc[:, tt, dg * 512:(dg + 1) * 512], in_=PS[:, tt, :]))(tt, dg),
                          reads=[('ps', tt)], writes=[('acc', tt, dg)])
        p.barrier()
        G2B = A.view(131072, [D], F32)
        FGB = A.view(131072 + 16384, [D], F32)
        x1t = A.view(P_DYN + 1024, [D], F32)
        dtmp2 = A.view(P_DYN, [128], F32)

        for c in range(KC):
            b = 2 + (c // 4) % 2
            q = c % 4
            p.dve((lambda c: lambda e: e.tensor_scalar(out=dtmp2, in0=ident_f, scalar1=gate2[:, c:c + 1], scalar2=None, op0=ALU.mult))(c),
                  reads=['ident_f', 'modT'], writes=['dtmp2'])
            p.pe((lambda b, q: lambda e: e.matmul(PS[:, b, q * 128:(q + 1) * 128], lhsT=ones_f, rhs=dtmp2, start=True, stop=True))(b, q),
                 reads=['dtmp2', 'ones_f'], writes=[('ps', b)])
            if q == 3:
                p.act((lambda b, c: lambda e: e.activation(out=G2B[:, (c - 3) * 128:(c + 1) * 128], in_=PS[:, b, :], func=AF.Identity))(b, c),
                      reads=[('ps', b)], writes=['G2B'])
        p.dma('sp', 'fgb', lambda e: e.dma_start(out=FGB, in_=fg_d.partition_broadcast(128)[:, 0, :]), writes=['FGB'])
        for tt in range(8):
            sc = small[:, 16 + 4 * (tt % 2):20 + 4 * (tt % 2)]
            sk = ('fsc', tt % 2)
            at = acc[:, tt, :]
            ak = ('accf', tt)
            p.dma('sp', 'x1ld', (lambda tt: lambda e: e.dma_start(out=x1t, in_=x1s[tt * 128:(tt + 1) * 128, :]))(tt), reads=['x1s'], writes=['x1t'])
            p.dve((lambda at: lambda e: e.tensor_tensor(out=at, in0=at, in1=G2B, op=ALU.mult))(at), reads=['G2B'], writes=[ak])
            p.pool((lambda at: lambda e: e.tensor_tensor(out=at, in0=at, in1=x1t, op=ALU.add))(at), reads=['x1t', ak], writes=[ak])
            p.act((lambda at, sc: lambda e: e.activation(out=PS[:, :, :], in_=at.rearrange("p (b t) -> p b t", b=8), func=AF.Square, accum_out=sc[:, 0:1]))(at, sc),
                  reads=[ak], writes=['junkf', sk])
            p.dve((lambda sc: lambda e: e.tensor_scalar(out=sc[:, 1:2], in0=sc[:, 0:1], scalar1=1.0 / D, scalar2=EPS, op0=ALU.mult, op1=ALU.add))(sc), reads=[sk], writes=[sk])
            p.act((lambda sc: lambda e: e.activation(out=sc[:, 2:3], in_=sc[:, 1:2], func=AF.Sqrt))(sc), reads=[sk], writes=[sk])
            p.dve((lambda sc: lambda e: e.reciprocal(out=sc[:, 3:4], in_=sc[:, 2:3]))(sc), reads=[sk], writes=[sk])
            p.dve((lambda at, sc: lambda e: e.scalar_tensor_tensor(out=at, in0=at, scalar=sc[:, 3:4], in1=FGB, op0=ALU.mult, op1=ALU.mult))(at, sc),
                  reads=[ak, sk, 'FGB'], writes=[ak])
            p.dma('sp', ('ost', tt % 2), (lambda tt, at: lambda e: e.dma_start(out=out_d[tt * 128:(tt + 1) * 128, :], in_=at))(tt, at), reads=[ak], writes=[ak, 'out'])
        p.barrier()
        p.emit(st)
    return nc, p


def t5_bucket_np(dist):
    max_exact = 16
    d = np.maximum(dist, 0)
    lr = np.log(np.maximum(d, 1).astype(np.float32) / max_exact) / math.log(128 / max_exact)
    large = max_exact + (lr * (32 - max_exact)).astype(np.int32)
    large = np.minimum(large, 31)
    return np.where(d < max_exact, d, large)


def make_in_maps(inputs, cores=range(8)):
    f = lambda a: np.ascontiguousarray(np.asarray(a, dtype=np.float32))
    x = f(inputs["x"]); c = f(inputs["c"])
    fm = lambda v, n: np.ascontiguousarray(v.reshape(n, 128).T)
    onehot = np.zeros((32, 128), np.float32)
    onehot[t5_bucket_np(np.arange(128)), np.arange(128)] = 1.0
    sel = np.zeros((24, 8, 128), np.float32)
    for h in range(8):
        for part in range(3):
            sel[part * 8 + h, h, :] = 1.0
    keys = f(inputs["peer_keys"])[0]
    keys_t = np.ascontiguousarray(keys.transpose(3, 0, 1, 2).reshape(128, 16 * 128))
    shared = {
        "w_ada": f(inputs["w_ada"])[0],
        "b_ada_t": fm(f(inputs["b_ada"])[0], 192),
        "g1_t": fm(f(inputs["norm1_g"])[0], 32),
        "g2_t": fm(f(inputs["norm2_g"])[0], 32),
        "w_in": f(inputs["w_in"])[0],
        "sinks": f(inputs["attn_sinks"])[0].reshape(1, 16),
        "rel_bias": f(inputs["rel_bias"]),
        "onehot": onehot,
        "gk2_w": f(inputs["gla_w_gk2"])[0],
        "gk2_b": f(inputs["gla_b_gk2"])[0].reshape(1, 1024),
        "gla_norm_g": f(inputs["gla_norm_g"])[0].reshape(1, 512),
        "w_out": f(inputs["w_out"])[0],
        "w_q": f(inputs["peer_w_q"])[0],
        "keys_t": keys_t,
        "u_t": np.ascontiguousarray(f(inputs["peer_u"])[0].T),
        "v": f(inputs["peer_v"])[0],
        "final_g": f(inputs["final_g"]).reshape(1, D),
        "sel": sel.reshape(24, 8 * 128),
    }
    maps = []
    for i in cores:
        b, half = i // 2, i % 2
        m = dict(shared)
        m["x_main"] = np.ascontiguousarray(x[b, half * TOK:(half + 1) * TOK])
        m["x_pre"] = np.ascontiguousarray(x[b, 0:TOK])
        m["flag"] = np.full((128, 1), float(half), np.float32)
        m["c_t"] = fm(c[b], 32)
        maps.append(m)
    return maps


_CACHE = {}


def kernel(**inputs):
    if "nc" not in _CACHE:
        _CACHE["nc"] = build_program()[0]
    nc = _CACHE["nc"]
    maps = make_in_maps(inputs)
    res = run_bass_kernel_spmd(nc, maps, core_ids=list(range(8)))
    out = np.empty((4, 2048, D), np.float32)
    for i in range(8):
        b, half = i // 2, i % 2
        out[b, half * TOK:(half + 1) * TOK] = res.results[i]["out"]
    return out
```
